# Optimizing a Trainium2 kernel written in Bass

```python
import math
import jax, jax.numpy as jnp
from jax import lax
import numpy as np

D_MODEL = 1024
BATCH = 8
SEQ = 4096
DEPTH = 1

SSM_WIDTH = D_MODEL // 2
SSM_GROUP = 16
SSM_GROUPS = SSM_WIDTH // SSM_GROUP
SSM_STATE = 64
DT_MIN = 1e-3
DT_MAX = 1e-1
ATTN_HEADS = 8
ATTN_KV_HEADS = 2
HEAD_DIM = 64
ATTN_WIDTH = ATTN_HEADS * HEAD_DIM
IDX_HEADS = 8
IDX_DIM = 32
TOPK_MAX = 256
Q_BLOCK = 128
ROPE_THETA = 10000.0
NEG_BIG = -1e30
PEER_HEADS = 8
PEER_KEYS = 128
PEER_EXPERTS = PEER_KEYS * PEER_KEYS
PEER_KEY_DIM = 128
PEER_TOPK = 16
PEER_CHUNK = 128
NORM_EPS = 1e-6

IN_SPLITS = (SSM_WIDTH, ATTN_WIDTH, ATTN_KV_HEADS * HEAD_DIM, ATTN_KV_HEADS * HEAD_DIM,
             IDX_HEADS * IDX_DIM, IDX_DIM, IDX_HEADS, D_MODEL, D_MODEL)
IN_WIDTH = sum(IN_SPLITS)

kernel_name = "hybrid_s5_dsa_peer_block"


def rmsnorm(x, g):
    xf = x.astype(jnp.float32)
    y = xf * lax.rsqrt(jnp.mean(xf * xf, axis=-1, keepdims=True) + NORM_EPS)
    return (y * g.astype(jnp.float32)).astype(x.dtype)


def rope_tables(seq, dim):
    pos = jnp.arange(seq, dtype=jnp.float32)
    inv = ROPE_THETA ** (-jnp.arange(0, dim, 2, dtype=jnp.float32) / dim)
    ang = pos[:, None] * inv[None, :]
    return jnp.cos(ang), jnp.sin(ang)


def apply_rope(x, cos, sin):
    half = x.shape[-1] // 2
    shape = (cos.shape[0],) + (1,) * (x.ndim - 3) + (half,)
    c = cos.reshape(shape)
    s = sin.reshape(shape)
    xf = x.astype(jnp.float32)
    x1, x2 = xf[..., :half], xf[..., half:]
    return jnp.concatenate([x1 * c - x2 * s, x2 * c + x1 * s], axis=-1).astype(x.dtype)


def s5_branch(u, a_re, a_im, log_dt, b_re, b_im, c_re, c_im, d_skip, w_glu):
    bsz, seq, _ = u.shape
    f32 = jnp.float32
    uf = u.astype(f32).reshape(bsz, seq, SSM_GROUPS, SSM_GROUP)
    lam = lax.complex(a_re.astype(f32), a_im.astype(f32))
    dt = jnp.exp(log_dt.astype(f32))[:, None]
    a_bar = jnp.exp(lam * dt)
    b = lax.complex(b_re.astype(f32), b_im.astype(f32))
    b_bar = ((a_bar - 1.0) / lam)[..., None] * b
    bu = jnp.einsum('gpc,bsgc->bsgp', b_bar, uf)
    a_seq = jnp.broadcast_to(a_bar, bu.shape)

    def combine(left, right):
        a1, s1 = left
        a2, s2 = right
        return a2 * a1, a2 * s1 + s2

    _, states = lax.associative_scan(combine, (a_seq, bu), axis=1)
    c = lax.complex(c_re.astype(f32), c_im.astype(f32))
    y = jnp.real(jnp.einsum('gcp,bsgp->bsgc', c, states))
    y = y + d_skip.astype(f32).reshape(SSM_GROUPS, SSM_GROUP) * uf
    y = jax.nn.gelu(y.reshape(bsz, seq, SSM_WIDTH))
    y = y * jax.nn.sigmoid(y @ w_glu.astype(f32))
    return y.astype(u.dtype)


def dsa_branch(q, k, v, qi, ki, wi):
    bsz, seq = q.shape[0], q.shape[1]
    topk = min(TOPK_MAX, seq // 4)
    nb = seq // Q_BLOCK
    grp = ATTN_HEADS // ATTN_KV_HEADS
    f32 = jnp.float32
    kf = ki.astype(f32)
    idx_scale = (IDX_HEADS ** -0.5) * (IDX_DIM ** -0.5)
    s_pos = jnp.arange(seq)

    def to_blocks(t):
        return t.reshape((bsz, nb, Q_BLOCK) + t.shape[2:]).swapaxes(0, 1)

    def one_block(args):
        qb, qib, wib, blk = args
        t_pos = blk * Q_BLOCK + jnp.arange(Q_BLOCK)
        rel = jax.nn.relu(jnp.einsum('bthd,bsd->bths', qib.astype(f32), kf))
        score = jnp.einsum('bths,bth->bts', rel, wib.astype(f32)) * idx_scale
        causal = s_pos[None, :] <= t_pos[:, None]
        score = jnp.where(causal[None], score, NEG_BIG)
        _, sel = lax.top_k(score, topk)
        valid = sel <= t_pos[None, :, None]
        kg = jax.vmap(lambda kk, ii: kk[ii])(k, sel)
        vg = jax.vmap(lambda vv, ii: vv[ii])(v, sel)
        qg = qb.reshape(bsz, Q_BLOCK, ATTN_KV_HEADS, grp, HEAD_DIM).astype(f32)
        logits = jnp.einsum('btngd,btknd->btngk', qg, kg.astype(f32)) * (HEAD_DIM ** -0.5)
        logits = jnp.where(valid[:, :, None, None, :], logits, NEG_BIG)
        p = jax.nn.softmax(logits, axis=-1)
        o = jnp.einsum('btngk,btknd->btngd', p, vg.astype(f32))
        return o.reshape(bsz, Q_BLOCK, ATTN_WIDTH).astype(qb.dtype)

    out = lax.map(one_block, (to_blocks(q), to_blocks(qi), to_blocks(wi), jnp.arange(nb)))
    return out.swapaxes(0, 1).reshape(bsz, seq, ATTN_WIDTH)


def peer_ffn(x, w_q, sub_k1, sub_k2, u_tab, v_tab):
    bsz, seq, d = x.shape
    f32 = jnp.float32
    q = (x @ w_q).astype(f32).reshape(bsz, seq, PEER_HEADS, 2, PEER_KEY_DIM)
    s1 = jnp.einsum('bshd,hnd->bshn', q[..., 0, :], sub_k1.astype(f32))
    s2 = jnp.einsum('bshd,hnd->bshn', q[..., 1, :], sub_k2.astype(f32))
    v1, i1 = lax.top_k(s1, PEER_TOPK)
    v2, i2 = lax.top_k(s2, PEER_TOPK)
    cand_s = (v1[..., :, None] + v2[..., None, :]).reshape(bsz, seq, PEER_HEADS, PEER_TOPK * PEER_TOPK)
    cand_i = (i1[..., :, None] * PEER_KEYS + i2[..., None, :]).reshape(bsz, seq, PEER_HEADS, PEER_TOPK * PEER_TOPK)
    top_s, pos = lax.top_k(cand_s, PEER_TOPK)
    experts = jnp.take_along_axis(cand_i, pos, axis=-1)
    gates = jax.nn.softmax(top_s, axis=-1)
    n_tok = bsz * seq
    n_chunks = n_tok // PEER_CHUNK
    n_sel = PEER_HEADS * PEER_TOPK
    xc_all = x.reshape(n_chunks, PEER_CHUNK, d)
    ec_all = experts.reshape(n_chunks, PEER_CHUNK, n_sel)
    gc_all = gates.reshape(n_chunks, PEER_CHUNK, n_sel)

    def chunk(args):
        xc, ec, gc = args
        u = u_tab[ec].astype(f32)
        act = jax.nn.gelu(jnp.einsum('ced,cd->ce', u, xc.astype(f32))) * gc
        return jnp.einsum('ce,ced->cd', act, v_tab[ec].astype(f32)).astype(x.dtype)

    out = lax.map(chunk, (xc_all, ec_all, gc_all))
    return out.reshape(bsz, seq, d)


def setup_inputs(seed: int = 0) -> dict:
    key = jax.random.key(seed)
    ks = jax.random.split(key, 24)
    f32 = jnp.float32
    L, D, G, P, N = DEPTH, D_MODEL, SSM_GROUPS, SSM_STATE, SSM_GROUP
    nrm = lambda k, shape, s: jax.random.normal(k, shape, f32) * s
    x = jax.random.normal(ks[0], (BATCH, SEQ, D), f32)
    norm1_g = 1.0 + nrm(ks[1], (L, D), 0.02)
    w_in = nrm(ks[2], (L, D, IN_WIDTH), D ** -0.5)
    a_re = -0.5 * (1.0 + nrm(ks[3], (L, G, P), 0.01))
    a_im = jnp.broadcast_to(math.pi * jnp.arange(P, dtype=f32), (L, G, P)) + nrm(ks[4], (L, G, P), 0.01)
    log_dt = jax.random.uniform(ks[5], (L, G), f32, math.log(DT_MIN), math.log(DT_MAX))
    b_re = nrm(ks[6], (L, G, P, N), (2.0 * N) ** -0.5)
    b_im = nrm(ks[7], (L, G, P, N), (2.0 * N) ** -0.5)
    c_re = nrm(ks[8], (L, G, N, P), P ** -0.5)
    c_im = nrm(ks[9], (L, G, N, P), P ** -0.5)
    d_skip = nrm(ks[10], (L, SSM_WIDTH), 1.0)
    w_glu = nrm(ks[11], (L, SSM_WIDTH, SSM_WIDTH), SSM_WIDTH ** -0.5)
    w_ssm_up = nrm(ks[12], (L, SSM_WIDTH, D), SSM_WIDTH ** -0.5)
    w_attn_up = nrm(ks[13], (L, ATTN_WIDTH, D), ATTN_WIDTH ** -0.5)
    w_out = nrm(ks[14], (L, D, D), D ** -0.5)
    norm2_g = 1.0 + nrm(ks[15], (L, D), 0.02)
    peer_wq = nrm(ks[16], (L, D, PEER_HEADS * 2 * PEER_KEY_DIM), D ** -0.5)
    peer_k1 = nrm(ks[17], (L, PEER_HEADS, PEER_KEYS, PEER_KEY_DIM), PEER_KEY_DIM ** -0.5)
    peer_k2 = nrm(ks[18], (L, PEER_HEADS, PEER_KEYS, PEER_KEY_DIM), PEER_KEY_DIM ** -0.5)
    peer_u = nrm(ks[19], (L, PEER_EXPERTS, D), D ** -0.5)
    peer_v = nrm(ks[20], (L, PEER_EXPERTS, D), PEER_HEADS ** -0.5)
    norm_f_g = 1.0 + nrm(ks[21], (D,), 0.02)
    return {"x": x, "norm1_g": norm1_g, "w_in": w_in, "a_re": a_re, "a_im": a_im,
            "log_dt": log_dt, "b_re": b_re, "b_im": b_im, "c_re": c_re, "c_im": c_im,
            "d_skip": d_skip, "w_glu": w_glu, "w_ssm_up": w_ssm_up, "w_attn_up": w_attn_up,
            "w_out": w_out, "norm2_g": norm2_g, "peer_wq": peer_wq, "peer_k1": peer_k1,
            "peer_k2": peer_k2, "peer_u": peer_u, "peer_v": peer_v, "norm_f_g": norm_f_g}


def reference(x, norm1_g, w_in, a_re, a_im, log_dt, b_re, b_im, c_re, c_im, d_skip, w_glu,
              w_ssm_up, w_attn_up, w_out, norm2_g, peer_wq, peer_k1, peer_k2, peer_u, peer_v,
              norm_f_g):
    bsz, seq, _ = x.shape
    cos_a, sin_a = rope_tables(seq, HEAD_DIM)
    cos_i, sin_i = rope_tables(seq, IDX_DIM)
    offsets = np.cumsum(IN_SPLITS)[:-1].tolist()
    h = x
    for layer in range(DEPTH):
        xn = rmsnorm(h, norm1_g[layer])
        proj = xn @ w_in[layer]
        u, q, k, v, qi, ki, wi, g_ssm, g_attn = jnp.split(proj, offsets, axis=-1)
        y_ssm = s5_branch(u, a_re[layer], a_im[layer], log_dt[layer], b_re[layer], b_im[layer],
                          c_re[layer], c_im[layer], d_skip[layer], w_glu[layer])
        q = apply_rope(q.reshape(bsz, seq, ATTN_HEADS, HEAD_DIM), cos_a, sin_a)
        k = apply_rope(k.reshape(bsz, seq, ATTN_KV_HEADS, HEAD_DIM), cos_a, sin_a)
        v = v.reshape(bsz, seq, ATTN_KV_HEADS, HEAD_DIM)
        qi = apply_rope(qi.reshape(bsz, seq, IDX_HEADS, IDX_DIM), cos_i, sin_i)
        ki = apply_rope(ki, cos_i, sin_i)
        y_attn = dsa_branch(q, k, v, qi, ki, wi)
        merged = (jax.nn.sigmoid(g_ssm) * (y_ssm @ w_ssm_up[layer])
                  + jax.nn.sigmoid(g_attn) * (y_attn @ w_attn_up[layer]))
        h = h + merged @ w_out[layer]
        hn = rmsnorm(h, norm2_g[layer])
        h = h + peer_ffn(hn, peer_wq[layer], peer_k1[layer], peer_k2[layer], peer_u[layer], peer_v[layer])
    return rmsnorm(h, norm_f_g)
```

```python
import math
from contextlib import ExitStack

import numpy as np
import concourse.bass as bass
import concourse.mybir as mybir
from concourse.bass_utils import run_bass_kernel_spmd

F32 = mybir.dt.float32
BF16 = mybir.dt.bfloat16
I32 = mybir.dt.int32
U32 = mybir.dt.uint32
ALU = mybir.AluOpType
AF = mybir.ActivationFunctionType
AX = mybir.AxisListType

D = 1024
NCORES = 8
SSM_W = 512
NG = 32
NP_ = 64
LCH = 16
EPS = 1e-6
NEG = -1.0e30
STRICT = False
DUMP2 = False
OUTK = ("out", "accum_out", "out_max", "out_indices")


class V:
    __slots__ = ("t", "ap")

    def __init__(self, t, ap):
        self.t = t
        self.ap = ap

    def __getitem__(self, k):
        return V(self.t, self.ap[k])

    def rr(self, pat, **kw):
        return V(self.t, self.ap.rearrange(pat, **kw))

    def bc(self, shape):
        return V(self.t, self.ap.to_broadcast(list(shape)))

    def cast(self, dt):
        return V(self.t, self.ap.bitcast(dt))

    def pat(self, off, pattern):
        a = self.ap
        return V(self.t, bass.AP(a.tensor, a.offset + off, [list(a.ap[0])] + [list(p) for p in pattern]))

    @property
    def shape(self):
        return self.ap.shape


class Tl:
    def __init__(self, base_ap, name, dram=False):
        self.base = base_ap
        self.name = name
        self.dram = dram
        self.w = None
        self.r = {}
        self.dsem = None

    def __getitem__(self, k):
        return V(self, self.base[k])

    def all(self):
        return V(self, self.base)


class Prog:
    EPOCH = 14000

    def __init__(self, nc, es):
        self.nc = nc
        self.es = es
        self.eng = {"pe": nc.tensor, "dve": nc.vector, "act": nc.scalar, "pool": nc.gpsimd, "sp": nc.sync}
        self.sems = []
        self.semeng = []
        self.cur = {}
        self.cnt = {}
        self.known = {e: {} for e in self.eng}
        self.dcnt = {}
        self.ninstr = 0
        self.freed = {}
        self.log = None
        for e in self.eng:
            self._newsem(e)

    def _newsem(self, e):
        s = self.es.enter_context(self.nc.semaphore(f"s{len(self.sems)}"))
        self.sems.append(s)
        self.semeng.append(e)
        idx = len(self.sems) - 1
        if e is not None:
            self.cur[e] = idx
            self.cnt[e] = 0
        else:
            self.dcnt[idx] = 0
        return idx

    def sb(self, es, name, shape, dt):
        self.uid = getattr(self, "uid", 0) + 1
        h = es.enter_context(self.nc.sbuf_tensor(f"sb{self.uid}_" + name, list(shape), dt))
        t = Tl(h[:], name)
        t.r = dict(self.freed)
        es.callback(self._on_free, t)
        return t

    def _on_free(self, t):
        toks = dict(t.r)
        if t.w is not None:
            toks[t.w[0]] = max(toks.get(t.w[0], 0), t.w[1])
        for si, val in toks.items():
            if self.freed.get(si, 0) < val:
                self.freed[si] = val

    def ps(self, es, name, shape, dt):
        h = es.enter_context(self.nc.psum_tensor("ps_" + name, list(shape), dt))
        return Tl(h[:], name)

    def dram(self, name, shape, dt, kind):
        h = self.nc.dram_tensor(name, list(shape), dt, kind=kind)
        return Tl(h.ap(), name, dram=True)

    def _need(self, e, tok, raw, dma=False):
        if tok is None:
            return
        si, val = tok
        owner = self.semeng[si]
        if owner == e and (e == "pe" or (not raw and not STRICT)) and not dma:
            return
        k = self.known[e]
        if k.get(si, 0) >= val:
            return
        self.eng[e].wait_ge(self.sems[si], val)
        self.ninstr += 1
        k[si] = val
        if self.log is not None:
            self.log.append(f"{e}: WAIT s{si}({self.semeng[si]}) >= {val}")

    def _deps(self, e, reads, writes, skip_w_sem=None, dma=False):
        for t in reads:
            self._need(e, t.w, True, dma)
        for t in writes:
            if t.w is not None and t.w[0] != skip_w_sem:
                self._need(e, t.w, False, dma)
            for si, val in t.r.items():
                self._need(e, (si, val), False, dma)

    def _mark(self, tok, reads, writes):
        si, val = tok
        for t in reads:
            if t.r.get(si, 0) < val:
                t.r[si] = val
        for t in writes:
            t.w = tok
            t.r = {}

    def op(self, e, fn, **kw):
        reads, writes, args = [], [], {}
        for k, v in kw.items():
            if isinstance(v, V):
                (writes if k in OUTK else reads).append(v.t)
                args[k] = v.ap
            else:
                args[k] = v
        self._deps(e, reads, writes)
        ins = getattr(self.eng[e], fn)(**args)
        if self.log is not None:
            self.log.append(f"{e}: {fn} W={[t.name for t in writes]} R={[t.name for t in reads]} -> {self.cnt[e] + 1}")
        if self.cnt[e] >= self.EPOCH:
            self._newsem(e)
        si = self.cur[e]
        self.cnt[e] += 1
        ins.then_inc(self.sems[si], 1)
        self.ninstr += 1
        self._mark((si, self.cnt[e]), reads, writes)
        return ins

    def _dsem(self, t):
        if t.dsem is None:
            t.dsem = self._newsem(None)
        return t.dsem

    def dma(self, q, out, in_, semtile=None, extra_reads=(), **kw):
        sbt = semtile if semtile is not None else (out.t if not out.t.dram else in_.t)
        ds = self._dsem(sbt)
        reads = [in_.t] + [x.t for x in extra_reads]
        writes = [out.t]
        self._deps(q, reads, writes, skip_w_sem=ds, dma=True)
        ins = self.eng[q].dma_start(out=out.ap, in_=in_.ap, **kw)
        self.dcnt[ds] += 16
        ins.then_inc(self.sems[ds], 16)
        self.ninstr += 1
        self._mark((ds, self.dcnt[ds]), reads, writes)

    def gather(self, out, table, idx):
        ds = self._dsem(out.t)
        reads = [table.t, idx.t]
        writes = [out.t]
        self._deps("pool", reads, writes, skip_w_sem=ds, dma=True)
        ins = self.nc.gpsimd.indirect_dma_start(
            out=out.ap, out_offset=None, in_=table.ap,
            in_offset=bass.IndirectOffsetOnAxis(ap=idx.ap, axis=0))
        self.dcnt[ds] += 16
        ins.then_inc(self.sems[ds], 16)
        self.ninstr += 1
        self._mark((ds, self.dcnt[ds]), reads, writes)

    def finish(self, tiles):
        for t in tiles:
            self._need("sp", t.w, True)

    def mm(self, out, lhsT, rhs, start, stop, **kw):
        return self.op("pe", "matmul", out=out, lhsT=lhsT, rhs=rhs, start=start, stop=stop, **kw)

    def tr(self, out, in_, ident):
        return self.op("pe", "transpose", out=out, in_=in_, identity=ident)

    def act(self, out, in_, func, **kw):
        return self.op("act", "activation", out=out, in_=in_, func=func, **kw)

    def tt(self, e, out, in0, in1, op):
        return self.op(e, "tensor_tensor", out=out, in0=in0, in1=in1, op=op)

    def ts(self, e, out, in0, s1, op0, s2=None, op1=None, **kw):
        if op1 is None:
            return self.op(e, "tensor_scalar", out=out, in0=in0, scalar1=s1, scalar2=None, op0=op0, **kw)
        if isinstance(s1, V) != isinstance(s2, V):
            self.op(e, "tensor_scalar", out=out, in0=in0, scalar1=s1, scalar2=None, op0=op0)
            return self.op(e, "tensor_scalar", out=out, in0=out, scalar1=s2, scalar2=None, op0=op1, **kw)
        return self.op(e, "tensor_scalar", out=out, in0=in0, scalar1=s1, scalar2=s2, op0=op0, op1=op1, **kw)

    def stt(self, e, out, in0, scalar, in1, op0, op1, **kw):
        return self.op(e, "scalar_tensor_tensor", out=out, in0=in0, scalar=scalar, in1=in1, op0=op0, op1=op1, **kw)

    def copy(self, e, out, in_):
        if e == "act":
            return self.act(out, in_, AF.Copy)
        return self.op(e, "tensor_copy", out=out, in_=in_)

    def memset(self, e, out, val):
        return self.op(e, "memset", ap=out, constant=val) if False else self._memset(e, out, val)

    def _memset(self, e, out, val):
        self._deps(e, [], [out.t])
        ins = self.eng[e].memset(out.ap, val)
        if self.cnt[e] >= self.EPOCH:
            self._newsem(e)
        si = self.cur[e]
        self.cnt[e] += 1
        ins.then_inc(self.sems[si], 1)
        self.ninstr += 1
        self._mark((si, self.cnt[e]), [], [out.t])


OFF_U, OFF_Q, OFF_K, OFF_V, OFF_QI, OFF_KI, OFF_WI, OFF_GS, OFF_GA = 0, 512, 1024, 1152, 1280, 1536, 1568, 1576, 2600
IN_W = 3624
TWO_PI = 2.0 * math.pi


class Ctx:
    pass


def rope_tables(P, es, S, c):
    tabs = {fn + nm: P.sb(es, f"rope_{fn}{nm}", [128, S], BF16) for nm in ("A", "I") for fn in ("cos", "sin")}
    tmp = ExitStack()
    pid = P.sb(tmp, "rt_pid", [128, 1], I32)
    pm = P.sb(tmp, "rt_pm", [128, 1], I32)
    pf = P.sb(tmp, "rt_pf", [128, 1], F32)
    inv = P.sb(tmp, "rt_inv", [128, 2], F32)
    posi = P.sb(tmp, "rt_posi", [128, S], I32)
    pos = P.sb(tmp, "rt_pos", [128, S], F32)
    ang = P.sb(tmp, "rt_ang", [128, S], F32)
    t1 = P.sb(tmp, "rt_t1", [128, S], F32)
    ti = P.sb(tmp, "rt_ti", [128, S], I32)
    P.op("pool", "iota", out=pid.all(), pattern=[[0, 1]], base=0, channel_multiplier=1)
    P.op("pool", "iota", out=posi.all(), pattern=[[1, S]], base=0, channel_multiplier=0)
    P.copy("dve", pos.all(), posi.all())
    for j, (msk, dim) in enumerate(((31, 64), (15, 32))):
        P.op("dve", "tensor_single_scalar", out=pm.all(), in_=pid.all(), scalar=msk, op=ALU.bitwise_and)
        P.copy("dve", pf.all(), pm.all())
        P.act(inv[:, j:j + 1], pf.all(), AF.Exp, scale=-math.log(10000.0) * 2.0 / dim)
    outs = {}
    for j, nm in enumerate(("A", "I")):
        for k, (fn, shift) in enumerate((("cos", math.pi / 2), ("sin", 0.0))):
            tab = tabs[fn + nm]
            P.ts("dve", ang.all(), pos.all(), inv[:, j:j + 1], ALU.mult, shift, ALU.add)
            range_reduce_sin(P, tab.all(), ang.all(), t1.all(), ti.all())
            outs[fn + nm] = tab
    tmp.close()
    c.rope = outs


def range_reduce_sin(P, out, ang, t1, ti):
    P.ts("dve", t1, ang, 1.0 / TWO_PI, ALU.mult)
    P.copy("dve", ti, t1)
    P.copy("dve", t1, ti)
    P.stt("dve", t1, t1, -TWO_PI, ang, ALU.mult, ALU.add)
    P.ts("dve", ang, t1, math.pi, ALU.is_gt)
    P.stt("dve", t1, ang, -TWO_PI, t1, ALU.mult, ALU.add)
    P.ts("dve", t1, t1, 3.141592, ALU.min, -3.141592, ALU.max)
    P.act(out, t1, AF.Sin)


def load_w_cols(P, c, dst_fn, col0, ncols, w_in_d, gcol, stage):
    for k in range(8):
        st = stage[k % 2]
        P.dma("sp" if k % 2 == 0 else "act", st[:, 0:ncols], w_in_d[k * 128:(k + 1) * 128, col0:col0 + ncols])
        dst_fn(k, st)


def cpow(P, es, name, lr, th, jv, G, J, order="gj"):
    shp = [128, G, J] if order == "gj" else [128, J, G]
    Pr = P.sb(es, name + "_r", shp, F32)
    Pi = P.sb(es, name + "_i", shp, F32)
    tmp = ExitStack()
    mag = P.sb(tmp, name + "_mag", shp, F32)
    ang = P.sb(tmp, name + "_ang", shp, F32)
    t1 = P.sb(tmp, name + "_t1", shp, F32)
    ti = P.sb(tmp, name + "_ti", shp, I32)
    if order == "gj":
        lb, jb = lr.pat(0, [(1, G), (0, J)]), jv.pat(0, [(0, G), (1, J)])
        tb = th.pat(0, [(1, G), (0, J)])
    else:
        lb, jb = lr.pat(0, [(0, J), (1, G)]), jv.pat(0, [(1, J), (0, G)])
        tb = th.pat(0, [(0, J), (1, G)])
    P.tt("dve", mag.all(), lb, jb, ALU.mult)
    P.act(mag.all(), mag.all(), AF.Exp)
    fl = "p a b -> p (a b)"
    for dst, shift in ((Pi, 0.0), (Pr, math.pi / 2)):
        P.tt("dve", ang.all(), tb, jb, ALU.mult)
        if shift:
            P.ts("dve", ang.all(), ang.all(), shift, ALU.add)
        range_reduce_sin(P, dst.all().rr(fl), ang.all().rr(fl), t1.all().rr(fl), ti.all().rr(fl))
        P.tt("dve", dst.all(), dst.all(), mag.all(), ALU.mult)
    tmp.close()
    return Pr, Pi


def kappa(P, es, name, ar, ai, lr, th, G):
    kr = P.sb(es, name + "_kr", [128, G], F32)
    ki = P.sb(es, name + "_ki", [128, G], F32)
    tmp = ExitStack()
    one = P.sb(tmp, name + "_one", [128, 1], F32)
    P.memset("dve", one.all(), 1.0)
    Ar, Ai = cpow(P, tmp, name + "_a1", lr, th, one.all(), G, 1)
    den = P.sb(tmp, name + "_den", [128, G], F32)
    t = P.sb(tmp, name + "_t", [128, G], F32)
    arm = P.sb(tmp, name + "_arm", [128, G], F32)
    A_r, A_i = Ar.all().rr("p g j -> p (g j)"), Ai.all().rr("p g j -> p (g j)")
    P.tt("dve", den.all(), ar, ar, ALU.mult)
    P.tt("dve", t.all(), ai, ai, ALU.mult)
    P.tt("dve", den.all(), den.all(), t.all(), ALU.add)
    P.op("dve", "reciprocal", out=den.all(), in_=den.all())
    P.ts("dve", arm.all(), A_r, -1.0, ALU.add)
    P.tt("dve", kr.all(), arm.all(), ar, ALU.mult)
    P.tt("dve", t.all(), A_i, ai, ALU.mult)
    P.tt("dve", kr.all(), kr.all(), t.all(), ALU.add)
    P.tt("dve", kr.all(), kr.all(), den.all(), ALU.mult)
    P.tt("dve", ki.all(), A_i, ar, ALU.mult)
    P.tt("dve", t.all(), arm.all(), ai, ALU.mult)
    P.tt("dve", ki.all(), ki.all(), t.all(), ALU.subtract)
    P.tt("dve", ki.all(), ki.all(), den.all(), ALU.mult)
    tmp.close()
    return kr, ki


def ssm_phase(P, c, S, uys, sp):
    NCH = S // LCH
    psb = c.psb
    uT = ysT = y2 = uys
    ph = ExitStack()
    sg = P.sb(ph, "sg", [128, 1], F32)
    nsg = P.sb(ph, "nsg", [128, 1], F32)
    P.ts("dve", sg.all(), c.pid_f.all(), 63.5, ALU.is_gt, -2.0, ALU.mult)
    P.ts("dve", sg.all(), sg.all(), 1.0, ALU.add)
    P.ts("dve", nsg.all(), sg.all(), -1.0, ALU.mult)
    jv = P.sb(ph, "jv", [128, 256], F32)
    jvi = P.sb(ph, "jvi", [128, 256], I32)
    P.op("pool", "iota", out=jvi.all(), pattern=[[1, 256]], base=0, channel_multiplier=0)
    P.copy("dve", jv.all(), jvi.all())
    jrev = P.sb(ph, "jrev", [128, 16], F32)
    P.ts("dve", jrev.all(), jv[:, 0:16], -1.0, ALU.mult, 15.0, ALU.add)
    bm = P.sb(ph, "bm", [128, 8], F32)
    t8 = P.sb(ph, "t8", [128, 8], F32)
    P.ts("dve", t8.all(), jv[:, 0:8], 16.0, ALU.mult)
    P.ts("dve", bm.all(), t8.all(), c.pid_f[:, 0:1], ALU.subtract)
    P.ts("dve", t8.all(), bm.all(), 0.5, ALU.is_gt, -1.0, ALU.mult)
    P.ts("dve", t8.all(), t8.all(), 1.0, ALU.add)
    P.ts("dve", bm.all(), bm.all(), -15.5, ALU.is_gt)
    P.tt("dve", bm.all(), bm.all(), t8.all(), ALU.mult)
    eye8 = P.sb(ph, "eye8", [128, 8, 8], F32)
    P.tt("dve", eye8.all(), jv[:, 0:8].pat(0, [(1, 8), (0, 8)]), jv[:, 0:8].pat(0, [(0, 8), (1, 8)]), ALU.is_equal)
    pswapb = P.sb(ph, "pswapb", [128, 128], BF16)
    pswap = P.sb(ph, "pswap", [128, 128], F32)
    P.ts("dve", pswap.all(), c.iota_f.all(), c.pid_f[:, 0:1], ALU.subtract)
    P.tt("dve", pswap.all(), pswap.all(), pswap.all(), ALU.mult)
    P.ts("dve", pswap.all(), pswap.all(), 4096.0, ALU.is_equal)
    P.copy("dve", pswapb.all(), pswap.all())
    dsk = P.sb(ph, "dsk", [128, 4], F32)
    P.dma("sp", dsk.all(), sp["dskip"].all())
    wglu = P.sb(ph, "wglu", [128, 4, 512], BF16)
    stgw = P.sb(ph, "stgw", [128, 512], F32)
    for k in range(4):
        P.dma("sp", stgw.all(), sp["w_glu"][k * 128:(k + 1) * 128, :])
        P.copy("act", wglu[:, k, :], stgw.all())

    for o in range(4):
        oc = ExitStack()
        BD = P.sb(oc, "BD", [128, 16, 128], BF16)
        Wb = P.sb(oc, "Wb", [128, 8, 16, 128], BF16)
        Wc = P.sb(oc, "Wc", [128, 8, 16, 128], BF16)
        tc_ = P.sb(oc, "tabc", [128, 8, NCH], BF16)
        tsn = P.sb(oc, "tabs", [128, 8, NCH], BF16)
        rho = P.sb(oc, "rho", [128, 8], F32)
        pr = ExitStack()
        prm = P.sb(pr, "prm", [128, 3, 8], F32)
        for i, nm in enumerate(("ar_sm", "ai_sm", "ldt_sm")):
            P.dma("sp", prm[:, i, :], sp[nm][:, o * 8:(o + 1) * 8])
        dt = P.sb(pr, "dt", [128, 8], F32)
        lr = P.sb(pr, "lr", [128, 8], F32)
        th = P.sb(pr, "th", [128, 8], F32)
        P.act(dt.all(), prm[:, 2, :], AF.Exp)
        P.tt("dve", lr.all(), prm[:, 0, :], dt.all(), ALU.mult)
        P.tt("dve", th.all(), prm[:, 1, :], dt.all(), ALU.mult)
        kr, ki = kappa(P, pr, "ksm", prm[:, 0, :], prm[:, 1, :], lr.all(), th.all(), 8)
        Ar, Ai = cpow(P, pr, "apw", lr.all(), th.all(), jv[:, 0:17], 8, 17)
        prA = pr
        pr = ExitStack()
        U = P.sb(pr, "U12", [128, 2, 8, 16], F32)
        T12 = P.sb(pr, "T12", [128, 2, 8, 16], F32)
        for i, nm in enumerate(("bU1", "bU2")):
            P.dma("sp", U[:, i], sp[nm][:, o * 8:(o + 1) * 8, :])
        for i, nm in enumerate(("cT1", "cT2")):
            P.dma("act", T12[:, i], sp[nm][:, o * 8:(o + 1) * 8, :])
        X = P.sb(pr, "X", [128, 8, 16], F32)
        tx = P.sb(pr, "tx", [128, 8, 16], F32)
        kib = P.sb(pr, "kib", [128, 8], F32)
        P.ts("dve", kib.all(), ki.all(), nsg[:, 0:1], ALU.mult)
        P.tt("dve", X.all(), U[:, 0], kr.all().pat(0, [(1, 8), (0, 16)]), ALU.mult)
        P.tt("dve", tx.all(), U[:, 1], kib.all().pat(0, [(1, 8), (0, 16)]), ALU.mult)
        P.tt("dve", X.all(), X.all(), tx.all(), ALU.add)
        if getattr(c, "dump", None) and o == 0 and DUMP2:
            c.dump("kr", kr.all(), F32)
            c.dump("ki", ki.all(), F32)
            c.dump("X", X.all().rr("p a b -> p (a b)"), F32)
            c.dump("Ar", Ar.all().rr("p a b -> p (a b)"), F32)
        Y = P.sb(pr, "Y", [128, 8, 17, 16], F32)
        ty = P.sb(pr, "ty", [128, 8, 17, 16], F32)
        for g in range(8):
            P.tt("dve", Y[:, g], T12[:, 0, g, :].pat(0, [(0, 17), (1, 16)]), Ar[:, g, :].pat(0, [(1, 17), (0, 16)]), ALU.mult)
            P.tt("dve", ty[:, g], T12[:, 1, g, :].pat(0, [(0, 17), (1, 16)]), Ai[:, g, :].pat(0, [(1, 17), (0, 16)]), ALU.mult)
        P.stt("dve", Y.all().rr("p g t c -> p (g t c)"), Y.all().rr("p g t c -> p (g t c)"), sg[:, 0:1],
              ty.all().rr("p g t c -> p (g t c)"), ALU.mult, ALU.subtract)
        if getattr(c, "dump", None) and o == 0 and DUMP2:
            c.dump("Y", Y.all().rr("p a b c -> p (a b c)"), F32)
        for g in range(8):
            P.tt("dve", Wc[:, g].rr("p t (a c) -> p t a c", a=8),
                 Y[:, g, 1:17, :].pat(0, [(16, 16), (0, 8), (1, 16)]),
                 eye8[:, g, :].pat(0, [(0, 16), (1, 8), (0, 16)]), ALU.mult)
        Xpad = P.sb(pr, "Xpad", [128, 8, 8, 16], BF16)
        Yb = P.sb(pr, "Yb", [128, 8, 16, 16], BF16)
        P.copy("dve", Yb.all(), Y[:, :, 0:16, :])
        for g in range(8):
            P.tt("dve", Xpad[:, g], X[:, g, :].pat(0, [(0, 8), (1, 16)]), eye8[:, g, :].pat(0, [(1, 8), (0, 16)]), ALU.mult)
        for g in range(8):
            P.mm(psb[0][:, 0:256], Xpad[:, g].rr("p a c -> p (a c)"), Yb[:, g].rr("p t c -> p (t c)"), g == 0, g == 7)
        Rsb = P.sb(pr, "Rsb", [128, 16, 16], F32)
        P.copy("act", Rsb.all().rr("p t c -> p (t c)"), psb[0][:, 0:256])
        if getattr(c, "dump", None) and o == 0 and DUMP2:
            c.dump("Rsb", Rsb.all().rr("p a b -> p (a b)"), F32)
            c.dump("Xpad", Xpad.all().rr("p a b c -> p (a b c)"), BF16)
        for tau in range(16):
            P.tt("dve", BD[:, tau, :].rr("p (a c) -> p a c", a=8), Rsb[:, tau, :].pat(0, [(0, 8), (1, 16)]),
                 bm.all().pat(0, [(1, 8), (0, 16)]), ALU.mult)
        pr.close()
        pr = ExitStack()
        l16 = P.sb(pr, "l16", [128, 8], F32)
        P.act(rho.all(), lr.all(), AF.Exp, scale=float(LCH))
        P.ts("dve", l16.all(), th.all(), float(LCH), ALU.mult)
        zero8 = P.sb(pr, "zero8", [128, 8], F32)
        P.memset("dve", zero8.all(), 0.0)
        assert NCH <= 256
        for h4 in range(2):
            pq = ExitStack()
            Er, Ei = cpow(P, pq, f"rot{h4}", zero8[:, h4 * 4:h4 * 4 + 4], l16[:, h4 * 4:h4 * 4 + 4], jv[:, 0:NCH], 4, NCH)
            P.copy("act", tc_[:, h4 * 4:h4 * 4 + 4, :], Er.all())
            P.copy("act", tsn[:, h4 * 4:h4 * 4 + 4, :], Ei.all())
            pq.close()
        pr.close()
        pr = ExitStack()
        pcm = P.sb(pr, "pcm", [128, 5, 64], F32)
        for i, nm in enumerate(("ar_cm", "ai_cm", "ldt_cm", "br_cm", "bi_cm")):
            P.dma("sp", pcm[:, i, :], sp[nm][:, o, :])
        dtc = P.sb(pr, "dtc", [128, 64], F32)
        lrc = P.sb(pr, "lrc", [128, 64], F32)
        thc = P.sb(pr, "thc", [128, 64], F32)
        P.act(dtc.all(), pcm[:, 2, :], AF.Exp)
        P.tt("dve", lrc.all(), pcm[:, 0, :], dtc.all(), ALU.mult)
        P.tt("dve", thc.all(), pcm[:, 1, :], dtc.all(), ALU.mult)
        krc, kic = kappa(P, pr, "kcm", pcm[:, 0, :], pcm[:, 1, :], lrc.all(), thc.all(), 64)
        Bbr = P.sb(pr, "Bbr", [128, 64], F32)
        Bbi = P.sb(pr, "Bbi", [128, 64], F32)
        tb = P.sb(pr, "tb", [128, 64], F32)
        P.tt("dve", Bbr.all(), krc.all(), pcm[:, 3, :], ALU.mult)
        P.tt("dve", tb.all(), kic.all(), pcm[:, 4, :], ALU.mult)
        P.tt("dve", Bbr.all(), Bbr.all(), tb.all(), ALU.subtract)
        P.tt("dve", Bbi.all(), krc.all(), pcm[:, 4, :], ALU.mult)
        P.tt("dve", tb.all(), kic.all(), pcm[:, 3, :], ALU.mult)
        P.tt("dve", Bbi.all(), Bbi.all(), tb.all(), ALU.add)
        Pr_, Pi_ = cpow(P, pr, "apc", lrc.all(), thc.all(), jrev.all(), 64, 16, order="jg")
        Z = P.sb(pr, "Z", [128, 16, 2, 64], F32)
        tz = P.sb(pr, "tz", [128, 16, 64], F32)
        bb = lambda t: t.all().pat(0, [(0, 16), (1, 64)])
        P.tt("dve", Z[:, :, 0, :], Pr_.all(), bb(Bbr), ALU.mult)
        P.tt("dve", tz.all(), Pi_.all(), bb(Bbi), ALU.mult)
        P.tt("dve", Z[:, :, 0, :], Z[:, :, 0, :], tz.all(), ALU.subtract)
        P.tt("dve", Z[:, :, 1, :], Pr_.all(), bb(Bbi), ALU.mult)
        P.tt("dve", tz.all(), Pi_.all(), bb(Bbr), ALU.mult)
        P.tt("dve", Z[:, :, 1, :], Z[:, :, 1, :], tz.all(), ALU.add)
        if getattr(c, "dump", None) and o == 0 and DUMP2:
            c.dump("Z", Z.all().rr("p s r m -> p (s r m)"), F32)
            c.dump("Bbr", Bbr.all(), F32)
            c.dump("Prc", Pr_.all().rr("p a b -> p (a b)"), F32)
            c.dump("krc", krc.all(), F32)
            c.dump("pcm", pcm.all().rr("p a b -> p (a b)"), F32)
            c.dump("lrc", lrc.all(), F32)
        for g in range(8):
            P.ts("dve", Wb[:, g].rr("p s m -> p (s m)"), Z.all().rr("p s r m -> p (s r m)"), bm[:, g:g + 1], ALU.mult)
        pr.close()
        prA.close()
        if getattr(c, "dump", None) and o == 0:
            c.dump("BD", BD.all().rr("p a b -> p (a b)"), BF16)
            c.dump("Wb", Wb.all().rr("p a b m -> p (a b m)"), BF16)
            c.dump("Wc", Wc.all().rr("p a b m -> p (a b m)"), BF16)
            c.dump("tabc", tc_.all().rr("p a b -> p (a b)"), BF16)
            c.dump("tabs", tsn.all().rr("p a b -> p (a b)"), BF16)
            c.dump("rho", rho.all(), F32)

        wk = ExitStack()
        SA = P.sb(wk, "SA", [128, 8, NCH], F32)
        VA = P.sb(wk, "VA", [128, 8, NCH], F32)
        VB = P.sb(wk, "VB", [128, 8, NCH], F32)
        t1 = P.sb(wk, "l2t1", [128, 8, NCH], F32)
        t2 = P.sb(wk, "l2t2", [128, 8, NCH], F32)
        H = P.sb(wk, "H", [128, 8, NCH], BF16)
        SAh = P.sb(wk, "SAh", [128, 8, NCH], BF16)
        SAl = P.sb(wk, "SAl", [128, 8, NCH], BF16)
        uo = uT[:, o, :].rr("p (k s) -> p s k", s=LCH)
        for g in range(8):
            bank = psb[1 + (g % 2)]
            for q0 in range(0, NCH, 512):
                qn = min(512, NCH - q0)
                for s_ in range(LCH):
                    P.mm(bank[:, 0:qn], Wb[:, g, s_, :], uo[:, s_, q0:q0 + qn], s_ == 0, s_ == LCH - 1)
                P.copy("act", SA[:, g, q0:q0 + qn], bank[:, 0:qn])
        fl = "p g k -> p (g k)"
        cb, sb_ = tc_.all().rr(fl), tsn.all().rr(fl)
        for g in range(8):
            for q0 in range(0, NCH, 512):
                qn = min(512, NCH - q0)
                bank = psb[3 + (g % 2)]
                P.copy("dve", SAh[:, g, q0:q0 + qn], SA[:, g, q0:q0 + qn])
                P.tt("dve", SAl[:, g, q0:q0 + qn], SA[:, g, q0:q0 + qn], SAh[:, g, q0:q0 + qn], ALU.subtract)
                P.mm(bank[:, 0:qn], pswapb.all(), SAh[:, g, q0:q0 + qn], True, False)
                P.mm(bank[:, 0:qn], pswapb.all(), SAl[:, g, q0:q0 + qn], False, True)
                sl = slice(q0, q0 + qn)
                A_, B_ = SA[:, g, sl], bank[:, 0:qn]
                cg, sgn_ = tc_[:, g, sl], tsn[:, g, sl]
                P.tt("dve", t1[:, g, sl], A_, cg, ALU.mult)
                P.tt("dve", t2[:, g, sl], B_, sgn_, ALU.mult)
                P.stt("dve", VA[:, g, sl], t2[:, g, sl], sg[:, 0:1], t1[:, g, sl], ALU.mult, ALU.add)
                P.tt("dve", t1[:, g, sl], B_, cg, ALU.mult)
                P.tt("dve", t2[:, g, sl], A_, sgn_, ALU.mult)
                P.stt("dve", VB[:, g, sl], t2[:, g, sl], nsg[:, 0:1], t1[:, g, sl], ALU.mult, ALU.add)
        for g in range(8):
            rb = rho[:, g:g + 1].pat(0, [(0, NCH)])
            P.op("dve", "tensor_tensor_scan", out=VA[:, g, :], data0=rb, data1=VA[:, g, :], initial=0.0, op0=ALU.mult, op1=ALU.add)
            P.op("dve", "tensor_tensor_scan", out=VB[:, g, :], data0=rb, data1=VB[:, g, :], initial=0.0, op0=ALU.mult, op1=ALU.add)
        P.tt("dve", t1.all().rr(fl), VA.all().rr(fl), cb, ALU.mult)
        P.tt("dve", t2.all().rr(fl), VB.all().rr(fl), sb_, ALU.mult)
        P.stt("dve", H.all().rr(fl), t2.all().rr(fl), nsg[:, 0:1], t1.all().rr(fl), ALU.mult, ALU.add)
        if getattr(c, "dump", None) and o == 0:
            c.dump("SA", SA.all().rr("p a b -> p (a b)"), F32)
            c.dump("H", H.all().rr("p a b -> p (a b)"), BF16)
        yo = uo
        for t in range(LCH - 1, -1, -1):
            bank = psb[5 + (t % 3)]
            for q0 in range(0, NCH, 512):
                qn = min(512, NCH - q0)
                for s_ in range(t + 1):
                    P.mm(bank[:, 0:qn], BD[:, t - s_, :], uo[:, s_, q0:q0 + qn], s_ == 0, False)
                for g in range(8):
                    lo = 1 if q0 == 0 else 0
                    P.mm(bank[:, lo:qn], Wc[:, g, t, :], H[:, g, q0 + lo - 1:q0 + qn - 1], False, g == 7)
                P.stt("dve", yo[:, t, q0:q0 + qn], uo[:, t, q0:q0 + qn], dsk[:, o:o + 1], bank[:, 0:qn], ALU.mult, ALU.add)
        wk.close()
        oc.close()
    if getattr(c, "dump", None):
        c.dump("ypre", y2.all().rr("p a b -> p (a b)"), BF16)
    gl = ExitStack()
    g1 = P.sb(gl, "g1", [128, S], F32)
    g2 = P.sb(gl, "g2", [128, S], F32)
    for o in range(4):
        gelu_inplace(P, y2[:, o, :], g1.all(), g2.all())
    gate = P.sb(gl, "gate", [128, 4, 512], BF16)
    for q0 in range(0, S, 512):
        for n in range(4):
            bank = psb[n]
            for k in range(4):
                P.mm(bank[:, 0:512], wglu[:, k, n * 128:(n + 1) * 128], y2[:, k, q0:q0 + 512], k == 0, k == 3)
            P.act(gate[:, n, :], bank[:, 0:512], AF.Sigmoid)
        P.tt("dve", ysT[:, :, q0:q0 + 512], gate.all(), y2[:, :, q0:q0 + 512], ALU.mult)
    gl.close()
    ph.close()


def gelu_inplace(P, x, t1, t2, eng="dve"):
    P.tt(eng, t1, x, x, ALU.mult)
    P.ts(eng, t1, t1, 0.044715 * 1.5957691216, ALU.mult, 1.5957691216, ALU.add)
    P.tt(eng, t1, t1, x, ALU.mult)
    P.act(t2, t1, AF.Sigmoid)
    P.tt(eng, x, x, t2, ALU.mult)


def attn_phase(P, c, S, x_d, w_in_d, g1c, ng1c, kTd, kiT4, vaug, ya_d, stop_at=None, dump=None):
    NT = S // 128
    TOPK = min(256, S // 4)
    psb, ident = c.psb, c.ident
    ph = ExitStack()
    rope_tables(P, ph, S, c)
    w_q = P.sb(ph, "w_q", [128, 8, 8, 128], BF16)
    w_qi = P.sb(ph, "w_qi", [128, 8, 6, 128], BF16)
    P.memset("pool", w_qi.all(), 0.0)
    w_wi = P.sb(ph, "w_wi", [128, 8, 8], BF16)
    stg = [P.sb(ph, f"astg{i}", [128, 512], F32) for i in range(2)]

    def cvt(dst, src, k, neg=False):
        P.act(dst, src, AF.Copy, scale=(ng1c if neg else g1c)[:, k:k + 1])

    def q_cvt(k, st):
        for m in range(4):
            cvt(w_q[:, k, m, :], st[:, m * 128:(m + 1) * 128], k)
            for e in range(2):
                base = m * 128 + e * 64
                cvt(w_q[:, k, 4 + m, e * 64:e * 64 + 32], st[:, base + 32:base + 64], k, neg=True)
                cvt(w_q[:, k, 4 + m, e * 64 + 32:e * 64 + 64], st[:, base:base + 32], k)
    load_w_cols(P, c, q_cvt, OFF_Q, 512, w_in_d, g1c, stg)

    def qi_cvt(k, st):
        for h in range(8):
            tl, pos = h // 3, h % 3
            cvt(w_qi[:, k, tl, pos * 32:(pos + 1) * 32], st[:, h * 32:(h + 1) * 32], k)
            cvt(w_qi[:, k, 3 + tl, pos * 32:pos * 32 + 16], st[:, h * 32 + 16:h * 32 + 32], k, neg=True)
            cvt(w_qi[:, k, 3 + tl, pos * 32 + 16:pos * 32 + 32], st[:, h * 32:h * 32 + 16], k)
    load_w_cols(P, c, qi_cvt, OFF_QI, 256, w_in_d, g1c, stg)
    load_w_cols(P, c, lambda k, st: cvt(w_wi[:, k, :], st[:, 0:8], k), OFF_WI, 8, w_in_d, g1c, stg)

    xt = [P.sb(ph, f"axt{i}", [128, D], F32) for i in range(2)]
    xh = P.sb(ph, "axh", [128, D], BF16)
    xhT = P.sb(ph, "axhT", [128, 8, 128], BF16)
    junk = P.sb(ph, "ajunk", [128, D], BF16)
    ss = P.sb(ph, "ass", [128, 1], F32)
    rstd = P.sb(ph, "arstd", [128, 1], F32)
    qT = P.sb(ph, "qT", [128, 4, 128], BF16)
    qiT = P.sb(ph, "qiT", [128, 3, 128], BF16)
    t1 = P.sb(ph, "at1", [128, 4, 128], F32)
    t2 = P.sb(ph, "at2", [128, 4, 128], F32)
    wsg = P.sb(ph, "wsg", [128, 8], F32)
    wsc = P.sb(ph, "wsc", [128, 8], F32)
    acc = P.sb(ph, "acc", [128, S], F32)
    msk = P.sb(ph, "msk", [128, S], BF16)
    mT = P.sb(ph, "mT", [128, NT, 128], BF16)
    rr = [P.sb(ph, f"rr{i}", [128, 512], F32) for i in range(2)]
    lo = P.sb(ph, "lo", [128, 1], F32)
    mid = P.sb(ph, "mid", [128, 1], F32)
    cnt = P.sb(ph, "cnt", [128, 1], F32)
    cntb = P.sb(ph, "cntb", [128, 1], F32)
    ge = P.sb(ph, "ge", [128, 1], F32)
    eT = [[P.sb(ph, f"eT{i}{n}", [128, 4, 128], BF16) for n in range(2)] for i in range(2)]
    pT = [[P.sb(ph, f"pT{i}{n}", [128, 4, 128], BF16) for n in range(2)] for i in range(2)]
    rden = P.sb(ph, "rden", [128, 512], F32)
    rdh = P.sb(ph, "rdh", [128, 512], BF16)
    rdl = P.sb(ph, "rdl", [128, 512], BF16)
    ones_bf = P.sb(ph, "ones_bf", [128, 64], BF16)
    P.memset("dve", ones_bf.all(), 1.0)
    bcs = P.sb(ph, "bcs", [64, 512], F32)
    ya = [P.sb(ph, f"ya{i}", [64, 8, 128], BF16) for i in range(2)]
    m4 = "p (m t) -> p m t"

    for b in range(NT):
        tok = slice(b * 128, (b + 1) * 128)
        Sc = (b + 1) * 128
        xb = xt[b % 2]
        P.dma("sp", xb.all(), x_d[tok, :])
        c.norm_and_transpose(b, xb, xh, xhT, junk, ss, rstd, psb[0])
        for m in range(8):
            bank = psb[1] if m < 4 else psb[2]
            for k in range(8):
                P.mm(bank[:, (m % 4) * 128:(m % 4 + 1) * 128], w_q[:, k, m, :], xhT[:, k, :], k == 0, k == 7)
        P.tt("dve", t1.all(), psb[1].all().rr(m4, m=4), c.rope["cosA"][:, tok].pat(0, [(0, 4), (1, 128)]), ALU.mult)
        P.tt("dve", t2.all(), psb[2].all().rr(m4, m=4), c.rope["sinA"][:, tok].pat(0, [(0, 4), (1, 128)]), ALU.mult)
        P.tt("dve", qT.all(), t1.all(), t2.all(), ALU.add)
        for m in range(6):
            bank = psb[3] if m < 3 else psb[6]
            for k in range(8):
                P.mm(bank[:, (m % 3) * 128:(m % 3 + 1) * 128], w_qi[:, k, m, :], xhT[:, k, :], k == 0, k == 7)
        P.tt("dve", t1[:, 0:3, :], psb[3][:, 0:384].rr(m4, m=3), c.rope["cosI"][:, tok].pat(0, [(0, 3), (1, 128)]), ALU.mult)
        P.tt("dve", t2[:, 0:3, :], psb[6][:, 0:384].rr(m4, m=3), c.rope["sinI"][:, tok].pat(0, [(0, 3), (1, 128)]), ALU.mult)
        P.tt("dve", qiT.all(), t1[:, 0:3, :], t2[:, 0:3, :], ALU.add)
        for k in range(8):
            P.mm(psb[7][:, 0:8], xhT[:, k, :], w_wi[:, k, :], k == 0, k == 7)
        P.ts("dve", wsg.all(), psb[7][:, 0:8], 0.0, ALU.is_gt, 2.0, ALU.mult)
        P.ts("dve", wsg.all(), wsg.all(), -1.0, ALU.add)
        P.tt("dve", wsc.all(), psb[7][:, 0:8], wsg.all(), ALU.mult)
        P.ts("dve", wsc.all(), wsc.all(), 1.0 / 16.0, ALU.mult)
        if stop_at == "proj":
            continue
        ibanks = [psb[1], psb[2], psb[3], psb[6]]
        ci = 0
        for q0 in range(0, Sc, 512):
            qn = min(512, Sc - q0)
            for h in range(8):
                bank, r = ibanks[h % 3], rr[ci % 2]
                ci += 1
                pb = 32 * (h % 3)
                P.mm(bank[:, 0:qn], qiT[pb:pb + 32, h // 3, :], kiT4[pb:pb + 32, q0:q0 + qn], True, True)
                P.act(r[:, 0:qn], bank[:, 0:qn], AF.Relu, scale=wsc[:, h:h + 1])
                if h == 0:
                    P.ts("dve", acc[:, q0:q0 + qn], r[:, 0:qn], wsg[:, 0:1], ALU.mult)
                else:
                    P.stt("dve", acc[:, q0:q0 + qn], r[:, 0:qn], wsg[:, h:h + 1], acc[:, q0:q0 + qn], ALU.mult, ALU.add)
        P.tt("dve", acc[:, b * 128:Sc], acc[:, b * 128:Sc], c.caus.all(), ALU.add)
        if stop_at == "idx":
            continue
        if Sc > TOPK:
            c1 = (int(Sc * 0.42) // 128) * 128 if Sc >= 1024 else Sc
            nB = Sc - c1
            half = 32.0
            P.memset("dve", mid.all(), 0.0)
            NSTEP = 22
            for it in range(NSTEP):
                P.op("dve", "tensor_scalar", out=msk[:, 0:c1], in0=acc[:, 0:c1], scalar1=mid[:, 0:1], scalar2=0.0,
                     op0=ALU.is_ge, op1=ALU.add, accum_out=cnt.all())
                if nB:
                    P.act(mT.all().rr("p j t -> p (j t)")[:, 0:nB], acc[:, c1:Sc], AF.Sign, bias=mid[:, 0:1], scale=-1.0,
                          accum_out=cntb.all())
                    P.stt("dve", cnt.all(), cntb.all(), -0.5, cnt.all(), ALU.mult, ALU.add)
                P.ts("dve", ge.all(), cnt.all(), TOPK - 0.5 - nB / 2.0, ALU.is_ge, half, ALU.mult)
                nxt = half / 2 if it < NSTEP - 1 else half
                P.stt("dve", mid.all(), ge.all(), -nxt, mid.all(), ALU.add, ALU.add)
                half = half / 2
            P.ts("dve", msk[:, 0:Sc], acc[:, 0:Sc], mid[:, 0:1], ALU.is_ge)
        else:
            P.ts("dve", msk[:, 0:Sc], acc[:, 0:Sc], -1.0e29, ALU.is_ge)
        if stop_at == "thr":
            continue
        pbf = psb[0].all().cast(BF16)
        for j0 in range(0, b + 1, 8):
            jn = min(8, b + 1 - j0)
            for jj in range(jn):
                P.tr(pbf[:, jj * 128:(jj + 1) * 128], msk[:, (j0 + jj) * 128:(j0 + jj + 1) * 128], ident.all())
            P.copy("act", mT[:, j0:j0 + jn, :].rr("p j t -> p (j t)"), pbf[:, 0:jn * 128])
        if stop_at == "mt":
            continue
        obank = [psb[4], psb[5]]
        dbank = [psb[7], psb[0]]
        for j in range(b + 1):
            ks = slice(j * 128, (j + 1) * 128)
            lb = [psb[1], psb[2]] if j % 2 == 0 else [psb[3], psb[6]]
            for h in range(8):
                n, m_, e = h // 4, h // 2, h % 2
                i = h // 2
                P.mm(lb[e][:, i * 128:(i + 1) * 128], kTd[64 * e:64 * e + 64, n, ks], qT[64 * e:64 * e + 64, m_, :], True, True)
            for e in range(2):
                et, pt = eT[j % 2][e], pT[j % 2][e]
                P.act(et.all().rr("p h t -> p (h t)"), lb[e].all(), AF.Exp, scale=0.125)
                P.tt("dve", pt.all(), et.all(), mT[:, j, :].pat(0, [(0, 4), (1, 128)]), ALU.mult)
            if stop_at != "lg":
                for e in range(2):
                    pt = pT[j % 2][e]
                    for n in range(2):
                        rhs = pt[:, 2 * n:2 * n + 2, :].rr("p h t -> p (h t)")
                        cs = slice(2 * e * 128, (2 * e + 2) * 128)
                        st_, sp_ = (j == 0 and e == 0), (j == b and e == 1)
                        P.mm(obank[n][0:64, cs], vaug[:, j, n, 0:64], rhs, st_, sp_, skip_group_check=True)
                        P.mm(dbank[n][0:64, cs], ones_bf.all(), rhs, st_, sp_, skip_group_check=True)
        if stop_at in ("pv", "lg"):
            continue
        yab = ya[b % 2]
        for n in range(2):
            P.op("dve", "reciprocal", out=bcs.all(), in_=dbank[n][0:64, :])
            for e in range(2):
                cs = slice(2 * e * 128, (2 * e + 2) * 128)
                P.tt("dve", yab[:, 4 * n + e:4 * n + e + 3:2, :], obank[n][0:64, cs].rr("p (i t) -> p i t", i=2),
                     bcs[:, cs].rr("p (i t) -> p i t", i=2), ALU.mult)
        P.dma("sp", ya_d[:, :, tok], yab.all())
    if dump is not None:
        dump("qT", qT.all().rr("p a b -> p (a b)"), BF16)
        dump("qiT", qiT.all().rr("p a b -> p (a b)"), BF16)
        dump("wsc", wsc.all(), F32)
        dump("wsg", wsg.all(), F32)
        if stop_at != "proj":
            dump("acc", acc.all(), F32)
        if stop_at not in ("proj", "idx"):
            dump("msk", msk.all(), BF16)
            dump("lo", mid.all(), F32)
        if stop_at == "lg":
            dump("pT", pT[(NT - 1) % 2][1].all().rr("p a b -> p (a b)"), BF16)
        if stop_at not in ("proj", "idx", "thr"):
            dump("mT", mT.all().rr("p a b -> p (a b)"), BF16)
        if stop_at == "pv":
            for n in range(2):
                P.copy("act", rden.all(), psb[4 + n].all())
                dump(f"oT{n}", rden.all(), F32)
    ph.close()


def merge_phase(P, c, S, x_d, w_in_d, g1c, ysT, ya_d, h_d, wd, uvb_d=None):
    NT = S // 128
    psb = c.psb
    ph = ExitStack()
    w_gs = P.sb(ph, "w_gs", [128, 8, 1024], BF16)
    w_ga = P.sb(ph, "w_ga", [128, 8, 1024], BF16)
    w_sup = P.sb(ph, "w_sup", [128, 4, 1024], BF16)
    w_aup = P.sb(ph, "w_aup", [64, 8, 1024], BF16)
    w_out = P.sb(ph, "w_out", [128, 8, 1024], BF16)
    stg = [P.sb(ph, f"bstg{i}", [128, 1024], F32) for i in range(2)]
    qs = ("sp", "act")

    def cvt(dst, src, k):
        P.act(dst, src, AF.Copy, scale=g1c[:, k:k + 1])
    load_w_cols(P, c, lambda k, st: cvt(w_gs[:, k, :], st[:, 0:1024], k), OFF_GS, 1024, w_in_d, g1c, stg)
    load_w_cols(P, c, lambda k, st: cvt(w_ga[:, k, :], st[:, 0:1024], k), OFF_GA, 1024, w_in_d, g1c, stg)
    for k in range(4):
        P.dma(qs[k % 2], stg[k % 2].all(), wd["w_ssm_up"][k * 128:(k + 1) * 128, :])
        P.copy("act", w_sup[:, k, :], stg[k % 2].all())
    for h in range(8):
        P.dma(qs[h % 2], stg[h % 2][0:64, :], wd["w_attn_up"][h * 64:(h + 1) * 64, :])
        P.copy("act", w_aup[:, h, :], stg[h % 2][0:64, :])
    for k in range(8):
        P.dma(qs[k % 2], stg[k % 2].all(), wd["w_out"][k * 128:(k + 1) * 128, :])
        P.copy("act", w_out[:, k, :], stg[k % 2].all())

    xt = [P.sb(ph, f"bxt{i}", [128, D], F32) for i in range(2)]
    xh = P.sb(ph, "bxh", [128, D], BF16)
    xhT = P.sb(ph, "bxhT", [128, 8, 128], BF16)
    junk = P.sb(ph, "bjunk", [128, D], BF16)
    ss = P.sb(ph, "bss", [128, 1], F32)
    rstd = P.sb(ph, "brstd", [128, 1], F32)
    yat = [P.sb(ph, f"yat{i}", [64, 8, 128], BF16) for i in range(2)]
    sgs = P.sb(ph, "sgs", [128, 512], F32)
    sga = P.sb(ph, "sga", [128, 512], F32)
    m1 = P.sb(ph, "m1", [128, 512], F32)
    m2 = P.sb(ph, "m2", [128, 512], F32)
    merged = P.sb(ph, "merged", [128, 8, 128], BF16)
    ht = [P.sb(ph, f"bht{i}", [128, D], F32) for i in range(2)]

    NCONV = 16384 // 128
    cvf = [P.sb(ph, f"cvf{i}", [128, 2048], F32) for i in range(2)]
    cvb = [P.sb(ph, f"cvb{i}", [128, 2048], BF16) for i in range(2)]

    def conv_step(i):
        rows = slice(i * 128, (i + 1) * 128)
        P.dma("pool", cvf[i % 2].all(), wd["peer_uv"][rows, :])
        P.copy("pool", cvb[i % 2].all(), cvf[i % 2].all())
        P.dma("pool", uvb_d[rows, :], cvb[i % 2].all())
    conv_per_tile = (NCONV + NT - 1) // NT
    conv_i = 0

    for b in range(NT):
        for _ in range(conv_per_tile):
            if uvb_d is not None and conv_i < NCONV:
                conv_step(conv_i)
                conv_i += 1
        tok = slice(b * 128, (b + 1) * 128)
        xb = xt[b % 2]
        P.dma("sp", xb.all(), x_d[tok, :])
        ya = yat[b % 2]
        P.dma("sp", ya.all(), ya_d[:, :, tok])
        c.norm_and_transpose(b, xb, xh, xhT, junk, ss, rstd, psb[0])
        for half in range(2):
            for cc in range(4):
                ci = half * 4 + cc
                cols = slice(ci * 128, (ci + 1) * 128)
                reg = slice(cc * 128, (cc + 1) * 128)
                for k in range(8):
                    P.mm(psb[1][:, reg], w_gs[:, k, cols], xhT[:, k, :], k == 0, k == 7)
                for k in range(8):
                    P.mm(psb[2][:, reg], w_ga[:, k, cols], xhT[:, k, :], k == 0, k == 7)
                for k in range(4):
                    P.mm(psb[3][:, reg], w_sup[:, k, cols], ysT[:, k, tok], k == 0, k == 3)
                for h in range(8):
                    P.mm(psb[4][:, reg], w_aup[:, h, cols], ya[:, h, :], h == 0, h == 7)
            P.act(sgs.all(), psb[1].all(), AF.Sigmoid)
            P.act(sga.all(), psb[2].all(), AF.Sigmoid)
            P.tt("dve", m1.all(), sgs.all(), psb[3].all(), ALU.mult)
            P.tt("dve", m2.all(), sga.all(), psb[4].all(), ALU.mult)
            P.tt("dve", merged[:, half * 4:half * 4 + 4, :].rr("p c t -> p (c t)"), m1.all(), m2.all(), ALU.add)
        hb = ht[b % 2]
        for n2 in range(2):
            cs = slice(n2 * 512, (n2 + 1) * 512)
            for k in range(8):
                P.mm(psb[5 + n2].all(), merged[:, k, :], w_out[:, k, cs], k == 0, k == 7)
            P.tt("dve", hb[:, cs], psb[5 + n2].all(), xb[:, cs], ALU.add)
        P.dma("sp", h_d[tok, :], hb.all())
    ph.close()


def top16(P, src, vals_out, idx_out, tl, par):
    m8a, i8a, m8b, i8b = tl["m8a"][par], tl["i8a"][par], tl["m8b"][par], tl["i8b"][par]
    n = src.shape[-1]
    sc2 = tl["sc2"][par][:, 0:n]
    P.op("dve", "max", out=m8a.all(), in_=src)
    P.op("dve", "max_index", out=i8a.all(), in_max=m8a.all(), in_values=src)
    P.op("dve", "match_replace", out=sc2, in_to_replace=m8a.all(), in_values=src, imm_value=-1.0e30)
    P.op("dve", "max", out=m8b.all(), in_=sc2)
    P.op("dve", "max_index", out=i8b.all(), in_max=m8b.all(), in_values=sc2)
    P.copy("act", vals_out[:, 0:8], m8a.all())
    P.copy("act", vals_out[:, 8:16], m8b.all())
    P.copy("dve", idx_out[:, 0:8], i8a.all().cast(I32))
    P.copy("dve", idx_out[:, 8:16], i8b.all().cast(I32))


def peer_phase(P, c, S, h_d, out_d, pd, uvb_d, NB=16, dump=None):
    NT = S // 128
    psb, ident = c.psb, c.ident
    ph = ExitStack()
    wq = P.sb(ph, "wq", [128, 8, 2048], BF16)
    pk = P.sb(ph, "pk", [128, 16, 128], BF16)
    g2c = P.sb(ph, "g2c", [128, 8], F32)
    g2b = P.sb(ph, "g2b", [128, D], F32)
    gfb = P.sb(ph, "gfb", [128, D], F32)
    P.dma("sp", g2c.all(), pd["g2c"].all())
    P.dma("sp", g2b.all(), pd["g2b"].all())
    P.dma("sp", gfb.all(), pd["gfb"].all())
    hts = [P.sb(ph, f"cht{i}", [128, D], F32) for i in range(2)]
    hh = P.sb(ph, "chh", [128, D], BF16)
    hT = P.sb(ph, "chT", [128, 8, 128], BF16)
    junk = P.sb(ph, "cjunk", [128, D], BF16)
    hn = P.sb(ph, "chn", [128, D], F32)
    ss = P.sb(ph, "css", [128, 1], F32)
    rstd = P.sb(ph, "crstd", [128, 1], F32)
    qT = P.sb(ph, "cqT", [128, 16, 128], BF16)
    SC = P.sb(ph, "SC", [128, 16, 128], F32)
    V16 = P.sb(ph, "V16", [128, 16, 16], F32)
    I16 = P.sb(ph, "I16", [128, 16, 16], F32)
    CS = P.sb(ph, "CS", [128, 8, 256], F32)
    TS = P.sb(ph, "TS", [128, 8, 16], F32)
    POSf = P.sb(ph, "POSf", [128, 8, 16], F32)
    POSi = P.sb(ph, "POSi", [128, 8, 16], I32)
    rowi = P.sb(ph, "rowi", [128, 8, 16], I32)
    coli = P.sb(ph, "coli", [128, 8, 16], I32)
    rowf = P.sb(ph, "rowf", [128, 8, 16], F32)
    colf = P.sb(ph, "colf", [128, 8, 16], F32)
    E = P.sb(ph, "E", [128, 8, 16], F32)
    G = P.sb(ph, "G", [128, 8, 16], F32)
    sm = P.sb(ph, "sm", [128, 8], F32)
    OH = P.sb(ph, "OH", [128, 8, 16, 16], F32)
    OH2 = P.sb(ph, "OH2", [128, 8, 16, 16], F32)
    i1s = P.sb(ph, "i1s", [128, 8, 16], F32)
    i2s = P.sb(ph, "i2s", [128, 8, 16], F32)
    eidf = P.sb(ph, "eidf", [128, 128], F32)
    EID = P.sb(ph, "EID", [128, 128], I32)
    dots = P.sb(ph, "dots", [128, 128], F32)
    actw = P.sb(ph, "actw", [128, 128], F32)
    resd = P.sb(ph, "cres", [128, D], F32)
    outt = [P.sb(ph, f"cout{i}", [128, D], F32) for i in range(2)]
    tl = {k: [P.sb(ph, f"{k}{i}", [128, 8], dt) for i in range(2)]
          for k, dt in (("m8a", F32), ("i8a", U32), ("m8b", F32), ("i8b", U32))}
    tl["sc2"] = [P.sb(ph, f"sc2{i}", [128, 256], F32) for i in range(2)]
    wst = ExitStack()
    stg = [P.sb(wst, f"cstg{i}", [128, 2048], F32) for i in range(2)]
    qs = ("sp", "act")
    for k in range(8):
        P.dma(qs[k % 2], stg[k % 2].all(), pd["peer_wq"][k * 128:(k + 1) * 128, :])
        P.act(wq[:, k, :], stg[k % 2].all(), AF.Copy, scale=g2c[:, k:k + 1])
    P.dma("sp", stg[0].all(), pd["pk"][0:128].rr("p a n -> p (a n)"))
    P.copy("act", pk.all().rr("p a n -> p (a n)"), stg[0].all())
    wst.close()
    UV = [P.sb(ph, f"UV{i}", [128, 2048], BF16) for i in range(NB)]
    dg = [P.sb(ph, f"dg{i}", [128, 128], BF16) for i in range(4)]
    hnb = P.sb(ph, "hnb", [128, D], BF16)
    junkb = P.sb(ph, "cjunkb", [128, D], F32)
    tuv = uvb_d.all()

    EIDs = [EID, P.sb(ph, "EID1", [128, 128], I32)]
    Gs = [G, P.sb(ph, "G1", [128, 8, 16], F32)]
    hnbs = [hnb, P.sb(ph, "hnb1", [128, D], BF16)]
    ssf = P.sb(ph, "cssf", [128, 1], F32)
    rstdf = P.sb(ph, "crstdf", [128, 1], F32)
    pacc = [psb[6], psb[7]]

    def front(b):
        tok = slice(b * 128, (b + 1) * 128)
        ht, EIDb, Gb, hnbb = hts[b % 2], EIDs[b % 2], Gs[b % 2], hnbs[b % 2]
        P.dma("sp", ht.all(), h_d[tok, :])
        P.act(junk.all(), ht.all(), AF.Square, accum_out=ss.all())
        P.ts("dve", ss.all(), ss.all(), 1.0 / D, ALU.mult, EPS, ALU.add)
        P.act(ss.all(), ss.all(), AF.Sqrt)
        P.op("dve", "reciprocal", out=rstd.all(), in_=ss.all())
        P.ts("dve", hh.all(), ht.all(), rstd[:, 0:1], ALU.mult)
        yield
        P.stt("dve", hn.all(), ht.all(), rstd[:, 0:1], g2b.all(), ALU.mult, ALU.mult)
        P.copy("act", hnbb.all(), hn.all())
        pbf = psb[0].all().cast(BF16)
        for k in range(8):
            P.tr(pbf[:, k * 128:(k + 1) * 128], hh[:, k * 128:(k + 1) * 128], ident.all())
        P.copy("act", hT.all().rr("p k t -> p (k t)"), pbf)
        yield
        for ch in range(16):
            bank = psb[1 + (ch // 4) % 2]
            for k in range(8):
                P.mm(bank[:, (ch % 4) * 128:(ch % 4 + 1) * 128], wq[:, k, ch * 128:(ch + 1) * 128], hT[:, k, :], k == 0, k == 7)
            if ch % 4 == 3:
                P.copy("act", qT[:, ch - 3:ch + 1, :].rr("p a t -> p (a t)"), bank.all())
                yield
        sbanks = [psb[3], psb[4], psb[5], psb[0]]
        for ch in range(16):
            bank = sbanks[ch // 4]
            P.mm(bank[:, (ch % 4) * 128:(ch % 4 + 1) * 128], qT[:, ch, :], pk[:, ch, :], True, True)
            if ch % 4 == 3:
                P.copy("act", SC[:, ch - 3:ch + 1, :].rr("p a n -> p (a n)"), bank.all())
        yield
        for ch in range(16):
            top16(P, SC[:, ch, :], V16[:, ch, :], I16[:, ch, :], tl, ch % 2)
            if ch % 2 == 1:
                yield
        P.tt("dve", CS.all().rr("p h (i j) -> p h i j", i=16), V16.all().pat(0, [(32, 8), (1, 16), (0, 16)]),
             V16.all().pat(16, [(32, 8), (0, 16), (1, 16)]), ALU.add)
        for h in range(8):
            top16(P, CS[:, h, :], TS[:, h, :], POSf[:, h, :], tl, h % 2)
            if h % 2 == 1:
                yield
        P.tt("dve", E.all(), TS.all(), TS.all().pat(0, [(16, 8), (0, 16)]), ALU.subtract)
        P.act(E.all(), E.all(), AF.Exp)
        P.op("dve", "tensor_reduce", out=sm.all(), in_=E.all(), axis=AX.X, op=ALU.add)
        P.op("dve", "reciprocal", out=sm.all(), in_=sm.all())
        P.tt("dve", Gb.all(), E.all(), sm.all().pat(0, [(1, 8), (0, 16)]), ALU.mult)
        yield
        P.copy("dve", POSi.all(), POSf.all())
        P.op("dve", "tensor_single_scalar", out=rowi.all(), in_=POSi.all(), scalar=4, op=ALU.arith_shift_right)
        P.op("dve", "tensor_single_scalar", out=coli.all(), in_=POSi.all(), scalar=15, op=ALU.bitwise_and)
        P.copy("dve", rowf.all(), rowi.all())
        P.copy("dve", colf.all(), coli.all())
        io16 = c.iota_f[:, 0:16].pat(0, [(0, 8), (0, 16), (1, 16)])
        for src, off, dst, oh in ((rowf, 0, i1s, OH), (colf, 16, i2s, OH2)):
            P.tt("dve", oh.all(), src.all().pat(0, [(16, 8), (1, 16), (0, 16)]), io16, ALU.is_equal)
            P.tt("dve", oh.all(), oh.all(), I16.all().pat(off, [(32, 8), (0, 16), (1, 16)]), ALU.mult)
            P.op("dve", "tensor_reduce", out=dst.all().rr("p h k -> p (h k)"), in_=oh.all().rr("p h k i -> p (h k) i"),
                 axis=AX.X, op=ALU.add)
            yield
        P.stt("dve", eidf.all(), i1s.all().rr("p h k -> p (h k)"), 128.0, i2s.all().rr("p h k -> p (h k)"), ALU.mult, ALU.add)
        P.copy("dve", EIDb.all(), eidf.all())

    def advance(gen, n):
        if gen is None:
            return None
        try:
            for _ in range(n):
                next(gen)
        except StopIteration:
            return None
        return gen

    GS = NB // 2
    NGRP = 128 // GS
    advance(front(0), 1000)
    for b in range(NT):
        tok = slice(b * 128, (b + 1) * 128)
        ht, EIDb, hnbb = hts[b % 2], EIDs[b % 2], hnbs[b % 2]
        Gf = Gs[b % 2].all().rr("p a b -> p (a b)")
        nxt = front(b + 1) if b + 1 < NT else None
        for g in range(NGRP):
            gs = slice(g * GS, (g + 1) * GS)
            for e in range(g * GS, (g + 1) * GS):
                uv = UV[e % NB]
                P.gather(uv.all(), tuv, EIDb[:, e:e + 1])
                P.stt("dve", junkb.all(), uv[:, 0:D], 1.0, hnbb.all(), ALU.mult, ALU.mult, accum_out=dots[:, e:e + 1])
            P.act(actw[:, gs], dots[:, gs], AF.Gelu_apprx_tanh)
            P.tt("dve", actw[:, gs], actw[:, gs], Gf[:, gs], ALU.mult)
            for e in range(g * GS, (g + 1) * GS):
                uv, dgt = UV[e % NB], dg[e % 4]
                P.act(dgt.all(), ident.all(), AF.Copy, scale=actw[:, e:e + 1])
                for n2 in range(2):
                    P.mm(pacc[n2].all(), dgt.all(), uv[:, D + n2 * 512:D + (n2 + 1) * 512], e == 0, e == 127)
            nxt = advance(nxt, 2)
        advance(nxt, 1000)
        for n2 in range(2):
            P.tt("dve", resd[:, n2 * 512:(n2 + 1) * 512], pacc[n2].all(), ht[:, n2 * 512:(n2 + 1) * 512], ALU.add)
        P.act(junk.all(), resd.all(), AF.Square, accum_out=ssf.all())
        P.ts("dve", ssf.all(), ssf.all(), 1.0 / D, ALU.mult, EPS, ALU.add)
        P.act(ssf.all(), ssf.all(), AF.Sqrt)
        P.op("dve", "reciprocal", out=rstdf.all(), in_=ssf.all())
        ot = outt[b % 2]
        P.stt("dve", ot.all(), resd.all(), rstdf[:, 0:1], gfb.all(), ALU.mult, ALU.mult)
        P.dma("sp", out_d[tok, :], ot.all())
    ph.close()


def late_param_shapes():
    return {"w_ssm_up": [513, 1024], "w_attn_up": [513, 1024], "w_out": [1025, 1024], "g2c": [128, 8],
            "g2b": [128, 1024], "gfb": [128, 1024], "peer_wq": [1025, 2048], "pk": [129, 16, 128],
            "peer_uv": [16385, 2048]}


def late_host_layout(inp):
    g2 = np.asarray(inp["norm2_g"], dtype=np.float32)[0]
    gf = np.asarray(inp["norm_f_g"], dtype=np.float32)
    k1, k2 = np.asarray(inp["peer_k1"])[0], np.asarray(inp["peer_k2"])[0]
    pk = np.stack([k1, k2], 1).reshape(16, 128, 128).transpose(2, 0, 1)
    d = {"w_ssm_up": np.asarray(inp["w_ssm_up"])[0], "w_attn_up": np.asarray(inp["w_attn_up"])[0],
         "w_out": np.asarray(inp["w_out"])[0], "g2c": g2.reshape(8, 128).T,
         "g2b": np.broadcast_to(g2[None, :], (128, 1024)), "gfb": np.broadcast_to(gf[None, :], (128, 1024)),
         "peer_wq": np.asarray(inp["peer_wq"])[0], "pk": pk,
         "peer_uv": np.concatenate([np.asarray(inp["peer_u"])[0], np.asarray(inp["peer_v"])[0]], 1)}
    return {k: np.ascontiguousarray(v, dtype=np.float32) for k, v in d.items()}


def ssm_param_shapes():
    return {"ar_sm": [128, 32], "ai_sm": [128, 32], "ldt_sm": [128, 32],
            "bU1": [128, 32, 16], "bU2": [128, 32, 16], "cT1": [128, 32, 16], "cT2": [128, 32, 16],
            "ar_cm": [128, 4, 64], "ai_cm": [128, 4, 64], "ldt_cm": [128, 4, 64],
            "br_cm": [128, 4, 64], "bi_cm": [128, 4, 64], "dskip": [128, 4], "w_glu": [513, 512]}


def ssm_host_layout(inp):
    a_re, a_im, log_dt = inp["a_re"][0], inp["a_im"][0], inp["log_dt"][0]
    b_re, b_im, c_re, c_im = inp["b_re"][0], inp["b_im"][0], inp["c_re"][0], inp["c_im"][0]
    d = {}
    d["ar_sm"] = np.concatenate([a_re.T, a_re.T], 0)
    d["ai_sm"] = np.concatenate([a_im.T, a_im.T], 0)
    d["ldt_sm"] = np.broadcast_to(log_dt[None, :], (128, 32))
    brT, biT = b_re.transpose(1, 0, 2), b_im.transpose(1, 0, 2)
    d["bU1"] = np.concatenate([brT, biT], 0)
    d["bU2"] = np.concatenate([biT, brT], 0)
    crT, ciT = c_re.transpose(2, 0, 1), c_im.transpose(2, 0, 1)
    d["cT1"] = np.concatenate([crT, ciT], 0)
    d["cT2"] = np.concatenate([ciT, crT], 0)
    q = np.arange(128)
    gq = (np.arange(4)[None, :] * 8 + (q // 16)[:, None])
    d["ar_cm"] = a_re[gq]
    d["ai_cm"] = a_im[gq]
    d["ldt_cm"] = np.broadcast_to(log_dt[gq][:, :, None], (128, 4, 64))
    d["br_cm"] = b_re[gq, :, (q % 16)[:, None]]
    d["bi_cm"] = b_im[gq, :, (q % 16)[:, None]]
    d["dskip"] = inp["d_skip"][0].reshape(4, 128).T
    d["w_glu"] = inp["w_glu"][0]
    return {k: np.ascontiguousarray(v, dtype=np.float32) for k, v in d.items()}


def build(nc, S, stage_stop=None, dbg=None):
    NT = S // 128
    NCH = S // LCH
    TOPK = min(256, S // 4)
    es = ExitStack()
    P = Prog(nc, es)
    c = Ctx()
    c.P = P
    dbg = dbg if dbg is not None else {}

    x_d = P.dram("x", [S, D], F32, "ExternalInput")
    g1c_d = P.dram("g1c", [128, 8], F32, "ExternalInput")
    w_in_d = P.dram("w_in", [D + 1, IN_W], F32, "ExternalInput")
    out_d = P.dram("out", [S, D], F32, "ExternalOutput")

    def dbg_out(name, shape, dt=F32):
        t = P.dram("dbg_" + name, shape, dt, "ExternalOutput")
        dbg[name] = t
        return t

    blk = es.enter_context(nc.Block())
    holder = {}

    def body(_sync):
        glob = ExitStack()
        ident = P.sb(glob, "ident", [128, 128], BF16)
        identf = P.sb(glob, "identf", [128, 128], F32)
        iota_i = P.sb(glob, "iota_i", [128, 128], I32)
        pid_i = P.sb(glob, "pid_i", [128, 1], I32)
        pid_f = P.sb(glob, "pid_f", [128, 1], F32)
        iota_f = P.sb(glob, "iota_f", [128, 128], F32)
        P.op("pool", "iota", out=iota_i.all(), pattern=[[1, 128]], base=0, channel_multiplier=0)
        P.op("pool", "iota", out=pid_i.all(), pattern=[[0, 1]], base=0, channel_multiplier=1)
        P.copy("dve", iota_f.all(), iota_i.all())
        P.copy("dve", pid_f.all(), pid_i.all())
        P.ts("dve", identf.all(), iota_f.all(), pid_f[:, 0:1], ALU.is_equal)
        P.copy("dve", ident.all(), identf.all())
        caus = P.sb(glob, "caus", [128, 128], F32)
        P.ts("dve", caus.all(), iota_f.all(), pid_f[:, 0:1], ALU.is_gt, NEG, ALU.mult)
        g1c = P.sb(glob, "g1c", [128, 8], F32)
        ng1c = P.sb(glob, "ng1c", [128, 8], F32)
        P.dma("sp", g1c.all(), g1c_d.all())
        P.ts("dve", ng1c.all(), g1c.all(), -1.0, ALU.mult)
        c.ident, c.identf, c.iota_f, c.pid_f, c.caus = ident, identf, iota_f, pid_f, caus

        psb = [P.ps(glob, f"psb{i}", [128, 512], F32) for i in range(8)]
        c.psb = psb

        res = ExitStack()
        uT = P.sb(res, "uys", [128, 4, S], BF16)
        res_a = ExitStack()
        res_a_close = res_a.close
        kTd = P.sb(res_a, "kTd", [128, 2, S], BF16)
        kiT4 = P.sb(res_a, "kiT4", [128, S], BF16)
        vaug = P.sb(res_a, "vaug", [128, NT, 2, 80], BF16)
        P.memset("pool", vaug.all(), 1.0)

        s1 = ExitStack()
        rope_tables(P, s1, S, c)
        stg = [P.sb(s1, f"stg{i}", [128, 1024], F32) for i in range(2)]
        w_u = P.sb(s1, "w_u", [128, 8, 512], BF16)
        w_kd = P.sb(s1, "w_kd", [128, 8, 4, 128], BF16)
        w_ki = P.sb(s1, "w_ki", [128, 8, 2, 128], BF16)
        w_v = P.sb(s1, "w_v", [128, 8, 128], BF16)

        def cvt(dst, src, k, neg=False):
            P.act(dst, src, AF.Copy, scale=(ng1c if neg else g1c)[:, k:k + 1])

        load_w_cols(P, c, lambda k, st: cvt(w_u[:, k, :], st[:, 0:512], k), OFF_U, 512, w_in_d, g1c, stg)

        def k_cvt(k, st):
            for n in range(2):
                for dup in range(2):
                    cvt(w_kd[:, k, n, dup * 64:(dup + 1) * 64], st[:, n * 64:(n + 1) * 64], k)
                    cvt(w_kd[:, k, 2 + n, dup * 64:dup * 64 + 32], st[:, n * 64 + 32:n * 64 + 64], k, neg=True)
                    cvt(w_kd[:, k, 2 + n, dup * 64 + 32:dup * 64 + 64], st[:, n * 64:n * 64 + 32], k)
        load_w_cols(P, c, k_cvt, OFF_K, 128, w_in_d, g1c, stg)

        def ki_cvt(k, st):
            for r in range(4):
                cvt(w_ki[:, k, 0, r * 32:(r + 1) * 32], st[:, 0:32], k)
                cvt(w_ki[:, k, 1, r * 32:r * 32 + 16], st[:, 16:32], k, neg=True)
                cvt(w_ki[:, k, 1, r * 32 + 16:r * 32 + 32], st[:, 0:16], k)
        load_w_cols(P, c, ki_cvt, OFF_KI, 32, w_in_d, g1c, stg)
        load_w_cols(P, c, lambda k, st: cvt(w_v[:, k, :], st[:, 0:128], k), OFF_V, 128, w_in_d, g1c, stg)

        xt = [P.sb(s1, f"xt{i}", [128, D], F32) for i in range(2)]
        xh = P.sb(s1, "xh", [128, D], BF16)
        xhT = P.sb(s1, "xhT", [128, 8, 128], BF16)
        junk = P.sb(s1, "junk", [128, D], BF16)
        ss = P.sb(s1, "ss", [128, 1], F32)
        rstd = P.sb(s1, "rstd", [128, 1], F32)
        r1 = P.sb(s1, "r1", [128, 128], F32)
        r2 = P.sb(s1, "r2", [128, 128], F32)

        def norm_and_transpose(b, xt_b, xh, xhT, junk, ss, rstd, psT):
            P.act(junk.all(), xt_b.all(), AF.Square, accum_out=ss.all())
            P.act(ss.all(), ss.all(), AF.Sqrt, scale=1.0 / D, bias=EPS) if False else None
            P.ts("dve", ss.all(), ss.all(), 1.0 / D, ALU.mult, EPS, ALU.add)
            P.act(ss.all(), ss.all(), AF.Sqrt)
            P.op("dve", "reciprocal", out=rstd.all(), in_=ss.all())
            P.ts("dve", xh.all(), xt_b.all(), rstd[:, 0:1], ALU.mult)
            pb = psT.all().cast(BF16)
            for k in range(8):
                P.tr(pb[:, k * 128:(k + 1) * 128], xh[:, k * 128:(k + 1) * 128], ident.all())
            P.copy("act", xhT.all().rr("p k t -> p (k t)"), pb)
        c.norm_and_transpose = norm_and_transpose

        for b in range(NT):
            xb = xt[b % 2]
            P.dma("sp", xb.all(), x_d[b * 128:(b + 1) * 128, :])
            norm_and_transpose(b, xb, xh, xhT, junk, ss, rstd, psb[0])
            tok = slice(b * 128, (b + 1) * 128)
            for m in range(4):
                for k in range(8):
                    P.mm(psb[1][:, m * 128:(m + 1) * 128], w_u[:, k, m * 128:(m + 1) * 128], xhT[:, k, :], k == 0, k == 7)
            P.copy("act", uT[:, :, tok], psb[1].all().rr("p (m t) -> p m t", m=4))
            for m in range(4):
                for k in range(8):
                    P.mm(psb[2][:, m * 128:(m + 1) * 128], w_kd[:, k, m, :], xhT[:, k, :], k == 0, k == 7)
            for n in range(2):
                P.tt("dve", r1.all(), psb[2][:, n * 128:(n + 1) * 128], c.rope["cosA"][:, tok], ALU.mult)
                P.tt("dve", r2.all(), psb[2][:, (2 + n) * 128:(3 + n) * 128], c.rope["sinA"][:, tok], ALU.mult)
                P.tt("dve", kTd[:, n, tok], r1.all(), r2.all(), ALU.add)
            for m in range(2):
                for k in range(8):
                    P.mm(psb[3][:, m * 128:(m + 1) * 128], w_ki[:, k, m, :], xhT[:, k, :], k == 0, k == 7)
            for k in range(8):
                P.mm(psb[3][:, 256:384], xhT[:, k, :], w_v[:, k, :], k == 0, k == 7)
            P.tt("dve", r1.all(), psb[3][:, 0:128], c.rope["cosI"][:, tok], ALU.mult)
            P.tt("dve", r2.all(), psb[3][:, 128:256], c.rope["sinI"][:, tok], ALU.mult)
            P.tt("dve", kiT4[:, tok], r1.all(), r2.all(), ALU.add)
            P.copy("act", vaug[:, b, :, 0:64], psb[3][:, 256:384].rr("p (n d) -> p n d", n=2))
        if stage_stop == "s1":
            for nm in ("cosA", "sinA", "cosI", "sinI"):
                t = dbg_out(nm, [128, S], BF16)
                P.dma("sp", t.all(), c.rope[nm].all())
        s1.close()

        if stage_stop == "s1":
            t = dbg_out("uT", [128, 4 * S], BF16)
            P.dma("sp", t.all(), uT.all().rr("p m t -> p (m t)"))
            t = dbg_out("kTd", [128, 2 * S], BF16)
            P.dma("sp", t.all(), kTd.all().rr("p m t -> p (m t)"))
            t = dbg_out("kiT4", [128, S], BF16)
            P.dma("sp", t.all(), kiT4.all())
            t = dbg_out("vaug", [128, NT * 160], BF16)
            P.dma("sp", t.all(), vaug.all().rr("p a n d -> p (a n d)"))
            P.finish(list(dbg.values()))
            res_a.close()
            res.close()
            glob.close()
            return

        sp = {nm: P.dram(nm, shp, F32, "ExternalInput") for nm, shp in ssm_param_shapes().items()}
        if stage_stop == "s2":
            def dump(name, view, dt):
                t = dbg_out(name, list(view.shape), dt)
                P.dma("sp", t.all(), view)
            c.dump = dump
        ssm_phase(P, c, S, uT, sp)
        if stage_stop == "s2":
            t = dbg_out("ysT", [128, 4 * S], BF16)
            P.dma("sp", t.all(), uT.all().rr("p m t -> p (m t)"))
            P.finish(list(dbg.values()))
            res_a.close()
            res.close()
            glob.close()
            return
        ya_d = P.dram("ya_scr", [64, 8, S], BF16, "ExternalOutput" if stage_stop == "a" else "Internal")
        astop = stage_stop[2:] if (stage_stop or "").startswith("a:") else None

        def adump(name, view, dt):
            t = dbg_out(name, list(view.shape), dt)
            P.dma("sp", t.all(), view)
        attn_phase(P, c, S, x_d, w_in_d, g1c, ng1c, kTd, kiT4, vaug, ya_d, stop_at=astop, dump=adump if astop else None)
        res_a.close()
        if astop:
            P.finish(list(dbg.values()))
            res.close()
            glob.close()
            return
        if stage_stop == "a":
            dbg["ya_scr"] = ya_d
            P.finish([ya_d])
            res.close()
            glob.close()
            return
        pd = {nm: P.dram(nm, shp, F32, "ExternalInput") for nm, shp in late_param_shapes().items()}
        h_d = P.dram("h_scr", [S, D], F32, "ExternalOutput" if stage_stop == "b" else "Internal")
        uvb_d = P.dram("uvb_scr", [16384, 2048], BF16, "ExternalOutput" if stage_stop == "cdbg" else "Internal")
        merge_phase(P, c, S, x_d, w_in_d, g1c, uT, ya_d, h_d, pd, uvb_d)
        res.close()
        if stage_stop == "b":
            dbg["h_scr"] = h_d
            P.finish([h_d])
            glob.close()
            return
        def cdump(name, view, dt):
            t = dbg_out(name, list(view.shape), dt)
            P.dma("sp", t.all(), view)
        peer_phase(P, c, S, h_d, out_d, pd, uvb_d, dump=cdump if stage_stop == "cdbg" else None)
        P.finish([out_d] + list(dbg.values()) + ([uvb_d] if stage_stop == "cdbg" else []))
        glob.close()

    holder["rest"] = lambda P, c, env: None
    blk.sync(body)
    es.close()
    return P, dbg


PADDED = ("w_in", "w_glu", "w_ssm_up", "w_attn_up", "w_out", "peer_wq", "pk", "peer_uv")


def core_inputs(shared, xb):
    im = dict(shared)
    im["x"] = np.ascontiguousarray(xb, dtype=np.float32)
    flat = im["x"].reshape(-1)
    for nm in PADDED:
        a = shared[nm]
        row = flat[:a[0].size].reshape((1,) + a.shape[1:])
        im[nm] = np.concatenate([a, row], 0)
    return im


def kernel(**inputs):
    inputs = {k: np.asarray(v) for k, v in inputs.items()}
    B, S, _ = inputs["x"].shape
    assert B == NCORES
    g1 = inputs["norm1_g"].astype(np.float32)[0]
    shared = {"g1c": np.ascontiguousarray(g1.reshape(8, 128).T),
              "w_in": np.ascontiguousarray(inputs["w_in"].astype(np.float32)[0])}
    shared.update(ssm_host_layout(inputs))
    shared.update(late_host_layout(inputs))
    nc = bass.Bass("TRN2", target_bir_lowering=False)
    build(nc, S)
    x = inputs["x"].astype(np.float32)
    in_maps = [core_inputs(shared, x[b]) for b in range(NCORES)]
    res = run_bass_kernel_spmd(nc, in_maps, core_ids=list(range(NCORES)))
    out = np.stack([np.asarray(res.results[b]["out"], dtype=np.float32) for b in range(NCORES)], 0)
    return out
```

```python
import math
from contextlib import ExitStack

import numpy as np
import concourse.bass as bass
import concourse.mybir as mybir
from concourse.bass_utils import run_bass_kernel_spmd

F32 = mybir.dt.float32
BF16 = mybir.dt.bfloat16
I32 = mybir.dt.int32
U32 = mybir.dt.uint32
ALU = mybir.AluOpType
AF = mybir.ActivationFunctionType
AX = mybir.AxisListType

D = 1024
NCORES = 8
SSM_W = 512
NG = 32
NP_ = 64
LCH = 16
EPS = 1e-6
NEG = -1.0e30
STRICT = True
DUMP2 = False
OUTK = ("out", "accum_out", "out_max", "out_indices")


class V:
    __slots__ = ("t", "ap")

    def __init__(self, t, ap):
        self.t = t
        self.ap = ap

    def __getitem__(self, k):
        return V(self.t, self.ap[k])

    def rr(self, pat, **kw):
        return V(self.t, self.ap.rearrange(pat, **kw))

    def bc(self, shape):
        return V(self.t, self.ap.to_broadcast(list(shape)))

    def cast(self, dt):
        return V(self.t, self.ap.bitcast(dt))

    def pat(self, off, pattern):
        a = self.ap
        return V(self.t, bass.AP(a.tensor, a.offset + off, [list(a.ap[0])] + [list(p) for p in pattern]))

    @property
    def shape(self):
        return self.ap.shape


class Tl:
    def __init__(self, base_ap, name, dram=False):
        self.base = base_ap
        self.name = name
        self.dram = dram
        self.w = None
        self.r = {}
        self.dsem = None

    def __getitem__(self, k):
        return V(self, self.base[k])

    def all(self):
        return V(self, self.base)


class Prog:
    EPOCH = 14000

    def __init__(self, nc, es):
        self.nc = nc
        self.es = es
        self.eng = {"pe": nc.tensor, "dve": nc.vector, "act": nc.scalar, "pool": nc.gpsimd, "sp": nc.sync}
        self.sems = []
        self.semeng = []
        self.cur = {}
        self.cnt = {}
        self.known = {e: {} for e in self.eng}
        self.dcnt = {}
        self.ninstr = 0
        self.freed = {}
        self.log = None
        for e in self.eng:
            self._newsem(e)

    def _newsem(self, e):
        s = self.es.enter_context(self.nc.semaphore(f"s{len(self.sems)}"))
        self.sems.append(s)
        self.semeng.append(e)
        idx = len(self.sems) - 1
        if e is not None:
            self.cur[e] = idx
            self.cnt[e] = 0
        else:
            self.dcnt[idx] = 0
        return idx

    def sb(self, es, name, shape, dt):
        self.uid = getattr(self, "uid", 0) + 1
        h = es.enter_context(self.nc.sbuf_tensor(f"sb{self.uid}_" + name, list(shape), dt))
        t = Tl(h[:], name)
        t.r = dict(self.freed)
        es.callback(self._on_free, t)
        return t

    def _on_free(self, t):
        toks = dict(t.r)
        if t.w is not None:
            toks[t.w[0]] = max(toks.get(t.w[0], 0), t.w[1])
        for si, val in toks.items():
            if self.freed.get(si, 0) < val:
                self.freed[si] = val

    def ps(self, es, name, shape, dt):
        h = es.enter_context(self.nc.psum_tensor("ps_" + name, list(shape), dt))
        return Tl(h[:], name)

    def dram(self, name, shape, dt, kind):
        h = self.nc.dram_tensor(name, list(shape), dt, kind=kind)
        return Tl(h.ap(), name, dram=True)

    def _need(self, e, tok, raw, dma=False):
        if tok is None:
            return
        si, val = tok
        owner = self.semeng[si]
        if owner == e and (e == "pe" or (not raw and not STRICT)) and not dma:
            return
        k = self.known[e]
        if k.get(si, 0) >= val:
            return
        self.eng[e].wait_ge(self.sems[si], val)
        self.ninstr += 1
        k[si] = val
        if self.log is not None:
            self.log.append(f"{e}: WAIT s{si}({self.semeng[si]}) >= {val}")

    def _deps(self, e, reads, writes, skip_w_sem=None, dma=False):
        for t in reads:
            self._need(e, t.w, True, dma)
        for t in writes:
            if t.w is not None and t.w[0] != skip_w_sem:
                self._need(e, t.w, False, dma)
            for si, val in t.r.items():
                self._need(e, (si, val), False, dma)

    def _mark(self, tok, reads, writes):
        si, val = tok
        for t in reads:
            if t.r.get(si, 0) < val:
                t.r[si] = val
        for t in writes:
            t.w = tok
            t.r = {}

    def op(self, e, fn, **kw):
        reads, writes, args = [], [], {}
        for k, v in kw.items():
            if isinstance(v, V):
                (writes if k in OUTK else reads).append(v.t)
                args[k] = v.ap
            else:
                args[k] = v
        self._deps(e, reads, writes)
        ins = getattr(self.eng[e], fn)(**args)
        if self.log is not None:
            self.log.append(f"{e}: {fn} W={[t.name for t in writes]} R={[t.name for t in reads]} -> {self.cnt[e] + 1}")
        if self.cnt[e] >= self.EPOCH:
            self._newsem(e)
        si = self.cur[e]
        self.cnt[e] += 1
        ins.then_inc(self.sems[si], 1)
        self.ninstr += 1
        self._mark((si, self.cnt[e]), reads, writes)
        return ins

    def _dsem(self, t):
        if t.dsem is None:
            t.dsem = self._newsem(None)
        return t.dsem

    def dma(self, q, out, in_, semtile=None, extra_reads=(), **kw):
        sbt = semtile if semtile is not None else (out.t if not out.t.dram else in_.t)
        ds = self._dsem(sbt)
        reads = [in_.t] + [x.t for x in extra_reads]
        writes = [out.t]
        self._deps(q, reads, writes, skip_w_sem=ds, dma=True)
        ins = self.eng[q].dma_start(out=out.ap, in_=in_.ap, **kw)
        self.dcnt[ds] += 16
        ins.then_inc(self.sems[ds], 16)
        self.ninstr += 1
        self._mark((ds, self.dcnt[ds]), reads, writes)

    def gather(self, out, table, idx):
        ds = self._dsem(out.t)
        reads = [table.t, idx.t]
        writes = [out.t]
        self._deps("pool", reads, writes, skip_w_sem=ds, dma=True)
        ins = self.nc.gpsimd.indirect_dma_start(
            out=out.ap, out_offset=None, in_=table.ap,
            in_offset=bass.IndirectOffsetOnAxis(ap=idx.ap, axis=0))
        self.dcnt[ds] += 16
        ins.then_inc(self.sems[ds], 16)
        self.ninstr += 1
        self._mark((ds, self.dcnt[ds]), reads, writes)

    def finish(self, tiles):
        for t in tiles:
            self._need("sp", t.w, True)

    def mm(self, out, lhsT, rhs, start, stop, **kw):
        return self.op("pe", "matmul", out=out, lhsT=lhsT, rhs=rhs, start=start, stop=stop, **kw)

    def tr(self, out, in_, ident):
        return self.op("pe", "transpose", out=out, in_=in_, identity=ident)

    def act(self, out, in_, func, **kw):
        return self.op("act", "activation", out=out, in_=in_, func=func, **kw)

    def tt(self, e, out, in0, in1, op):
        return self.op(e, "tensor_tensor", out=out, in0=in0, in1=in1, op=op)

    def ts(self, e, out, in0, s1, op0, s2=None, op1=None, **kw):
        if op1 is None:
            return self.op(e, "tensor_scalar", out=out, in0=in0, scalar1=s1, scalar2=None, op0=op0, **kw)
        if isinstance(s1, V) != isinstance(s2, V):
            self.op(e, "tensor_scalar", out=out, in0=in0, scalar1=s1, scalar2=None, op0=op0)
            return self.op(e, "tensor_scalar", out=out, in0=out, scalar1=s2, scalar2=None, op0=op1, **kw)
        return self.op(e, "tensor_scalar", out=out, in0=in0, scalar1=s1, scalar2=s2, op0=op0, op1=op1, **kw)

    def stt(self, e, out, in0, scalar, in1, op0, op1, **kw):
        return self.op(e, "scalar_tensor_tensor", out=out, in0=in0, scalar=scalar, in1=in1, op0=op0, op1=op1, **kw)

    def copy(self, e, out, in_):
        if e == "act":
            return self.act(out, in_, AF.Copy)
        return self.op(e, "tensor_copy", out=out, in_=in_)

    def memset(self, e, out, val):
        return self.op(e, "memset", ap=out, constant=val) if False else self._memset(e, out, val)

    def _memset(self, e, out, val):
        self._deps(e, [], [out.t])
        ins = self.eng[e].memset(out.ap, val)
        if self.cnt[e] >= self.EPOCH:
            self._newsem(e)
        si = self.cur[e]
        self.cnt[e] += 1
        ins.then_inc(self.sems[si], 1)
        self.ninstr += 1
        self._mark((si, self.cnt[e]), [], [out.t])


OFF_U, OFF_Q, OFF_K, OFF_V, OFF_QI, OFF_KI, OFF_WI, OFF_GS, OFF_GA = 0, 512, 1024, 1152, 1280, 1536, 1568, 1576, 2600
IN_W = 3624
TWO_PI = 2.0 * math.pi


class Ctx:
    pass


def rope_tables(P, es, S, c):
    tabs = {fn + nm: P.sb(es, f"rope_{fn}{nm}", [128, S], BF16) for nm in ("A", "I") for fn in ("cos", "sin")}
    tmp = ExitStack()
    pid = P.sb(tmp, "rt_pid", [128, 1], I32)
    pm = P.sb(tmp, "rt_pm", [128, 1], I32)
    pf = P.sb(tmp, "rt_pf", [128, 1], F32)
    inv = P.sb(tmp, "rt_inv", [128, 2], F32)
    posi = P.sb(tmp, "rt_posi", [128, S], I32)
    pos = P.sb(tmp, "rt_pos", [128, S], F32)
    ang = P.sb(tmp, "rt_ang", [128, S], F32)
    t1 = P.sb(tmp, "rt_t1", [128, S], F32)
    ti = P.sb(tmp, "rt_ti", [128, S], I32)
    P.op("pool", "iota", out=pid.all(), pattern=[[0, 1]], base=0, channel_multiplier=1)
    P.op("pool", "iota", out=posi.all(), pattern=[[1, S]], base=0, channel_multiplier=0)
    P.copy("dve", pos.all(), posi.all())
    for j, (msk, dim) in enumerate(((31, 64), (15, 32))):
        P.op("dve", "tensor_single_scalar", out=pm.all(), in_=pid.all(), scalar=msk, op=ALU.bitwise_and)
        P.copy("dve", pf.all(), pm.all())
        P.act(inv[:, j:j + 1], pf.all(), AF.Exp, scale=-math.log(10000.0) * 2.0 / dim)
    outs = {}
    for j, nm in enumerate(("A", "I")):
        for k, (fn, shift) in enumerate((("cos", math.pi / 2), ("sin", 0.0))):
            tab = tabs[fn + nm]
            P.ts("dve", ang.all(), pos.all(), inv[:, j:j + 1], ALU.mult, shift, ALU.add)
            range_reduce_sin(P, tab.all(), ang.all(), t1.all(), ti.all())
            outs[fn + nm] = tab
    tmp.close()
    c.rope = outs


def range_reduce_sin(P, out, ang, t1, ti):
    P.ts("dve", t1, ang, 1.0 / TWO_PI, ALU.mult)
    P.copy("dve", ti, t1)
    P.copy("dve", t1, ti)
    P.stt("dve", t1, t1, -TWO_PI, ang, ALU.mult, ALU.add)
    P.ts("dve", ang, t1, math.pi, ALU.is_gt)
    P.stt("dve", t1, ang, -TWO_PI, t1, ALU.mult, ALU.add)
    P.ts("dve", t1, t1, 3.141592, ALU.min, -3.141592, ALU.max)
    P.act(out, t1, AF.Sin)


def load_w_cols(P, c, dst_fn, col0, ncols, w_in_d, gcol, stage):
    for k in range(8):
        st = stage[k % 2]
        P.dma("sp" if k % 2 == 0 else "act", st[:, 0:ncols], w_in_d[k * 128:(k + 1) * 128, col0:col0 + ncols])
        dst_fn(k, st)


def cpow(P, es, name, lr, th, jv, G, J, order="gj"):
    shp = [128, G, J] if order == "gj" else [128, J, G]
    Pr = P.sb(es, name + "_r", shp, F32)
    Pi = P.sb(es, name + "_i", shp, F32)
    tmp = ExitStack()
    mag = P.sb(tmp, name + "_mag", shp, F32)
    ang = P.sb(tmp, name + "_ang", shp, F32)
    t1 = P.sb(tmp, name + "_t1", shp, F32)
    ti = P.sb(tmp, name + "_ti", shp, I32)
    if order == "gj":
        lb, jb = lr.pat(0, [(1, G), (0, J)]), jv.pat(0, [(0, G), (1, J)])
        tb = th.pat(0, [(1, G), (0, J)])
    else:
        lb, jb = lr.pat(0, [(0, J), (1, G)]), jv.pat(0, [(1, J), (0, G)])
        tb = th.pat(0, [(0, J), (1, G)])
    P.tt("dve", mag.all(), lb, jb, ALU.mult)
    P.act(mag.all(), mag.all(), AF.Exp)
    fl = "p a b -> p (a b)"
    for dst, shift in ((Pi, 0.0), (Pr, math.pi / 2)):
        P.tt("dve", ang.all(), tb, jb, ALU.mult)
        if shift:
            P.ts("dve", ang.all(), ang.all(), shift, ALU.add)
        range_reduce_sin(P, dst.all().rr(fl), ang.all().rr(fl), t1.all().rr(fl), ti.all().rr(fl))
        P.tt("dve", dst.all(), dst.all(), mag.all(), ALU.mult)
    tmp.close()
    return Pr, Pi


def kappa(P, es, name, ar, ai, lr, th, G):
    kr = P.sb(es, name + "_kr", [128, G], F32)
    ki = P.sb(es, name + "_ki", [128, G], F32)
    tmp = ExitStack()
    one = P.sb(tmp, name + "_one", [128, 1], F32)
    P.memset("dve", one.all(), 1.0)
    Ar, Ai = cpow(P, tmp, name + "_a1", lr, th, one.all(), G, 1)
    den = P.sb(tmp, name + "_den", [128, G], F32)
    t = P.sb(tmp, name + "_t", [128, G], F32)
    arm = P.sb(tmp, name + "_arm", [128, G], F32)
    A_r, A_i = Ar.all().rr("p g j -> p (g j)"), Ai.all().rr("p g j -> p (g j)")
    P.tt("dve", den.all(), ar, ar, ALU.mult)
    P.tt("dve", t.all(), ai, ai, ALU.mult)
    P.tt("dve", den.all(), den.all(), t.all(), ALU.add)
    P.op("dve", "reciprocal", out=den.all(), in_=den.all())
    P.ts("dve", arm.all(), A_r, -1.0, ALU.add)
    P.tt("dve", kr.all(), arm.all(), ar, ALU.mult)
    P.tt("dve", t.all(), A_i, ai, ALU.mult)
    P.tt("dve", kr.all(), kr.all(), t.all(), ALU.add)
    P.tt("dve", kr.all(), kr.all(), den.all(), ALU.mult)
    P.tt("dve", ki.all(), A_i, ar, ALU.mult)
    P.tt("dve", t.all(), arm.all(), ai, ALU.mult)
    P.tt("dve", ki.all(), ki.all(), t.all(), ALU.subtract)
    P.tt("dve", ki.all(), ki.all(), den.all(), ALU.mult)
    tmp.close()
    return kr, ki


def ssm_phase(P, c, S, uys, sp):
    NCH = S // LCH
    psb = c.psb
    uT = ysT = y2 = uys
    ph = ExitStack()
    sg = P.sb(ph, "sg", [128, 1], F32)
    nsg = P.sb(ph, "nsg", [128, 1], F32)
    P.ts("dve", sg.all(), c.pid_f.all(), 63.5, ALU.is_gt, -2.0, ALU.mult)
    P.ts("dve", sg.all(), sg.all(), 1.0, ALU.add)
    P.ts("dve", nsg.all(), sg.all(), -1.0, ALU.mult)
    jv = P.sb(ph, "jv", [128, 256], F32)
    jvi = P.sb(ph, "jvi", [128, 256], I32)
    P.op("pool", "iota", out=jvi.all(), pattern=[[1, 256]], base=0, channel_multiplier=0)
    P.copy("dve", jv.all(), jvi.all())
    jrev = P.sb(ph, "jrev", [128, 16], F32)
    P.ts("dve", jrev.all(), jv[:, 0:16], -1.0, ALU.mult, 15.0, ALU.add)
    bm = P.sb(ph, "bm", [128, 8], F32)
    t8 = P.sb(ph, "t8", [128, 8], F32)
    P.ts("dve", t8.all(), jv[:, 0:8], 16.0, ALU.mult)
    P.ts("dve", bm.all(), t8.all(), c.pid_f[:, 0:1], ALU.subtract)
    P.ts("dve", t8.all(), bm.all(), 0.5, ALU.is_gt, -1.0, ALU.mult)
    P.ts("dve", t8.all(), t8.all(), 1.0, ALU.add)
    P.ts("dve", bm.all(), bm.all(), -15.5, ALU.is_gt)
    P.tt("dve", bm.all(), bm.all(), t8.all(), ALU.mult)
    eye8 = P.sb(ph, "eye8", [128, 8, 8], F32)
    P.tt("dve", eye8.all(), jv[:, 0:8].pat(0, [(1, 8), (0, 8)]), jv[:, 0:8].pat(0, [(0, 8), (1, 8)]), ALU.is_equal)
    pswapb = P.sb(ph, "pswapb", [128, 128], BF16)
    pswap = P.sb(ph, "pswap", [128, 128], F32)
    P.ts("dve", pswap.all(), c.iota_f.all(), c.pid_f[:, 0:1], ALU.subtract)
    P.tt("dve", pswap.all(), pswap.all(), pswap.all(), ALU.mult)
    P.ts("dve", pswap.all(), pswap.all(), 4096.0, ALU.is_equal)
    P.copy("dve", pswapb.all(), pswap.all())
    dsk = P.sb(ph, "dsk", [128, 4], F32)
    P.dma("sp", dsk.all(), sp["dskip"].all())
    wglu = P.sb(ph, "wglu", [128, 4, 512], BF16)
    stgw = P.sb(ph, "stgw", [128, 512], F32)
    for k in range(4):
        P.dma("sp", stgw.all(), sp["w_glu"][k * 128:(k + 1) * 128, :])
        P.copy("act", wglu[:, k, :], stgw.all())

    for o in range(4):
        oc = ExitStack()
        BD = P.sb(oc, "BD", [128, 16, 128], BF16)
        Wb = P.sb(oc, "Wb", [128, 8, 16, 128], BF16)
        Wc = P.sb(oc, "Wc", [128, 8, 16, 128], BF16)
        tc_ = P.sb(oc, "tabc", [128, 8, NCH], BF16)
        tsn = P.sb(oc, "tabs", [128, 8, NCH], BF16)
        rho = P.sb(oc, "rho", [128, 8], F32)
        pr = ExitStack()
        prm = P.sb(pr, "prm", [128, 3, 8], F32)
        for i, nm in enumerate(("ar_sm", "ai_sm", "ldt_sm")):
            P.dma("sp", prm[:, i, :], sp[nm][:, o * 8:(o + 1) * 8])
        dt = P.sb(pr, "dt", [128, 8], F32)
        lr = P.sb(pr, "lr", [128, 8], F32)
        th = P.sb(pr, "th", [128, 8], F32)
        P.act(dt.all(), prm[:, 2, :], AF.Exp)
        P.tt("dve", lr.all(), prm[:, 0, :], dt.all(), ALU.mult)
        P.tt("dve", th.all(), prm[:, 1, :], dt.all(), ALU.mult)
        kr, ki = kappa(P, pr, "ksm", prm[:, 0, :], prm[:, 1, :], lr.all(), th.all(), 8)
        Ar, Ai = cpow(P, pr, "apw", lr.all(), th.all(), jv[:, 0:17], 8, 17)
        prA = pr
        pr = ExitStack()
        U = P.sb(pr, "U12", [128, 2, 8, 16], F32)
        T12 = P.sb(pr, "T12", [128, 2, 8, 16], F32)
        for i, nm in enumerate(("bU1", "bU2")):
            P.dma("sp", U[:, i], sp[nm][:, o * 8:(o + 1) * 8, :])
        for i, nm in enumerate(("cT1", "cT2")):
            P.dma("act", T12[:, i], sp[nm][:, o * 8:(o + 1) * 8, :])
        X = P.sb(pr, "X", [128, 8, 16], F32)
        tx = P.sb(pr, "tx", [128, 8, 16], F32)
        kib = P.sb(pr, "kib", [128, 8], F32)
        P.ts("dve", kib.all(), ki.all(), nsg[:, 0:1], ALU.mult)
        P.tt("dve", X.all(), U[:, 0], kr.all().pat(0, [(1, 8), (0, 16)]), ALU.mult)
        P.tt("dve", tx.all(), U[:, 1], kib.all().pat(0, [(1, 8), (0, 16)]), ALU.mult)
        P.tt("dve", X.all(), X.all(), tx.all(), ALU.add)
        if getattr(c, "dump", None) and o == 0 and DUMP2:
            c.dump("kr", kr.all(), F32)
            c.dump("ki", ki.all(), F32)
            c.dump("X", X.all().rr("p a b -> p (a b)"), F32)
            c.dump("Ar", Ar.all().rr("p a b -> p (a b)"), F32)
        Y = P.sb(pr, "Y", [128, 8, 17, 16], F32)
        ty = P.sb(pr, "ty", [128, 8, 17, 16], F32)
        for g in range(8):
            P.tt("dve", Y[:, g], T12[:, 0, g, :].pat(0, [(0, 17), (1, 16)]), Ar[:, g, :].pat(0, [(1, 17), (0, 16)]), ALU.mult)
            P.tt("dve", ty[:, g], T12[:, 1, g, :].pat(0, [(0, 17), (1, 16)]), Ai[:, g, :].pat(0, [(1, 17), (0, 16)]), ALU.mult)
        P.stt("dve", Y.all().rr("p g t c -> p (g t c)"), Y.all().rr("p g t c -> p (g t c)"), sg[:, 0:1],
              ty.all().rr("p g t c -> p (g t c)"), ALU.mult, ALU.subtract)
        if getattr(c, "dump", None) and o == 0 and DUMP2:
            c.dump("Y", Y.all().rr("p a b c -> p (a b c)"), F32)
        for g in range(8):
            P.tt("dve", Wc[:, g].rr("p t (a c) -> p t a c", a=8),
                 Y[:, g, 1:17, :].pat(0, [(16, 16), (0, 8), (1, 16)]),
                 eye8[:, g, :].pat(0, [(0, 16), (1, 8), (0, 16)]), ALU.mult)
        Xpad = P.sb(pr, "Xpad", [128, 8, 8, 16], BF16)
        Yb = P.sb(pr, "Yb", [128, 8, 16, 16], BF16)
        P.copy("dve", Yb.all(), Y[:, :, 0:16, :])
        for g in range(8):
            P.tt("dve", Xpad[:, g], X[:, g, :].pat(0, [(0, 8), (1, 16)]), eye8[:, g, :].pat(0, [(1, 8), (0, 16)]), ALU.mult)
        for g in range(8):
            P.mm(psb[0][:, 0:256], Xpad[:, g].rr("p a c -> p (a c)"), Yb[:, g].rr("p t c -> p (t c)"), g == 0, g == 7)
        Rsb = P.sb(pr, "Rsb", [128, 16, 16], F32)
        P.copy("act", Rsb.all().rr("p t c -> p (t c)"), psb[0][:, 0:256])
        if getattr(c, "dump", None) and o == 0 and DUMP2:
            c.dump("Rsb", Rsb.all().rr("p a b -> p (a b)"), F32)
            c.dump("Xpad", Xpad.all().rr("p a b c -> p (a b c)"), BF16)
        for tau in range(16):
            P.tt("dve", BD[:, tau, :].rr("p (a c) -> p a c", a=8), Rsb[:, tau, :].pat(0, [(0, 8), (1, 16)]),
                 bm.all().pat(0, [(1, 8), (0, 16)]), ALU.mult)
        pr.close()
        pr = ExitStack()
        l16 = P.sb(pr, "l16", [128, 8], F32)
        P.act(rho.all(), lr.all(), AF.Exp, scale=float(LCH))
        P.ts("dve", l16.all(), th.all(), float(LCH), ALU.mult)
        zero8 = P.sb(pr, "zero8", [128, 8], F32)
        P.memset("dve", zero8.all(), 0.0)
        assert NCH <= 256
        for h4 in range(2):
            pq = ExitStack()
            Er, Ei = cpow(P, pq, f"rot{h4}", zero8[:, h4 * 4:h4 * 4 + 4], l16[:, h4 * 4:h4 * 4 + 4], jv[:, 0:NCH], 4, NCH)
            P.copy("act", tc_[:, h4 * 4:h4 * 4 + 4, :], Er.all())
            P.copy("act", tsn[:, h4 * 4:h4 * 4 + 4, :], Ei.all())
            pq.close()
        pr.close()
        pr = ExitStack()
        pcm = P.sb(pr, "pcm", [128, 5, 64], F32)
        for i, nm in enumerate(("ar_cm", "ai_cm", "ldt_cm", "br_cm", "bi_cm")):
            P.dma("sp", pcm[:, i, :], sp[nm][:, o, :])
        dtc = P.sb(pr, "dtc", [128, 64], F32)
        lrc = P.sb(pr, "lrc", [128, 64], F32)
        thc = P.sb(pr, "thc", [128, 64], F32)
        P.act(dtc.all(), pcm[:, 2, :], AF.Exp)
        P.tt("dve", lrc.all(), pcm[:, 0, :], dtc.all(), ALU.mult)
        P.tt("dve", thc.all(), pcm[:, 1, :], dtc.all(), ALU.mult)
        krc, kic = kappa(P, pr, "kcm", pcm[:, 0, :], pcm[:, 1, :], lrc.all(), thc.all(), 64)
        Bbr = P.sb(pr, "Bbr", [128, 64], F32)
        Bbi = P.sb(pr, "Bbi", [128, 64], F32)
        tb = P.sb(pr, "tb", [128, 64], F32)
        P.tt("dve", Bbr.all(), krc.all(), pcm[:, 3, :], ALU.mult)
        P.tt("dve", tb.all(), kic.all(), pcm[:, 4, :], ALU.mult)
        P.tt("dve", Bbr.all(), Bbr.all(), tb.all(), ALU.subtract)
        P.tt("dve", Bbi.all(), krc.all(), pcm[:, 4, :], ALU.mult)
        P.tt("dve", tb.all(), kic.all(), pcm[:, 3, :], ALU.mult)
        P.tt("dve", Bbi.all(), Bbi.all(), tb.all(), ALU.add)
        Pr_, Pi_ = cpow(P, pr, "apc", lrc.all(), thc.all(), jrev.all(), 64, 16, order="jg")
        Z = P.sb(pr, "Z", [128, 16, 2, 64], F32)
        tz = P.sb(pr, "tz", [128, 16, 64], F32)
        bb = lambda t: t.all().pat(0, [(0, 16), (1, 64)])
        P.tt("dve", Z[:, :, 0, :], Pr_.all(), bb(Bbr), ALU.mult)
        P.tt("dve", tz.all(), Pi_.all(), bb(Bbi), ALU.mult)
        P.tt("dve", Z[:, :, 0, :], Z[:, :, 0, :], tz.all(), ALU.subtract)
        P.tt("dve", Z[:, :, 1, :], Pr_.all(), bb(Bbi), ALU.mult)
        P.tt("dve", tz.all(), Pi_.all(), bb(Bbr), ALU.mult)
        P.tt("dve", Z[:, :, 1, :], Z[:, :, 1, :], tz.all(), ALU.add)
        if getattr(c, "dump", None) and o == 0 and DUMP2:
            c.dump("Z", Z.all().rr("p s r m -> p (s r m)"), F32)
            c.dump("Bbr", Bbr.all(), F32)
            c.dump("Prc", Pr_.all().rr("p a b -> p (a b)"), F32)
            c.dump("krc", krc.all(), F32)
            c.dump("pcm", pcm.all().rr("p a b -> p (a b)"), F32)
            c.dump("lrc", lrc.all(), F32)
        for g in range(8):
            P.ts("dve", Wb[:, g].rr("p s m -> p (s m)"), Z.all().rr("p s r m -> p (s r m)"), bm[:, g:g + 1], ALU.mult)
        pr.close()
        prA.close()
        if getattr(c, "dump", None) and o == 0:
            c.dump("BD", BD.all().rr("p a b -> p (a b)"), BF16)
            c.dump("Wb", Wb.all().rr("p a b m -> p (a b m)"), BF16)
            c.dump("Wc", Wc.all().rr("p a b m -> p (a b m)"), BF16)
            c.dump("tabc", tc_.all().rr("p a b -> p (a b)"), BF16)
            c.dump("tabs", tsn.all().rr("p a b -> p (a b)"), BF16)
            c.dump("rho", rho.all(), F32)

        wk = ExitStack()
        SA = P.sb(wk, "SA", [128, 8, NCH], F32)
        VA = P.sb(wk, "VA", [128, 8, NCH], F32)
        VB = P.sb(wk, "VB", [128, 8, NCH], F32)
        t1 = P.sb(wk, "l2t1", [128, 8, NCH], F32)
        t2 = P.sb(wk, "l2t2", [128, 8, NCH], F32)
        H = P.sb(wk, "H", [128, 8, NCH], BF16)
        SAh = P.sb(wk, "SAh", [128, 8, NCH], BF16)
        SAl = P.sb(wk, "SAl", [128, 8, NCH], BF16)
        uo = uT[:, o, :].rr("p (k s) -> p s k", s=LCH)
        for g in range(8):
            bank = psb[1 + (g % 2)]
            for q0 in range(0, NCH, 512):
                qn = min(512, NCH - q0)
                for s_ in range(LCH):
                    P.mm(bank[:, 0:qn], Wb[:, g, s_, :], uo[:, s_, q0:q0 + qn], s_ == 0, s_ == LCH - 1)
                P.copy("act", SA[:, g, q0:q0 + qn], bank[:, 0:qn])
        fl = "p g k -> p (g k)"
        cb, sb_ = tc_.all().rr(fl), tsn.all().rr(fl)
        for g in range(8):
            for q0 in range(0, NCH, 512):
                qn = min(512, NCH - q0)
                bank = psb[3 + (g % 2)]
                P.copy("dve", SAh[:, g, q0:q0 + qn], SA[:, g, q0:q0 + qn])
                P.tt("dve", SAl[:, g, q0:q0 + qn], SA[:, g, q0:q0 + qn], SAh[:, g, q0:q0 + qn], ALU.subtract)
                P.mm(bank[:, 0:qn], pswapb.all(), SAh[:, g, q0:q0 + qn], True, False)
                P.mm(bank[:, 0:qn], pswapb.all(), SAl[:, g, q0:q0 + qn], False, True)
                sl = slice(q0, q0 + qn)
                A_, B_ = SA[:, g, sl], bank[:, 0:qn]
                cg, sgn_ = tc_[:, g, sl], tsn[:, g, sl]
                P.tt("dve", t1[:, g, sl], A_, cg, ALU.mult)
                P.tt("dve", t2[:, g, sl], B_, sgn_, ALU.mult)
                P.stt("dve", VA[:, g, sl], t2[:, g, sl], sg[:, 0:1], t1[:, g, sl], ALU.mult, ALU.add)
                P.tt("dve", t1[:, g, sl], B_, cg, ALU.mult)
                P.tt("dve", t2[:, g, sl], A_, sgn_, ALU.mult)
                P.stt("dve", VB[:, g, sl], t2[:, g, sl], nsg[:, 0:1], t1[:, g, sl], ALU.mult, ALU.add)
        for g in range(8):
            rb = rho[:, g:g + 1].pat(0, [(0, NCH)])
            P.op("dve", "tensor_tensor_scan", out=VA[:, g, :], data0=rb, data1=VA[:, g, :], initial=0.0, op0=ALU.mult, op1=ALU.add)
            P.op("dve", "tensor_tensor_scan", out=VB[:, g, :], data0=rb, data1=VB[:, g, :], initial=0.0, op0=ALU.mult, op1=ALU.add)
        P.tt("dve", t1.all().rr(fl), VA.all().rr(fl), cb, ALU.mult)
        P.tt("dve", t2.all().rr(fl), VB.all().rr(fl), sb_, ALU.mult)
        P.stt("dve", H.all().rr(fl), t2.all().rr(fl), nsg[:, 0:1], t1.all().rr(fl), ALU.mult, ALU.add)
        if getattr(c, "dump", None) and o == 0:
            c.dump("SA", SA.all().rr("p a b -> p (a b)"), F32)
            c.dump("H", H.all().rr("p a b -> p (a b)"), BF16)
        yo = uo
        for t in range(LCH - 1, -1, -1):
            bank = psb[5 + (t % 3)]
            for q0 in range(0, NCH, 512):
                qn = min(512, NCH - q0)
                for s_ in range(t + 1):
                    P.mm(bank[:, 0:qn], BD[:, t - s_, :], uo[:, s_, q0:q0 + qn], s_ == 0, False)
                for g in range(8):
                    lo = 1 if q0 == 0 else 0
                    P.mm(bank[:, lo:qn], Wc[:, g, t, :], H[:, g, q0 + lo - 1:q0 + qn - 1], False, g == 7)
                P.stt("dve", yo[:, t, q0:q0 + qn], uo[:, t, q0:q0 + qn], dsk[:, o:o + 1], bank[:, 0:qn], ALU.mult, ALU.add)
        wk.close()
        oc.close()
    if getattr(c, "dump", None):
        c.dump("ypre", y2.all().rr("p a b -> p (a b)"), BF16)
    gl = ExitStack()
    g1 = P.sb(gl, "g1", [128, S], F32)
    g2 = P.sb(gl, "g2", [128, S], F32)
    for o in range(4):
        gelu_inplace(P, y2[:, o, :], g1.all(), g2.all())
    gate = P.sb(gl, "gate", [128, 4, 512], BF16)
    for q0 in range(0, S, 512):
        for n in range(4):
            bank = psb[n]
            for k in range(4):
                P.mm(bank[:, 0:512], wglu[:, k, n * 128:(n + 1) * 128], y2[:, k, q0:q0 + 512], k == 0, k == 3)
            P.act(gate[:, n, :], bank[:, 0:512], AF.Sigmoid)
        P.tt("dve", ysT[:, :, q0:q0 + 512], gate.all(), y2[:, :, q0:q0 + 512], ALU.mult)
    gl.close()
    ph.close()


def gelu_inplace(P, x, t1, t2, eng="dve"):
    P.tt(eng, t1, x, x, ALU.mult)
    P.ts(eng, t1, t1, 0.044715 * 1.5957691216, ALU.mult, 1.5957691216, ALU.add)
    P.tt(eng, t1, t1, x, ALU.mult)
    P.act(t2, t1, AF.Sigmoid)
    P.tt(eng, x, x, t2, ALU.mult)


def attn_phase(P, c, S, x_d, w_in_d, g1c, ng1c, kTd, kiT4, vaug, ya_d, stop_at=None, dump=None):
    NT = S // 128
    TOPK = min(256, S // 4)
    psb, ident = c.psb, c.ident
    ph = ExitStack()
    rope_tables(P, ph, S, c)
    w_q = P.sb(ph, "w_q", [128, 8, 8, 128], BF16)
    w_qi = P.sb(ph, "w_qi", [128, 8, 6, 128], BF16)
    P.memset("pool", w_qi.all(), 0.0)
    w_wi = P.sb(ph, "w_wi", [128, 8, 8], BF16)
    wst = ExitStack()
    stg = [P.sb(wst, f"astg{i}", [128, 512], F32) for i in range(2)]

    def cvt(dst, src, k, neg=False):
        P.act(dst, src, AF.Copy, scale=(ng1c if neg else g1c)[:, k:k + 1])

    def q_cvt(k, st):
        for m in range(4):
            cvt(w_q[:, k, m, :], st[:, m * 128:(m + 1) * 128], k)
            for e in range(2):
                base = m * 128 + e * 64
                cvt(w_q[:, k, 4 + m, e * 64:e * 64 + 32], st[:, base + 32:base + 64], k, neg=True)
                cvt(w_q[:, k, 4 + m, e * 64 + 32:e * 64 + 64], st[:, base:base + 32], k)
    load_w_cols(P, c, q_cvt, OFF_Q, 512, w_in_d, g1c, stg)

    def qi_cvt(k, st):
        for h in range(8):
            tl, pos = h // 3, h % 3
            cvt(w_qi[:, k, tl, pos * 32:(pos + 1) * 32], st[:, h * 32:(h + 1) * 32], k)
            cvt(w_qi[:, k, 3 + tl, pos * 32:pos * 32 + 16], st[:, h * 32 + 16:h * 32 + 32], k, neg=True)
            cvt(w_qi[:, k, 3 + tl, pos * 32 + 16:pos * 32 + 32], st[:, h * 32:h * 32 + 16], k)
    load_w_cols(P, c, qi_cvt, OFF_QI, 256, w_in_d, g1c, stg)
    load_w_cols(P, c, lambda k, st: cvt(w_wi[:, k, :], st[:, 0:8], k), OFF_WI, 8, w_in_d, g1c, stg)
    wst.close()

    xt = [P.sb(ph, "axt0", [128, D], F32)] * 2
    xh = P.sb(ph, "axh", [128, D], BF16)
    xhT = P.sb(ph, "axhT", [128, 8, 128], BF16)
    junk = P.sb(ph, "ajunk", [128, D], BF16)
    ss = P.sb(ph, "ass", [128, 1], F32)
    rstd = P.sb(ph, "arstd", [128, 1], F32)
    qT = P.sb(ph, "qT", [128, 4, 128], BF16)
    qiT = P.sb(ph, "qiT", [128, 3, 128], BF16)
    t1 = P.sb(ph, "at1", [128, 4, 128], F32)
    t2 = P.sb(ph, "at2", [128, 4, 128], F32)
    wsg = P.sb(ph, "wsg", [128, 8], F32)
    wsc = P.sb(ph, "wsc", [128, 8], F32)
    acc = P.sb(ph, "acc", [128, S], F32)
    msk = P.sb(ph, "msk", [128, S], BF16)
    mT = P.sb(ph, "mT", [128, NT, 128], BF16)
    rr = [P.sb(ph, f"rr{i}", [128, 512], F32) for i in range(2)]
    lo = P.sb(ph, "lo", [128, 1], F32)
    mid = P.sb(ph, "mid", [128, 1], F32)
    cnt = P.sb(ph, "cnt", [128, 1], F32)
    cntb = P.sb(ph, "cntb", [128, 1], F32)
    ge = P.sb(ph, "ge", [128, 1], F32)
    eT = [[P.sb(ph, f"eT{i}{n}", [128, 4, 128], BF16) for n in range(2)] for i in range(2)]
    pT = [[P.sb(ph, f"pT{i}{n}", [128, 4, 128], BF16) for n in range(2)] for i in range(2)]
    rden = P.sb(ph, "rden", [128, 512], F32)
    rdh = P.sb(ph, "rdh", [128, 512], BF16)
    rdl = P.sb(ph, "rdl", [128, 512], BF16)
    ones_bf = P.sb(ph, "ones_bf", [128, 64], BF16)
    P.memset("dve", ones_bf.all(), 1.0)
    bcs = P.sb(ph, "bcs", [64, 512], F32)
    ya = [P.sb(ph, f"ya{i}", [64, 8, 128], BF16) for i in range(2)]
    m4 = "p (m t) -> p m t"

    qTs = [qT, P.sb(ph, "qT1", [128, 4, 128], BF16)]
    sjunk = P.sb(ph, "sjunk", [128, 2432], BF16)
    PIPE = stop_at is None

    def stage_I(b):
        tok = slice(b * 128, (b + 1) * 128)
        Sc = (b + 1) * 128
        qTb = qTs[b % 2]
        xb = xt[b % 2]
        P.dma("sp", xb.all(), x_d[tok, :])
        c.norm_and_transpose(b, xb, xh, xhT, junk, ss, rstd, psb[0])
        for m in range(8):
            bank = psb[1] if m < 4 else psb[2]
            for k in range(8):
                P.mm(bank[:, (m % 4) * 128:(m % 4 + 1) * 128], w_q[:, k, m, :], xhT[:, k, :], k == 0, k == 7)
        P.tt("dve", t1.all(), psb[1].all().rr(m4, m=4), c.rope["cosA"][:, tok].pat(0, [(0, 4), (1, 128)]), ALU.mult)
        P.tt("dve", t2.all(), psb[2].all().rr(m4, m=4), c.rope["sinA"][:, tok].pat(0, [(0, 4), (1, 128)]), ALU.mult)
        P.tt("dve", qTb.all(), t1.all(), t2.all(), ALU.add)
        for m in range(6):
            bank = psb[3] if m < 3 else psb[6]
            for k in range(8):
                P.mm(bank[:, (m % 3) * 128:(m % 3 + 1) * 128], w_qi[:, k, m, :], xhT[:, k, :], k == 0, k == 7)
        P.tt("dve", t1[:, 0:3, :], psb[3][:, 0:384].rr(m4, m=3), c.rope["cosI"][:, tok].pat(0, [(0, 3), (1, 128)]), ALU.mult)
        P.tt("dve", t2[:, 0:3, :], psb[6][:, 0:384].rr(m4, m=3), c.rope["sinI"][:, tok].pat(0, [(0, 3), (1, 128)]), ALU.mult)
        P.tt("dve", qiT.all(), t1[:, 0:3, :], t2[:, 0:3, :], ALU.add)
        for k in range(8):
            P.mm(psb[7][:, 0:8], xhT[:, k, :], w_wi[:, k, :], k == 0, k == 7)
        P.ts("dve", wsg.all(), psb[7][:, 0:8], 0.0, ALU.is_gt, 2.0, ALU.mult)
        P.ts("dve", wsg.all(), wsg.all(), -1.0, ALU.add)
        P.tt("dve", wsc.all(), psb[7][:, 0:8], wsg.all(), ALU.mult)
        P.ts("dve", wsc.all(), wsc.all(), 1.0 / 16.0, ALU.mult)
        if stop_at == "proj":
            return
        ibanks = [psb[1], psb[2], psb[3], psb[6]]
        ci = 0
        for q0 in range(0, Sc, 512):
            qn = min(512, Sc - q0)
            for h in range(8):
                bank, r = ibanks[h % 3], rr[ci % 2]
                ci += 1
                pb = 32 * (h % 3)
                P.mm(bank[:, 0:qn], qiT[pb:pb + 32, h // 3, :], kiT4[pb:pb + 32, q0:q0 + qn], True, True)
                P.act(r[:, 0:qn], bank[:, 0:qn], AF.Relu, scale=wsc[:, h:h + 1])
                if h == 0:
                    P.ts("dve", acc[:, q0:q0 + qn], r[:, 0:qn], wsg[:, 0:1], ALU.mult)
                else:
                    P.stt("dve", acc[:, q0:q0 + qn], r[:, 0:qn], wsg[:, h:h + 1], acc[:, q0:q0 + qn], ALU.mult, ALU.add)
        P.tt("dve", acc[:, b * 128:Sc], acc[:, b * 128:Sc], c.caus.all(), ALU.add)

    def stage_B(b):
        Sc = (b + 1) * 128
        if Sc > TOPK:
            c1 = (int(Sc * 0.42) // 128) * 128 if Sc >= 1024 else Sc
            nB = Sc - c1
            half = 32.0
            P.memset("dve", mid.all(), 0.0)
            NSTEP = 22
            for it in range(NSTEP):
                P.op("dve", "tensor_scalar", out=msk[:, 0:c1], in0=acc[:, 0:c1], scalar1=mid[:, 0:1], scalar2=0.0,
                     op0=ALU.is_ge, op1=ALU.add, accum_out=cnt.all())
                if nB:
                    P.act(sjunk[:, 0:nB], acc[:, c1:Sc], AF.Sign, bias=mid[:, 0:1], scale=-1.0, accum_out=cntb.all())
                    P.stt("dve", cnt.all(), cntb.all(), -0.5, cnt.all(), ALU.mult, ALU.add)
                P.ts("dve", ge.all(), cnt.all(), TOPK - 0.5 - nB / 2.0, ALU.is_ge, half, ALU.mult)
                nxt = half / 2 if it < NSTEP - 1 else half
                P.stt("dve", mid.all(), ge.all(), -nxt, mid.all(), ALU.add, ALU.add)
                half = half / 2
                yield
            P.ts("dve", msk[:, 0:Sc], acc[:, 0:Sc], mid[:, 0:1], ALU.is_ge)
        else:
            P.ts("dve", msk[:, 0:Sc], acc[:, 0:Sc], -1.0e29, ALU.is_ge)

    def stage_T(b):
        pbf = psb[0].all().cast(BF16)
        for j0 in range(0, b + 1, 8):
            jn = min(8, b + 1 - j0)
            for jj in range(jn):
                P.tr(pbf[:, jj * 128:(jj + 1) * 128], msk[:, (j0 + jj) * 128:(j0 + jj + 1) * 128], ident.all())
            P.copy("act", mT[:, j0:j0 + jn, :].rr("p j t -> p (j t)"), pbf[:, 0:jn * 128])

    def stage_A(b):
        tok = slice(b * 128, (b + 1) * 128)
        qTb = qTs[b % 2]
        obank = [psb[4], psb[5]]
        dbank = [psb[7], psb[0]]
        meng = "pool" if PIPE else "dve"
        for j in range(b + 1):
            ks = slice(j * 128, (j + 1) * 128)
            lb = [psb[1], psb[2]] if j % 2 == 0 else [psb[3], psb[6]]
            for h in range(8):
                n, m_, e = h // 4, h // 2, h % 2
                i = h // 2
                P.mm(lb[e][:, i * 128:(i + 1) * 128], kTd[64 * e:64 * e + 64, n, ks], qTb[64 * e:64 * e + 64, m_, :], True, True)
            for e in range(2):
                et, pt = eT[j % 2][e], pT[j % 2][e]
                P.act(et.all().rr("p h t -> p (h t)"), lb[e].all(), AF.Exp, scale=0.125)
                P.tt(meng, pt.all(), et.all(), mT[:, j, :].pat(0, [(0, 4), (1, 128)]), ALU.mult)
            if stop_at != "lg":
                for e in range(2):
                    pt = pT[j % 2][e]
                    for n in range(2):
                        rhs = pt[:, 2 * n:2 * n + 2, :].rr("p h t -> p (h t)")
                        cs = slice(2 * e * 128, (2 * e + 2) * 128)
                        st_, sp_ = (j == 0 and e == 0), (j == b and e == 1)
                        P.mm(obank[n][0:64, cs], vaug[:, j, n, 0:64], rhs, st_, sp_, skip_group_check=True)
                        P.mm(dbank[n][0:64, cs], ones_bf.all(), rhs, st_, sp_, skip_group_check=True)
            yield
        if stop_at in ("pv", "lg"):
            return
        yab = ya[b % 2]
        for n in range(2):
            P.op("dve", "reciprocal", out=bcs.all(), in_=dbank[n][0:64, :])
            for e in range(2):
                cs = slice(2 * e * 128, (2 * e + 2) * 128)
                P.tt("dve", yab[:, 4 * n + e:4 * n + e + 3:2, :], obank[n][0:64, cs].rr("p (i t) -> p i t", i=2),
                     bcs[:, cs].rr("p (i t) -> p i t", i=2), ALU.mult)
        P.dma("sp", ya_d[:, :, tok], yab.all())

    def run(gen):
        if gen is not None:
            for _ in gen:
                pass

    def step(gen):
        if gen is None:
            return None
        try:
            next(gen)
            return gen
        except StopIteration:
            return None

    if not PIPE:
        for b in range(NT):
            stage_I(b)
            if stop_at in ("proj", "idx"):
                continue
            run(stage_B(b))
            if stop_at == "thr":
                continue
            stage_T(b)
            if stop_at == "mt":
                continue
            run(stage_A(b))
    else:
        stage_I(0)
        run(stage_B(0))
        stage_T(0)
        for b in range(NT):
            gB = None
            if b + 1 < NT:
                stage_I(b + 1)
                gB = stage_B(b + 1)
            gA = stage_A(b)
            while gA is not None or gB is not None:
                gA = step(gA)
                gB = step(gB)
                gB = step(gB)
            if b + 1 < NT:
                stage_T(b + 1)
    if dump is not None:
        dump("qT", qT.all().rr("p a b -> p (a b)"), BF16)
        dump("qiT", qiT.all().rr("p a b -> p (a b)"), BF16)
        dump("wsc", wsc.all(), F32)
        dump("wsg", wsg.all(), F32)
        if stop_at != "proj":
            dump("acc", acc.all(), F32)
        if stop_at not in ("proj", "idx"):
            dump("msk", msk.all(), BF16)
            dump("lo", mid.all(), F32)
        if stop_at == "lg":
            dump("pT", pT[(NT - 1) % 2][1].all().rr("p a b -> p (a b)"), BF16)
        if stop_at not in ("proj", "idx", "thr"):
            dump("mT", mT.all().rr("p a b -> p (a b)"), BF16)
        if stop_at == "pv":
            for n in range(2):
                P.copy("act", rden.all(), psb[4 + n].all())
                dump(f"oT{n}", rden.all(), F32)
    ph.close()


def merge_phase(P, c, S, x_d, w_in_d, g1c, ysT, ya_d, h_d, wd, uvb_d=None):
    NT = S // 128
    psb = c.psb
    ph = ExitStack()
    w_gs = P.sb(ph, "w_gs", [128, 8, 1024], BF16)
    w_ga = P.sb(ph, "w_ga", [128, 8, 1024], BF16)
    w_sup = P.sb(ph, "w_sup", [128, 4, 1024], BF16)
    w_aup = P.sb(ph, "w_aup", [64, 8, 1024], BF16)
    w_out = P.sb(ph, "w_out", [128, 8, 1024], BF16)
    stg = [P.sb(ph, f"bstg{i}", [128, 1024], F32) for i in range(2)]
    qs = ("sp", "act")

    def cvt(dst, src, k):
        P.act(dst, src, AF.Copy, scale=g1c[:, k:k + 1])
    load_w_cols(P, c, lambda k, st: cvt(w_gs[:, k, :], st[:, 0:1024], k), OFF_GS, 1024, w_in_d, g1c, stg)
    load_w_cols(P, c, lambda k, st: cvt(w_ga[:, k, :], st[:, 0:1024], k), OFF_GA, 1024, w_in_d, g1c, stg)
    for k in range(4):
        P.dma(qs[k % 2], stg[k % 2].all(), wd["w_ssm_up"][k * 128:(k + 1) * 128, :])
        P.copy("act", w_sup[:, k, :], stg[k % 2].all())
    for h in range(8):
        P.dma(qs[h % 2], stg[h % 2][0:64, :], wd["w_attn_up"][h * 64:(h + 1) * 64, :])
        P.copy("act", w_aup[:, h, :], stg[h % 2][0:64, :])
    for k in range(8):
        P.dma(qs[k % 2], stg[k % 2].all(), wd["w_out"][k * 128:(k + 1) * 128, :])
        P.copy("act", w_out[:, k, :], stg[k % 2].all())

    xt = [P.sb(ph, f"bxt{i}", [128, D], F32) for i in range(2)]
    xh = P.sb(ph, "bxh", [128, D], BF16)
    xhT = P.sb(ph, "bxhT", [128, 8, 128], BF16)
    junk = P.sb(ph, "bjunk", [128, D], BF16)
    ss = P.sb(ph, "bss", [128, 1], F32)
    rstd = P.sb(ph, "brstd", [128, 1], F32)
    yat = [P.sb(ph, f"yat{i}", [64, 8, 128], BF16) for i in range(2)]
    sgs = P.sb(ph, "sgs", [128, 512], F32)
    sga = P.sb(ph, "sga", [128, 512], F32)
    m1 = P.sb(ph, "m1", [128, 512], F32)
    m2 = P.sb(ph, "m2", [128, 512], F32)
    merged = P.sb(ph, "merged", [128, 8, 128], BF16)
    ht = [P.sb(ph, f"bht{i}", [128, D], F32) for i in range(2)]

    NCONV = 16384 // 128
    cvf = [P.sb(ph, f"cvf{i}", [128, 2048], F32) for i in range(2)]
    cvb = [P.sb(ph, f"cvb{i}", [128, 2048], BF16) for i in range(2)]

    def conv_step(i):
        rows = slice(i * 128, (i + 1) * 128)
        P.dma("pool", cvf[i % 2].all(), wd["peer_uv"][rows, :])
        P.copy("pool", cvb[i % 2].all(), cvf[i % 2].all())
        P.dma("pool", uvb_d[rows, :], cvb[i % 2].all())
    conv_per_tile = (NCONV + NT - 1) // NT
    conv_i = 0

    for b in range(NT):
        for _ in range(conv_per_tile):
            if uvb_d is not None and conv_i < NCONV:
                conv_step(conv_i)
                conv_i += 1
        tok = slice(b * 128, (b + 1) * 128)
        xb = xt[b % 2]
        P.dma("sp", xb.all(), x_d[tok, :])
        ya = yat[b % 2]
        P.dma("sp", ya.all(), ya_d[:, :, tok])
        c.norm_and_transpose(b, xb, xh, xhT, junk, ss, rstd, psb[0])
        for half in range(2):
            for cc in range(4):
                ci = half * 4 + cc
                cols = slice(ci * 128, (ci + 1) * 128)
                reg = slice(cc * 128, (cc + 1) * 128)
                for k in range(8):
                    P.mm(psb[1][:, reg], w_gs[:, k, cols], xhT[:, k, :], k == 0, k == 7)
                for k in range(8):
                    P.mm(psb[2][:, reg], w_ga[:, k, cols], xhT[:, k, :], k == 0, k == 7)
                for k in range(4):
                    P.mm(psb[3][:, reg], w_sup[:, k, cols], ysT[:, k, tok], k == 0, k == 3)
                for h in range(8):
                    P.mm(psb[4][:, reg], w_aup[:, h, cols], ya[:, h, :], h == 0, h == 7)
            P.act(sgs.all(), psb[1].all(), AF.Sigmoid)
            P.act(sga.all(), psb[2].all(), AF.Sigmoid)
            P.tt("dve", m1.all(), sgs.all(), psb[3].all(), ALU.mult)
            P.tt("dve", m2.all(), sga.all(), psb[4].all(), ALU.mult)
            P.tt("dve", merged[:, half * 4:half * 4 + 4, :].rr("p c t -> p (c t)"), m1.all(), m2.all(), ALU.add)
        hb = ht[b % 2]
        for n2 in range(2):
            cs = slice(n2 * 512, (n2 + 1) * 512)
            for k in range(8):
                P.mm(psb[5 + n2].all(), merged[:, k, :], w_out[:, k, cs], k == 0, k == 7)
            P.tt("dve", hb[:, cs], psb[5 + n2].all(), xb[:, cs], ALU.add)
        P.dma("sp", h_d[tok, :], hb.all())
    ph.close()


def top16(P, src, vals_out, idx_out, tl, par):
    m8a, i8a, m8b, i8b = tl["m8a"][par], tl["i8a"][par], tl["m8b"][par], tl["i8b"][par]
    n = src.shape[-1]
    sc2 = tl["sc2"][par][:, 0:n]
    P.op("dve", "max", out=m8a.all(), in_=src)
    P.op("dve", "max_index", out=i8a.all(), in_max=m8a.all(), in_values=src)
    P.op("dve", "match_replace", out=sc2, in_to_replace=m8a.all(), in_values=src, imm_value=-1.0e30)
    P.op("dve", "max", out=m8b.all(), in_=sc2)
    P.op("dve", "max_index", out=i8b.all(), in_max=m8b.all(), in_values=sc2)
    P.copy("act", vals_out[:, 0:8], m8a.all())
    P.copy("act", vals_out[:, 8:16], m8b.all())
    P.copy("dve", idx_out[:, 0:8], i8a.all().cast(I32))
    P.copy("dve", idx_out[:, 8:16], i8b.all().cast(I32))


def peer_phase(P, c, S, h_d, out_d, pd, uvb_d, NB=16, dump=None):
    NT = S // 128
    psb, ident = c.psb, c.ident
    ph = ExitStack()
    wq = P.sb(ph, "wq", [128, 8, 2048], BF16)
    pk = P.sb(ph, "pk", [128, 16, 128], BF16)
    g2c = P.sb(ph, "g2c", [128, 8], F32)
    g2b = P.sb(ph, "g2b", [128, D], F32)
    gfb = P.sb(ph, "gfb", [128, D], F32)
    P.dma("sp", g2c.all(), pd["g2c"].all())
    P.dma("sp", g2b.all(), pd["g2b"].all())
    P.dma("sp", gfb.all(), pd["gfb"].all())
    hts = [P.sb(ph, f"cht{i}", [128, D], F32) for i in range(2)]
    hh = P.sb(ph, "chh", [128, D], BF16)
    hT = P.sb(ph, "chT", [128, 8, 128], BF16)
    junk = P.sb(ph, "cjunk", [128, D], BF16)
    hn = P.sb(ph, "chn", [128, D], F32)
    ss = P.sb(ph, "css", [128, 1], F32)
    rstd = P.sb(ph, "crstd", [128, 1], F32)
    qT = P.sb(ph, "cqT", [128, 16, 128], BF16)
    SC = P.sb(ph, "SC", [128, 16, 128], F32)
    V16 = P.sb(ph, "V16", [128, 16, 16], F32)
    I16 = P.sb(ph, "I16", [128, 16, 16], F32)
    CS = P.sb(ph, "CS", [128, 8, 256], F32)
    TS = P.sb(ph, "TS", [128, 8, 16], F32)
    POSf = P.sb(ph, "POSf", [128, 8, 16], F32)
    POSi = P.sb(ph, "POSi", [128, 8, 16], I32)
    rowi = P.sb(ph, "rowi", [128, 8, 16], I32)
    coli = P.sb(ph, "coli", [128, 8, 16], I32)
    rowf = P.sb(ph, "rowf", [128, 8, 16], F32)
    colf = P.sb(ph, "colf", [128, 8, 16], F32)
    E = P.sb(ph, "E", [128, 8, 16], F32)
    G = P.sb(ph, "G", [128, 8, 16], F32)
    sm = P.sb(ph, "sm", [128, 8], F32)
    OH = P.sb(ph, "OH", [128, 8, 16, 16], F32)
    OH2 = P.sb(ph, "OH2", [128, 8, 16, 16], F32)
    i1s = P.sb(ph, "i1s", [128, 8, 16], F32)
    i2s = P.sb(ph, "i2s", [128, 8, 16], F32)
    eidf = P.sb(ph, "eidf", [128, 128], F32)
    EID = P.sb(ph, "EID", [128, 128], I32)
    dots = P.sb(ph, "dots", [128, 128], F32)
    actw = P.sb(ph, "actw", [128, 128], F32)
    resd = P.sb(ph, "cres", [128, D], F32)
    outt = [P.sb(ph, f"cout{i}", [128, D], F32) for i in range(2)]
    tl = {k: [P.sb(ph, f"{k}{i}", [128, 8], dt) for i in range(2)]
          for k, dt in (("m8a", F32), ("i8a", U32), ("m8b", F32), ("i8b", U32))}
    tl["sc2"] = [P.sb(ph, f"sc2{i}", [128, 256], F32) for i in range(2)]
    wst = ExitStack()
    stg = [P.sb(wst, f"cstg{i}", [128, 2048], F32) for i in range(2)]
    qs = ("sp", "act")
    for k in range(8):
        P.dma(qs[k % 2], stg[k % 2].all(), pd["peer_wq"][k * 128:(k + 1) * 128, :])
        P.act(wq[:, k, :], stg[k % 2].all(), AF.Copy, scale=g2c[:, k:k + 1])
    P.dma("sp", stg[0].all(), pd["pk"][0:128].rr("p a n -> p (a n)"))
    P.copy("act", pk.all().rr("p a n -> p (a n)"), stg[0].all())
    wst.close()
    UV = [P.sb(ph, f"UV{i}", [128, 2048], BF16) for i in range(NB)]
    dg = [P.sb(ph, f"dg{i}", [128, 128], BF16) for i in range(4)]
    hnb = P.sb(ph, "hnb", [128, D], BF16)
    junkb = P.sb(ph, "cjunkb", [128, D], F32)
    tuv = uvb_d.all()

    EIDs = [EID, P.sb(ph, "EID1", [128, 128], I32)]
    Gs = [G, P.sb(ph, "G1", [128, 8, 16], F32)]
    hnbs = [hnb, P.sb(ph, "hnb1", [128, D], BF16)]
    ssf = P.sb(ph, "cssf", [128, 1], F32)
    rstdf = P.sb(ph, "crstdf", [128, 1], F32)
    pacc = [psb[6], psb[7]]

    def front(b):
        tok = slice(b * 128, (b + 1) * 128)
        ht, EIDb, Gb, hnbb = hts[b % 2], EIDs[b % 2], Gs[b % 2], hnbs[b % 2]
        P.dma("sp", ht.all(), h_d[tok, :])
        P.act(junk.all(), ht.all(), AF.Square, accum_out=ss.all())
        P.ts("dve", ss.all(), ss.all(), 1.0 / D, ALU.mult, EPS, ALU.add)
        P.act(ss.all(), ss.all(), AF.Sqrt)
        P.op("dve", "reciprocal", out=rstd.all(), in_=ss.all())
        P.ts("dve", hh.all(), ht.all(), rstd[:, 0:1], ALU.mult)
        yield
        P.stt("dve", hn.all(), ht.all(), rstd[:, 0:1], g2b.all(), ALU.mult, ALU.mult)
        P.copy("act", hnbb.all(), hn.all())
        pbf = psb[0].all().cast(BF16)
        for k in range(8):
            P.tr(pbf[:, k * 128:(k + 1) * 128], hh[:, k * 128:(k + 1) * 128], ident.all())
        P.copy("act", hT.all().rr("p k t -> p (k t)"), pbf)
        yield
        for ch in range(16):
            bank = psb[1 + (ch // 4) % 2]
            for k in range(8):
                P.mm(bank[:, (ch % 4) * 128:(ch % 4 + 1) * 128], wq[:, k, ch * 128:(ch + 1) * 128], hT[:, k, :], k == 0, k == 7)
            if ch % 4 == 3:
                P.copy("act", qT[:, ch - 3:ch + 1, :].rr("p a t -> p (a t)"), bank.all())
                yield
        sbanks = [psb[3], psb[4], psb[5], psb[0]]
        for ch in range(16):
            bank = sbanks[ch // 4]
            P.mm(bank[:, (ch % 4) * 128:(ch % 4 + 1) * 128], qT[:, ch, :], pk[:, ch, :], True, True)
            if ch % 4 == 3:
                P.copy("act", SC[:, ch - 3:ch + 1, :].rr("p a n -> p (a n)"), bank.all())
        yield
        for ch in range(16):
            top16(P, SC[:, ch, :], V16[:, ch, :], I16[:, ch, :], tl, ch % 2)
            if ch % 2 == 1:
                yield
        P.tt("dve", CS.all().rr("p h (i j) -> p h i j", i=16), V16.all().pat(0, [(32, 8), (1, 16), (0, 16)]),
             V16.all().pat(16, [(32, 8), (0, 16), (1, 16)]), ALU.add)
        for h in range(8):
            top16(P, CS[:, h, :], TS[:, h, :], POSf[:, h, :], tl, h % 2)
            if h % 2 == 1:
                yield
        P.tt("dve", E.all(), TS.all(), TS.all().pat(0, [(16, 8), (0, 16)]), ALU.subtract)
        P.act(E.all(), E.all(), AF.Exp)
        P.op("dve", "tensor_reduce", out=sm.all(), in_=E.all(), axis=AX.X, op=ALU.add)
        P.op("dve", "reciprocal", out=sm.all(), in_=sm.all())
        P.tt("dve", Gb.all(), E.all(), sm.all().pat(0, [(1, 8), (0, 16)]), ALU.mult)
        yield
        P.copy("dve", POSi.all(), POSf.all())
        P.op("dve", "tensor_single_scalar", out=rowi.all(), in_=POSi.all(), scalar=4, op=ALU.arith_shift_right)
        P.op("dve", "tensor_single_scalar", out=coli.all(), in_=POSi.all(), scalar=15, op=ALU.bitwise_and)
        P.copy("dve", rowf.all(), rowi.all())
        P.copy("dve", colf.all(), coli.all())
        io16 = c.iota_f[:, 0:16].pat(0, [(0, 8), (0, 16), (1, 16)])
        for src, off, dst, oh in ((rowf, 0, i1s, OH), (colf, 16, i2s, OH2)):
            P.tt("dve", oh.all(), src.all().pat(0, [(16, 8), (1, 16), (0, 16)]), io16, ALU.is_equal)
            P.tt("dve", oh.all(), oh.all(), I16.all().pat(off, [(32, 8), (0, 16), (1, 16)]), ALU.mult)
            P.op("dve", "tensor_reduce", out=dst.all().rr("p h k -> p (h k)"), in_=oh.all().rr("p h k i -> p (h k) i"),
                 axis=AX.X, op=ALU.add)
            yield
        P.stt("dve", eidf.all(), i1s.all().rr("p h k -> p (h k)"), 128.0, i2s.all().rr("p h k -> p (h k)"), ALU.mult, ALU.add)
        P.copy("dve", EIDb.all(), eidf.all())

    def advance(gen, n):
        if gen is None:
            return None
        try:
            for _ in range(n):
                next(gen)
        except StopIteration:
            return None
        return gen

    GS = NB // 2
    NGRP = 128 // GS
    advance(front(0), 1000)
    for b in range(NT):
        tok = slice(b * 128, (b + 1) * 128)
        ht, EIDb, hnbb = hts[b % 2], EIDs[b % 2], hnbs[b % 2]
        Gf = Gs[b % 2].all().rr("p a b -> p (a b)")
        nxt = front(b + 1) if b + 1 < NT else None
        for g in range(NGRP):
            gs = slice(g * GS, (g + 1) * GS)
            for e in range(g * GS, (g + 1) * GS):
                uv = UV[e % NB]
                P.gather(uv.all(), tuv, EIDb[:, e:e + 1])
                P.stt("dve", junkb.all(), uv[:, 0:D], 1.0, hnbb.all(), ALU.mult, ALU.mult, accum_out=dots[:, e:e + 1])
            P.act(actw[:, gs], dots[:, gs], AF.Gelu_apprx_tanh)
            P.tt("dve", actw[:, gs], actw[:, gs], Gf[:, gs], ALU.mult)
            for e in range(g * GS, (g + 1) * GS):
                uv, dgt = UV[e % NB], dg[e % 4]
                P.act(dgt.all(), ident.all(), AF.Copy, scale=actw[:, e:e + 1])
                for n2 in range(2):
                    P.mm(pacc[n2].all(), dgt.all(), uv[:, D + n2 * 512:D + (n2 + 1) * 512], e == 0, e == 127)
            nxt = advance(nxt, 2)
        advance(nxt, 1000)
        for n2 in range(2):
            P.tt("dve", resd[:, n2 * 512:(n2 + 1) * 512], pacc[n2].all(), ht[:, n2 * 512:(n2 + 1) * 512], ALU.add)
        P.act(junk.all(), resd.all(), AF.Square, accum_out=ssf.all())
        P.ts("dve", ssf.all(), ssf.all(), 1.0 / D, ALU.mult, EPS, ALU.add)
        P.act(ssf.all(), ssf.all(), AF.Sqrt)
        P.op("dve", "reciprocal", out=rstdf.all(), in_=ssf.all())
        ot = outt[b % 2]
        P.stt("dve", ot.all(), resd.all(), rstdf[:, 0:1], gfb.all(), ALU.mult, ALU.mult)
        P.dma("sp", out_d[tok, :], ot.all())
    ph.close()


def late_param_shapes():
    return {"w_ssm_up": [513, 1024], "w_attn_up": [513, 1024], "w_out": [1025, 1024], "g2c": [128, 8],
            "g2b": [128, 1024], "gfb": [128, 1024], "peer_wq": [1025, 2048], "pk": [129, 16, 128],
            "peer_uv": [16385, 2048]}


def late_host_layout(inp):
    g2 = np.asarray(inp["norm2_g"], dtype=np.float32)[0]
    gf = np.asarray(inp["norm_f_g"], dtype=np.float32)
    k1, k2 = np.asarray(inp["peer_k1"])[0], np.asarray(inp["peer_k2"])[0]
    pk = np.stack([k1, k2], 1).reshape(16, 128, 128).transpose(2, 0, 1)
    d = {"w_ssm_up": np.asarray(inp["w_ssm_up"])[0], "w_attn_up": np.asarray(inp["w_attn_up"])[0],
         "w_out": np.asarray(inp["w_out"])[0], "g2c": g2.reshape(8, 128).T,
         "g2b": np.broadcast_to(g2[None, :], (128, 1024)), "gfb": np.broadcast_to(gf[None, :], (128, 1024)),
         "peer_wq": np.asarray(inp["peer_wq"])[0], "pk": pk,
         "peer_uv": np.concatenate([np.asarray(inp["peer_u"])[0], np.asarray(inp["peer_v"])[0]], 1)}
    return {k: np.ascontiguousarray(v, dtype=np.float32) for k, v in d.items()}


def ssm_param_shapes():
    return {"ar_sm": [128, 32], "ai_sm": [128, 32], "ldt_sm": [128, 32],
            "bU1": [128, 32, 16], "bU2": [128, 32, 16], "cT1": [128, 32, 16], "cT2": [128, 32, 16],
            "ar_cm": [128, 4, 64], "ai_cm": [128, 4, 64], "ldt_cm": [128, 4, 64],
            "br_cm": [128, 4, 64], "bi_cm": [128, 4, 64], "dskip": [128, 4], "w_glu": [513, 512]}


def ssm_host_layout(inp):
    a_re, a_im, log_dt = inp["a_re"][0], inp["a_im"][0], inp["log_dt"][0]
    b_re, b_im, c_re, c_im = inp["b_re"][0], inp["b_im"][0], inp["c_re"][0], inp["c_im"][0]
    d = {}
    d["ar_sm"] = np.concatenate([a_re.T, a_re.T], 0)
    d["ai_sm"] = np.concatenate([a_im.T, a_im.T], 0)
    d["ldt_sm"] = np.broadcast_to(log_dt[None, :], (128, 32))
    brT, biT = b_re.transpose(1, 0, 2), b_im.transpose(1, 0, 2)
    d["bU1"] = np.concatenate([brT, biT], 0)
    d["bU2"] = np.concatenate([biT, brT], 0)
    crT, ciT = c_re.transpose(2, 0, 1), c_im.transpose(2, 0, 1)
    d["cT1"] = np.concatenate([crT, ciT], 0)
    d["cT2"] = np.concatenate([ciT, crT], 0)
    q = np.arange(128)
    gq = (np.arange(4)[None, :] * 8 + (q // 16)[:, None])
    d["ar_cm"] = a_re[gq]
    d["ai_cm"] = a_im[gq]
    d["ldt_cm"] = np.broadcast_to(log_dt[gq][:, :, None], (128, 4, 64))
    d["br_cm"] = b_re[gq, :, (q % 16)[:, None]]
    d["bi_cm"] = b_im[gq, :, (q % 16)[:, None]]
    d["dskip"] = inp["d_skip"][0].reshape(4, 128).T
    d["w_glu"] = inp["w_glu"][0]
    return {k: np.ascontiguousarray(v, dtype=np.float32) for k, v in d.items()}


def build(nc, S, stage_stop=None, dbg=None):
    NT = S // 128
    NCH = S // LCH
    TOPK = min(256, S // 4)
    es = ExitStack()
    P = Prog(nc, es)
    c = Ctx()
    c.P = P
    dbg = dbg if dbg is not None else {}

    x_d = P.dram("x", [S, D], F32, "ExternalInput")
    g1c_d = P.dram("g1c", [128, 8], F32, "ExternalInput")
    w_in_d = P.dram("w_in", [D + 1, IN_W], F32, "ExternalInput")
    out_d = P.dram("out", [S, D], F32, "ExternalOutput")

    def dbg_out(name, shape, dt=F32):
        t = P.dram("dbg_" + name, shape, dt, "ExternalOutput")
        dbg[name] = t
        return t

    blk = es.enter_context(nc.Block())
    holder = {}

    def body(_sync):
        glob = ExitStack()
        ident = P.sb(glob, "ident", [128, 128], BF16)
        identf = P.sb(glob, "identf", [128, 128], F32)
        iota_i = P.sb(glob, "iota_i", [128, 128], I32)
        pid_i = P.sb(glob, "pid_i", [128, 1], I32)
        pid_f = P.sb(glob, "pid_f", [128, 1], F32)
        iota_f = P.sb(glob, "iota_f", [128, 128], F32)
        P.op("pool", "iota", out=iota_i.all(), pattern=[[1, 128]], base=0, channel_multiplier=0)
        P.op("pool", "iota", out=pid_i.all(), pattern=[[0, 1]], base=0, channel_multiplier=1)
        P.copy("dve", iota_f.all(), iota_i.all())
        P.copy("dve", pid_f.all(), pid_i.all())
        P.ts("dve", identf.all(), iota_f.all(), pid_f[:, 0:1], ALU.is_equal)
        P.copy("dve", ident.all(), identf.all())
        caus = P.sb(glob, "caus", [128, 128], F32)
        P.ts("dve", caus.all(), iota_f.all(), pid_f[:, 0:1], ALU.is_gt, NEG, ALU.mult)
        g1c = P.sb(glob, "g1c", [128, 8], F32)
        ng1c = P.sb(glob, "ng1c", [128, 8], F32)
        P.dma("sp", g1c.all(), g1c_d.all())
        P.ts("dve", ng1c.all(), g1c.all(), -1.0, ALU.mult)
        c.ident, c.identf, c.iota_f, c.pid_f, c.caus = ident, identf, iota_f, pid_f, caus

        psb = [P.ps(glob, f"psb{i}", [128, 512], F32) for i in range(8)]
        c.psb = psb

        res = ExitStack()
        uT = P.sb(res, "uys", [128, 4, S], BF16)
        res_a = ExitStack()
        res_a_close = res_a.close
        kTd = P.sb(res_a, "kTd", [128, 2, S], BF16)
        kiT4 = P.sb(res_a, "kiT4", [128, S], BF16)
        vaug = P.sb(res_a, "vaug", [128, NT, 2, 80], BF16)
        P.memset("pool", vaug.all(), 1.0)

        s1 = ExitStack()
        rope_tables(P, s1, S, c)
        stg = [P.sb(s1, f"stg{i}", [128, 1024], F32) for i in range(2)]
        w_u = P.sb(s1, "w_u", [128, 8, 512], BF16)
        w_kd = P.sb(s1, "w_kd", [128, 8, 4, 128], BF16)
        w_ki = P.sb(s1, "w_ki", [128, 8, 2, 128], BF16)
        w_v = P.sb(s1, "w_v", [128, 8, 128], BF16)

        def cvt(dst, src, k, neg=False):
            P.act(dst, src, AF.Copy, scale=(ng1c if neg else g1c)[:, k:k + 1])

        load_w_cols(P, c, lambda k, st: cvt(w_u[:, k, :], st[:, 0:512], k), OFF_U, 512, w_in_d, g1c, stg)

        def k_cvt(k, st):
            for n in range(2):
                for dup in range(2):
                    cvt(w_kd[:, k, n, dup * 64:(dup + 1) * 64], st[:, n * 64:(n + 1) * 64], k)
                    cvt(w_kd[:, k, 2 + n, dup * 64:dup * 64 + 32], st[:, n * 64 + 32:n * 64 + 64], k, neg=True)
                    cvt(w_kd[:, k, 2 + n, dup * 64 + 32:dup * 64 + 64], st[:, n * 64:n * 64 + 32], k)
        load_w_cols(P, c, k_cvt, OFF_K, 128, w_in_d, g1c, stg)

        def ki_cvt(k, st):
            for r in range(4):
                cvt(w_ki[:, k, 0, r * 32:(r + 1) * 32], st[:, 0:32], k)
                cvt(w_ki[:, k, 1, r * 32:r * 32 + 16], st[:, 16:32], k, neg=True)
                cvt(w_ki[:, k, 1, r * 32 + 16:r * 32 + 32], st[:, 0:16], k)
        load_w_cols(P, c, ki_cvt, OFF_KI, 32, w_in_d, g1c, stg)
        load_w_cols(P, c, lambda k, st: cvt(w_v[:, k, :], st[:, 0:128], k), OFF_V, 128, w_in_d, g1c, stg)

        xt = [P.sb(s1, f"xt{i}", [128, D], F32) for i in range(2)]
        xh = P.sb(s1, "xh", [128, D], BF16)
        xhT = P.sb(s1, "xhT", [128, 8, 128], BF16)
        junk = P.sb(s1, "junk", [128, D], BF16)
        ss = P.sb(s1, "ss", [128, 1], F32)
        rstd = P.sb(s1, "rstd", [128, 1], F32)
        r1 = P.sb(s1, "r1", [128, 128], F32)
        r2 = P.sb(s1, "r2", [128, 128], F32)

        def norm_and_transpose(b, xt_b, xh, xhT, junk, ss, rstd, psT):
            P.act(junk.all(), xt_b.all(), AF.Square, accum_out=ss.all())
            P.act(ss.all(), ss.all(), AF.Sqrt, scale=1.0 / D, bias=EPS) if False else None
            P.ts("dve", ss.all(), ss.all(), 1.0 / D, ALU.mult, EPS, ALU.add)
            P.act(ss.all(), ss.all(), AF.Sqrt)
            P.op("dve", "reciprocal", out=rstd.all(), in_=ss.all())
            P.ts("dve", xh.all(), xt_b.all(), rstd[:, 0:1], ALU.mult)
            pb = psT.all().cast(BF16)
            for k in range(8):
                P.tr(pb[:, k * 128:(k + 1) * 128], xh[:, k * 128:(k + 1) * 128], ident.all())
            P.copy("act", xhT.all().rr("p k t -> p (k t)"), pb)
        c.norm_and_transpose = norm_and_transpose

        for b in range(NT):
            xb = xt[b % 2]
            P.dma("sp", xb.all(), x_d[b * 128:(b + 1) * 128, :])
            norm_and_transpose(b, xb, xh, xhT, junk, ss, rstd, psb[0])
            tok = slice(b * 128, (b + 1) * 128)
            for m in range(4):
                for k in range(8):
                    P.mm(psb[1][:, m * 128:(m + 1) * 128], w_u[:, k, m * 128:(m + 1) * 128], xhT[:, k, :], k == 0, k == 7)
            P.copy("act", uT[:, :, tok], psb[1].all().rr("p (m t) -> p m t", m=4))
            for m in range(4):
                for k in range(8):
                    P.mm(psb[2][:, m * 128:(m + 1) * 128], w_kd[:, k, m, :], xhT[:, k, :], k == 0, k == 7)
            for n in range(2):
                P.tt("dve", r1.all(), psb[2][:, n * 128:(n + 1) * 128], c.rope["cosA"][:, tok], ALU.mult)
                P.tt("dve", r2.all(), psb[2][:, (2 + n) * 128:(3 + n) * 128], c.rope["sinA"][:, tok], ALU.mult)
                P.tt("dve", kTd[:, n, tok], r1.all(), r2.all(), ALU.add)
            for m in range(2):
                for k in range(8):
                    P.mm(psb[3][:, m * 128:(m + 1) * 128], w_ki[:, k, m, :], xhT[:, k, :], k == 0, k == 7)
            for k in range(8):
                P.mm(psb[3][:, 256:384], xhT[:, k, :], w_v[:, k, :], k == 0, k == 7)
            P.tt("dve", r1.all(), psb[3][:, 0:128], c.rope["cosI"][:, tok], ALU.mult)
            P.tt("dve", r2.all(), psb[3][:, 128:256], c.rope["sinI"][:, tok], ALU.mult)
            P.tt("dve", kiT4[:, tok], r1.all(), r2.all(), ALU.add)
            P.copy("act", vaug[:, b, :, 0:64], psb[3][:, 256:384].rr("p (n d) -> p n d", n=2))
        if stage_stop == "s1":
            for nm in ("cosA", "sinA", "cosI", "sinI"):
                t = dbg_out(nm, [128, S], BF16)
                P.dma("sp", t.all(), c.rope[nm].all())
        s1.close()

        if stage_stop == "s1":
            t = dbg_out("uT", [128, 4 * S], BF16)
            P.dma("sp", t.all(), uT.all().rr("p m t -> p (m t)"))
            t = dbg_out("kTd", [128, 2 * S], BF16)
            P.dma("sp", t.all(), kTd.all().rr("p m t -> p (m t)"))
            t = dbg_out("kiT4", [128, S], BF16)
            P.dma("sp", t.all(), kiT4.all())
            t = dbg_out("vaug", [128, NT * 160], BF16)
            P.dma("sp", t.all(), vaug.all().rr("p a n d -> p (a n d)"))
            P.finish(list(dbg.values()))
            res_a.close()
            res.close()
            glob.close()
            return

        sp = {nm: P.dram(nm, shp, F32, "ExternalInput") for nm, shp in ssm_param_shapes().items()}
        if stage_stop == "s2":
            def dump(name, view, dt):
                t = dbg_out(name, list(view.shape), dt)
                P.dma("sp", t.all(), view)
            c.dump = dump
        ssm_phase(P, c, S, uT, sp)
        if stage_stop == "s2":
            t = dbg_out("ysT", [128, 4 * S], BF16)
            P.dma("sp", t.all(), uT.all().rr("p m t -> p (m t)"))
            P.finish(list(dbg.values()))
            res_a.close()
            res.close()
            glob.close()
            return
        ya_d = P.dram("ya_scr", [64, 8, S], BF16, "ExternalOutput" if stage_stop == "a" else "Internal")
        astop = stage_stop[2:] if (stage_stop or "").startswith("a:") else None

        def adump(name, view, dt):
            t = dbg_out(name, list(view.shape), dt)
            P.dma("sp", t.all(), view)
        attn_phase(P, c, S, x_d, w_in_d, g1c, ng1c, kTd, kiT4, vaug, ya_d, stop_at=astop, dump=adump if astop else None)
        res_a.close()
        if astop:
            P.finish(list(dbg.values()))
            res.close()
            glob.close()
            return
        if stage_stop == "a":
            dbg["ya_scr"] = ya_d
            P.finish([ya_d])
            res.close()
            glob.close()
            return
        pd = {nm: P.dram(nm, shp, F32, "ExternalInput") for nm, shp in late_param_shapes().items()}
        h_d = P.dram("h_scr", [S, D], F32, "ExternalOutput" if stage_stop == "b" else "Internal")
        uvb_d = P.dram("uvb_scr", [16384, 2048], BF16, "ExternalOutput" if stage_stop == "cdbg" else "Internal")
        merge_phase(P, c, S, x_d, w_in_d, g1c, uT, ya_d, h_d, pd, uvb_d)
        res.close()
        if stage_stop == "b":
            dbg["h_scr"] = h_d
            P.finish([h_d])
            glob.close()
            return
        def cdump(name, view, dt):
            t = dbg_out(name, list(view.shape), dt)
            P.dma("sp", t.all(), view)
        peer_phase(P, c, S, h_d, out_d, pd, uvb_d, dump=cdump if stage_stop == "cdbg" else None)
        P.finish([out_d] + list(dbg.values()) + ([uvb_d] if stage_stop == "cdbg" else []))
        glob.close()

    holder["rest"] = lambda P, c, env: None
    blk.sync(body)
    es.close()
    return P, dbg


PADDED = ("w_in", "w_glu", "w_ssm_up", "w_attn_up", "w_out", "peer_wq", "pk", "peer_uv")


def core_inputs(shared, xb):
    im = dict(shared)
    im["x"] = np.ascontiguousarray(xb, dtype=np.float32)
    flat = im["x"].reshape(-1)
    for nm in PADDED:
        a = shared[nm]
        row = flat[:a[0].size].reshape((1,) + a.shape[1:])
        im[nm] = np.concatenate([a, row], 0)
    return im


def kernel(**inputs):
    inputs = {k: np.asarray(v) for k, v in inputs.items()}
    B, S, _ = inputs["x"].shape
    assert B == NCORES
    g1 = inputs["norm1_g"].astype(np.float32)[0]
    shared = {"g1c": np.ascontiguousarray(g1.reshape(8, 128).T),
              "w_in": np.ascontiguousarray(inputs["w_in"].astype(np.float32)[0])}
    shared.update(ssm_host_layout(inputs))
    shared.update(late_host_layout(inputs))
    nc = bass.Bass("TRN2", target_bir_lowering=False)
    build(nc, S)
    x = inputs["x"].astype(np.float32)
    in_maps = [core_inputs(shared, x[b]) for b in range(NCORES)]
    res = run_bass_kernel_spmd(nc, in_maps, core_ids=list(range(NCORES)))
    out = np.stack([np.asarray(res.results[b]["out"], dtype=np.float32) for b in range(NCORES)], 0)
    return out
```

```python
import math
from contextlib import ExitStack

import numpy as np
import concourse.bass as bass
import concourse.mybir as mybir
from concourse.bass_utils import run_bass_kernel_spmd

F32 = mybir.dt.float32
BF16 = mybir.dt.bfloat16
I32 = mybir.dt.int32
U32 = mybir.dt.uint32
ALU = mybir.AluOpType
AF = mybir.ActivationFunctionType
AX = mybir.AxisListType

D = 1024
NCORES = 8
SSM_W = 512
NG = 32
NP_ = 64
LCH = 16
EPS = 1e-6
NEG = -1.0e30
STRICT = True
DUMP2 = False
OUTK = ("out", "accum_out", "out_max", "out_indices")


class V:
    __slots__ = ("t", "ap")

    def __init__(self, t, ap):
        self.t = t
        self.ap = ap

    def __getitem__(self, k):
        return V(self.t, self.ap[k])

    def rr(self, pat, **kw):
        return V(self.t, self.ap.rearrange(pat, **kw))

    def bc(self, shape):
        return V(self.t, self.ap.to_broadcast(list(shape)))

    def cast(self, dt):
        return V(self.t, self.ap.bitcast(dt))

    def pat(self, off, pattern):
        a = self.ap
        return V(self.t, bass.AP(a.tensor, a.offset + off, [list(a.ap[0])] + [list(p) for p in pattern]))

    @property
    def shape(self):
        return self.ap.shape


class Tl:
    def __init__(self, base_ap, name, dram=False):
        self.base = base_ap
        self.name = name
        self.dram = dram
        self.w = None
        self.r = {}
        self.dsem = None

    def __getitem__(self, k):
        return V(self, self.base[k])

    def all(self):
        return V(self, self.base)


class Prog:
    EPOCH = 14000

    def __init__(self, nc, es):
        self.nc = nc
        self.es = es
        self.eng = {"pe": nc.tensor, "dve": nc.vector, "act": nc.scalar, "pool": nc.gpsimd, "sp": nc.sync}
        self.sems = []
        self.semeng = []
        self.cur = {}
        self.cnt = {}
        self.known = {e: {} for e in self.eng}
        self.dcnt = {}
        self.ninstr = 0
        self.freed = {}
        self.log = None
        for e in self.eng:
            self._newsem(e)

    def _newsem(self, e):
        s = self.es.enter_context(self.nc.semaphore(f"s{len(self.sems)}"))
        self.sems.append(s)
        self.semeng.append(e)
        idx = len(self.sems) - 1
        if e is not None:
            self.cur[e] = idx
            self.cnt[e] = 0
        else:
            self.dcnt[idx] = 0
        return idx

    def sb(self, es, name, shape, dt):
        self.uid = getattr(self, "uid", 0) + 1
        h = es.enter_context(self.nc.sbuf_tensor(f"sb{self.uid}_" + name, list(shape), dt))
        t = Tl(h[:], name)
        t.r = dict(self.freed)
        es.callback(self._on_free, t)
        return t

    def _on_free(self, t):
        toks = dict(t.r)
        if t.w is not None:
            toks[t.w[0]] = max(toks.get(t.w[0], 0), t.w[1])
        for si, val in toks.items():
            if self.freed.get(si, 0) < val:
                self.freed[si] = val

    def ps(self, es, name, shape, dt):
        h = es.enter_context(self.nc.psum_tensor("ps_" + name, list(shape), dt))
        return Tl(h[:], name)

    def dram(self, name, shape, dt, kind):
        h = self.nc.dram_tensor(name, list(shape), dt, kind=kind)
        return Tl(h.ap(), name, dram=True)

    def _need(self, e, tok, raw, dma=False):
        if tok is None:
            return
        si, val = tok
        owner = self.semeng[si]
        if owner == e and (e == "pe" or (not raw and not STRICT)) and not dma:
            return
        k = self.known[e]
        if k.get(si, 0) >= val:
            return
        self.eng[e].wait_ge(self.sems[si], val)
        self.ninstr += 1
        k[si] = val
        if self.log is not None:
            self.log.append(f"{e}: WAIT s{si}({self.semeng[si]}) >= {val}")

    def _deps(self, e, reads, writes, skip_w_sem=None, dma=False):
        for t in reads:
            self._need(e, t.w, True, dma)
        for t in writes:
            if t.w is not None and t.w[0] != skip_w_sem:
                self._need(e, t.w, False, dma)
            for si, val in t.r.items():
                self._need(e, (si, val), False, dma)

    def _mark(self, tok, reads, writes):
        si, val = tok
        for t in reads:
            if t.r.get(si, 0) < val:
                t.r[si] = val
        for t in writes:
            t.w = tok
            t.r = {}

    def op(self, e, fn, **kw):
        reads, writes, args = [], [], {}
        for k, v in kw.items():
            if isinstance(v, V):
                (writes if k in OUTK else reads).append(v.t)
                args[k] = v.ap
            else:
                args[k] = v
        self._deps(e, reads, writes)
        ins = getattr(self.eng[e], fn)(**args)
        if self.log is not None:
            self.log.append(f"{e}: {fn} W={[t.name for t in writes]} R={[t.name for t in reads]} -> {self.cnt[e] + 1}")
        if self.cnt[e] >= self.EPOCH:
            self._newsem(e)
        si = self.cur[e]
        self.cnt[e] += 1
        ins.then_inc(self.sems[si], 1)
        self.ninstr += 1
        self._mark((si, self.cnt[e]), reads, writes)
        return ins

    def _dsem(self, t):
        if t.dsem is None:
            t.dsem = self._newsem(None)
        return t.dsem

    def dma(self, q, out, in_, semtile=None, extra_reads=(), **kw):
        sbt = semtile if semtile is not None else (out.t if not out.t.dram else in_.t)
        ds = self._dsem(sbt)
        reads = [in_.t] + [x.t for x in extra_reads]
        writes = [out.t]
        self._deps(q, reads, writes, skip_w_sem=ds, dma=True)
        ins = self.eng[q].dma_start(out=out.ap, in_=in_.ap, **kw)
        self.dcnt[ds] += 16
        ins.then_inc(self.sems[ds], 16)
        self.ninstr += 1
        self._mark((ds, self.dcnt[ds]), reads, writes)

    def gather(self, out, table, idx):
        ds = self._dsem(out.t)
        reads = [table.t, idx.t]
        writes = [out.t]
        self._deps("pool", reads, writes, skip_w_sem=ds, dma=True)
        ins = self.nc.gpsimd.indirect_dma_start(
            out=out.ap, out_offset=None, in_=table.ap,
            in_offset=bass.IndirectOffsetOnAxis(ap=idx.ap, axis=0))
        self.dcnt[ds] += 16
        ins.then_inc(self.sems[ds], 16)
        self.ninstr += 1
        self._mark((ds, self.dcnt[ds]), reads, writes)

    def finish(self, tiles):
        for t in tiles:
            self._need("sp", t.w, True)

    def mm(self, out, lhsT, rhs, start, stop, **kw):
        return self.op("pe", "matmul", out=out, lhsT=lhsT, rhs=rhs, start=start, stop=stop, **kw)

    def tr(self, out, in_, ident):
        return self.op("pe", "transpose", out=out, in_=in_, identity=ident)

    def act(self, out, in_, func, **kw):
        return self.op("act", "activation", out=out, in_=in_, func=func, **kw)

    def tt(self, e, out, in0, in1, op):
        return self.op(e, "tensor_tensor", out=out, in0=in0, in1=in1, op=op)

    def ts(self, e, out, in0, s1, op0, s2=None, op1=None, **kw):
        if op1 is None:
            return self.op(e, "tensor_scalar", out=out, in0=in0, scalar1=s1, scalar2=None, op0=op0, **kw)
        if isinstance(s1, V) != isinstance(s2, V):
            self.op(e, "tensor_scalar", out=out, in0=in0, scalar1=s1, scalar2=None, op0=op0)
            return self.op(e, "tensor_scalar", out=out, in0=out, scalar1=s2, scalar2=None, op0=op1, **kw)
        return self.op(e, "tensor_scalar", out=out, in0=in0, scalar1=s1, scalar2=s2, op0=op0, op1=op1, **kw)

    def stt(self, e, out, in0, scalar, in1, op0, op1, **kw):
        return self.op(e, "scalar_tensor_tensor", out=out, in0=in0, scalar=scalar, in1=in1, op0=op0, op1=op1, **kw)

    def copy(self, e, out, in_):
        if e == "act":
            return self.act(out, in_, AF.Copy)
        return self.op(e, "tensor_copy", out=out, in_=in_)

    def memset(self, e, out, val):
        return self.op(e, "memset", ap=out, constant=val) if False else self._memset(e, out, val)

    def _memset(self, e, out, val):
        self._deps(e, [], [out.t])
        ins = self.eng[e].memset(out.ap, val)
        if self.cnt[e] >= self.EPOCH:
            self._newsem(e)
        si = self.cur[e]
        self.cnt[e] += 1
        ins.then_inc(self.sems[si], 1)
        self.ninstr += 1
        self._mark((si, self.cnt[e]), [], [out.t])


OFF_U, OFF_Q, OFF_K, OFF_V, OFF_QI, OFF_KI, OFF_WI, OFF_GS, OFF_GA = 0, 512, 1024, 1152, 1280, 1536, 1568, 1576, 2600
IN_W = 3624
TWO_PI = 2.0 * math.pi


class Ctx:
    pass


def rope_tables(P, es, S, c):
    tabs = {fn + nm: P.sb(es, f"rope_{fn}{nm}", [128, S], BF16) for nm in ("A", "I") for fn in ("cos", "sin")}
    tmp = ExitStack()
    pid = P.sb(tmp, "rt_pid", [128, 1], I32)
    pm = P.sb(tmp, "rt_pm", [128, 1], I32)
    pf = P.sb(tmp, "rt_pf", [128, 1], F32)
    inv = P.sb(tmp, "rt_inv", [128, 2], F32)
    posi = P.sb(tmp, "rt_posi", [128, S], I32)
    pos = P.sb(tmp, "rt_pos", [128, S], F32)
    ang = P.sb(tmp, "rt_ang", [128, S], F32)
    t1 = P.sb(tmp, "rt_t1", [128, S], F32)
    ti = P.sb(tmp, "rt_ti", [128, S], I32)
    P.op("pool", "iota", out=pid.all(), pattern=[[0, 1]], base=0, channel_multiplier=1)
    P.op("pool", "iota", out=posi.all(), pattern=[[1, S]], base=0, channel_multiplier=0)
    P.copy("dve", pos.all(), posi.all())
    for j, (msk, dim) in enumerate(((31, 64), (15, 32))):
        P.op("dve", "tensor_single_scalar", out=pm.all(), in_=pid.all(), scalar=msk, op=ALU.bitwise_and)
        P.copy("dve", pf.all(), pm.all())
        P.act(inv[:, j:j + 1], pf.all(), AF.Exp, scale=-math.log(10000.0) * 2.0 / dim)
    outs = {}
    for j, nm in enumerate(("A", "I")):
        for k, (fn, shift) in enumerate((("cos", math.pi / 2), ("sin", 0.0))):
            tab = tabs[fn + nm]
            P.ts("dve", ang.all(), pos.all(), inv[:, j:j + 1], ALU.mult, shift, ALU.add)
            range_reduce_sin(P, tab.all(), ang.all(), t1.all(), ti.all())
            outs[fn + nm] = tab
    tmp.close()
    c.rope = outs


def range_reduce_sin(P, out, ang, t1, ti):
    P.ts("dve", t1, ang, 1.0 / TWO_PI, ALU.mult)
    P.copy("dve", ti, t1)
    P.copy("dve", t1, ti)
    P.stt("dve", t1, t1, -TWO_PI, ang, ALU.mult, ALU.add)
    P.ts("dve", ang, t1, math.pi, ALU.is_gt)
    P.stt("dve", t1, ang, -TWO_PI, t1, ALU.mult, ALU.add)
    P.ts("dve", t1, t1, 3.141592, ALU.min, -3.141592, ALU.max)
    P.act(out, t1, AF.Sin)


def load_w_cols(P, c, dst_fn, col0, ncols, w_in_d, gcol, stage):
    for k in range(8):
        st = stage[k % 2]
        P.dma("sp" if k % 2 == 0 else "act", st[:, 0:ncols], w_in_d[k * 128:(k + 1) * 128, col0:col0 + ncols])
        dst_fn(k, st)


def cpow(P, es, name, lr, th, jv, G, J, order="gj"):
    shp = [128, G, J] if order == "gj" else [128, J, G]
    Pr = P.sb(es, name + "_r", shp, F32)
    Pi = P.sb(es, name + "_i", shp, F32)
    tmp = ExitStack()
    mag = P.sb(tmp, name + "_mag", shp, F32)
    ang = P.sb(tmp, name + "_ang", shp, F32)
    t1 = P.sb(tmp, name + "_t1", shp, F32)
    ti = P.sb(tmp, name + "_ti", shp, I32)
    if order == "gj":
        lb, jb = lr.pat(0, [(1, G), (0, J)]), jv.pat(0, [(0, G), (1, J)])
        tb = th.pat(0, [(1, G), (0, J)])
    else:
        lb, jb = lr.pat(0, [(0, J), (1, G)]), jv.pat(0, [(1, J), (0, G)])
        tb = th.pat(0, [(0, J), (1, G)])
    P.tt("dve", mag.all(), lb, jb, ALU.mult)
    P.act(mag.all(), mag.all(), AF.Exp)
    fl = "p a b -> p (a b)"
    for dst, shift in ((Pi, 0.0), (Pr, math.pi / 2)):
        P.tt("dve", ang.all(), tb, jb, ALU.mult)
        if shift:
            P.ts("dve", ang.all(), ang.all(), shift, ALU.add)
        range_reduce_sin(P, dst.all().rr(fl), ang.all().rr(fl), t1.all().rr(fl), ti.all().rr(fl))
        P.tt("dve", dst.all(), dst.all(), mag.all(), ALU.mult)
    tmp.close()
    return Pr, Pi


def kappa(P, es, name, ar, ai, lr, th, G):
    kr = P.sb(es, name + "_kr", [128, G], F32)
    ki = P.sb(es, name + "_ki", [128, G], F32)
    tmp = ExitStack()
    one = P.sb(tmp, name + "_one", [128, 1], F32)
    P.memset("dve", one.all(), 1.0)
    Ar, Ai = cpow(P, tmp, name + "_a1", lr, th, one.all(), G, 1)
    den = P.sb(tmp, name + "_den", [128, G], F32)
    t = P.sb(tmp, name + "_t", [128, G], F32)
    arm = P.sb(tmp, name + "_arm", [128, G], F32)
    A_r, A_i = Ar.all().rr("p g j -> p (g j)"), Ai.all().rr("p g j -> p (g j)")
    P.tt("dve", den.all(), ar, ar, ALU.mult)
    P.tt("dve", t.all(), ai, ai, ALU.mult)
    P.tt("dve", den.all(), den.all(), t.all(), ALU.add)
    P.op("dve", "reciprocal", out=den.all(), in_=den.all())
    P.ts("dve", arm.all(), A_r, -1.0, ALU.add)
    P.tt("dve", kr.all(), arm.all(), ar, ALU.mult)
    P.tt("dve", t.all(), A_i, ai, ALU.mult)
    P.tt("dve", kr.all(), kr.all(), t.all(), ALU.add)
    P.tt("dve", kr.all(), kr.all(), den.all(), ALU.mult)
    P.tt("dve", ki.all(), A_i, ar, ALU.mult)
    P.tt("dve", t.all(), arm.all(), ai, ALU.mult)
    P.tt("dve", ki.all(), ki.all(), t.all(), ALU.subtract)
    P.tt("dve", ki.all(), ki.all(), den.all(), ALU.mult)
    tmp.close()
    return kr, ki


def ssm_phase(P, c, S, uys, sp):
    NCH = S // LCH
    psb = c.psb
    uT = ysT = y2 = uys
    ph = ExitStack()
    sg = P.sb(ph, "sg", [128, 1], F32)
    nsg = P.sb(ph, "nsg", [128, 1], F32)
    P.ts("dve", sg.all(), c.pid_f.all(), 63.5, ALU.is_gt, -2.0, ALU.mult)
    P.ts("dve", sg.all(), sg.all(), 1.0, ALU.add)
    P.ts("dve", nsg.all(), sg.all(), -1.0, ALU.mult)
    jv = P.sb(ph, "jv", [128, 256], F32)
    jvi = P.sb(ph, "jvi", [128, 256], I32)
    P.op("pool", "iota", out=jvi.all(), pattern=[[1, 256]], base=0, channel_multiplier=0)
    P.copy("dve", jv.all(), jvi.all())
    jrev = P.sb(ph, "jrev", [128, 16], F32)
    P.ts("dve", jrev.all(), jv[:, 0:16], -1.0, ALU.mult, 15.0, ALU.add)
    bm = P.sb(ph, "bm", [128, 8], F32)
    t8 = P.sb(ph, "t8", [128, 8], F32)
    P.ts("dve", t8.all(), jv[:, 0:8], 16.0, ALU.mult)
    P.ts("dve", bm.all(), t8.all(), c.pid_f[:, 0:1], ALU.subtract)
    P.ts("dve", t8.all(), bm.all(), 0.5, ALU.is_gt, -1.0, ALU.mult)
    P.ts("dve", t8.all(), t8.all(), 1.0, ALU.add)
    P.ts("dve", bm.all(), bm.all(), -15.5, ALU.is_gt)
    P.tt("dve", bm.all(), bm.all(), t8.all(), ALU.mult)
    eye8 = P.sb(ph, "eye8", [128, 8, 8], F32)
    P.tt("dve", eye8.all(), jv[:, 0:8].pat(0, [(1, 8), (0, 8)]), jv[:, 0:8].pat(0, [(0, 8), (1, 8)]), ALU.is_equal)
    pswapb = P.sb(ph, "pswapb", [128, 128], BF16)
    pswap = P.sb(ph, "pswap", [128, 128], F32)
    P.ts("dve", pswap.all(), c.iota_f.all(), c.pid_f[:, 0:1], ALU.subtract)
    P.tt("dve", pswap.all(), pswap.all(), pswap.all(), ALU.mult)
    P.ts("dve", pswap.all(), pswap.all(), 4096.0, ALU.is_equal)
    P.copy("dve", pswapb.all(), pswap.all())
    dsk = P.sb(ph, "dsk", [128, 4], F32)
    P.dma("sp", dsk.all(), sp["dskip"].all())
    wglu = P.sb(ph, "wglu", [128, 4, 512], BF16)
    stgw = P.sb(ph, "stgw", [128, 512], F32)
    for k in range(4):
        P.dma("sp", stgw.all(), sp["w_glu"][k * 128:(k + 1) * 128, :])
        P.copy("act", wglu[:, k, :], stgw.all())

    for o in range(4):
        oc = ExitStack()
        BD = P.sb(oc, "BD", [128, 16, 128], BF16)
        Wb = P.sb(oc, "Wb", [128, 8, 16, 128], BF16)
        Wc = P.sb(oc, "Wc", [128, 8, 16, 128], BF16)
        tc_ = P.sb(oc, "tabc", [128, 8, NCH], BF16)
        tsn = P.sb(oc, "tabs", [128, 8, NCH], BF16)
        rho = P.sb(oc, "rho", [128, 8], F32)
        pr = ExitStack()
        prm = P.sb(pr, "prm", [128, 3, 8], F32)
        for i, nm in enumerate(("ar_sm", "ai_sm", "ldt_sm")):
            P.dma("sp", prm[:, i, :], sp[nm][:, o * 8:(o + 1) * 8])
        dt = P.sb(pr, "dt", [128, 8], F32)
        lr = P.sb(pr, "lr", [128, 8], F32)
        th = P.sb(pr, "th", [128, 8], F32)
        P.act(dt.all(), prm[:, 2, :], AF.Exp)
        P.tt("dve", lr.all(), prm[:, 0, :], dt.all(), ALU.mult)
        P.tt("dve", th.all(), prm[:, 1, :], dt.all(), ALU.mult)
        kr, ki = kappa(P, pr, "ksm", prm[:, 0, :], prm[:, 1, :], lr.all(), th.all(), 8)
        Ar, Ai = cpow(P, pr, "apw", lr.all(), th.all(), jv[:, 0:17], 8, 17)
        prA = pr
        pr = ExitStack()
        U = P.sb(pr, "U12", [128, 2, 8, 16], F32)
        T12 = P.sb(pr, "T12", [128, 2, 8, 16], F32)
        for i, nm in enumerate(("bU1", "bU2")):
            P.dma("sp", U[:, i], sp[nm][:, o * 8:(o + 1) * 8, :])
        for i, nm in enumerate(("cT1", "cT2")):
            P.dma("act", T12[:, i], sp[nm][:, o * 8:(o + 1) * 8, :])
        X = P.sb(pr, "X", [128, 8, 16], F32)
        tx = P.sb(pr, "tx", [128, 8, 16], F32)
        kib = P.sb(pr, "kib", [128, 8], F32)
        P.ts("dve", kib.all(), ki.all(), nsg[:, 0:1], ALU.mult)
        P.tt("dve", X.all(), U[:, 0], kr.all().pat(0, [(1, 8), (0, 16)]), ALU.mult)
        P.tt("dve", tx.all(), U[:, 1], kib.all().pat(0, [(1, 8), (0, 16)]), ALU.mult)
        P.tt("dve", X.all(), X.all(), tx.all(), ALU.add)
        if getattr(c, "dump", None) and o == 0 and DUMP2:
            c.dump("kr", kr.all(), F32)
            c.dump("ki", ki.all(), F32)
            c.dump("X", X.all().rr("p a b -> p (a b)"), F32)
            c.dump("Ar", Ar.all().rr("p a b -> p (a b)"), F32)
        Y = P.sb(pr, "Y", [128, 8, 17, 16], F32)
        ty = P.sb(pr, "ty", [128, 8, 17, 16], F32)
        for g in range(8):
            P.tt("dve", Y[:, g], T12[:, 0, g, :].pat(0, [(0, 17), (1, 16)]), Ar[:, g, :].pat(0, [(1, 17), (0, 16)]), ALU.mult)
            P.tt("dve", ty[:, g], T12[:, 1, g, :].pat(0, [(0, 17), (1, 16)]), Ai[:, g, :].pat(0, [(1, 17), (0, 16)]), ALU.mult)
        P.stt("dve", Y.all().rr("p g t c -> p (g t c)"), Y.all().rr("p g t c -> p (g t c)"), sg[:, 0:1],
              ty.all().rr("p g t c -> p (g t c)"), ALU.mult, ALU.subtract)
        if getattr(c, "dump", None) and o == 0 and DUMP2:
            c.dump("Y", Y.all().rr("p a b c -> p (a b c)"), F32)
        for g in range(8):
            P.tt("dve", Wc[:, g].rr("p t (a c) -> p t a c", a=8),
                 Y[:, g, 1:17, :].pat(0, [(16, 16), (0, 8), (1, 16)]),
                 eye8[:, g, :].pat(0, [(0, 16), (1, 8), (0, 16)]), ALU.mult)
        Xpad = P.sb(pr, "Xpad", [128, 8, 8, 16], BF16)
        Yb = P.sb(pr, "Yb", [128, 8, 16, 16], BF16)
        P.copy("dve", Yb.all(), Y[:, :, 0:16, :])
        for g in range(8):
            P.tt("dve", Xpad[:, g], X[:, g, :].pat(0, [(0, 8), (1, 16)]), eye8[:, g, :].pat(0, [(1, 8), (0, 16)]), ALU.mult)
        for g in range(8):
            P.mm(psb[0][:, 0:256], Xpad[:, g].rr("p a c -> p (a c)"), Yb[:, g].rr("p t c -> p (t c)"), g == 0, g == 7)
        Rsb = P.sb(pr, "Rsb", [128, 16, 16], F32)
        P.copy("act", Rsb.all().rr("p t c -> p (t c)"), psb[0][:, 0:256])
        if getattr(c, "dump", None) and o == 0 and DUMP2:
            c.dump("Rsb", Rsb.all().rr("p a b -> p (a b)"), F32)
            c.dump("Xpad", Xpad.all().rr("p a b c -> p (a b c)"), BF16)
        for tau in range(16):
            P.tt("dve", BD[:, tau, :].rr("p (a c) -> p a c", a=8), Rsb[:, tau, :].pat(0, [(0, 8), (1, 16)]),
                 bm.all().pat(0, [(1, 8), (0, 16)]), ALU.mult)
        pr.close()
        pr = ExitStack()
        l16 = P.sb(pr, "l16", [128, 8], F32)
        P.act(rho.all(), lr.all(), AF.Exp, scale=float(LCH))
        P.ts("dve", l16.all(), th.all(), float(LCH), ALU.mult)
        zero8 = P.sb(pr, "zero8", [128, 8], F32)
        P.memset("dve", zero8.all(), 0.0)
        assert NCH <= 256
        for h4 in range(2):
            pq = ExitStack()
            Er, Ei = cpow(P, pq, f"rot{h4}", zero8[:, h4 * 4:h4 * 4 + 4], l16[:, h4 * 4:h4 * 4 + 4], jv[:, 0:NCH], 4, NCH)
            P.copy("act", tc_[:, h4 * 4:h4 * 4 + 4, :], Er.all())
            P.copy("act", tsn[:, h4 * 4:h4 * 4 + 4, :], Ei.all())
            pq.close()
        pr.close()
        pr = ExitStack()
        pcm = P.sb(pr, "pcm", [128, 5, 64], F32)
        for i, nm in enumerate(("ar_cm", "ai_cm", "ldt_cm", "br_cm", "bi_cm")):
            P.dma("sp", pcm[:, i, :], sp[nm][:, o, :])
        dtc = P.sb(pr, "dtc", [128, 64], F32)
        lrc = P.sb(pr, "lrc", [128, 64], F32)
        thc = P.sb(pr, "thc", [128, 64], F32)
        P.act(dtc.all(), pcm[:, 2, :], AF.Exp)
        P.tt("dve", lrc.all(), pcm[:, 0, :], dtc.all(), ALU.mult)
        P.tt("dve", thc.all(), pcm[:, 1, :], dtc.all(), ALU.mult)
        krc, kic = kappa(P, pr, "kcm", pcm[:, 0, :], pcm[:, 1, :], lrc.all(), thc.all(), 64)
        Bbr = P.sb(pr, "Bbr", [128, 64], F32)
        Bbi = P.sb(pr, "Bbi", [128, 64], F32)
        tb = P.sb(pr, "tb", [128, 64], F32)
        P.tt("dve", Bbr.all(), krc.all(), pcm[:, 3, :], ALU.mult)
        P.tt("dve", tb.all(), kic.all(), pcm[:, 4, :], ALU.mult)
        P.tt("dve", Bbr.all(), Bbr.all(), tb.all(), ALU.subtract)
        P.tt("dve", Bbi.all(), krc.all(), pcm[:, 4, :], ALU.mult)
        P.tt("dve", tb.all(), kic.all(), pcm[:, 3, :], ALU.mult)
        P.tt("dve", Bbi.all(), Bbi.all(), tb.all(), ALU.add)
        Pr_, Pi_ = cpow(P, pr, "apc", lrc.all(), thc.all(), jrev.all(), 64, 16, order="jg")
        Z = P.sb(pr, "Z", [128, 16, 2, 64], F32)
        tz = P.sb(pr, "tz", [128, 16, 64], F32)
        bb = lambda t: t.all().pat(0, [(0, 16), (1, 64)])
        P.tt("dve", Z[:, :, 0, :], Pr_.all(), bb(Bbr), ALU.mult)
        P.tt("dve", tz.all(), Pi_.all(), bb(Bbi), ALU.mult)
        P.tt("dve", Z[:, :, 0, :], Z[:, :, 0, :], tz.all(), ALU.subtract)
        P.tt("dve", Z[:, :, 1, :], Pr_.all(), bb(Bbi), ALU.mult)
        P.tt("dve", tz.all(), Pi_.all(), bb(Bbr), ALU.mult)
        P.tt("dve", Z[:, :, 1, :], Z[:, :, 1, :], tz.all(), ALU.add)
        if getattr(c, "dump", None) and o == 0 and DUMP2:
            c.dump("Z", Z.all().rr("p s r m -> p (s r m)"), F32)
            c.dump("Bbr", Bbr.all(), F32)
            c.dump("Prc", Pr_.all().rr("p a b -> p (a b)"), F32)
            c.dump("krc", krc.all(), F32)
            c.dump("pcm", pcm.all().rr("p a b -> p (a b)"), F32)
            c.dump("lrc", lrc.all(), F32)
        for g in range(8):
            P.ts("dve", Wb[:, g].rr("p s m -> p (s m)"), Z.all().rr("p s r m -> p (s r m)"), bm[:, g:g + 1], ALU.mult)
        pr.close()
        prA.close()
        if getattr(c, "dump", None) and o == 0:
            c.dump("BD", BD.all().rr("p a b -> p (a b)"), BF16)
            c.dump("Wb", Wb.all().rr("p a b m -> p (a b m)"), BF16)
            c.dump("Wc", Wc.all().rr("p a b m -> p (a b m)"), BF16)
            c.dump("tabc", tc_.all().rr("p a b -> p (a b)"), BF16)
            c.dump("tabs", tsn.all().rr("p a b -> p (a b)"), BF16)
            c.dump("rho", rho.all(), F32)

        wk = ExitStack()
        SA = P.sb(wk, "SA", [128, 8, NCH], F32)
        VA = P.sb(wk, "VA", [128, 8, NCH], F32)
        VB = P.sb(wk, "VB", [128, 8, NCH], F32)
        t1 = P.sb(wk, "l2t1", [128, 8, NCH], F32)
        t2 = P.sb(wk, "l2t2", [128, 8, NCH], F32)
        H = P.sb(wk, "H", [128, 8, NCH], BF16)
        SAh = P.sb(wk, "SAh", [128, 8, NCH], BF16)
        SAl = P.sb(wk, "SAl", [128, 8, NCH], BF16)
        uo = uT[:, o, :].rr("p (k s) -> p s k", s=LCH)
        for g in range(8):
            bank = psb[1 + (g % 2)]
            for q0 in range(0, NCH, 512):
                qn = min(512, NCH - q0)
                for s_ in range(LCH):
                    P.mm(bank[:, 0:qn], Wb[:, g, s_, :], uo[:, s_, q0:q0 + qn], s_ == 0, s_ == LCH - 1)
                P.copy("act", SA[:, g, q0:q0 + qn], bank[:, 0:qn])
        fl = "p g k -> p (g k)"
        cb, sb_ = tc_.all().rr(fl), tsn.all().rr(fl)
        for g in range(8):
            for q0 in range(0, NCH, 512):
                qn = min(512, NCH - q0)
                bank = psb[3 + (g % 2)]
                P.copy("dve", SAh[:, g, q0:q0 + qn], SA[:, g, q0:q0 + qn])
                P.tt("dve", SAl[:, g, q0:q0 + qn], SA[:, g, q0:q0 + qn], SAh[:, g, q0:q0 + qn], ALU.subtract)
                P.mm(bank[:, 0:qn], pswapb.all(), SAh[:, g, q0:q0 + qn], True, False)
                P.mm(bank[:, 0:qn], pswapb.all(), SAl[:, g, q0:q0 + qn], False, True)
                sl = slice(q0, q0 + qn)
                A_, B_ = SA[:, g, sl], bank[:, 0:qn]
                cg, sgn_ = tc_[:, g, sl], tsn[:, g, sl]
                P.tt("dve", t1[:, g, sl], A_, cg, ALU.mult)
                P.tt("dve", t2[:, g, sl], B_, sgn_, ALU.mult)
                P.stt("dve", VA[:, g, sl], t2[:, g, sl], sg[:, 0:1], t1[:, g, sl], ALU.mult, ALU.add)
                P.tt("dve", t1[:, g, sl], B_, cg, ALU.mult)
                P.tt("dve", t2[:, g, sl], A_, sgn_, ALU.mult)
                P.stt("dve", VB[:, g, sl], t2[:, g, sl], nsg[:, 0:1], t1[:, g, sl], ALU.mult, ALU.add)
        for g in range(8):
            rb = rho[:, g:g + 1].pat(0, [(0, NCH)])
            P.op("dve", "tensor_tensor_scan", out=VA[:, g, :], data0=rb, data1=VA[:, g, :], initial=0.0, op0=ALU.mult, op1=ALU.add)
            P.op("dve", "tensor_tensor_scan", out=VB[:, g, :], data0=rb, data1=VB[:, g, :], initial=0.0, op0=ALU.mult, op1=ALU.add)
        P.tt("dve", t1.all().rr(fl), VA.all().rr(fl), cb, ALU.mult)
        P.tt("dve", t2.all().rr(fl), VB.all().rr(fl), sb_, ALU.mult)
        P.stt("dve", H.all().rr(fl), t2.all().rr(fl), nsg[:, 0:1], t1.all().rr(fl), ALU.mult, ALU.add)
        if getattr(c, "dump", None) and o == 0:
            c.dump("SA", SA.all().rr("p a b -> p (a b)"), F32)
            c.dump("H", H.all().rr("p a b -> p (a b)"), BF16)
        yo = uo
        for t in range(LCH - 1, -1, -1):
            bank = psb[5 + (t % 3)]
            for q0 in range(0, NCH, 512):
                qn = min(512, NCH - q0)
                for s_ in range(t + 1):
                    P.mm(bank[:, 0:qn], BD[:, t - s_, :], uo[:, s_, q0:q0 + qn], s_ == 0, False)
                for g in range(8):
                    lo = 1 if q0 == 0 else 0
                    P.mm(bank[:, lo:qn], Wc[:, g, t, :], H[:, g, q0 + lo - 1:q0 + qn - 1], False, g == 7)
                P.stt("dve", yo[:, t, q0:q0 + qn], uo[:, t, q0:q0 + qn], dsk[:, o:o + 1], bank[:, 0:qn], ALU.mult, ALU.add)
        wk.close()
        oc.close()
    if getattr(c, "dump", None):
        c.dump("ypre", y2.all().rr("p a b -> p (a b)"), BF16)
    gl = ExitStack()
    g1 = P.sb(gl, "g1", [128, S], F32)
    g2 = P.sb(gl, "g2", [128, S], F32)
    for o in range(4):
        gelu_inplace(P, y2[:, o, :], g1.all(), g2.all())
    gate = P.sb(gl, "gate", [128, 4, 512], BF16)
    for q0 in range(0, S, 512):
        for n in range(4):
            bank = psb[n]
            for k in range(4):
                P.mm(bank[:, 0:512], wglu[:, k, n * 128:(n + 1) * 128], y2[:, k, q0:q0 + 512], k == 0, k == 3)
            P.act(gate[:, n, :], bank[:, 0:512], AF.Sigmoid)
        P.tt("dve", ysT[:, :, q0:q0 + 512], gate.all(), y2[:, :, q0:q0 + 512], ALU.mult)
    gl.close()
    ph.close()


def gelu_inplace(P, x, t1, t2, eng="dve"):
    P.tt(eng, t1, x, x, ALU.mult)
    P.ts(eng, t1, t1, 0.044715 * 1.5957691216, ALU.mult, 1.5957691216, ALU.add)
    P.tt(eng, t1, t1, x, ALU.mult)
    P.act(t2, t1, AF.Sigmoid)
    P.tt(eng, x, x, t2, ALU.mult)


def attn_phase(P, c, S, x_d, w_in_d, g1c, ng1c, kTd, kiT4, vaug, ya_d, stop_at=None, dump=None):
    NT = S // 128
    TOPK = min(256, S // 4)
    psb, ident = c.psb, c.ident
    ph = ExitStack()
    rope_tables(P, ph, S, c)
    w_q = P.sb(ph, "w_q", [128, 8, 8, 128], BF16)
    w_qi = P.sb(ph, "w_qi", [128, 8, 6, 128], BF16)
    P.memset("pool", w_qi.all(), 0.0)
    w_wi = P.sb(ph, "w_wi", [128, 8, 8], BF16)
    wst = ExitStack()
    stg = [P.sb(wst, f"astg{i}", [128, 512], F32) for i in range(2)]

    def cvt(dst, src, k, neg=False):
        P.act(dst, src, AF.Copy, scale=(ng1c if neg else g1c)[:, k:k + 1])

    def q_cvt(k, st):
        for m in range(4):
            cvt(w_q[:, k, m, :], st[:, m * 128:(m + 1) * 128], k)
            for e in range(2):
                base = m * 128 + e * 64
                cvt(w_q[:, k, 4 + m, e * 64:e * 64 + 32], st[:, base + 32:base + 64], k, neg=True)
                cvt(w_q[:, k, 4 + m, e * 64 + 32:e * 64 + 64], st[:, base:base + 32], k)
    load_w_cols(P, c, q_cvt, OFF_Q, 512, w_in_d, g1c, stg)

    def qi_cvt(k, st):
        for h in range(8):
            tl, pos = h // 3, h % 3
            cvt(w_qi[:, k, tl, pos * 32:(pos + 1) * 32], st[:, h * 32:(h + 1) * 32], k)
            cvt(w_qi[:, k, 3 + tl, pos * 32:pos * 32 + 16], st[:, h * 32 + 16:h * 32 + 32], k, neg=True)
            cvt(w_qi[:, k, 3 + tl, pos * 32 + 16:pos * 32 + 32], st[:, h * 32:h * 32 + 16], k)
    load_w_cols(P, c, qi_cvt, OFF_QI, 256, w_in_d, g1c, stg)
    load_w_cols(P, c, lambda k, st: cvt(w_wi[:, k, :], st[:, 0:8], k), OFF_WI, 8, w_in_d, g1c, stg)
    wst.close()

    xt = [P.sb(ph, "axt0", [128, D], F32)] * 2
    xh = P.sb(ph, "axh", [128, D], BF16)
    xhT = P.sb(ph, "axhT", [128, 8, 128], BF16)
    junk = P.sb(ph, "ajunk", [128, D], BF16)
    ss = P.sb(ph, "ass", [128, 1], F32)
    rstd = P.sb(ph, "arstd", [128, 1], F32)
    qT = P.sb(ph, "qT", [128, 4, 128], BF16)
    qiT = P.sb(ph, "qiT", [128, 3, 128], BF16)
    t1 = P.sb(ph, "at1", [128, 4, 128], F32)
    t2 = P.sb(ph, "at2", [128, 4, 128], F32)
    wsg = P.sb(ph, "wsg", [128, 8], F32)
    wsc = P.sb(ph, "wsc", [128, 8], F32)
    acc = P.sb(ph, "acc", [128, S], F32)
    msk = P.sb(ph, "msk", [128, S], BF16)
    mT = P.sb(ph, "mT", [128, NT, 128], BF16)
    rr = [P.sb(ph, f"rr{i}", [128, 512], F32) for i in range(2)]
    lo = P.sb(ph, "lo", [128, 1], F32)
    mid = P.sb(ph, "mid", [128, 1], F32)
    cnt = P.sb(ph, "cnt", [128, 1], F32)
    cntb = P.sb(ph, "cntb", [128, 1], F32)
    ge = P.sb(ph, "ge", [128, 1], F32)
    eT = [[P.sb(ph, f"eT{i}{n}", [128, 4, 128], BF16) for n in range(2)] for i in range(2)]
    pT = [[P.sb(ph, f"pT{i}{n}", [128, 4, 128], BF16) for n in range(2)] for i in range(2)]
    rden = P.sb(ph, "rden", [128, 512], F32)
    rdh = P.sb(ph, "rdh", [128, 512], BF16)
    rdl = P.sb(ph, "rdl", [128, 512], BF16)
    ones_bf = P.sb(ph, "ones_bf", [128, 64], BF16)
    P.memset("dve", ones_bf.all(), 1.0)
    bcs = P.sb(ph, "bcs", [64, 512], F32)
    ya = [P.sb(ph, f"ya{i}", [64, 8, 128], BF16) for i in range(2)]
    m4 = "p (m t) -> p m t"

    qTs = [qT, P.sb(ph, "qT1", [128, 4, 128], BF16)]
    sjunk = P.sb(ph, "sjunk", [128, 2432], BF16)
    PIPE = stop_at is None

    def stage_I(b):
        tok = slice(b * 128, (b + 1) * 128)
        Sc = (b + 1) * 128
        qTb = qTs[b % 2]
        xb = xt[b % 2]
        P.dma("sp", xb.all(), x_d[tok, :])
        c.norm_and_transpose(b, xb, xh, xhT, junk, ss, rstd, psb[0])
        for m in range(8):
            bank = psb[1] if m < 4 else psb[2]
            for k in range(8):
                P.mm(bank[:, (m % 4) * 128:(m % 4 + 1) * 128], w_q[:, k, m, :], xhT[:, k, :], k == 0, k == 7)
        P.tt("dve", t1.all(), psb[1].all().rr(m4, m=4), c.rope["cosA"][:, tok].pat(0, [(0, 4), (1, 128)]), ALU.mult)
        P.tt("dve", t2.all(), psb[2].all().rr(m4, m=4), c.rope["sinA"][:, tok].pat(0, [(0, 4), (1, 128)]), ALU.mult)
        P.tt("dve", qTb.all(), t1.all(), t2.all(), ALU.add)
        for m in range(6):
            bank = psb[3] if m < 3 else psb[6]
            for k in range(8):
                P.mm(bank[:, (m % 3) * 128:(m % 3 + 1) * 128], w_qi[:, k, m, :], xhT[:, k, :], k == 0, k == 7)
        P.tt("dve", t1[:, 0:3, :], psb[3][:, 0:384].rr(m4, m=3), c.rope["cosI"][:, tok].pat(0, [(0, 3), (1, 128)]), ALU.mult)
        P.tt("dve", t2[:, 0:3, :], psb[6][:, 0:384].rr(m4, m=3), c.rope["sinI"][:, tok].pat(0, [(0, 3), (1, 128)]), ALU.mult)
        P.tt("dve", qiT.all(), t1[:, 0:3, :], t2[:, 0:3, :], ALU.add)
        for k in range(8):
            P.mm(psb[7][:, 0:8], xhT[:, k, :], w_wi[:, k, :], k == 0, k == 7)
        P.ts("dve", wsg.all(), psb[7][:, 0:8], 0.0, ALU.is_gt, 2.0, ALU.mult)
        P.ts("dve", wsg.all(), wsg.all(), -1.0, ALU.add)
        P.tt("dve", wsc.all(), psb[7][:, 0:8], wsg.all(), ALU.mult)
        P.ts("dve", wsc.all(), wsc.all(), 1.0 / 16.0, ALU.mult)
        if stop_at == "proj":
            return
        ibanks = [psb[1], psb[2], psb[3], psb[6]]
        ci = 0
        for q0 in range(0, Sc, 512):
            qn = min(512, Sc - q0)
            for h in range(8):
                bank, r = ibanks[h % 3], rr[ci % 2]
                ci += 1
                pb = 32 * (h % 3)
                P.mm(bank[:, 0:qn], qiT[pb:pb + 32, h // 3, :], kiT4[pb:pb + 32, q0:q0 + qn], True, True)
                P.act(r[:, 0:qn], bank[:, 0:qn], AF.Relu, scale=wsc[:, h:h + 1])
                if h == 0:
                    P.ts("dve", acc[:, q0:q0 + qn], r[:, 0:qn], wsg[:, 0:1], ALU.mult)
                else:
                    P.stt("dve", acc[:, q0:q0 + qn], r[:, 0:qn], wsg[:, h:h + 1], acc[:, q0:q0 + qn], ALU.mult, ALU.add)
        P.tt("dve", acc[:, b * 128:Sc], acc[:, b * 128:Sc], c.caus.all(), ALU.add)

    def stage_B(b):
        Sc = (b + 1) * 128
        if Sc > TOPK:
            c1 = (int(Sc * 0.42) // 128) * 128 if Sc >= 1024 else Sc
            nB = Sc - c1
            half = 8.0
            P.memset("dve", mid.all(), 0.0)
            NSTEP = 19
            for it in range(NSTEP):
                P.op("dve", "tensor_scalar", out=msk[:, 0:c1], in0=acc[:, 0:c1], scalar1=mid[:, 0:1], scalar2=0.0,
                     op0=ALU.is_ge, op1=ALU.add, accum_out=cnt.all())
                if nB:
                    P.act(sjunk[:, 0:nB], acc[:, c1:Sc], AF.Sign, bias=mid[:, 0:1], scale=-1.0, accum_out=cntb.all())
                    P.stt("dve", cnt.all(), cntb.all(), -0.5, cnt.all(), ALU.mult, ALU.add)
                P.ts("dve", ge.all(), cnt.all(), TOPK - 0.5 - nB / 2.0, ALU.is_ge, half, ALU.mult)
                nxt = half / 2 if it < NSTEP - 1 else half
                P.stt("dve", mid.all(), ge.all(), -nxt, mid.all(), ALU.add, ALU.add)
                half = half / 2
                yield
            P.ts("dve", msk[:, 0:Sc], acc[:, 0:Sc], mid[:, 0:1], ALU.is_ge)
        else:
            P.ts("dve", msk[:, 0:Sc], acc[:, 0:Sc], -1.0e29, ALU.is_ge)

    def stage_T(b):
        pbf = psb[0].all().cast(BF16)
        for j0 in range(0, b + 1, 8):
            jn = min(8, b + 1 - j0)
            for jj in range(jn):
                P.tr(pbf[:, jj * 128:(jj + 1) * 128], msk[:, (j0 + jj) * 128:(j0 + jj + 1) * 128], ident.all())
            P.copy("act", mT[:, j0:j0 + jn, :].rr("p j t -> p (j t)"), pbf[:, 0:jn * 128])

    def stage_A(b):
        tok = slice(b * 128, (b + 1) * 128)
        qTb = qTs[b % 2]
        obank = [psb[4], psb[5]]
        dbank = [psb[7], psb[0]]
        meng = "pool" if PIPE else "dve"
        for j in range(b + 1):
            ks = slice(j * 128, (j + 1) * 128)
            lb = [psb[1], psb[2]] if j % 2 == 0 else [psb[3], psb[6]]
            for e in range(2):
                for n in range(2):
                    P.mm(lb[e][:, 2 * n * 128:(2 * n + 2) * 128], kTd[64 * e:64 * e + 64, n, ks],
                         qTb[64 * e:64 * e + 64, 2 * n:2 * n + 2, :].rr("p m t -> p (m t)"), True, True)
            for e in range(2):
                et, pt = eT[j % 2][e], pT[j % 2][e]
                P.act(et.all().rr("p h t -> p (h t)"), lb[e].all(), AF.Exp, scale=0.125)
                P.tt(meng, pt.all(), et.all(), mT[:, j, :].pat(0, [(0, 4), (1, 128)]), ALU.mult)
            if stop_at != "lg":
                for e in range(2):
                    pt = pT[j % 2][e]
                    for n in range(2):
                        rhs = pt[:, 2 * n:2 * n + 2, :].rr("p h t -> p (h t)")
                        cs = slice(2 * e * 128, (2 * e + 2) * 128)
                        st_, sp_ = (j == 0 and e == 0), (j == b and e == 1)
                        P.mm(obank[n][0:64, cs], vaug[:, j, n, 0:64], rhs, st_, sp_, skip_group_check=True)
                        P.mm(dbank[n][0:64, cs], ones_bf.all(), rhs, st_, sp_, skip_group_check=True)
            yield
        if stop_at in ("pv", "lg"):
            return
        yab = ya[b % 2]
        for n in range(2):
            P.op("dve", "reciprocal", out=bcs.all(), in_=dbank[n][0:64, :])
            for e in range(2):
                cs = slice(2 * e * 128, (2 * e + 2) * 128)
                P.tt("dve", yab[:, 4 * n + e:4 * n + e + 3:2, :], obank[n][0:64, cs].rr("p (i t) -> p i t", i=2),
                     bcs[:, cs].rr("p (i t) -> p i t", i=2), ALU.mult)
        P.dma("sp", ya_d[:, :, tok], yab.all())

    def run(gen):
        if gen is not None:
            for _ in gen:
                pass

    def step(gen):
        if gen is None:
            return None
        try:
            next(gen)
            return gen
        except StopIteration:
            return None

    if not PIPE:
        for b in range(NT):
            stage_I(b)
            if stop_at in ("proj", "idx"):
                continue
            run(stage_B(b))
            if stop_at == "thr":
                continue
            stage_T(b)
            if stop_at == "mt":
                continue
            run(stage_A(b))
    else:
        stage_I(0)
        run(stage_B(0))
        stage_T(0)
        for b in range(NT):
            gB = None
            if b + 1 < NT:
                stage_I(b + 1)
                gB = stage_B(b + 1)
            gA = stage_A(b)
            while gA is not None or gB is not None:
                gA = step(gA)
                gB = step(gB)
                gB = step(gB)
            if b + 1 < NT:
                stage_T(b + 1)
    if dump is not None:
        dump("qT", qT.all().rr("p a b -> p (a b)"), BF16)
        dump("qiT", qiT.all().rr("p a b -> p (a b)"), BF16)
        dump("wsc", wsc.all(), F32)
        dump("wsg", wsg.all(), F32)
        if stop_at != "proj":
            dump("acc", acc.all(), F32)
        if stop_at not in ("proj", "idx"):
            dump("msk", msk.all(), BF16)
            dump("lo", mid.all(), F32)
        if stop_at == "lg":
            dump("pT", pT[(NT - 1) % 2][1].all().rr("p a b -> p (a b)"), BF16)
        if stop_at not in ("proj", "idx", "thr"):
            dump("mT", mT.all().rr("p a b -> p (a b)"), BF16)
        if stop_at == "pv":
            for n in range(2):
                P.copy("act", rden.all(), psb[4 + n].all())
                dump(f"oT{n}", rden.all(), F32)
    ph.close()


def merge_phase(P, c, S, x_d, w_in_d, g1c, ysT, ya_d, h_d, wd, uvb_d=None):
    NT = S // 128
    psb = c.psb
    ph = ExitStack()
    w_gs = P.sb(ph, "w_gs", [128, 8, 1024], BF16)
    w_ga = P.sb(ph, "w_ga", [128, 8, 1024], BF16)
    w_sup = P.sb(ph, "w_sup", [128, 4, 1024], BF16)
    w_aup = P.sb(ph, "w_aup", [64, 8, 1024], BF16)
    w_out = P.sb(ph, "w_out", [128, 8, 1024], BF16)
    stg = [P.sb(ph, f"bstg{i}", [128, 1024], F32) for i in range(2)]
    qs = ("sp", "act")

    def cvt(dst, src, k):
        P.act(dst, src, AF.Copy, scale=g1c[:, k:k + 1])
    load_w_cols(P, c, lambda k, st: cvt(w_gs[:, k, :], st[:, 0:1024], k), OFF_GS, 1024, w_in_d, g1c, stg)
    load_w_cols(P, c, lambda k, st: cvt(w_ga[:, k, :], st[:, 0:1024], k), OFF_GA, 1024, w_in_d, g1c, stg)
    for k in range(4):
        P.dma(qs[k % 2], stg[k % 2].all(), wd["w_ssm_up"][k * 128:(k + 1) * 128, :])
        P.copy("act", w_sup[:, k, :], stg[k % 2].all())
    for h in range(8):
        P.dma(qs[h % 2], stg[h % 2][0:64, :], wd["w_attn_up"][h * 64:(h + 1) * 64, :])
        P.copy("act", w_aup[:, h, :], stg[h % 2][0:64, :])
    for k in range(8):
        P.dma(qs[k % 2], stg[k % 2].all(), wd["w_out"][k * 128:(k + 1) * 128, :])
        P.copy("act", w_out[:, k, :], stg[k % 2].all())

    xt = [P.sb(ph, f"bxt{i}", [128, D], F32) for i in range(2)]
    xh = P.sb(ph, "bxh", [128, D], BF16)
    xhT = P.sb(ph, "bxhT", [128, 8, 128], BF16)
    junk = P.sb(ph, "bjunk", [128, D], BF16)
    ss = P.sb(ph, "bss", [128, 1], F32)
    rstd = P.sb(ph, "brstd", [128, 1], F32)
    yat = [P.sb(ph, f"yat{i}", [64, 8, 128], BF16) for i in range(2)]
    sgs = P.sb(ph, "sgs", [128, 512], F32)
    sga = P.sb(ph, "sga", [128, 512], F32)
    m1 = P.sb(ph, "m1", [128, 512], F32)
    m2 = P.sb(ph, "m2", [128, 512], F32)
    merged = P.sb(ph, "merged", [128, 8, 128], BF16)
    ht = [P.sb(ph, f"bht{i}", [128, D], F32) for i in range(2)]

    NCONV = 16384 // 128
    cvf = [P.sb(ph, f"cvf{i}", [128, 2048], F32) for i in range(2)]
    cvb = [P.sb(ph, f"cvb{i}", [128, 2048], BF16) for i in range(2)]

    def conv_step(i):
        rows = slice(i * 128, (i + 1) * 128)
        P.dma("pool", cvf[i % 2].all(), wd["peer_uv"][rows, :])
        P.copy("pool", cvb[i % 2].all(), cvf[i % 2].all())
        P.dma("pool", uvb_d[rows, :], cvb[i % 2].all())
    conv_per_tile = (NCONV + NT - 1) // NT
    conv_i = 0

    for b in range(NT):
        for _ in range(conv_per_tile):
            if uvb_d is not None and conv_i < NCONV:
                conv_step(conv_i)
                conv_i += 1
        tok = slice(b * 128, (b + 1) * 128)
        xb = xt[b % 2]
        P.dma("sp", xb.all(), x_d[tok, :])
        ya = yat[b % 2]
        P.dma("sp", ya.all(), ya_d[:, :, tok])
        c.norm_and_transpose(b, xb, xh, xhT, junk, ss, rstd, psb[0])
        for half in range(2):
            for cc in range(4):
                ci = half * 4 + cc
                cols = slice(ci * 128, (ci + 1) * 128)
                reg = slice(cc * 128, (cc + 1) * 128)
                for k in range(8):
                    P.mm(psb[1][:, reg], w_gs[:, k, cols], xhT[:, k, :], k == 0, k == 7)
                for k in range(8):
                    P.mm(psb[2][:, reg], w_ga[:, k, cols], xhT[:, k, :], k == 0, k == 7)
                for k in range(4):
                    P.mm(psb[3][:, reg], w_sup[:, k, cols], ysT[:, k, tok], k == 0, k == 3)
                for h in range(8):
                    P.mm(psb[4][:, reg], w_aup[:, h, cols], ya[:, h, :], h == 0, h == 7)
            P.act(sgs.all(), psb[1].all(), AF.Sigmoid)
            P.act(sga.all(), psb[2].all(), AF.Sigmoid)
            P.tt("dve", m1.all(), sgs.all(), psb[3].all(), ALU.mult)
            P.tt("dve", m2.all(), sga.all(), psb[4].all(), ALU.mult)
            P.tt("dve", merged[:, half * 4:half * 4 + 4, :].rr("p c t -> p (c t)"), m1.all(), m2.all(), ALU.add)
        hb = ht[b % 2]
        for n2 in range(2):
            cs = slice(n2 * 512, (n2 + 1) * 512)
            for k in range(8):
                P.mm(psb[5 + n2].all(), merged[:, k, :], w_out[:, k, cs], k == 0, k == 7)
            P.tt("dve", hb[:, cs], psb[5 + n2].all(), xb[:, cs], ALU.add)
        P.dma("sp", h_d[tok, :], hb.all())
    ph.close()


def top16(P, src, vals_out, idx_out, tl, par):
    m8a, i8a, m8b, i8b = tl["m8a"][par], tl["i8a"][par], tl["m8b"][par], tl["i8b"][par]
    n = src.shape[-1]
    sc2 = tl["sc2"][par][:, 0:n]
    P.op("dve", "max", out=m8a.all(), in_=src)
    P.op("dve", "max_index", out=i8a.all(), in_max=m8a.all(), in_values=src)
    P.op("dve", "match_replace", out=sc2, in_to_replace=m8a.all(), in_values=src, imm_value=-1.0e30)
    P.op("dve", "max", out=m8b.all(), in_=sc2)
    P.op("dve", "max_index", out=i8b.all(), in_max=m8b.all(), in_values=sc2)
    P.copy("act", vals_out[:, 0:8], m8a.all())
    P.copy("act", vals_out[:, 8:16], m8b.all())
    P.copy("dve", idx_out[:, 0:8], i8a.all().cast(I32))
    P.copy("dve", idx_out[:, 8:16], i8b.all().cast(I32))


def peer_phase(P, c, S, h_d, out_d, pd, uvb_d, NB=16, dump=None):
    NT = S // 128
    psb, ident = c.psb, c.ident
    ph = ExitStack()
    wq = P.sb(ph, "wq", [128, 8, 2048], BF16)
    pk = P.sb(ph, "pk", [128, 16, 128], BF16)
    g2c = P.sb(ph, "g2c", [128, 8], F32)
    g2b = P.sb(ph, "g2b", [128, D], F32)
    gfb = P.sb(ph, "gfb", [128, D], F32)
    P.dma("sp", g2c.all(), pd["g2c"].all())
    P.dma("sp", g2b.all(), pd["g2b"].all())
    P.dma("sp", gfb.all(), pd["gfb"].all())
    hts = [P.sb(ph, f"cht{i}", [128, D], F32) for i in range(2)]
    hh = P.sb(ph, "chh", [128, D], BF16)
    hT = P.sb(ph, "chT", [128, 8, 128], BF16)
    junk = P.sb(ph, "cjunk", [128, D], BF16)
    hn = P.sb(ph, "chn", [128, D], F32)
    ss = P.sb(ph, "css", [128, 1], F32)
    rstd = P.sb(ph, "crstd", [128, 1], F32)
    qT = P.sb(ph, "cqT", [128, 16, 128], BF16)
    SC = P.sb(ph, "SC", [128, 16, 128], F32)
    V16 = P.sb(ph, "V16", [128, 16, 16], F32)
    I16 = P.sb(ph, "I16", [128, 16, 16], F32)
    CS = P.sb(ph, "CS", [128, 8, 256], F32)
    TS = P.sb(ph, "TS", [128, 8, 16], F32)
    POSf = P.sb(ph, "POSf", [128, 8, 16], F32)
    POSi = P.sb(ph, "POSi", [128, 8, 16], I32)
    rowi = P.sb(ph, "rowi", [128, 8, 16], I32)
    coli = P.sb(ph, "coli", [128, 8, 16], I32)
    rowf = P.sb(ph, "rowf", [128, 8, 16], F32)
    colf = P.sb(ph, "colf", [128, 8, 16], F32)
    E = P.sb(ph, "E", [128, 8, 16], F32)
    G = P.sb(ph, "G", [128, 8, 16], F32)
    sm = P.sb(ph, "sm", [128, 8], F32)
    OH = P.sb(ph, "OH", [128, 8, 16, 16], F32)
    OH2 = P.sb(ph, "OH2", [128, 8, 16, 16], F32)
    i1s = P.sb(ph, "i1s", [128, 8, 16], F32)
    i2s = P.sb(ph, "i2s", [128, 8, 16], F32)
    eidf = P.sb(ph, "eidf", [128, 128], F32)
    EID = P.sb(ph, "EID", [128, 128], I32)
    dots = P.sb(ph, "dots", [128, 128], F32)
    actw = P.sb(ph, "actw", [128, 128], F32)
    resd = P.sb(ph, "cres", [128, D], F32)
    outt = [P.sb(ph, f"cout{i}", [128, D], F32) for i in range(2)]
    tl = {k: [P.sb(ph, f"{k}{i}", [128, 8], dt) for i in range(2)]
          for k, dt in (("m8a", F32), ("i8a", U32), ("m8b", F32), ("i8b", U32))}
    tl["sc2"] = [P.sb(ph, f"sc2{i}", [128, 256], F32) for i in range(2)]
    wst = ExitStack()
    stg = [P.sb(wst, f"cstg{i}", [128, 2048], F32) for i in range(2)]
    qs = ("sp", "act")
    for k in range(8):
        P.dma(qs[k % 2], stg[k % 2].all(), pd["peer_wq"][k * 128:(k + 1) * 128, :])
        P.act(wq[:, k, :], stg[k % 2].all(), AF.Copy, scale=g2c[:, k:k + 1])
    P.dma("sp", stg[0].all(), pd["pk"][0:128].rr("p a n -> p (a n)"))
    P.copy("act", pk.all().rr("p a n -> p (a n)"), stg[0].all())
    wst.close()
    UV = [P.sb(ph, f"UV{i}", [128, 2048], BF16) for i in range(NB)]
    dg = [P.sb(ph, f"dg{i}", [128, 128], BF16) for i in range(4)]
    hnb = P.sb(ph, "hnb", [128, D], BF16)
    junkb = P.sb(ph, "cjunkb", [128, D], F32)
    tuv = uvb_d.all()

    EIDs = [EID, P.sb(ph, "EID1", [128, 128], I32)]
    Gs = [G, P.sb(ph, "G1", [128, 8, 16], F32)]
    hnbs = [hnb, P.sb(ph, "hnb1", [128, D], BF16)]
    ssf = P.sb(ph, "cssf", [128, 1], F32)
    rstdf = P.sb(ph, "crstdf", [128, 1], F32)
    pacc = [psb[6], psb[7]]

    def front(b):
        tok = slice(b * 128, (b + 1) * 128)
        ht, EIDb, Gb, hnbb = hts[b % 2], EIDs[b % 2], Gs[b % 2], hnbs[b % 2]
        P.dma("sp", ht.all(), h_d[tok, :])
        P.act(junk.all(), ht.all(), AF.Square, accum_out=ss.all())
        P.ts("dve", ss.all(), ss.all(), 1.0 / D, ALU.mult, EPS, ALU.add)
        P.act(ss.all(), ss.all(), AF.Sqrt)
        P.op("dve", "reciprocal", out=rstd.all(), in_=ss.all())
        P.ts("dve", hh.all(), ht.all(), rstd[:, 0:1], ALU.mult)
        yield
        P.stt("dve", hn.all(), ht.all(), rstd[:, 0:1], g2b.all(), ALU.mult, ALU.mult)
        P.copy("act", hnbb.all(), hn.all())
        pbf = psb[0].all().cast(BF16)
        for k in range(8):
            P.tr(pbf[:, k * 128:(k + 1) * 128], hh[:, k * 128:(k + 1) * 128], ident.all())
        P.copy("act", hT.all().rr("p k t -> p (k t)"), pbf)
        yield
        for ch in range(16):
            bank = psb[1 + (ch // 4) % 2]
            for k in range(8):
                P.mm(bank[:, (ch % 4) * 128:(ch % 4 + 1) * 128], wq[:, k, ch * 128:(ch + 1) * 128], hT[:, k, :], k == 0, k == 7)
            if ch % 4 == 3:
                P.copy("act", qT[:, ch - 3:ch + 1, :].rr("p a t -> p (a t)"), bank.all())
                yield
        sbanks = [psb[3], psb[4], psb[5], psb[0]]
        for ch in range(16):
            bank = sbanks[ch // 4]
            P.mm(bank[:, (ch % 4) * 128:(ch % 4 + 1) * 128], qT[:, ch, :], pk[:, ch, :], True, True)
            if ch % 4 == 3:
                P.copy("act", SC[:, ch - 3:ch + 1, :].rr("p a n -> p (a n)"), bank.all())
        yield
        for ch in range(16):
            top16(P, SC[:, ch, :], V16[:, ch, :], I16[:, ch, :], tl, ch % 2)
            if ch % 2 == 1:
                yield
        P.tt("dve", CS.all().rr("p h (i j) -> p h i j", i=16), V16.all().pat(0, [(32, 8), (1, 16), (0, 16)]),
             V16.all().pat(16, [(32, 8), (0, 16), (1, 16)]), ALU.add)
        for h in range(8):
            top16(P, CS[:, h, :], TS[:, h, :], POSf[:, h, :], tl, h % 2)
            if h % 2 == 1:
                yield
        P.tt("dve", E.all(), TS.all(), TS.all().pat(0, [(16, 8), (0, 16)]), ALU.subtract)
        P.act(E.all(), E.all(), AF.Exp)
        P.op("dve", "tensor_reduce", out=sm.all(), in_=E.all(), axis=AX.X, op=ALU.add)
        P.op("dve", "reciprocal", out=sm.all(), in_=sm.all())
        P.tt("dve", Gb.all(), E.all(), sm.all().pat(0, [(1, 8), (0, 16)]), ALU.mult)
        yield
        P.copy("dve", POSi.all(), POSf.all())
        P.op("dve", "tensor_single_scalar", out=rowi.all(), in_=POSi.all(), scalar=4, op=ALU.arith_shift_right)
        P.op("dve", "tensor_single_scalar", out=coli.all(), in_=POSi.all(), scalar=15, op=ALU.bitwise_and)
        P.copy("dve", rowf.all(), rowi.all())
        P.copy("dve", colf.all(), coli.all())
        io16 = c.iota_f[:, 0:16].pat(0, [(0, 8), (0, 16), (1, 16)])
        for src, off, dst, oh in ((rowf, 0, i1s, OH), (colf, 16, i2s, OH2)):
            P.tt("dve", oh.all(), src.all().pat(0, [(16, 8), (1, 16), (0, 16)]), io16, ALU.is_equal)
            P.tt("dve", oh.all(), oh.all(), I16.all().pat(off, [(32, 8), (0, 16), (1, 16)]), ALU.mult)
            P.op("dve", "tensor_reduce", out=dst.all().rr("p h k -> p (h k)"), in_=oh.all().rr("p h k i -> p (h k) i"),
                 axis=AX.X, op=ALU.add)
            yield
        P.stt("dve", eidf.all(), i1s.all().rr("p h k -> p (h k)"), 128.0, i2s.all().rr("p h k -> p (h k)"), ALU.mult, ALU.add)
        P.copy("dve", EIDb.all(), eidf.all())

    def advance(gen, n):
        if gen is None:
            return None
        try:
            for _ in range(n):
                next(gen)
        except StopIteration:
            return None
        return gen

    GS = NB // 2
    NGRP = 128 // GS
    advance(front(0), 1000)
    for b in range(NT):
        tok = slice(b * 128, (b + 1) * 128)
        ht, EIDb, hnbb = hts[b % 2], EIDs[b % 2], hnbs[b % 2]
        Gf = Gs[b % 2].all().rr("p a b -> p (a b)")
        nxt = front(b + 1) if b + 1 < NT else None
        for g in range(NGRP):
            gs = slice(g * GS, (g + 1) * GS)
            for e in range(g * GS, (g + 1) * GS):
                uv = UV[e % NB]
                P.gather(uv.all(), tuv, EIDb[:, e:e + 1])
                P.stt("dve", junkb.all(), uv[:, 0:D], 1.0, hnbb.all(), ALU.mult, ALU.mult, accum_out=dots[:, e:e + 1])
            P.act(actw[:, gs], dots[:, gs], AF.Gelu_apprx_tanh)
            P.tt("dve", actw[:, gs], actw[:, gs], Gf[:, gs], ALU.mult)
            for e in range(g * GS, (g + 1) * GS):
                uv, dgt = UV[e % NB], dg[e % 4]
                P.act(dgt.all(), ident.all(), AF.Copy, scale=actw[:, e:e + 1])
                for n2 in range(2):
                    P.mm(pacc[n2].all(), dgt.all(), uv[:, D + n2 * 512:D + (n2 + 1) * 512], e == 0, e == 127)
            nxt = advance(nxt, 2)
        advance(nxt, 1000)
        for n2 in range(2):
            P.tt("dve", resd[:, n2 * 512:(n2 + 1) * 512], pacc[n2].all(), ht[:, n2 * 512:(n2 + 1) * 512], ALU.add)
        P.act(junk.all(), resd.all(), AF.Square, accum_out=ssf.all())
        P.ts("dve", ssf.all(), ssf.all(), 1.0 / D, ALU.mult, EPS, ALU.add)
        P.act(ssf.all(), ssf.all(), AF.Sqrt)
        P.op("dve", "reciprocal", out=rstdf.all(), in_=ssf.all())
        ot = outt[b % 2]
        P.stt("dve", ot.all(), resd.all(), rstdf[:, 0:1], gfb.all(), ALU.mult, ALU.mult)
        P.dma("sp", out_d[tok, :], ot.all())
    ph.close()


def late_param_shapes():
    return {"w_ssm_up": [513, 1024], "w_attn_up": [513, 1024], "w_out": [1025, 1024], "g2c": [128, 8],
            "g2b": [128, 1024], "gfb": [128, 1024], "peer_wq": [1025, 2048], "pk": [129, 16, 128],
            "peer_uv": [16385, 2048]}


def late_host_layout(inp):
    g2 = np.asarray(inp["norm2_g"], dtype=np.float32)[0]
    gf = np.asarray(inp["norm_f_g"], dtype=np.float32)
    k1, k2 = np.asarray(inp["peer_k1"])[0], np.asarray(inp["peer_k2"])[0]
    pk = np.stack([k1, k2], 1).reshape(16, 128, 128).transpose(2, 0, 1)
    d = {"w_ssm_up": np.asarray(inp["w_ssm_up"])[0], "w_attn_up": np.asarray(inp["w_attn_up"])[0],
         "w_out": np.asarray(inp["w_out"])[0], "g2c": g2.reshape(8, 128).T,
         "g2b": np.broadcast_to(g2[None, :], (128, 1024)), "gfb": np.broadcast_to(gf[None, :], (128, 1024)),
         "peer_wq": np.asarray(inp["peer_wq"])[0], "pk": pk,
         "peer_uv": np.concatenate([np.asarray(inp["peer_u"])[0], np.asarray(inp["peer_v"])[0]], 1)}
    return {k: np.ascontiguousarray(v, dtype=np.float32) for k, v in d.items()}


def ssm_param_shapes():
    return {"ar_sm": [128, 32], "ai_sm": [128, 32], "ldt_sm": [128, 32],
            "bU1": [128, 32, 16], "bU2": [128, 32, 16], "cT1": [128, 32, 16], "cT2": [128, 32, 16],
            "ar_cm": [128, 4, 64], "ai_cm": [128, 4, 64], "ldt_cm": [128, 4, 64],
            "br_cm": [128, 4, 64], "bi_cm": [128, 4, 64], "dskip": [128, 4], "w_glu": [513, 512]}


def ssm_host_layout(inp):
    a_re, a_im, log_dt = inp["a_re"][0], inp["a_im"][0], inp["log_dt"][0]
    b_re, b_im, c_re, c_im = inp["b_re"][0], inp["b_im"][0], inp["c_re"][0], inp["c_im"][0]
    d = {}
    d["ar_sm"] = np.concatenate([a_re.T, a_re.T], 0)
    d["ai_sm"] = np.concatenate([a_im.T, a_im.T], 0)
    d["ldt_sm"] = np.broadcast_to(log_dt[None, :], (128, 32))
    brT, biT = b_re.transpose(1, 0, 2), b_im.transpose(1, 0, 2)
    d["bU1"] = np.concatenate([brT, biT], 0)
    d["bU2"] = np.concatenate([biT, brT], 0)
    crT, ciT = c_re.transpose(2, 0, 1), c_im.transpose(2, 0, 1)
    d["cT1"] = np.concatenate([crT, ciT], 0)
    d["cT2"] = np.concatenate([ciT, crT], 0)
    q = np.arange(128)
    gq = (np.arange(4)[None, :] * 8 + (q // 16)[:, None])
    d["ar_cm"] = a_re[gq]
    d["ai_cm"] = a_im[gq]
    d["ldt_cm"] = np.broadcast_to(log_dt[gq][:, :, None], (128, 4, 64))
    d["br_cm"] = b_re[gq, :, (q % 16)[:, None]]
    d["bi_cm"] = b_im[gq, :, (q % 16)[:, None]]
    d["dskip"] = inp["d_skip"][0].reshape(4, 128).T
    d["w_glu"] = inp["w_glu"][0]
    return {k: np.ascontiguousarray(v, dtype=np.float32) for k, v in d.items()}


def build(nc, S, stage_stop=None, dbg=None):
    NT = S // 128
    NCH = S // LCH
    TOPK = min(256, S // 4)
    es = ExitStack()
    P = Prog(nc, es)
    c = Ctx()
    c.P = P
    dbg = dbg if dbg is not None else {}

    x_d = P.dram("x", [S, D], F32, "ExternalInput")
    g1c_d = P.dram("g1c", [128, 8], F32, "ExternalInput")
    w_in_d = P.dram("w_in", [D + 1, IN_W], F32, "ExternalInput")
    out_d = P.dram("out", [S, D], F32, "ExternalOutput")

    def dbg_out(name, shape, dt=F32):
        t = P.dram("dbg_" + name, shape, dt, "ExternalOutput")
        dbg[name] = t
        return t

    blk = es.enter_context(nc.Block())
    holder = {}

    def body(_sync):
        glob = ExitStack()
        ident = P.sb(glob, "ident", [128, 128], BF16)
        identf = P.sb(glob, "identf", [128, 128], F32)
        iota_i = P.sb(glob, "iota_i", [128, 128], I32)
        pid_i = P.sb(glob, "pid_i", [128, 1], I32)
        pid_f = P.sb(glob, "pid_f", [128, 1], F32)
        iota_f = P.sb(glob, "iota_f", [128, 128], F32)
        P.op("pool", "iota", out=iota_i.all(), pattern=[[1, 128]], base=0, channel_multiplier=0)
        P.op("pool", "iota", out=pid_i.all(), pattern=[[0, 1]], base=0, channel_multiplier=1)
        P.copy("dve", iota_f.all(), iota_i.all())
        P.copy("dve", pid_f.all(), pid_i.all())
        P.ts("dve", identf.all(), iota_f.all(), pid_f[:, 0:1], ALU.is_equal)
        P.copy("dve", ident.all(), identf.all())
        caus = P.sb(glob, "caus", [128, 128], F32)
        P.ts("dve", caus.all(), iota_f.all(), pid_f[:, 0:1], ALU.is_gt, NEG, ALU.mult)
        g1c = P.sb(glob, "g1c", [128, 8], F32)
        ng1c = P.sb(glob, "ng1c", [128, 8], F32)
        P.dma("sp", g1c.all(), g1c_d.all())
        P.ts("dve", ng1c.all(), g1c.all(), -1.0, ALU.mult)
        c.ident, c.identf, c.iota_f, c.pid_f, c.caus = ident, identf, iota_f, pid_f, caus

        psb = [P.ps(glob, f"psb{i}", [128, 512], F32) for i in range(8)]
        c.psb = psb

        res = ExitStack()
        uT = P.sb(res, "uys", [128, 4, S], BF16)
        res_a = ExitStack()
        res_a_close = res_a.close
        kTd = P.sb(res_a, "kTd", [128, 2, S], BF16)
        kiT4 = P.sb(res_a, "kiT4", [128, S], BF16)
        vaug = P.sb(res_a, "vaug", [128, NT, 2, 80], BF16)
        P.memset("pool", vaug.all(), 1.0)

        s1 = ExitStack()
        rope_tables(P, s1, S, c)
        stg = [P.sb(s1, f"stg{i}", [128, 1024], F32) for i in range(2)]
        w_u = P.sb(s1, "w_u", [128, 8, 512], BF16)
        w_kd = P.sb(s1, "w_kd", [128, 8, 4, 128], BF16)
        w_ki = P.sb(s1, "w_ki", [128, 8, 2, 128], BF16)
        w_v = P.sb(s1, "w_v", [128, 8, 128], BF16)

        def cvt(dst, src, k, neg=False):
            P.act(dst, src, AF.Copy, scale=(ng1c if neg else g1c)[:, k:k + 1])

        load_w_cols(P, c, lambda k, st: cvt(w_u[:, k, :], st[:, 0:512], k), OFF_U, 512, w_in_d, g1c, stg)

        def k_cvt(k, st):
            for n in range(2):
                for dup in range(2):
                    cvt(w_kd[:, k, n, dup * 64:(dup + 1) * 64], st[:, n * 64:(n + 1) * 64], k)
                    cvt(w_kd[:, k, 2 + n, dup * 64:dup * 64 + 32], st[:, n * 64 + 32:n * 64 + 64], k, neg=True)
                    cvt(w_kd[:, k, 2 + n, dup * 64 + 32:dup * 64 + 64], st[:, n * 64:n * 64 + 32], k)
        load_w_cols(P, c, k_cvt, OFF_K, 128, w_in_d, g1c, stg)

        def ki_cvt(k, st):
            for r in range(4):
                cvt(w_ki[:, k, 0, r * 32:(r + 1) * 32], st[:, 0:32], k)
                cvt(w_ki[:, k, 1, r * 32:r * 32 + 16], st[:, 16:32], k, neg=True)
                cvt(w_ki[:, k, 1, r * 32 + 16:r * 32 + 32], st[:, 0:16], k)
        load_w_cols(P, c, ki_cvt, OFF_KI, 32, w_in_d, g1c, stg)
        load_w_cols(P, c, lambda k, st: cvt(w_v[:, k, :], st[:, 0:128], k), OFF_V, 128, w_in_d, g1c, stg)

        xt = [P.sb(s1, f"xt{i}", [128, D], F32) for i in range(2)]
        xh = P.sb(s1, "xh", [128, D], BF16)
        xhT = P.sb(s1, "xhT", [128, 8, 128], BF16)
        junk = P.sb(s1, "junk", [128, D], BF16)
        ss = P.sb(s1, "ss", [128, 1], F32)
        rstd = P.sb(s1, "rstd", [128, 1], F32)
        r1 = P.sb(s1, "r1", [128, 128], F32)
        r2 = P.sb(s1, "r2", [128, 128], F32)

        def norm_and_transpose(b, xt_b, xh, xhT, junk, ss, rstd, psT):
            P.act(junk.all(), xt_b.all(), AF.Square, accum_out=ss.all())
            P.act(ss.all(), ss.all(), AF.Sqrt, scale=1.0 / D, bias=EPS) if False else None
            P.ts("dve", ss.all(), ss.all(), 1.0 / D, ALU.mult, EPS, ALU.add)
            P.act(ss.all(), ss.all(), AF.Sqrt)
            P.op("dve", "reciprocal", out=rstd.all(), in_=ss.all())
            P.ts("dve", xh.all(), xt_b.all(), rstd[:, 0:1], ALU.mult)
            pb = psT.all().cast(BF16)
            for k in range(8):
                P.tr(pb[:, k * 128:(k + 1) * 128], xh[:, k * 128:(k + 1) * 128], ident.all())
            P.copy("act", xhT.all().rr("p k t -> p (k t)"), pb)
        c.norm_and_transpose = norm_and_transpose

        for b in range(NT):
            xb = xt[b % 2]
            P.dma("sp", xb.all(), x_d[b * 128:(b + 1) * 128, :])
            norm_and_transpose(b, xb, xh, xhT, junk, ss, rstd, psb[0])
            tok = slice(b * 128, (b + 1) * 128)
            for m in range(4):
                for k in range(8):
                    P.mm(psb[1][:, m * 128:(m + 1) * 128], w_u[:, k, m * 128:(m + 1) * 128], xhT[:, k, :], k == 0, k == 7)
            P.copy("act", uT[:, :, tok], psb[1].all().rr("p (m t) -> p m t", m=4))
            for m in range(4):
                for k in range(8):
                    P.mm(psb[2][:, m * 128:(m + 1) * 128], w_kd[:, k, m, :], xhT[:, k, :], k == 0, k == 7)
            for n in range(2):
                P.tt("dve", r1.all(), psb[2][:, n * 128:(n + 1) * 128], c.rope["cosA"][:, tok], ALU.mult)
                P.tt("dve", r2.all(), psb[2][:, (2 + n) * 128:(3 + n) * 128], c.rope["sinA"][:, tok], ALU.mult)
                P.tt("dve", kTd[:, n, tok], r1.all(), r2.all(), ALU.add)
            for m in range(2):
                for k in range(8):
                    P.mm(psb[3][:, m * 128:(m + 1) * 128], w_ki[:, k, m, :], xhT[:, k, :], k == 0, k == 7)
            for k in range(8):
                P.mm(psb[3][:, 256:384], xhT[:, k, :], w_v[:, k, :], k == 0, k == 7)
            P.tt("dve", r1.all(), psb[3][:, 0:128], c.rope["cosI"][:, tok], ALU.mult)
            P.tt("dve", r2.all(), psb[3][:, 128:256], c.rope["sinI"][:, tok], ALU.mult)
            P.tt("dve", kiT4[:, tok], r1.all(), r2.all(), ALU.add)
            P.copy("act", vaug[:, b, :, 0:64], psb[3][:, 256:384].rr("p (n d) -> p n d", n=2))
        if stage_stop == "s1":
            for nm in ("cosA", "sinA", "cosI", "sinI"):
                t = dbg_out(nm, [128, S], BF16)
                P.dma("sp", t.all(), c.rope[nm].all())
        s1.close()

        if stage_stop == "s1":
            t = dbg_out("uT", [128, 4 * S], BF16)
            P.dma("sp", t.all(), uT.all().rr("p m t -> p (m t)"))
            t = dbg_out("kTd", [128, 2 * S], BF16)
            P.dma("sp", t.all(), kTd.all().rr("p m t -> p (m t)"))
            t = dbg_out("kiT4", [128, S], BF16)
            P.dma("sp", t.all(), kiT4.all())
            t = dbg_out("vaug", [128, NT * 160], BF16)
            P.dma("sp", t.all(), vaug.all().rr("p a n d -> p (a n d)"))
            P.finish(list(dbg.values()))
            res_a.close()
            res.close()
            glob.close()
            return

        sp = {nm: P.dram(nm, shp, F32, "ExternalInput") for nm, shp in ssm_param_shapes().items()}
        if stage_stop == "s2":
            def dump(name, view, dt):
                t = dbg_out(name, list(view.shape), dt)
                P.dma("sp", t.all(), view)
            c.dump = dump
        ssm_phase(P, c, S, uT, sp)
        if stage_stop == "s2":
            t = dbg_out("ysT", [128, 4 * S], BF16)
            P.dma("sp", t.all(), uT.all().rr("p m t -> p (m t)"))
            P.finish(list(dbg.values()))
            res_a.close()
            res.close()
            glob.close()
            return
        ya_d = P.dram("ya_scr", [64, 8, S], BF16, "ExternalOutput" if stage_stop == "a" else "Internal")
        astop = stage_stop[2:] if (stage_stop or "").startswith("a:") else None

        def adump(name, view, dt):
            t = dbg_out(name, list(view.shape), dt)
            P.dma("sp", t.all(), view)
        attn_phase(P, c, S, x_d, w_in_d, g1c, ng1c, kTd, kiT4, vaug, ya_d, stop_at=astop, dump=adump if astop else None)
        res_a.close()
        if astop:
            P.finish(list(dbg.values()))
            res.close()
            glob.close()
            return
        if stage_stop == "a":
            dbg["ya_scr"] = ya_d
            P.finish([ya_d])
            res.close()
            glob.close()
            return
        pd = {nm: P.dram(nm, shp, F32, "ExternalInput") for nm, shp in late_param_shapes().items()}
        h_d = P.dram("h_scr", [S, D], F32, "ExternalOutput" if stage_stop == "b" else "Internal")
        uvb_d = P.dram("uvb_scr", [16384, 2048], BF16, "ExternalOutput" if stage_stop == "cdbg" else "Internal")
        merge_phase(P, c, S, x_d, w_in_d, g1c, uT, ya_d, h_d, pd, uvb_d)
        res.close()
        if stage_stop == "b":
            dbg["h_scr"] = h_d
            P.finish([h_d])
            glob.close()
            return
        def cdump(name, view, dt):
            t = dbg_out(name, list(view.shape), dt)
            P.dma("sp", t.all(), view)
        peer_phase(P, c, S, h_d, out_d, pd, uvb_d, dump=cdump if stage_stop == "cdbg" else None)
        P.finish([out_d] + list(dbg.values()) + ([uvb_d] if stage_stop == "cdbg" else []))
        glob.close()

    holder["rest"] = lambda P, c, env: None
    blk.sync(body)
    es.close()
    return P, dbg


PADDED = ("w_in", "w_glu", "w_ssm_up", "w_attn_up", "w_out", "peer_wq", "pk", "peer_uv")


def core_inputs(shared, xb):
    im = dict(shared)
    im["x"] = np.ascontiguousarray(xb, dtype=np.float32)
    flat = im["x"].reshape(-1)
    for nm in PADDED:
        a = shared[nm]
        row = flat[:a[0].size].reshape((1,) + a.shape[1:])
        im[nm] = np.concatenate([a, row], 0)
    return im


def kernel(**inputs):
    inputs = {k: np.asarray(v) for k, v in inputs.items()}
    B, S, _ = inputs["x"].shape
    assert B == NCORES
    g1 = inputs["norm1_g"].astype(np.float32)[0]
    shared = {"g1c": np.ascontiguousarray(g1.reshape(8, 128).T),
              "w_in": np.ascontiguousarray(inputs["w_in"].astype(np.float32)[0])}
    shared.update(ssm_host_layout(inputs))
    shared.update(late_host_layout(inputs))
    nc = bass.Bass("TRN2", target_bir_lowering=False)
    build(nc, S)
    x = inputs["x"].astype(np.float32)
    in_maps = [core_inputs(shared, x[b]) for b in range(NCORES)]
    res = run_bass_kernel_spmd(nc, in_maps, core_ids=list(range(NCORES)))
    out = np.stack([np.asarray(res.results[b]["out"], dtype=np.float32) for b in range(NCORES)], 0)
    return out
```

```python
import math
from contextlib import ExitStack

import numpy as np
import concourse.bass as bass
import concourse.mybir as mybir
from concourse.bass_utils import run_bass_kernel_spmd

F32 = mybir.dt.float32
BF16 = mybir.dt.bfloat16
I32 = mybir.dt.int32
U32 = mybir.dt.uint32
ALU = mybir.AluOpType
AF = mybir.ActivationFunctionType
AX = mybir.AxisListType

D = 1024
NCORES = 8
SSM_W = 512
NG = 32
NP_ = 64
LCH = 16
EPS = 1e-6
NEG = -1.0e30
STRICT = True
DUMP2 = False
OUTK = ("out", "accum_out", "out_max", "out_indices")


class V:
    __slots__ = ("t", "ap")

    def __init__(self, t, ap):
        self.t = t
        self.ap = ap

    def __getitem__(self, k):
        return V(self.t, self.ap[k])

    def rr(self, pat, **kw):
        return V(self.t, self.ap.rearrange(pat, **kw))

    def bc(self, shape):
        return V(self.t, self.ap.to_broadcast(list(shape)))

    def cast(self, dt):
        return V(self.t, self.ap.bitcast(dt))

    def pat(self, off, pattern):
        a = self.ap
        return V(self.t, bass.AP(a.tensor, a.offset + off, [list(a.ap[0])] + [list(p) for p in pattern]))

    @property
    def shape(self):
        return self.ap.shape


class Tl:
    def __init__(self, base_ap, name, dram=False):
        self.base = base_ap
        self.name = name
        self.dram = dram
        self.w = None
        self.r = {}
        self.dsem = None

    def __getitem__(self, k):
        return V(self, self.base[k])

    def all(self):
        return V(self, self.base)


class Prog:
    EPOCH = 14000

    def __init__(self, nc, es):
        self.nc = nc
        self.es = es
        self.eng = {"pe": nc.tensor, "dve": nc.vector, "act": nc.scalar, "pool": nc.gpsimd, "sp": nc.sync}
        self.sems = []
        self.semeng = []
        self.cur = {}
        self.cnt = {}
        self.known = {e: {} for e in self.eng}
        self.dcnt = {}
        self.ninstr = 0
        self.freed = {}
        self.log = None
        for e in self.eng:
            self._newsem(e)

    def _newsem(self, e):
        s = self.es.enter_context(self.nc.semaphore(f"s{len(self.sems)}"))
        self.sems.append(s)
        self.semeng.append(e)
        idx = len(self.sems) - 1
        if e is not None:
            self.cur[e] = idx
            self.cnt[e] = 0
        else:
            self.dcnt[idx] = 0
        return idx

    def sb(self, es, name, shape, dt):
        self.uid = getattr(self, "uid", 0) + 1
        h = es.enter_context(self.nc.sbuf_tensor(f"sb{self.uid}_" + name, list(shape), dt))
        t = Tl(h[:], name)
        t.r = dict(self.freed)
        es.callback(self._on_free, t)
        return t

    def _on_free(self, t):
        toks = dict(t.r)
        if t.w is not None:
            toks[t.w[0]] = max(toks.get(t.w[0], 0), t.w[1])
        for si, val in toks.items():
            if self.freed.get(si, 0) < val:
                self.freed[si] = val

    def ps(self, es, name, shape, dt):
        h = es.enter_context(self.nc.psum_tensor("ps_" + name, list(shape), dt))
        return Tl(h[:], name)

    def dram(self, name, shape, dt, kind):
        h = self.nc.dram_tensor(name, list(shape), dt, kind=kind)
        return Tl(h.ap(), name, dram=True)

    def _need(self, e, tok, raw, dma=False):
        if tok is None:
            return
        si, val = tok
        owner = self.semeng[si]
        if owner == e and (e == "pe" or (not raw and not STRICT)) and not dma:
            return
        k = self.known[e]
        if k.get(si, 0) >= val:
            return
        self.eng[e].wait_ge(self.sems[si], val)
        self.ninstr += 1
        k[si] = val
        if self.log is not None:
            self.log.append(f"{e}: WAIT s{si}({self.semeng[si]}) >= {val}")

    def _deps(self, e, reads, writes, skip_w_sem=None, dma=False):
        for t in reads:
            self._need(e, t.w, True, dma)
        for t in writes:
            if t.w is not None and t.w[0] != skip_w_sem:
                self._need(e, t.w, False, dma)
            for si, val in t.r.items():
                self._need(e, (si, val), False, dma)

    def _mark(self, tok, reads, writes):
        si, val = tok
        for t in reads:
            if t.r.get(si, 0) < val:
                t.r[si] = val
        for t in writes:
            t.w = tok
            t.r = {}

    def op(self, e, fn, **kw):
        reads, writes, args = [], [], {}
        for k, v in kw.items():
            if isinstance(v, V):
                (writes if k in OUTK else reads).append(v.t)
                args[k] = v.ap
            else:
                args[k] = v
        self._deps(e, reads, writes)
        ins = getattr(self.eng[e], fn)(**args)
        if self.log is not None:
            self.log.append(f"{e}: {fn} W={[t.name for t in writes]} R={[t.name for t in reads]} -> {self.cnt[e] + 1}")
        if self.cnt[e] >= self.EPOCH:
            self._newsem(e)
        si = self.cur[e]
        self.cnt[e] += 1
        ins.then_inc(self.sems[si], 1)
        self.ninstr += 1
        self._mark((si, self.cnt[e]), reads, writes)
        return ins

    def _dsem(self, t):
        if t.dsem is None:
            t.dsem = self._newsem(None)
        return t.dsem

    def dma(self, q, out, in_, semtile=None, extra_reads=(), **kw):
        sbt = semtile if semtile is not None else (out.t if not out.t.dram else in_.t)
        ds = self._dsem(sbt)
        reads = [in_.t] + [x.t for x in extra_reads]
        writes = [out.t]
        self._deps(q, reads, writes, skip_w_sem=ds, dma=True)
        ins = self.eng[q].dma_start(out=out.ap, in_=in_.ap, **kw)
        self.dcnt[ds] += 16
        ins.then_inc(self.sems[ds], 16)
        self.ninstr += 1
        self._mark((ds, self.dcnt[ds]), reads, writes)

    def gather(self, out, table, idx):
        ds = self._dsem(out.t)
        reads = [table.t, idx.t]
        writes = [out.t]
        self._deps("pool", reads, writes, skip_w_sem=ds, dma=True)
        ins = self.nc.gpsimd.indirect_dma_start(
            out=out.ap, out_offset=None, in_=table.ap,
            in_offset=bass.IndirectOffsetOnAxis(ap=idx.ap, axis=0))
        self.dcnt[ds] += 16
        ins.then_inc(self.sems[ds], 16)
        self.ninstr += 1
        self._mark((ds, self.dcnt[ds]), reads, writes)

    def finish(self, tiles):
        for t in tiles:
            self._need("sp", t.w, True)

    def mm(self, out, lhsT, rhs, start, stop, **kw):
        return self.op("pe", "matmul", out=out, lhsT=lhsT, rhs=rhs, start=start, stop=stop, **kw)

    def tr(self, out, in_, ident):
        return self.op("pe", "transpose", out=out, in_=in_, identity=ident)

    def act(self, out, in_, func, **kw):
        return self.op("act", "activation", out=out, in_=in_, func=func, **kw)

    def tt(self, e, out, in0, in1, op):
        return self.op(e, "tensor_tensor", out=out, in0=in0, in1=in1, op=op)

    def ts(self, e, out, in0, s1, op0, s2=None, op1=None, **kw):
        if op1 is None:
            return self.op(e, "tensor_scalar", out=out, in0=in0, scalar1=s1, scalar2=None, op0=op0, **kw)
        if isinstance(s1, V) != isinstance(s2, V):
            self.op(e, "tensor_scalar", out=out, in0=in0, scalar1=s1, scalar2=None, op0=op0)
            return self.op(e, "tensor_scalar", out=out, in0=out, scalar1=s2, scalar2=None, op0=op1, **kw)
        return self.op(e, "tensor_scalar", out=out, in0=in0, scalar1=s1, scalar2=s2, op0=op0, op1=op1, **kw)

    def stt(self, e, out, in0, scalar, in1, op0, op1, **kw):
        return self.op(e, "scalar_tensor_tensor", out=out, in0=in0, scalar=scalar, in1=in1, op0=op0, op1=op1, **kw)

    def copy(self, e, out, in_):
        if e == "act":
            return self.act(out, in_, AF.Copy)
        return self.op(e, "tensor_copy", out=out, in_=in_)

    def memset(self, e, out, val):
        return self.op(e, "memset", ap=out, constant=val) if False else self._memset(e, out, val)

    def _memset(self, e, out, val):
        self._deps(e, [], [out.t])
        ins = self.eng[e].memset(out.ap, val)
        if self.cnt[e] >= self.EPOCH:
            self._newsem(e)
        si = self.cur[e]
        self.cnt[e] += 1
        ins.then_inc(self.sems[si], 1)
        self.ninstr += 1
        self._mark((si, self.cnt[e]), [], [out.t])


OFF_U, OFF_Q, OFF_K, OFF_V, OFF_QI, OFF_KI, OFF_WI, OFF_GS, OFF_GA = 0, 512, 1024, 1152, 1280, 1536, 1568, 1576, 2600
IN_W = 3624
TWO_PI = 2.0 * math.pi


class Ctx:
    pass


def rope_tables(P, es, S, c):
    tabs = {fn + nm: P.sb(es, f"rope_{fn}{nm}", [128, S], BF16) for nm in ("A", "I") for fn in ("cos", "sin")}
    tmp = ExitStack()
    pid = P.sb(tmp, "rt_pid", [128, 1], I32)
    pm = P.sb(tmp, "rt_pm", [128, 1], I32)
    pf = P.sb(tmp, "rt_pf", [128, 1], F32)
    inv = P.sb(tmp, "rt_inv", [128, 2], F32)
    posi = P.sb(tmp, "rt_posi", [128, S], I32)
    pos = P.sb(tmp, "rt_pos", [128, S], F32)
    ang = P.sb(tmp, "rt_ang", [128, S], F32)
    t1 = P.sb(tmp, "rt_t1", [128, S], F32)
    ti = P.sb(tmp, "rt_ti", [128, S], I32)
    P.op("pool", "iota", out=pid.all(), pattern=[[0, 1]], base=0, channel_multiplier=1)
    P.op("pool", "iota", out=posi.all(), pattern=[[1, S]], base=0, channel_multiplier=0)
    P.copy("dve", pos.all(), posi.all())
    for j, (msk, dim) in enumerate(((31, 64), (15, 32))):
        P.op("dve", "tensor_single_scalar", out=pm.all(), in_=pid.all(), scalar=msk, op=ALU.bitwise_and)
        P.copy("dve", pf.all(), pm.all())
        P.act(inv[:, j:j + 1], pf.all(), AF.Exp, scale=-math.log(10000.0) * 2.0 / dim)
    outs = {}
    for j, nm in enumerate(("A", "I")):
        for k, (fn, shift) in enumerate((("cos", math.pi / 2), ("sin", 0.0))):
            tab = tabs[fn + nm]
            P.ts("dve", ang.all(), pos.all(), inv[:, j:j + 1], ALU.mult, shift, ALU.add)
            range_reduce_sin(P, tab.all(), ang.all(), t1.all(), ti.all())
            outs[fn + nm] = tab
    tmp.close()
    c.rope = outs


def range_reduce_sin(P, out, ang, t1, ti):
    P.ts("dve", t1, ang, 1.0 / TWO_PI, ALU.mult)
    P.copy("dve", ti, t1)
    P.copy("dve", t1, ti)
    P.stt("dve", t1, t1, -TWO_PI, ang, ALU.mult, ALU.add)
    P.ts("dve", ang, t1, math.pi, ALU.is_gt)
    P.stt("dve", t1, ang, -TWO_PI, t1, ALU.mult, ALU.add)
    P.ts("dve", t1, t1, 3.141592, ALU.min, -3.141592, ALU.max)
    P.act(out, t1, AF.Sin)


def load_w_cols(P, c, dst_fn, col0, ncols, w_in_d, gcol, stage):
    for k in range(8):
        st = stage[k % 2]
        P.dma("sp" if k % 2 == 0 else "act", st[:, 0:ncols], w_in_d[k * 128:(k + 1) * 128, col0:col0 + ncols])
        dst_fn(k, st)


def cpow(P, es, name, lr, th, jv, G, J, order="gj"):
    shp = [128, G, J] if order == "gj" else [128, J, G]
    Pr = P.sb(es, name + "_r", shp, F32)
    Pi = P.sb(es, name + "_i", shp, F32)
    tmp = ExitStack()
    mag = P.sb(tmp, name + "_mag", shp, F32)
    ang = P.sb(tmp, name + "_ang", shp, F32)
    t1 = P.sb(tmp, name + "_t1", shp, F32)
    ti = P.sb(tmp, name + "_ti", shp, I32)
    if order == "gj":
        lb, jb = lr.pat(0, [(1, G), (0, J)]), jv.pat(0, [(0, G), (1, J)])
        tb = th.pat(0, [(1, G), (0, J)])
    else:
        lb, jb = lr.pat(0, [(0, J), (1, G)]), jv.pat(0, [(1, J), (0, G)])
        tb = th.pat(0, [(0, J), (1, G)])
    P.tt("dve", mag.all(), lb, jb, ALU.mult)
    P.act(mag.all(), mag.all(), AF.Exp)
    fl = "p a b -> p (a b)"
    for dst, shift in ((Pi, 0.0), (Pr, math.pi / 2)):
        P.tt("dve", ang.all(), tb, jb, ALU.mult)
        if shift:
            P.ts("dve", ang.all(), ang.all(), shift, ALU.add)
        range_reduce_sin(P, dst.all().rr(fl), ang.all().rr(fl), t1.all().rr(fl), ti.all().rr(fl))
        P.tt("dve", dst.all(), dst.all(), mag.all(), ALU.mult)
    tmp.close()
    return Pr, Pi


def kappa(P, es, name, ar, ai, lr, th, G):
    kr = P.sb(es, name + "_kr", [128, G], F32)
    ki = P.sb(es, name + "_ki", [128, G], F32)
    tmp = ExitStack()
    one = P.sb(tmp, name + "_one", [128, 1], F32)
    P.memset("dve", one.all(), 1.0)
    Ar, Ai = cpow(P, tmp, name + "_a1", lr, th, one.all(), G, 1)
    den = P.sb(tmp, name + "_den", [128, G], F32)
    t = P.sb(tmp, name + "_t", [128, G], F32)
    arm = P.sb(tmp, name + "_arm", [128, G], F32)
    A_r, A_i = Ar.all().rr("p g j -> p (g j)"), Ai.all().rr("p g j -> p (g j)")
    P.tt("dve", den.all(), ar, ar, ALU.mult)
    P.tt("dve", t.all(), ai, ai, ALU.mult)
    P.tt("dve", den.all(), den.all(), t.all(), ALU.add)
    P.op("dve", "reciprocal", out=den.all(), in_=den.all())
    P.ts("dve", arm.all(), A_r, -1.0, ALU.add)
    P.tt("dve", kr.all(), arm.all(), ar, ALU.mult)
    P.tt("dve", t.all(), A_i, ai, ALU.mult)
    P.tt("dve", kr.all(), kr.all(), t.all(), ALU.add)
    P.tt("dve", kr.all(), kr.all(), den.all(), ALU.mult)
    P.tt("dve", ki.all(), A_i, ar, ALU.mult)
    P.tt("dve", t.all(), arm.all(), ai, ALU.mult)
    P.tt("dve", ki.all(), ki.all(), t.all(), ALU.subtract)
    P.tt("dve", ki.all(), ki.all(), den.all(), ALU.mult)
    tmp.close()
    return kr, ki


def ssm_phase(P, c, S, uys, sp):
    NCH = S // LCH
    psb = c.psb
    uT = ysT = y2 = uys
    ph = ExitStack()
    sg = P.sb(ph, "sg", [128, 1], F32)
    nsg = P.sb(ph, "nsg", [128, 1], F32)
    P.ts("dve", sg.all(), c.pid_f.all(), 63.5, ALU.is_gt, -2.0, ALU.mult)
    P.ts("dve", sg.all(), sg.all(), 1.0, ALU.add)
    P.ts("dve", nsg.all(), sg.all(), -1.0, ALU.mult)
    jv = P.sb(ph, "jv", [128, 256], F32)
    jvi = P.sb(ph, "jvi", [128, 256], I32)
    P.op("pool", "iota", out=jvi.all(), pattern=[[1, 256]], base=0, channel_multiplier=0)
    P.copy("dve", jv.all(), jvi.all())
    jrev = P.sb(ph, "jrev", [128, 16], F32)
    P.ts("dve", jrev.all(), jv[:, 0:16], -1.0, ALU.mult, 15.0, ALU.add)
    bm = P.sb(ph, "bm", [128, 8], F32)
    t8 = P.sb(ph, "t8", [128, 8], F32)
    P.ts("dve", t8.all(), jv[:, 0:8], 16.0, ALU.mult)
    P.ts("dve", bm.all(), t8.all(), c.pid_f[:, 0:1], ALU.subtract)
    P.ts("dve", t8.all(), bm.all(), 0.5, ALU.is_gt, -1.0, ALU.mult)
    P.ts("dve", t8.all(), t8.all(), 1.0, ALU.add)
    P.ts("dve", bm.all(), bm.all(), -15.5, ALU.is_gt)
    P.tt("dve", bm.all(), bm.all(), t8.all(), ALU.mult)
    eye8 = P.sb(ph, "eye8", [128, 8, 8], F32)
    P.tt("dve", eye8.all(), jv[:, 0:8].pat(0, [(1, 8), (0, 8)]), jv[:, 0:8].pat(0, [(0, 8), (1, 8)]), ALU.is_equal)
    pswapb = P.sb(ph, "pswapb", [128, 128], BF16)
    pswap = P.sb(ph, "pswap", [128, 128], F32)
    P.ts("dve", pswap.all(), c.iota_f.all(), c.pid_f[:, 0:1], ALU.subtract)
    P.tt("dve", pswap.all(), pswap.all(), pswap.all(), ALU.mult)
    P.ts("dve", pswap.all(), pswap.all(), 4096.0, ALU.is_equal)
    P.copy("dve", pswapb.all(), pswap.all())
    dsk = P.sb(ph, "dsk", [128, 4], F32)
    P.dma("sp", dsk.all(), sp["dskip"].all())
    wglu = P.sb(ph, "wglu", [128, 4, 512], BF16)
    stgw = P.sb(ph, "stgw", [128, 512], F32)
    for k in range(4):
        P.dma("sp", stgw.all(), sp["w_glu"][k * 128:(k + 1) * 128, :])
        P.copy("act", wglu[:, k, :], stgw.all())

    for o in range(4):
        oc = ExitStack()
        BD = P.sb(oc, "BD", [128, 16, 128], BF16)
        Wb = P.sb(oc, "Wb", [128, 8, 16, 128], BF16)
        Wc = P.sb(oc, "Wc", [128, 8, 16, 128], BF16)
        tc_ = P.sb(oc, "tabc", [128, 8, NCH], BF16)
        tsn = P.sb(oc, "tabs", [128, 8, NCH], BF16)
        rho = P.sb(oc, "rho", [128, 8], F32)
        pr = ExitStack()
        prm = P.sb(pr, "prm", [128, 3, 8], F32)
        for i, nm in enumerate(("ar_sm", "ai_sm", "ldt_sm")):
            P.dma("sp", prm[:, i, :], sp[nm][:, o * 8:(o + 1) * 8])
        dt = P.sb(pr, "dt", [128, 8], F32)
        lr = P.sb(pr, "lr", [128, 8], F32)
        th = P.sb(pr, "th", [128, 8], F32)
        P.act(dt.all(), prm[:, 2, :], AF.Exp)
        P.tt("dve", lr.all(), prm[:, 0, :], dt.all(), ALU.mult)
        P.tt("dve", th.all(), prm[:, 1, :], dt.all(), ALU.mult)
        kr, ki = kappa(P, pr, "ksm", prm[:, 0, :], prm[:, 1, :], lr.all(), th.all(), 8)
        Ar, Ai = cpow(P, pr, "apw", lr.all(), th.all(), jv[:, 0:17], 8, 17)
        prA = pr
        pr = ExitStack()
        U = P.sb(pr, "U12", [128, 2, 8, 16], F32)
        T12 = P.sb(pr, "T12", [128, 2, 8, 16], F32)
        for i, nm in enumerate(("bU1", "bU2")):
            P.dma("sp", U[:, i], sp[nm][:, o * 8:(o + 1) * 8, :])
        for i, nm in enumerate(("cT1", "cT2")):
            P.dma("act", T12[:, i], sp[nm][:, o * 8:(o + 1) * 8, :])
        X = P.sb(pr, "X", [128, 8, 16], F32)
        tx = P.sb(pr, "tx", [128, 8, 16], F32)
        kib = P.sb(pr, "kib", [128, 8], F32)
        P.ts("dve", kib.all(), ki.all(), nsg[:, 0:1], ALU.mult)
        P.tt("dve", X.all(), U[:, 0], kr.all().pat(0, [(1, 8), (0, 16)]), ALU.mult)
        P.tt("dve", tx.all(), U[:, 1], kib.all().pat(0, [(1, 8), (0, 16)]), ALU.mult)
        P.tt("dve", X.all(), X.all(), tx.all(), ALU.add)
        if getattr(c, "dump", None) and o == 0 and DUMP2:
            c.dump("kr", kr.all(), F32)
            c.dump("ki", ki.all(), F32)
            c.dump("X", X.all().rr("p a b -> p (a b)"), F32)
            c.dump("Ar", Ar.all().rr("p a b -> p (a b)"), F32)
        Y = P.sb(pr, "Y", [128, 8, 17, 16], F32)
        ty = P.sb(pr, "ty", [128, 8, 17, 16], F32)
        for g in range(8):
            P.tt("dve", Y[:, g], T12[:, 0, g, :].pat(0, [(0, 17), (1, 16)]), Ar[:, g, :].pat(0, [(1, 17), (0, 16)]), ALU.mult)
            P.tt("dve", ty[:, g], T12[:, 1, g, :].pat(0, [(0, 17), (1, 16)]), Ai[:, g, :].pat(0, [(1, 17), (0, 16)]), ALU.mult)
        P.stt("dve", Y.all().rr("p g t c -> p (g t c)"), Y.all().rr("p g t c -> p (g t c)"), sg[:, 0:1],
              ty.all().rr("p g t c -> p (g t c)"), ALU.mult, ALU.subtract)
        if getattr(c, "dump", None) and o == 0 and DUMP2:
            c.dump("Y", Y.all().rr("p a b c -> p (a b c)"), F32)
        for g in range(8):
            P.tt("dve", Wc[:, g].rr("p t (a c) -> p t a c", a=8),
                 Y[:, g, 1:17, :].pat(0, [(16, 16), (0, 8), (1, 16)]),
                 eye8[:, g, :].pat(0, [(0, 16), (1, 8), (0, 16)]), ALU.mult)
        Xpad = P.sb(pr, "Xpad", [128, 8, 8, 16], BF16)
        Yb = P.sb(pr, "Yb", [128, 8, 16, 16], BF16)
        P.copy("dve", Yb.all(), Y[:, :, 0:16, :])
        for g in range(8):
            P.tt("dve", Xpad[:, g], X[:, g, :].pat(0, [(0, 8), (1, 16)]), eye8[:, g, :].pat(0, [(1, 8), (0, 16)]), ALU.mult)
        for g in range(8):
            P.mm(psb[0][:, 0:256], Xpad[:, g].rr("p a c -> p (a c)"), Yb[:, g].rr("p t c -> p (t c)"), g == 0, g == 7)
        Rsb = P.sb(pr, "Rsb", [128, 16, 16], F32)
        P.copy("act", Rsb.all().rr("p t c -> p (t c)"), psb[0][:, 0:256])
        if getattr(c, "dump", None) and o == 0 and DUMP2:
            c.dump("Rsb", Rsb.all().rr("p a b -> p (a b)"), F32)
            c.dump("Xpad", Xpad.all().rr("p a b c -> p (a b c)"), BF16)
        for tau in range(16):
            P.tt("dve", BD[:, tau, :].rr("p (a c) -> p a c", a=8), Rsb[:, tau, :].pat(0, [(0, 8), (1, 16)]),
                 bm.all().pat(0, [(1, 8), (0, 16)]), ALU.mult)
        pr.close()
        pr = ExitStack()
        l16 = P.sb(pr, "l16", [128, 8], F32)
        P.act(rho.all(), lr.all(), AF.Exp, scale=float(LCH))
        P.ts("dve", l16.all(), th.all(), float(LCH), ALU.mult)
        zero8 = P.sb(pr, "zero8", [128, 8], F32)
        P.memset("dve", zero8.all(), 0.0)
        assert NCH <= 256
        for h4 in range(2):
            pq = ExitStack()
            Er, Ei = cpow(P, pq, f"rot{h4}", zero8[:, h4 * 4:h4 * 4 + 4], l16[:, h4 * 4:h4 * 4 + 4], jv[:, 0:NCH], 4, NCH)
            P.copy("act", tc_[:, h4 * 4:h4 * 4 + 4, :], Er.all())
            P.copy("act", tsn[:, h4 * 4:h4 * 4 + 4, :], Ei.all())
            pq.close()
        pr.close()
        pr = ExitStack()
        pcm = P.sb(pr, "pcm", [128, 5, 64], F32)
        for i, nm in enumerate(("ar_cm", "ai_cm", "ldt_cm", "br_cm", "bi_cm")):
            P.dma("sp", pcm[:, i, :], sp[nm][:, o, :])
        dtc = P.sb(pr, "dtc", [128, 64], F32)
        lrc = P.sb(pr, "lrc", [128, 64], F32)
        thc = P.sb(pr, "thc", [128, 64], F32)
        P.act(dtc.all(), pcm[:, 2, :], AF.Exp)
        P.tt("dve", lrc.all(), pcm[:, 0, :], dtc.all(), ALU.mult)
        P.tt("dve", thc.all(), pcm[:, 1, :], dtc.all(), ALU.mult)
        krc, kic = kappa(P, pr, "kcm", pcm[:, 0, :], pcm[:, 1, :], lrc.all(), thc.all(), 64)
        Bbr = P.sb(pr, "Bbr", [128, 64], F32)
        Bbi = P.sb(pr, "Bbi", [128, 64], F32)
        tb = P.sb(pr, "tb", [128, 64], F32)
        P.tt("dve", Bbr.all(), krc.all(), pcm[:, 3, :], ALU.mult)
        P.tt("dve", tb.all(), kic.all(), pcm[:, 4, :], ALU.mult)
        P.tt("dve", Bbr.all(), Bbr.all(), tb.all(), ALU.subtract)
        P.tt("dve", Bbi.all(), krc.all(), pcm[:, 4, :], ALU.mult)
        P.tt("dve", tb.all(), kic.all(), pcm[:, 3, :], ALU.mult)
        P.tt("dve", Bbi.all(), Bbi.all(), tb.all(), ALU.add)
        Pr_, Pi_ = cpow(P, pr, "apc", lrc.all(), thc.all(), jrev.all(), 64, 16, order="jg")
        Z = P.sb(pr, "Z", [128, 16, 2, 64], F32)
        tz = P.sb(pr, "tz", [128, 16, 64], F32)
        bb = lambda t: t.all().pat(0, [(0, 16), (1, 64)])
        P.tt("dve", Z[:, :, 0, :], Pr_.all(), bb(Bbr), ALU.mult)
        P.tt("dve", tz.all(), Pi_.all(), bb(Bbi), ALU.mult)
        P.tt("dve", Z[:, :, 0, :], Z[:, :, 0, :], tz.all(), ALU.subtract)
        P.tt("dve", Z[:, :, 1, :], Pr_.all(), bb(Bbi), ALU.mult)
        P.tt("dve", tz.all(), Pi_.all(), bb(Bbr), ALU.mult)
        P.tt("dve", Z[:, :, 1, :], Z[:, :, 1, :], tz.all(), ALU.add)
        if getattr(c, "dump", None) and o == 0 and DUMP2:
            c.dump("Z", Z.all().rr("p s r m -> p (s r m)"), F32)
            c.dump("Bbr", Bbr.all(), F32)
            c.dump("Prc", Pr_.all().rr("p a b -> p (a b)"), F32)
            c.dump("krc", krc.all(), F32)
            c.dump("pcm", pcm.all().rr("p a b -> p (a b)"), F32)
            c.dump("lrc", lrc.all(), F32)
        for g in range(8):
            P.ts("dve", Wb[:, g].rr("p s m -> p (s m)"), Z.all().rr("p s r m -> p (s r m)"), bm[:, g:g + 1], ALU.mult)
        pr.close()
        prA.close()
        if getattr(c, "dump", None) and o == 0:
            c.dump("BD", BD.all().rr("p a b -> p (a b)"), BF16)
            c.dump("Wb", Wb.all().rr("p a b m -> p (a b m)"), BF16)
            c.dump("Wc", Wc.all().rr("p a b m -> p (a b m)"), BF16)
            c.dump("tabc", tc_.all().rr("p a b -> p (a b)"), BF16)
            c.dump("tabs", tsn.all().rr("p a b -> p (a b)"), BF16)
            c.dump("rho", rho.all(), F32)

        wk = ExitStack()
        SA = P.sb(wk, "SA", [128, 8, NCH], F32)
        VA = P.sb(wk, "VA", [128, 8, NCH], F32)
        VB = P.sb(wk, "VB", [128, 8, NCH], F32)
        t1 = P.sb(wk, "l2t1", [128, 8, NCH], F32)
        t2 = P.sb(wk, "l2t2", [128, 8, NCH], F32)
        H = P.sb(wk, "H", [128, 8, NCH], BF16)
        SAh = P.sb(wk, "SAh", [128, 8, NCH], BF16)
        SAl = P.sb(wk, "SAl", [128, 8, NCH], BF16)
        uo = uT[:, o, :].rr("p (k s) -> p s k", s=LCH)
        for g in range(8):
            bank = psb[1 + (g % 2)]
            for q0 in range(0, NCH, 512):
                qn = min(512, NCH - q0)
                for s_ in range(LCH):
                    P.mm(bank[:, 0:qn], Wb[:, g, s_, :], uo[:, s_, q0:q0 + qn], s_ == 0, s_ == LCH - 1)
                P.copy("act", SA[:, g, q0:q0 + qn], bank[:, 0:qn])
        fl = "p g k -> p (g k)"
        cb, sb_ = tc_.all().rr(fl), tsn.all().rr(fl)
        for g in range(8):
            for q0 in range(0, NCH, 512):
                qn = min(512, NCH - q0)
                bank = psb[3 + (g % 2)]
                P.copy("dve", SAh[:, g, q0:q0 + qn], SA[:, g, q0:q0 + qn])
                P.tt("dve", SAl[:, g, q0:q0 + qn], SA[:, g, q0:q0 + qn], SAh[:, g, q0:q0 + qn], ALU.subtract)
                P.mm(bank[:, 0:qn], pswapb.all(), SAh[:, g, q0:q0 + qn], True, False)
                P.mm(bank[:, 0:qn], pswapb.all(), SAl[:, g, q0:q0 + qn], False, True)
                sl = slice(q0, q0 + qn)
                A_, B_ = SA[:, g, sl], bank[:, 0:qn]
                cg, sgn_ = tc_[:, g, sl], tsn[:, g, sl]
                P.tt("dve", t1[:, g, sl], A_, cg, ALU.mult)
                P.tt("dve", t2[:, g, sl], B_, sgn_, ALU.mult)
                P.stt("dve", VA[:, g, sl], t2[:, g, sl], sg[:, 0:1], t1[:, g, sl], ALU.mult, ALU.add)
                P.tt("dve", t1[:, g, sl], B_, cg, ALU.mult)
                P.tt("dve", t2[:, g, sl], A_, sgn_, ALU.mult)
                P.stt("dve", VB[:, g, sl], t2[:, g, sl], nsg[:, 0:1], t1[:, g, sl], ALU.mult, ALU.add)
        for g in range(8):
            rb = rho[:, g:g + 1].pat(0, [(0, NCH)])
            P.op("dve", "tensor_tensor_scan", out=VA[:, g, :], data0=rb, data1=VA[:, g, :], initial=0.0, op0=ALU.mult, op1=ALU.add)
            P.op("dve", "tensor_tensor_scan", out=VB[:, g, :], data0=rb, data1=VB[:, g, :], initial=0.0, op0=ALU.mult, op1=ALU.add)
        P.tt("dve", t1.all().rr(fl), VA.all().rr(fl), cb, ALU.mult)
        P.tt("dve", t2.all().rr(fl), VB.all().rr(fl), sb_, ALU.mult)
        P.stt("dve", H.all().rr(fl), t2.all().rr(fl), nsg[:, 0:1], t1.all().rr(fl), ALU.mult, ALU.add)
        if getattr(c, "dump", None) and o == 0:
            c.dump("SA", SA.all().rr("p a b -> p (a b)"), F32)
            c.dump("H", H.all().rr("p a b -> p (a b)"), BF16)
        yo = uo
        for t in range(LCH - 1, -1, -1):
            bank = psb[5 + (t % 3)]
            for q0 in range(0, NCH, 512):
                qn = min(512, NCH - q0)
                for s_ in range(t + 1):
                    P.mm(bank[:, 0:qn], BD[:, t - s_, :], uo[:, s_, q0:q0 + qn], s_ == 0, False)
                for g in range(8):
                    lo = 1 if q0 == 0 else 0
                    P.mm(bank[:, lo:qn], Wc[:, g, t, :], H[:, g, q0 + lo - 1:q0 + qn - 1], False, g == 7)
                P.stt("dve", yo[:, t, q0:q0 + qn], uo[:, t, q0:q0 + qn], dsk[:, o:o + 1], bank[:, 0:qn], ALU.mult, ALU.add)
        wk.close()
        oc.close()
    if getattr(c, "dump", None):
        c.dump("ypre", y2.all().rr("p a b -> p (a b)"), BF16)
    gl = ExitStack()
    g1 = P.sb(gl, "g1", [128, S], F32)
    g2 = P.sb(gl, "g2", [128, S], F32)
    for o in range(4):
        P.act(y2[:, o, :], y2[:, o, :], AF.Gelu_apprx_tanh)
    gate = P.sb(gl, "gate", [128, 4, 512], BF16)
    for q0 in range(0, S, 512):
        for n in range(4):
            bank = psb[n]
            for k in range(4):
                P.mm(bank[:, 0:512], wglu[:, k, n * 128:(n + 1) * 128], y2[:, k, q0:q0 + 512], k == 0, k == 3)
            P.act(gate[:, n, :], bank[:, 0:512], AF.Sigmoid)
        P.tt("dve", ysT[:, :, q0:q0 + 512], gate.all(), y2[:, :, q0:q0 + 512], ALU.mult)
    gl.close()
    ph.close()


def gelu_inplace(P, x, t1, t2, eng="dve"):
    P.tt(eng, t1, x, x, ALU.mult)
    P.ts(eng, t1, t1, 0.044715 * 1.5957691216, ALU.mult, 1.5957691216, ALU.add)
    P.tt(eng, t1, t1, x, ALU.mult)
    P.act(t2, t1, AF.Sigmoid)
    P.tt(eng, x, x, t2, ALU.mult)


def attn_phase(P, c, S, x_d, w_in_d, g1c, ng1c, kTd, kiT4, vaug, ya_d, stop_at=None, dump=None):
    NT = S // 128
    TOPK = min(256, S // 4)
    psb, ident = c.psb, c.ident
    ph = ExitStack()
    rope_tables(P, ph, S, c)
    w_q = P.sb(ph, "w_q", [128, 8, 8, 128], BF16)
    w_qi = P.sb(ph, "w_qi", [128, 8, 6, 128], BF16)
    P.memset("pool", w_qi.all(), 0.0)
    w_wi = P.sb(ph, "w_wi", [128, 8, 8], BF16)
    wst = ExitStack()
    stg = [P.sb(wst, f"astg{i}", [128, 512], F32) for i in range(2)]

    def cvt(dst, src, k, neg=False):
        P.act(dst, src, AF.Copy, scale=(ng1c if neg else g1c)[:, k:k + 1])

    def q_cvt(k, st):
        for m in range(4):
            cvt(w_q[:, k, m, :], st[:, m * 128:(m + 1) * 128], k)
            for e in range(2):
                base = m * 128 + e * 64
                cvt(w_q[:, k, 4 + m, e * 64:e * 64 + 32], st[:, base + 32:base + 64], k, neg=True)
                cvt(w_q[:, k, 4 + m, e * 64 + 32:e * 64 + 64], st[:, base:base + 32], k)
    load_w_cols(P, c, q_cvt, OFF_Q, 512, w_in_d, g1c, stg)

    def qi_cvt(k, st):
        for h in range(8):
            tl, pos = h // 3, h % 3
            cvt(w_qi[:, k, tl, pos * 32:(pos + 1) * 32], st[:, h * 32:(h + 1) * 32], k)
            cvt(w_qi[:, k, 3 + tl, pos * 32:pos * 32 + 16], st[:, h * 32 + 16:h * 32 + 32], k, neg=True)
            cvt(w_qi[:, k, 3 + tl, pos * 32 + 16:pos * 32 + 32], st[:, h * 32:h * 32 + 16], k)
    load_w_cols(P, c, qi_cvt, OFF_QI, 256, w_in_d, g1c, stg)
    load_w_cols(P, c, lambda k, st: cvt(w_wi[:, k, :], st[:, 0:8], k), OFF_WI, 8, w_in_d, g1c, stg)
    wst.close()

    xt = [P.sb(ph, "axt0", [128, D], F32)] * 2
    xh = P.sb(ph, "axh", [128, D], BF16)
    xhT = P.sb(ph, "axhT", [128, 8, 128], BF16)
    junk = P.sb(ph, "ajunk", [128, D], BF16)
    ss = P.sb(ph, "ass", [128, 1], F32)
    rstd = P.sb(ph, "arstd", [128, 1], F32)
    qT = P.sb(ph, "qT", [128, 4, 128], BF16)
    qiT = P.sb(ph, "qiT", [128, 3, 128], BF16)
    t1 = P.sb(ph, "at1", [128, 4, 128], F32)
    t2 = P.sb(ph, "at2", [128, 4, 128], F32)
    wsg = P.sb(ph, "wsg", [128, 8], F32)
    wsc = P.sb(ph, "wsc", [128, 8], F32)
    acc = P.sb(ph, "acc", [128, S], F32)
    msk = P.sb(ph, "msk", [128, S], BF16)
    mT = P.sb(ph, "mT", [128, NT, 128], BF16)
    rr = [P.sb(ph, f"rr{i}", [128, 512], F32) for i in range(2)]
    lo = P.sb(ph, "lo", [128, 1], F32)
    mid = P.sb(ph, "mid", [128, 1], F32)
    cnt = P.sb(ph, "cnt", [128, 1], F32)
    cntb = P.sb(ph, "cntb", [128, 1], F32)
    ge = P.sb(ph, "ge", [128, 1], F32)
    eT = [[P.sb(ph, f"eT{i}{n}", [128, 4, 128], BF16) for n in range(2)] for i in range(2)]
    pT = [[P.sb(ph, f"pT{i}{n}", [128, 4, 128], BF16) for n in range(2)] for i in range(2)]
    rden = P.sb(ph, "rden", [128, 512], F32)
    rdh = P.sb(ph, "rdh", [128, 512], BF16)
    rdl = P.sb(ph, "rdl", [128, 512], BF16)
    ones_bf = P.sb(ph, "ones_bf", [128, 64], BF16)
    P.memset("dve", ones_bf.all(), 1.0)
    bcs = P.sb(ph, "bcs", [64, 512], F32)
    ya = [P.sb(ph, f"ya{i}", [64, 8, 128], BF16) for i in range(2)]
    m4 = "p (m t) -> p m t"

    qTs = [qT, P.sb(ph, "qT1", [128, 4, 128], BF16)]
    sjunk = P.sb(ph, "sjunk", [128, 2432], BF16)
    PIPE = stop_at is None

    def stage_I(b):
        tok = slice(b * 128, (b + 1) * 128)
        Sc = (b + 1) * 128
        qTb = qTs[b % 2]
        xb = xt[b % 2]
        P.dma("sp", xb.all(), x_d[tok, :])
        c.norm_and_transpose(b, xb, xh, xhT, junk, ss, rstd, psb[0])
        for m in range(8):
            bank = psb[1] if m < 4 else psb[2]
            for k in range(8):
                P.mm(bank[:, (m % 4) * 128:(m % 4 + 1) * 128], w_q[:, k, m, :], xhT[:, k, :], k == 0, k == 7)
        P.tt("dve", t1.all(), psb[1].all().rr(m4, m=4), c.rope["cosA"][:, tok].pat(0, [(0, 4), (1, 128)]), ALU.mult)
        P.tt("dve", t2.all(), psb[2].all().rr(m4, m=4), c.rope["sinA"][:, tok].pat(0, [(0, 4), (1, 128)]), ALU.mult)
        P.tt("dve", qTb.all(), t1.all(), t2.all(), ALU.add)
        for m in range(6):
            bank = psb[3] if m < 3 else psb[6]
            for k in range(8):
                P.mm(bank[:, (m % 3) * 128:(m % 3 + 1) * 128], w_qi[:, k, m, :], xhT[:, k, :], k == 0, k == 7)
        P.tt("dve", t1[:, 0:3, :], psb[3][:, 0:384].rr(m4, m=3), c.rope["cosI"][:, tok].pat(0, [(0, 3), (1, 128)]), ALU.mult)
        P.tt("dve", t2[:, 0:3, :], psb[6][:, 0:384].rr(m4, m=3), c.rope["sinI"][:, tok].pat(0, [(0, 3), (1, 128)]), ALU.mult)
        P.tt("dve", qiT.all(), t1[:, 0:3, :], t2[:, 0:3, :], ALU.add)
        for k in range(8):
            P.mm(psb[7][:, 0:8], xhT[:, k, :], w_wi[:, k, :], k == 0, k == 7)
        P.ts("dve", wsg.all(), psb[7][:, 0:8], 0.0, ALU.is_gt, 2.0, ALU.mult)
        P.ts("dve", wsg.all(), wsg.all(), -1.0, ALU.add)
        P.tt("dve", wsc.all(), psb[7][:, 0:8], wsg.all(), ALU.mult)
        P.ts("dve", wsc.all(), wsc.all(), 1.0 / 16.0, ALU.mult)
        if stop_at == "proj":
            return
        ibanks = [psb[1], psb[2], psb[3], psb[6]]
        ci = 0
        for q0 in range(0, Sc, 512):
            qn = min(512, Sc - q0)
            for h in range(8):
                bank, r = ibanks[h % 3], rr[ci % 2]
                ci += 1
                pb = 32 * (h % 3)
                P.mm(bank[:, 0:qn], qiT[pb:pb + 32, h // 3, :], kiT4[pb:pb + 32, q0:q0 + qn], True, True)
                P.act(r[:, 0:qn], bank[:, 0:qn], AF.Relu, scale=wsc[:, h:h + 1])
                if h == 0:
                    P.ts("dve", acc[:, q0:q0 + qn], r[:, 0:qn], wsg[:, 0:1], ALU.mult)
                else:
                    P.stt("dve", acc[:, q0:q0 + qn], r[:, 0:qn], wsg[:, h:h + 1], acc[:, q0:q0 + qn], ALU.mult, ALU.add)
        P.tt("dve", acc[:, b * 128:Sc], acc[:, b * 128:Sc], c.caus.all(), ALU.add)

    def stage_B(b):
        Sc = (b + 1) * 128
        if Sc > TOPK:
            c1 = (int(Sc * 0.42) // 128) * 128 if Sc >= 1024 else Sc
            nB = Sc - c1
            half = 8.0
            P.memset("dve", mid.all(), 0.0)
            NSTEP = 19
            for it in range(NSTEP):
                P.op("dve", "tensor_scalar", out=msk[:, 0:c1], in0=acc[:, 0:c1], scalar1=mid[:, 0:1], scalar2=0.0,
                     op0=ALU.is_ge, op1=ALU.add, accum_out=cnt.all())
                if nB:
                    P.act(sjunk[:, 0:nB], acc[:, c1:Sc], AF.Sign, bias=mid[:, 0:1], scale=-1.0, accum_out=cntb.all())
                    P.stt("dve", cnt.all(), cntb.all(), -0.5, cnt.all(), ALU.mult, ALU.add)
                P.ts("dve", ge.all(), cnt.all(), TOPK - 0.5 - nB / 2.0, ALU.is_ge, half, ALU.mult)
                nxt = half / 2 if it < NSTEP - 1 else half
                P.stt("dve", mid.all(), ge.all(), -nxt, mid.all(), ALU.add, ALU.add)
                half = half / 2
                yield
            P.ts("dve", msk[:, 0:Sc], acc[:, 0:Sc], mid[:, 0:1], ALU.is_ge)
        else:
            P.ts("dve", msk[:, 0:Sc], acc[:, 0:Sc], -1.0e29, ALU.is_ge)

    def stage_T(b):
        pbf = psb[0].all().cast(BF16)
        for j0 in range(0, b + 1, 8):
            jn = min(8, b + 1 - j0)
            for jj in range(jn):
                P.tr(pbf[:, jj * 128:(jj + 1) * 128], msk[:, (j0 + jj) * 128:(j0 + jj + 1) * 128], ident.all())
            P.copy("act", mT[:, j0:j0 + jn, :].rr("p j t -> p (j t)"), pbf[:, 0:jn * 128])

    def stage_A(b):
        tok = slice(b * 128, (b + 1) * 128)
        qTb = qTs[b % 2]
        obank = [psb[4], psb[5]]
        dbank = [psb[7], psb[0]]
        for j in range(b + 1):
            meng = "pool" if (PIPE and mstate["bisect"]) else "dve"
            ks = slice(j * 128, (j + 1) * 128)
            lb = [psb[1], psb[2]] if j % 2 == 0 else [psb[3], psb[6]]
            for e in range(2):
                for n in range(2):
                    P.mm(lb[e][:, 2 * n * 128:(2 * n + 2) * 128], kTd[64 * e:64 * e + 64, n, ks],
                         qTb[64 * e:64 * e + 64, 2 * n:2 * n + 2, :].rr("p m t -> p (m t)"), True, True)
            for e in range(2):
                et, pt = eT[j % 2][e], pT[j % 2][e]
                P.act(et.all().rr("p h t -> p (h t)"), lb[e].all(), AF.Exp, scale=0.125)
                P.tt(meng, pt.all(), et.all(), mT[:, j, :].pat(0, [(0, 4), (1, 128)]), ALU.mult)
            if stop_at != "lg":
                for e in range(2):
                    pt = pT[j % 2][e]
                    for n in range(2):
                        rhs = pt[:, 2 * n:2 * n + 2, :].rr("p h t -> p (h t)")
                        cs = slice(2 * e * 128, (2 * e + 2) * 128)
                        st_, sp_ = (j == 0 and e == 0), (j == b and e == 1)
                        P.mm(obank[n][0:64, cs], vaug[:, j, n, 0:64], rhs, st_, sp_, skip_group_check=True)
                        P.mm(dbank[n][0:64, cs], ones_bf.all(), rhs, st_, sp_, skip_group_check=True)
            yield
        if stop_at in ("pv", "lg"):
            return
        yab = ya[b % 2]
        for n in range(2):
            P.op("dve", "reciprocal", out=bcs.all(), in_=dbank[n][0:64, :])
            for e in range(2):
                cs = slice(2 * e * 128, (2 * e + 2) * 128)
                P.tt("dve", yab[:, 4 * n + e:4 * n + e + 3:2, :], obank[n][0:64, cs].rr("p (i t) -> p i t", i=2),
                     bcs[:, cs].rr("p (i t) -> p i t", i=2), ALU.mult)
        P.dma("sp", ya_d[:, :, tok], yab.all())

    mstate = {"bisect": False}

    def run(gen):
        if gen is not None:
            for _ in gen:
                pass

    def step(gen):
        if gen is None:
            return None
        try:
            next(gen)
            return gen
        except StopIteration:
            return None

    if not PIPE:
        for b in range(NT):
            stage_I(b)
            if stop_at in ("proj", "idx"):
                continue
            run(stage_B(b))
            if stop_at == "thr":
                continue
            stage_T(b)
            if stop_at == "mt":
                continue
            run(stage_A(b))
    else:
        stage_I(0)
        run(stage_B(0))
        stage_T(0)
        for b in range(NT):
            gB = None
            if b + 1 < NT:
                stage_I(b + 1)
                gB = stage_B(b + 1)
            gA = stage_A(b)
            while gA is not None or gB is not None:
                mstate["bisect"] = gB is not None
                gA = step(gA)
                gB = step(gB)
                gB = step(gB)
            if b + 1 < NT:
                stage_T(b + 1)
    if dump is not None:
        dump("qT", qT.all().rr("p a b -> p (a b)"), BF16)
        dump("qiT", qiT.all().rr("p a b -> p (a b)"), BF16)
        dump("wsc", wsc.all(), F32)
        dump("wsg", wsg.all(), F32)
        if stop_at != "proj":
            dump("acc", acc.all(), F32)
        if stop_at not in ("proj", "idx"):
            dump("msk", msk.all(), BF16)
            dump("lo", mid.all(), F32)
        if stop_at == "lg":
            dump("pT", pT[(NT - 1) % 2][1].all().rr("p a b -> p (a b)"), BF16)
        if stop_at not in ("proj", "idx", "thr"):
            dump("mT", mT.all().rr("p a b -> p (a b)"), BF16)
        if stop_at == "pv":
            for n in range(2):
                P.copy("act", rden.all(), psb[4 + n].all())
                dump(f"oT{n}", rden.all(), F32)
    ph.close()


def merge_phase(P, c, S, x_d, w_in_d, g1c, ysT, ya_d, h_d, wd, uvb_d=None):
    NT = S // 128
    psb = c.psb
    ph = ExitStack()
    w_gs = P.sb(ph, "w_gs", [128, 8, 1024], BF16)
    w_ga = P.sb(ph, "w_ga", [128, 8, 1024], BF16)
    w_sup = P.sb(ph, "w_sup", [128, 4, 1024], BF16)
    w_aup = P.sb(ph, "w_aup", [64, 8, 1024], BF16)
    w_out = P.sb(ph, "w_out", [128, 8, 1024], BF16)
    stg = [P.sb(ph, f"bstg{i}", [128, 1024], F32) for i in range(2)]
    qs = ("sp", "act")

    def cvt(dst, src, k):
        P.act(dst, src, AF.Copy, scale=g1c[:, k:k + 1])
    load_w_cols(P, c, lambda k, st: cvt(w_gs[:, k, :], st[:, 0:1024], k), OFF_GS, 1024, w_in_d, g1c, stg)
    load_w_cols(P, c, lambda k, st: cvt(w_ga[:, k, :], st[:, 0:1024], k), OFF_GA, 1024, w_in_d, g1c, stg)
    for k in range(4):
        P.dma(qs[k % 2], stg[k % 2].all(), wd["w_ssm_up"][k * 128:(k + 1) * 128, :])
        P.copy("act", w_sup[:, k, :], stg[k % 2].all())
    for h in range(8):
        P.dma(qs[h % 2], stg[h % 2][0:64, :], wd["w_attn_up"][h * 64:(h + 1) * 64, :])
        P.copy("act", w_aup[:, h, :], stg[h % 2][0:64, :])
    for k in range(8):
        P.dma(qs[k % 2], stg[k % 2].all(), wd["w_out"][k * 128:(k + 1) * 128, :])
        P.copy("act", w_out[:, k, :], stg[k % 2].all())

    xt = [P.sb(ph, f"bxt{i}", [128, D], F32) for i in range(2)]
    xh = P.sb(ph, "bxh", [128, D], BF16)
    xhT = P.sb(ph, "bxhT", [128, 8, 128], BF16)
    junk = P.sb(ph, "bjunk", [128, D], BF16)
    ss = P.sb(ph, "bss", [128, 1], F32)
    rstd = P.sb(ph, "brstd", [128, 1], F32)
    yat = [P.sb(ph, f"yat{i}", [64, 8, 128], BF16) for i in range(2)]
    sgs = P.sb(ph, "sgs", [128, 512], F32)
    sga = P.sb(ph, "sga", [128, 512], F32)
    m1 = P.sb(ph, "m1", [128, 512], F32)
    m2 = P.sb(ph, "m2", [128, 512], F32)
    merged = P.sb(ph, "merged", [128, 8, 128], BF16)
    ht = [P.sb(ph, f"bht{i}", [128, D], F32) for i in range(2)]

    NCONV = 16384 // 128
    cvf = [P.sb(ph, f"cvf{i}", [128, 2048], F32) for i in range(2)]
    cvb = [P.sb(ph, f"cvb{i}", [128, 2048], BF16) for i in range(2)]

    def conv_step(i):
        rows = slice(i * 128, (i + 1) * 128)
        P.dma("pool", cvf[i % 2].all(), wd["peer_uv"][rows, :])
        P.copy("pool", cvb[i % 2].all(), cvf[i % 2].all())
        P.dma("pool", uvb_d[rows, :], cvb[i % 2].all())
    conv_per_tile = (NCONV + NT - 1) // NT
    conv_i = 0

    for b in range(NT):
        for _ in range(conv_per_tile):
            if uvb_d is not None and conv_i < NCONV:
                conv_step(conv_i)
                conv_i += 1
        tok = slice(b * 128, (b + 1) * 128)
        xb = xt[b % 2]
        P.dma("sp", xb.all(), x_d[tok, :])
        ya = yat[b % 2]
        P.dma("sp", ya.all(), ya_d[:, :, tok])
        c.norm_and_transpose(b, xb, xh, xhT, junk, ss, rstd, psb[0])
        for half in range(2):
            for cc in range(4):
                ci = half * 4 + cc
                cols = slice(ci * 128, (ci + 1) * 128)
                reg = slice(cc * 128, (cc + 1) * 128)
                for k in range(8):
                    P.mm(psb[1][:, reg], w_gs[:, k, cols], xhT[:, k, :], k == 0, k == 7)
                for k in range(8):
                    P.mm(psb[2][:, reg], w_ga[:, k, cols], xhT[:, k, :], k == 0, k == 7)
                for k in range(4):
                    P.mm(psb[3][:, reg], w_sup[:, k, cols], ysT[:, k, tok], k == 0, k == 3)
                for h in range(8):
                    P.mm(psb[4][:, reg], w_aup[:, h, cols], ya[:, h, :], h == 0, h == 7)
            P.act(sgs.all(), psb[1].all(), AF.Sigmoid)
            P.act(sga.all(), psb[2].all(), AF.Sigmoid)
            P.tt("dve", m1.all(), sgs.all(), psb[3].all(), ALU.mult)
            P.tt("dve", m2.all(), sga.all(), psb[4].all(), ALU.mult)
            P.tt("dve", merged[:, half * 4:half * 4 + 4, :].rr("p c t -> p (c t)"), m1.all(), m2.all(), ALU.add)
        hb = ht[b % 2]
        for n2 in range(2):
            cs = slice(n2 * 512, (n2 + 1) * 512)
            for k in range(8):
                P.mm(psb[5 + n2].all(), merged[:, k, :], w_out[:, k, cs], k == 0, k == 7)
            P.tt("dve", hb[:, cs], psb[5 + n2].all(), xb[:, cs], ALU.add)
        P.dma("sp", h_d[tok, :], hb.all())
    ph.close()


def top16(P, src, vals_out, idx_out, tl, par):
    m8a, i8a, m8b, i8b = tl["m8a"][par], tl["i8a"][par], tl["m8b"][par], tl["i8b"][par]
    n = src.shape[-1]
    sc2 = tl["sc2"][par][:, 0:n]
    P.op("dve", "max", out=m8a.all(), in_=src)
    P.op("dve", "max_index", out=i8a.all(), in_max=m8a.all(), in_values=src)
    P.op("dve", "match_replace", out=sc2, in_to_replace=m8a.all(), in_values=src, imm_value=-1.0e30)
    P.op("dve", "max", out=m8b.all(), in_=sc2)
    P.op("dve", "max_index", out=i8b.all(), in_max=m8b.all(), in_values=sc2)
    P.copy("act", vals_out[:, 0:8], m8a.all())
    P.copy("act", vals_out[:, 8:16], m8b.all())
    P.copy("dve", idx_out[:, 0:8], i8a.all().cast(I32))
    P.copy("dve", idx_out[:, 8:16], i8b.all().cast(I32))


def peer_phase(P, c, S, h_d, out_d, pd, uvb_d, NB=16, dump=None):
    NT = S // 128
    psb, ident = c.psb, c.ident
    ph = ExitStack()
    wq = P.sb(ph, "wq", [128, 8, 2048], BF16)
    pk = P.sb(ph, "pk", [128, 16, 128], BF16)
    g2c = P.sb(ph, "g2c", [128, 8], F32)
    g2b = P.sb(ph, "g2b", [128, D], F32)
    gfb = P.sb(ph, "gfb", [128, D], F32)
    P.dma("sp", g2c.all(), pd["g2c"].all())
    P.dma("sp", g2b.all(), pd["g2b"].all())
    P.dma("sp", gfb.all(), pd["gfb"].all())
    hts = [P.sb(ph, f"cht{i}", [128, D], F32) for i in range(2)]
    hh = P.sb(ph, "chh", [128, D], BF16)
    hT = P.sb(ph, "chT", [128, 8, 128], BF16)
    junk = P.sb(ph, "cjunk", [128, D], BF16)
    hn = P.sb(ph, "chn", [128, D], F32)
    ss = P.sb(ph, "css", [128, 1], F32)
    rstd = P.sb(ph, "crstd", [128, 1], F32)
    qT = P.sb(ph, "cqT", [128, 16, 128], BF16)
    SC = P.sb(ph, "SC", [128, 16, 128], F32)
    V16 = P.sb(ph, "V16", [128, 16, 16], F32)
    I16 = P.sb(ph, "I16", [128, 16, 16], F32)
    CS = P.sb(ph, "CS", [128, 8, 256], F32)
    TS = P.sb(ph, "TS", [128, 8, 16], F32)
    POSf = P.sb(ph, "POSf", [128, 8, 16], F32)
    POSi = P.sb(ph, "POSi", [128, 8, 16], I32)
    rowi = P.sb(ph, "rowi", [128, 8, 16], I32)
    coli = P.sb(ph, "coli", [128, 8, 16], I32)
    rowf = P.sb(ph, "rowf", [128, 8, 16], F32)
    colf = P.sb(ph, "colf", [128, 8, 16], F32)
    E = P.sb(ph, "E", [128, 8, 16], F32)
    G = P.sb(ph, "G", [128, 8, 16], F32)
    sm = P.sb(ph, "sm", [128, 8], F32)
    OH = P.sb(ph, "OH", [128, 8, 16, 16], F32)
    OH2 = P.sb(ph, "OH2", [128, 8, 16, 16], F32)
    i1s = P.sb(ph, "i1s", [128, 8, 16], F32)
    i2s = P.sb(ph, "i2s", [128, 8, 16], F32)
    eidf = P.sb(ph, "eidf", [128, 128], F32)
    EID = P.sb(ph, "EID", [128, 128], I32)
    dots = P.sb(ph, "dots", [128, 128], F32)
    actw = P.sb(ph, "actw", [128, 128], F32)
    resd = P.sb(ph, "cres", [128, D], F32)
    outt = [P.sb(ph, f"cout{i}", [128, D], F32) for i in range(2)]
    tl = {k: [P.sb(ph, f"{k}{i}", [128, 8], dt) for i in range(2)]
          for k, dt in (("m8a", F32), ("i8a", U32), ("m8b", F32), ("i8b", U32))}
    tl["sc2"] = [P.sb(ph, f"sc2{i}", [128, 256], F32) for i in range(2)]
    wst = ExitStack()
    stg = [P.sb(wst, f"cstg{i}", [128, 2048], F32) for i in range(2)]
    qs = ("sp", "act")
    for k in range(8):
        P.dma(qs[k % 2], stg[k % 2].all(), pd["peer_wq"][k * 128:(k + 1) * 128, :])
        P.act(wq[:, k, :], stg[k % 2].all(), AF.Copy, scale=g2c[:, k:k + 1])
    P.dma("sp", stg[0].all(), pd["pk"][0:128].rr("p a n -> p (a n)"))
    P.copy("act", pk.all().rr("p a n -> p (a n)"), stg[0].all())
    wst.close()
    UV = [P.sb(ph, f"UV{i}", [128, 2048], BF16) for i in range(NB)]
    dg = [P.sb(ph, f"dg{i}", [128, 128], BF16) for i in range(4)]
    hnb = P.sb(ph, "hnb", [128, D], BF16)
    junkb = P.sb(ph, "cjunkb", [128, D], F32)
    tuv = uvb_d.all()

    EIDs = [EID, P.sb(ph, "EID1", [128, 128], I32)]
    Gs = [G, P.sb(ph, "G1", [128, 8, 16], F32)]
    hnbs = [hnb, P.sb(ph, "hnb1", [128, D], BF16)]
    ssf = P.sb(ph, "cssf", [128, 1], F32)
    rstdf = P.sb(ph, "crstdf", [128, 1], F32)
    pacc = [psb[6], psb[7]]

    def front(b):
        tok = slice(b * 128, (b + 1) * 128)
        ht, EIDb, Gb, hnbb = hts[b % 2], EIDs[b % 2], Gs[b % 2], hnbs[b % 2]
        P.dma("sp", ht.all(), h_d[tok, :])
        P.act(junk.all(), ht.all(), AF.Square, accum_out=ss.all())
        P.ts("dve", ss.all(), ss.all(), 1.0 / D, ALU.mult, EPS, ALU.add)
        P.act(ss.all(), ss.all(), AF.Sqrt)
        P.op("dve", "reciprocal", out=rstd.all(), in_=ss.all())
        P.ts("dve", hh.all(), ht.all(), rstd[:, 0:1], ALU.mult)
        yield
        P.stt("dve", hn.all(), ht.all(), rstd[:, 0:1], g2b.all(), ALU.mult, ALU.mult)
        P.copy("act", hnbb.all(), hn.all())
        pbf = psb[0].all().cast(BF16)
        for k in range(8):
            P.tr(pbf[:, k * 128:(k + 1) * 128], hh[:, k * 128:(k + 1) * 128], ident.all())
        P.copy("act", hT.all().rr("p k t -> p (k t)"), pbf)
        yield
        for ch in range(16):
            bank = psb[1 + (ch // 4) % 2]
            for k in range(8):
                P.mm(bank[:, (ch % 4) * 128:(ch % 4 + 1) * 128], wq[:, k, ch * 128:(ch + 1) * 128], hT[:, k, :], k == 0, k == 7)
            if ch % 4 == 3:
                P.copy("act", qT[:, ch - 3:ch + 1, :].rr("p a t -> p (a t)"), bank.all())
                yield
        sbanks = [psb[3], psb[4], psb[5], psb[0]]
        for ch in range(16):
            bank = sbanks[ch // 4]
            P.mm(bank[:, (ch % 4) * 128:(ch % 4 + 1) * 128], qT[:, ch, :], pk[:, ch, :], True, True)
            if ch % 4 == 3:
                P.copy("act", SC[:, ch - 3:ch + 1, :].rr("p a n -> p (a n)"), bank.all())
        yield
        for ch in range(16):
            top16(P, SC[:, ch, :], V16[:, ch, :], I16[:, ch, :], tl, ch % 2)
            if ch % 2 == 1:
                yield
        P.tt("dve", CS.all().rr("p h (i j) -> p h i j", i=16), V16.all().pat(0, [(32, 8), (1, 16), (0, 16)]),
             V16.all().pat(16, [(32, 8), (0, 16), (1, 16)]), ALU.add)
        for h in range(8):
            top16(P, CS[:, h, :], TS[:, h, :], POSf[:, h, :], tl, h % 2)
            if h % 2 == 1:
                yield
        P.tt("dve", E.all(), TS.all(), TS.all().pat(0, [(16, 8), (0, 16)]), ALU.subtract)
        P.act(E.all(), E.all(), AF.Exp)
        P.op("dve", "tensor_reduce", out=sm.all(), in_=E.all(), axis=AX.X, op=ALU.add)
        P.op("dve", "reciprocal", out=sm.all(), in_=sm.all())
        P.tt("dve", Gb.all(), E.all(), sm.all().pat(0, [(1, 8), (0, 16)]), ALU.mult)
        yield
        P.copy("dve", POSi.all(), POSf.all())
        P.op("dve", "tensor_single_scalar", out=rowi.all(), in_=POSi.all(), scalar=4, op=ALU.arith_shift_right)
        P.op("dve", "tensor_single_scalar", out=coli.all(), in_=POSi.all(), scalar=15, op=ALU.bitwise_and)
        P.copy("dve", rowf.all(), rowi.all())
        P.copy("dve", colf.all(), coli.all())
        io16 = c.iota_f[:, 0:16].pat(0, [(0, 8), (0, 16), (1, 16)])
        for src, off, dst, oh in ((rowf, 0, i1s, OH), (colf, 16, i2s, OH2)):
            P.tt("dve", oh.all(), src.all().pat(0, [(16, 8), (1, 16), (0, 16)]), io16, ALU.is_equal)
            P.tt("dve", oh.all(), oh.all(), I16.all().pat(off, [(32, 8), (0, 16), (1, 16)]), ALU.mult)
            P.op("dve", "tensor_reduce", out=dst.all().rr("p h k -> p (h k)"), in_=oh.all().rr("p h k i -> p (h k) i"),
                 axis=AX.X, op=ALU.add)
            yield
        P.stt("dve", eidf.all(), i1s.all().rr("p h k -> p (h k)"), 128.0, i2s.all().rr("p h k -> p (h k)"), ALU.mult, ALU.add)
        P.copy("dve", EIDb.all(), eidf.all())

    def advance(gen, n):
        if gen is None:
            return None
        try:
            for _ in range(n):
                next(gen)
        except StopIteration:
            return None
        return gen

    GS = NB // 2
    NGRP = 128 // GS
    advance(front(0), 1000)
    for b in range(NT):
        tok = slice(b * 128, (b + 1) * 128)
        ht, EIDb, hnbb = hts[b % 2], EIDs[b % 2], hnbs[b % 2]
        Gf = Gs[b % 2].all().rr("p a b -> p (a b)")
        nxt = front(b + 1) if b + 1 < NT else None
        for g in range(NGRP):
            gs = slice(g * GS, (g + 1) * GS)
            for e in range(g * GS, (g + 1) * GS):
                uv = UV[e % NB]
                P.gather(uv.all(), tuv, EIDb[:, e:e + 1])
                P.stt("dve", junkb.all(), uv[:, 0:D], 1.0, hnbb.all(), ALU.mult, ALU.mult, accum_out=dots[:, e:e + 1])
            P.act(actw[:, gs], dots[:, gs], AF.Gelu_apprx_tanh)
            P.tt("dve", actw[:, gs], actw[:, gs], Gf[:, gs], ALU.mult)
            for e in range(g * GS, (g + 1) * GS):
                uv, dgt = UV[e % NB], dg[e % 4]
                P.act(dgt.all(), ident.all(), AF.Copy, scale=actw[:, e:e + 1])
                for n2 in range(2):
                    P.mm(pacc[n2].all(), dgt.all(), uv[:, D + n2 * 512:D + (n2 + 1) * 512], e == 0, e == 127)
            nxt = advance(nxt, 2)
        advance(nxt, 1000)
        for n2 in range(2):
            P.tt("dve", resd[:, n2 * 512:(n2 + 1) * 512], pacc[n2].all(), ht[:, n2 * 512:(n2 + 1) * 512], ALU.add)
        P.act(junk.all(), resd.all(), AF.Square, accum_out=ssf.all())
        P.ts("dve", ssf.all(), ssf.all(), 1.0 / D, ALU.mult, EPS, ALU.add)
        P.act(ssf.all(), ssf.all(), AF.Sqrt)
        P.op("dve", "reciprocal", out=rstdf.all(), in_=ssf.all())
        ot = outt[b % 2]
        P.stt("dve", ot.all(), resd.all(), rstdf[:, 0:1], gfb.all(), ALU.mult, ALU.mult)
        P.dma("sp", out_d[tok, :], ot.all())
    ph.close()


def late_param_shapes():
    return {"w_ssm_up": [513, 1024], "w_attn_up": [513, 1024], "w_out": [1025, 1024], "g2c": [128, 8],
            "g2b": [128, 1024], "gfb": [128, 1024], "peer_wq": [1025, 2048], "pk": [129, 16, 128],
            "peer_uv": [16385, 2048]}


def late_host_layout(inp):
    g2 = np.asarray(inp["norm2_g"], dtype=np.float32)[0]
    gf = np.asarray(inp["norm_f_g"], dtype=np.float32)
    k1, k2 = np.asarray(inp["peer_k1"])[0], np.asarray(inp["peer_k2"])[0]
    pk = np.stack([k1, k2], 1).reshape(16, 128, 128).transpose(2, 0, 1)
    d = {"w_ssm_up": np.asarray(inp["w_ssm_up"])[0], "w_attn_up": np.asarray(inp["w_attn_up"])[0],
         "w_out": np.asarray(inp["w_out"])[0], "g2c": g2.reshape(8, 128).T,
         "g2b": np.broadcast_to(g2[None, :], (128, 1024)), "gfb": np.broadcast_to(gf[None, :], (128, 1024)),
         "peer_wq": np.asarray(inp["peer_wq"])[0], "pk": pk,
         "peer_uv": np.concatenate([np.asarray(inp["peer_u"])[0], np.asarray(inp["peer_v"])[0]], 1)}
    return {k: np.ascontiguousarray(v, dtype=np.float32) for k, v in d.items()}


def ssm_param_shapes():
    return {"ar_sm": [128, 32], "ai_sm": [128, 32], "ldt_sm": [128, 32],
            "bU1": [128, 32, 16], "bU2": [128, 32, 16], "cT1": [128, 32, 16], "cT2": [128, 32, 16],
            "ar_cm": [128, 4, 64], "ai_cm": [128, 4, 64], "ldt_cm": [128, 4, 64],
            "br_cm": [128, 4, 64], "bi_cm": [128, 4, 64], "dskip": [128, 4], "w_glu": [513, 512]}


def ssm_host_layout(inp):
    a_re, a_im, log_dt = inp["a_re"][0], inp["a_im"][0], inp["log_dt"][0]
    b_re, b_im, c_re, c_im = inp["b_re"][0], inp["b_im"][0], inp["c_re"][0], inp["c_im"][0]
    d = {}
    d["ar_sm"] = np.concatenate([a_re.T, a_re.T], 0)
    d["ai_sm"] = np.concatenate([a_im.T, a_im.T], 0)
    d["ldt_sm"] = np.broadcast_to(log_dt[None, :], (128, 32))
    brT, biT = b_re.transpose(1, 0, 2), b_im.transpose(1, 0, 2)
    d["bU1"] = np.concatenate([brT, biT], 0)
    d["bU2"] = np.concatenate([biT, brT], 0)
    crT, ciT = c_re.transpose(2, 0, 1), c_im.transpose(2, 0, 1)
    d["cT1"] = np.concatenate([crT, ciT], 0)
    d["cT2"] = np.concatenate([ciT, crT], 0)
    q = np.arange(128)
    gq = (np.arange(4)[None, :] * 8 + (q // 16)[:, None])
    d["ar_cm"] = a_re[gq]
    d["ai_cm"] = a_im[gq]
    d["ldt_cm"] = np.broadcast_to(log_dt[gq][:, :, None], (128, 4, 64))
    d["br_cm"] = b_re[gq, :, (q % 16)[:, None]]
    d["bi_cm"] = b_im[gq, :, (q % 16)[:, None]]
    d["dskip"] = inp["d_skip"][0].reshape(4, 128).T
    d["w_glu"] = inp["w_glu"][0]
    return {k: np.ascontiguousarray(v, dtype=np.float32) for k, v in d.items()}


def build(nc, S, stage_stop=None, dbg=None):
    NT = S // 128
    NCH = S // LCH
    TOPK = min(256, S // 4)
    es = ExitStack()
    P = Prog(nc, es)
    c = Ctx()
    c.P = P
    dbg = dbg if dbg is not None else {}

    x_d = P.dram("x", [S, D], F32, "ExternalInput")
    g1c_d = P.dram("g1c", [128, 8], F32, "ExternalInput")
    w_in_d = P.dram("w_in", [D + 1, IN_W], F32, "ExternalInput")
    out_d = P.dram("out", [S, D], F32, "ExternalOutput")

    def dbg_out(name, shape, dt=F32):
        t = P.dram("dbg_" + name, shape, dt, "ExternalOutput")
        dbg[name] = t
        return t

    blk = es.enter_context(nc.Block())
    holder = {}

    def body(_sync):
        glob = ExitStack()
        ident = P.sb(glob, "ident", [128, 128], BF16)
        identf = P.sb(glob, "identf", [128, 128], F32)
        iota_i = P.sb(glob, "iota_i", [128, 128], I32)
        pid_i = P.sb(glob, "pid_i", [128, 1], I32)
        pid_f = P.sb(glob, "pid_f", [128, 1], F32)
        iota_f = P.sb(glob, "iota_f", [128, 128], F32)
        P.op("pool", "iota", out=iota_i.all(), pattern=[[1, 128]], base=0, channel_multiplier=0)
        P.op("pool", "iota", out=pid_i.all(), pattern=[[0, 1]], base=0, channel_multiplier=1)
        P.copy("dve", iota_f.all(), iota_i.all())
        P.copy("dve", pid_f.all(), pid_i.all())
        P.ts("dve", identf.all(), iota_f.all(), pid_f[:, 0:1], ALU.is_equal)
        P.copy("dve", ident.all(), identf.all())
        caus = P.sb(glob, "caus", [128, 128], F32)
        P.ts("dve", caus.all(), iota_f.all(), pid_f[:, 0:1], ALU.is_gt, NEG, ALU.mult)
        g1c = P.sb(glob, "g1c", [128, 8], F32)
        ng1c = P.sb(glob, "ng1c", [128, 8], F32)
        P.dma("sp", g1c.all(), g1c_d.all())
        P.ts("dve", ng1c.all(), g1c.all(), -1.0, ALU.mult)
        c.ident, c.identf, c.iota_f, c.pid_f, c.caus = ident, identf, iota_f, pid_f, caus

        psb = [P.ps(glob, f"psb{i}", [128, 512], F32) for i in range(8)]
        c.psb = psb

        res = ExitStack()
        uT = P.sb(res, "uys", [128, 4, S], BF16)
        res_a = ExitStack()
        res_a_close = res_a.close
        kTd = P.sb(res_a, "kTd", [128, 2, S], BF16)
        kiT4 = P.sb(res_a, "kiT4", [128, S], BF16)
        vaug = P.sb(res_a, "vaug", [128, NT, 2, 80], BF16)
        P.memset("pool", vaug.all(), 1.0)

        s1 = ExitStack()
        rope_tables(P, s1, S, c)
        stg = [P.sb(s1, f"stg{i}", [128, 1024], F32) for i in range(2)]
        w_u = P.sb(s1, "w_u", [128, 8, 512], BF16)
        w_kd = P.sb(s1, "w_kd", [128, 8, 4, 128], BF16)
        w_ki = P.sb(s1, "w_ki", [128, 8, 2, 128], BF16)
        w_v = P.sb(s1, "w_v", [128, 8, 128], BF16)

        def cvt(dst, src, k, neg=False):
            P.act(dst, src, AF.Copy, scale=(ng1c if neg else g1c)[:, k:k + 1])

        load_w_cols(P, c, lambda k, st: cvt(w_u[:, k, :], st[:, 0:512], k), OFF_U, 512, w_in_d, g1c, stg)

        def k_cvt(k, st):
            for n in range(2):
                for dup in range(2):
                    cvt(w_kd[:, k, n, dup * 64:(dup + 1) * 64], st[:, n * 64:(n + 1) * 64], k)
                    cvt(w_kd[:, k, 2 + n, dup * 64:dup * 64 + 32], st[:, n * 64 + 32:n * 64 + 64], k, neg=True)
                    cvt(w_kd[:, k, 2 + n, dup * 64 + 32:dup * 64 + 64], st[:, n * 64:n * 64 + 32], k)
        load_w_cols(P, c, k_cvt, OFF_K, 128, w_in_d, g1c, stg)

        def ki_cvt(k, st):
            for r in range(4):
                cvt(w_ki[:, k, 0, r * 32:(r + 1) * 32], st[:, 0:32], k)
                cvt(w_ki[:, k, 1, r * 32:r * 32 + 16], st[:, 16:32], k, neg=True)
                cvt(w_ki[:, k, 1, r * 32 + 16:r * 32 + 32], st[:, 0:16], k)
        load_w_cols(P, c, ki_cvt, OFF_KI, 32, w_in_d, g1c, stg)
        load_w_cols(P, c, lambda k, st: cvt(w_v[:, k, :], st[:, 0:128], k), OFF_V, 128, w_in_d, g1c, stg)

        xt = [P.sb(s1, f"xt{i}", [128, D], F32) for i in range(2)]
        xh = P.sb(s1, "xh", [128, D], BF16)
        xhT = P.sb(s1, "xhT", [128, 8, 128], BF16)
        junk = P.sb(s1, "junk", [128, D], BF16)
        ss = P.sb(s1, "ss", [128, 1], F32)
        rstd = P.sb(s1, "rstd", [128, 1], F32)
        r1 = P.sb(s1, "r1", [128, 128], F32)
        r2 = P.sb(s1, "r2", [128, 128], F32)

        def norm_and_transpose(b, xt_b, xh, xhT, junk, ss, rstd, psT):
            P.act(junk.all(), xt_b.all(), AF.Square, accum_out=ss.all())
            P.act(ss.all(), ss.all(), AF.Sqrt, scale=1.0 / D, bias=EPS) if False else None
            P.ts("dve", ss.all(), ss.all(), 1.0 / D, ALU.mult, EPS, ALU.add)
            P.act(ss.all(), ss.all(), AF.Sqrt)
            P.op("dve", "reciprocal", out=rstd.all(), in_=ss.all())
            P.ts("dve", xh.all(), xt_b.all(), rstd[:, 0:1], ALU.mult)
            pb = psT.all().cast(BF16)
            for k in range(8):
                P.tr(pb[:, k * 128:(k + 1) * 128], xh[:, k * 128:(k + 1) * 128], ident.all())
            P.copy("act", xhT.all().rr("p k t -> p (k t)"), pb)
        c.norm_and_transpose = norm_and_transpose

        for b in range(NT):
            xb = xt[b % 2]
            P.dma("sp", xb.all(), x_d[b * 128:(b + 1) * 128, :])
            norm_and_transpose(b, xb, xh, xhT, junk, ss, rstd, psb[0])
            tok = slice(b * 128, (b + 1) * 128)
            for m in range(4):
                for k in range(8):
                    P.mm(psb[1][:, m * 128:(m + 1) * 128], w_u[:, k, m * 128:(m + 1) * 128], xhT[:, k, :], k == 0, k == 7)
            P.copy("act", uT[:, :, tok], psb[1].all().rr("p (m t) -> p m t", m=4))
            for m in range(4):
                for k in range(8):
                    P.mm(psb[2][:, m * 128:(m + 1) * 128], w_kd[:, k, m, :], xhT[:, k, :], k == 0, k == 7)
            for n in range(2):
                P.tt("dve", r1.all(), psb[2][:, n * 128:(n + 1) * 128], c.rope["cosA"][:, tok], ALU.mult)
                P.tt("dve", r2.all(), psb[2][:, (2 + n) * 128:(3 + n) * 128], c.rope["sinA"][:, tok], ALU.mult)
                P.tt("dve", kTd[:, n, tok], r1.all(), r2.all(), ALU.add)
            for m in range(2):
                for k in range(8):
                    P.mm(psb[3][:, m * 128:(m + 1) * 128], w_ki[:, k, m, :], xhT[:, k, :], k == 0, k == 7)
            for k in range(8):
                P.mm(psb[3][:, 256:384], xhT[:, k, :], w_v[:, k, :], k == 0, k == 7)
            P.tt("dve", r1.all(), psb[3][:, 0:128], c.rope["cosI"][:, tok], ALU.mult)
            P.tt("dve", r2.all(), psb[3][:, 128:256], c.rope["sinI"][:, tok], ALU.mult)
            P.tt("dve", kiT4[:, tok], r1.all(), r2.all(), ALU.add)
            P.copy("act", vaug[:, b, :, 0:64], psb[3][:, 256:384].rr("p (n d) -> p n d", n=2))
        if stage_stop == "s1":
            for nm in ("cosA", "sinA", "cosI", "sinI"):
                t = dbg_out(nm, [128, S], BF16)
                P.dma("sp", t.all(), c.rope[nm].all())
        s1.close()

        if stage_stop == "s1":
            t = dbg_out("uT", [128, 4 * S], BF16)
            P.dma("sp", t.all(), uT.all().rr("p m t -> p (m t)"))
            t = dbg_out("kTd", [128, 2 * S], BF16)
            P.dma("sp", t.all(), kTd.all().rr("p m t -> p (m t)"))
            t = dbg_out("kiT4", [128, S], BF16)
            P.dma("sp", t.all(), kiT4.all())
            t = dbg_out("vaug", [128, NT * 160], BF16)
            P.dma("sp", t.all(), vaug.all().rr("p a n d -> p (a n d)"))
            P.finish(list(dbg.values()))
            res_a.close()
            res.close()
            glob.close()
            return

        sp = {nm: P.dram(nm, shp, F32, "ExternalInput") for nm, shp in ssm_param_shapes().items()}
        if stage_stop == "s2":
            def dump(name, view, dt):
                t = dbg_out(name, list(view.shape), dt)
                P.dma("sp", t.all(), view)
            c.dump = dump
        ssm_phase(P, c, S, uT, sp)
        if stage_stop == "s2":
            t = dbg_out("ysT", [128, 4 * S], BF16)
            P.dma("sp", t.all(), uT.all().rr("p m t -> p (m t)"))
            P.finish(list(dbg.values()))
            res_a.close()
            res.close()
            glob.close()
            return
        ya_d = P.dram("ya_scr", [64, 8, S], BF16, "ExternalOutput" if stage_stop == "a" else "Internal")
        astop = stage_stop[2:] if (stage_stop or "").startswith("a:") else None

        def adump(name, view, dt):
            t = dbg_out(name, list(view.shape), dt)
            P.dma("sp", t.all(), view)
        attn_phase(P, c, S, x_d, w_in_d, g1c, ng1c, kTd, kiT4, vaug, ya_d, stop_at=astop, dump=adump if astop else None)
        res_a.close()
        if astop:
            P.finish(list(dbg.values()))
            res.close()
            glob.close()
            return
        if stage_stop == "a":
            dbg["ya_scr"] = ya_d
            P.finish([ya_d])
            res.close()
            glob.close()
            return
        pd = {nm: P.dram(nm, shp, F32, "ExternalInput") for nm, shp in late_param_shapes().items()}
        h_d = P.dram("h_scr", [S, D], F32, "ExternalOutput" if stage_stop == "b" else "Internal")
        uvb_d = P.dram("uvb_scr", [16384, 2048], BF16, "ExternalOutput" if stage_stop == "cdbg" else "Internal")
        merge_phase(P, c, S, x_d, w_in_d, g1c, uT, ya_d, h_d, pd, uvb_d)
        res.close()
        if stage_stop == "b":
            dbg["h_scr"] = h_d
            P.finish([h_d])
            glob.close()
            return
        def cdump(name, view, dt):
            t = dbg_out(name, list(view.shape), dt)
            P.dma("sp", t.all(), view)
        peer_phase(P, c, S, h_d, out_d, pd, uvb_d, dump=cdump if stage_stop == "cdbg" else None)
        P.finish([out_d] + list(dbg.values()) + ([uvb_d] if stage_stop == "cdbg" else []))
        glob.close()

    holder["rest"] = lambda P, c, env: None
    blk.sync(body)
    es.close()
    return P, dbg


PADDED = ("w_in", "w_glu", "w_ssm_up", "w_attn_up", "w_out", "peer_wq", "pk", "peer_uv")


def core_inputs(shared, xb):
    im = dict(shared)
    im["x"] = np.ascontiguousarray(xb, dtype=np.float32)
    flat = im["x"].reshape(-1)
    for nm in PADDED:
        a = shared[nm]
        row = flat[:a[0].size].reshape((1,) + a.shape[1:])
        im[nm] = np.concatenate([a, row], 0)
    return im


def kernel(**inputs):
    inputs = {k: np.asarray(v) for k, v in inputs.items()}
    B, S, _ = inputs["x"].shape
    assert B == NCORES
    g1 = inputs["norm1_g"].astype(np.float32)[0]
    shared = {"g1c": np.ascontiguousarray(g1.reshape(8, 128).T),
              "w_in": np.ascontiguousarray(inputs["w_in"].astype(np.float32)[0])}
    shared.update(ssm_host_layout(inputs))
    shared.update(late_host_layout(inputs))
    nc = bass.Bass("TRN2", target_bir_lowering=False)
    build(nc, S)
    x = inputs["x"].astype(np.float32)
    in_maps = [core_inputs(shared, x[b]) for b in range(NCORES)]
    res = run_bass_kernel_spmd(nc, in_maps, core_ids=list(range(NCORES)))
    out = np.stack([np.asarray(res.results[b]["out"], dtype=np.float32) for b in range(NCORES)], 0)
    return out
```

```python
import math
from contextlib import ExitStack

import numpy as np
import concourse.bass as bass
import concourse.mybir as mybir
from concourse.bass_utils import run_bass_kernel_spmd

F32 = mybir.dt.float32
BF16 = mybir.dt.bfloat16
I32 = mybir.dt.int32
U32 = mybir.dt.uint32
ALU = mybir.AluOpType
AF = mybir.ActivationFunctionType
AX = mybir.AxisListType

D = 1024
NCORES = 8
SSM_W = 512
NG = 32
NP_ = 64
LCH = 16
EPS = 1e-6
NEG = -1.0e30
STRICT = True
DUMP2 = False
OUTK = ("out", "accum_out", "out_max", "out_indices")


class V:
    __slots__ = ("t", "ap")

    def __init__(self, t, ap):
        self.t = t
        self.ap = ap

    def __getitem__(self, k):
        return V(self.t, self.ap[k])

    def rr(self, pat, **kw):
        return V(self.t, self.ap.rearrange(pat, **kw))

    def bc(self, shape):
        return V(self.t, self.ap.to_broadcast(list(shape)))

    def cast(self, dt):
        return V(self.t, self.ap.bitcast(dt))

    def pat(self, off, pattern):
        a = self.ap
        return V(self.t, bass.AP(a.tensor, a.offset + off, [list(a.ap[0])] + [list(p) for p in pattern]))

    @property
    def shape(self):
        return self.ap.shape


class Tl:
    def __init__(self, base_ap, name, dram=False):
        self.base = base_ap
        self.name = name
        self.dram = dram
        self.w = None
        self.r = {}
        self.dsem = None

    def __getitem__(self, k):
        return V(self, self.base[k])

    def all(self):
        return V(self, self.base)


class Prog:
    EPOCH = 14000

    def __init__(self, nc, es):
        self.nc = nc
        self.es = es
        self.eng = {"pe": nc.tensor, "dve": nc.vector, "act": nc.scalar, "pool": nc.gpsimd, "sp": nc.sync}
        self.sems = []
        self.semeng = []
        self.cur = {}
        self.cnt = {}
        self.known = {e: {} for e in self.eng}
        self.dcnt = {}
        self.ninstr = 0
        self.freed = {}
        self.log = None
        for e in self.eng:
            self._newsem(e)

    def _newsem(self, e):
        s = self.es.enter_context(self.nc.semaphore(f"s{len(self.sems)}"))
        self.sems.append(s)
        self.semeng.append(e)
        idx = len(self.sems) - 1
        if e is not None:
            self.cur[e] = idx
            self.cnt[e] = 0
        else:
            self.dcnt[idx] = 0
        return idx

    def sb(self, es, name, shape, dt):
        self.uid = getattr(self, "uid", 0) + 1
        h = es.enter_context(self.nc.sbuf_tensor(f"sb{self.uid}_" + name, list(shape), dt))
        t = Tl(h[:], name)
        t.r = dict(self.freed)
        es.callback(self._on_free, t)
        return t

    def _on_free(self, t):
        toks = dict(t.r)
        if t.w is not None:
            toks[t.w[0]] = max(toks.get(t.w[0], 0), t.w[1])
        for si, val in toks.items():
            if self.freed.get(si, 0) < val:
                self.freed[si] = val

    def ps(self, es, name, shape, dt):
        h = es.enter_context(self.nc.psum_tensor("ps_" + name, list(shape), dt))
        return Tl(h[:], name)

    def dram(self, name, shape, dt, kind):
        h = self.nc.dram_tensor(name, list(shape), dt, kind=kind)
        return Tl(h.ap(), name, dram=True)

    def _need(self, e, tok, raw, dma=False):
        if tok is None:
            return
        si, val = tok
        owner = self.semeng[si]
        if owner == e and (e == "pe" or (not raw and not STRICT)) and not dma:
            return
        k = self.known[e]
        if k.get(si, 0) >= val:
            return
        self.eng[e].wait_ge(self.sems[si], val)
        self.ninstr += 1
        k[si] = val
        if self.log is not None:
            self.log.append(f"{e}: WAIT s{si}({self.semeng[si]}) >= {val}")

    def _deps(self, e, reads, writes, skip_w_sem=None, dma=False):
        for t in reads:
            self._need(e, t.w, True, dma)
        for t in writes:
            if t.w is not None and t.w[0] != skip_w_sem:
                self._need(e, t.w, False, dma)
            for si, val in t.r.items():
                self._need(e, (si, val), False, dma)

    def _mark(self, tok, reads, writes):
        si, val = tok
        for t in reads:
            if t.r.get(si, 0) < val:
                t.r[si] = val
        for t in writes:
            t.w = tok
            t.r = {}

    def op(self, e, fn, **kw):
        reads, writes, args = [], [], {}
        for k, v in kw.items():
            if isinstance(v, V):
                (writes if k in OUTK else reads).append(v.t)
                args[k] = v.ap
            else:
                args[k] = v
        self._deps(e, reads, writes)
        ins = getattr(self.eng[e], fn)(**args)
        if self.log is not None:
            self.log.append(f"{e}: {fn} W={[t.name for t in writes]} R={[t.name for t in reads]} -> {self.cnt[e] + 1}")
        if self.cnt[e] >= self.EPOCH:
            self._newsem(e)
        si = self.cur[e]
        self.cnt[e] += 1
        ins.then_inc(self.sems[si], 1)
        self.ninstr += 1
        self._mark((si, self.cnt[e]), reads, writes)
        return ins

    def _dsem(self, t):
        if t.dsem is None:
            t.dsem = self._newsem(None)
        return t.dsem

    def dma(self, q, out, in_, semtile=None, extra_reads=(), **kw):
        sbt = semtile if semtile is not None else (out.t if not out.t.dram else in_.t)
        ds = self._dsem(sbt)
        reads = [in_.t] + [x.t for x in extra_reads]
        writes = [out.t]
        self._deps(q, reads, writes, skip_w_sem=ds, dma=True)
        ins = self.eng[q].dma_start(out=out.ap, in_=in_.ap, **kw)
        self.dcnt[ds] += 16
        ins.then_inc(self.sems[ds], 16)
        self.ninstr += 1
        self._mark((ds, self.dcnt[ds]), reads, writes)

    def gather(self, out, table, idx):
        ds = self._dsem(out.t)
        reads = [table.t, idx.t]
        writes = [out.t]
        self._deps("pool", reads, writes, skip_w_sem=ds, dma=True)
        ins = self.nc.gpsimd.indirect_dma_start(
            out=out.ap, out_offset=None, in_=table.ap,
            in_offset=bass.IndirectOffsetOnAxis(ap=idx.ap, axis=0))
        self.dcnt[ds] += 16
        ins.then_inc(self.sems[ds], 16)
        self.ninstr += 1
        self._mark((ds, self.dcnt[ds]), reads, writes)

    def finish(self, tiles):
        for t in tiles:
            self._need("sp", t.w, True)

    def mm(self, out, lhsT, rhs, start, stop, **kw):
        return self.op("pe", "matmul", out=out, lhsT=lhsT, rhs=rhs, start=start, stop=stop, **kw)

    def tr(self, out, in_, ident):
        return self.op("pe", "transpose", out=out, in_=in_, identity=ident)

    def act(self, out, in_, func, **kw):
        return self.op("act", "activation", out=out, in_=in_, func=func, **kw)

    def tt(self, e, out, in0, in1, op):
        return self.op(e, "tensor_tensor", out=out, in0=in0, in1=in1, op=op)

    def ts(self, e, out, in0, s1, op0, s2=None, op1=None, **kw):
        if op1 is None:
            return self.op(e, "tensor_scalar", out=out, in0=in0, scalar1=s1, scalar2=None, op0=op0, **kw)
        if isinstance(s1, V) != isinstance(s2, V):
            self.op(e, "tensor_scalar", out=out, in0=in0, scalar1=s1, scalar2=None, op0=op0)
            return self.op(e, "tensor_scalar", out=out, in0=out, scalar1=s2, scalar2=None, op0=op1, **kw)
        return self.op(e, "tensor_scalar", out=out, in0=in0, scalar1=s1, scalar2=s2, op0=op0, op1=op1, **kw)

    def stt(self, e, out, in0, scalar, in1, op0, op1, **kw):
        return self.op(e, "scalar_tensor_tensor", out=out, in0=in0, scalar=scalar, in1=in1, op0=op0, op1=op1, **kw)

    def copy(self, e, out, in_):
        if e == "act":
            return self.act(out, in_, AF.Copy)
        return self.op(e, "tensor_copy", out=out, in_=in_)

    def memset(self, e, out, val):
        return self.op(e, "memset", ap=out, constant=val) if False else self._memset(e, out, val)

    def _memset(self, e, out, val):
        self._deps(e, [], [out.t])
        ins = self.eng[e].memset(out.ap, val)
        if self.cnt[e] >= self.EPOCH:
            self._newsem(e)
        si = self.cur[e]
        self.cnt[e] += 1
        ins.then_inc(self.sems[si], 1)
        self.ninstr += 1
        self._mark((si, self.cnt[e]), [], [out.t])


OFF_U, OFF_Q, OFF_K, OFF_V, OFF_QI, OFF_KI, OFF_WI, OFF_GS, OFF_GA = 0, 512, 1024, 1152, 1280, 1536, 1568, 1576, 2600
IN_W = 3624
TWO_PI = 2.0 * math.pi


class Ctx:
    pass


def rope_tables(P, es, S, c):
    tabs = {fn + nm: P.sb(es, f"rope_{fn}{nm}", [128, S], BF16) for nm in ("A", "I") for fn in ("cos", "sin")}
    tmp = ExitStack()
    pid = P.sb(tmp, "rt_pid", [128, 1], I32)
    pm = P.sb(tmp, "rt_pm", [128, 1], I32)
    pf = P.sb(tmp, "rt_pf", [128, 1], F32)
    inv = P.sb(tmp, "rt_inv", [128, 2], F32)
    posi = P.sb(tmp, "rt_posi", [128, S], I32)
    pos = P.sb(tmp, "rt_pos", [128, S], F32)
    ang = P.sb(tmp, "rt_ang", [128, S], F32)
    t1 = P.sb(tmp, "rt_t1", [128, S], F32)
    ti = P.sb(tmp, "rt_ti", [128, S], I32)
    P.op("pool", "iota", out=pid.all(), pattern=[[0, 1]], base=0, channel_multiplier=1)
    P.op("pool", "iota", out=posi.all(), pattern=[[1, S]], base=0, channel_multiplier=0)
    P.copy("dve", pos.all(), posi.all())
    for j, (msk, dim) in enumerate(((31, 64), (15, 32))):
        P.op("dve", "tensor_single_scalar", out=pm.all(), in_=pid.all(), scalar=msk, op=ALU.bitwise_and)
        P.copy("dve", pf.all(), pm.all())
        P.act(inv[:, j:j + 1], pf.all(), AF.Exp, scale=-math.log(10000.0) * 2.0 / dim)
    outs = {}
    for j, nm in enumerate(("A", "I")):
        for k, (fn, shift) in enumerate((("cos", math.pi / 2), ("sin", 0.0))):
            tab = tabs[fn + nm]
            P.ts("dve", ang.all(), pos.all(), inv[:, j:j + 1], ALU.mult, shift, ALU.add)
            range_reduce_sin(P, tab.all(), ang.all(), t1.all(), ti.all())
            outs[fn + nm] = tab
    tmp.close()
    c.rope = outs


def range_reduce_sin(P, out, ang, t1, ti):
    P.ts("dve", t1, ang, 1.0 / TWO_PI, ALU.mult)
    P.copy("dve", ti, t1)
    P.copy("dve", t1, ti)
    P.stt("dve", t1, t1, -TWO_PI, ang, ALU.mult, ALU.add)
    P.ts("dve", ang, t1, math.pi, ALU.is_gt)
    P.stt("dve", t1, ang, -TWO_PI, t1, ALU.mult, ALU.add)
    P.ts("dve", t1, t1, 3.141592, ALU.min, -3.141592, ALU.max)
    P.act(out, t1, AF.Sin)


def load_w_cols(P, c, dst_fn, col0, ncols, w_in_d, gcol, stage):
    for k in range(8):
        st = stage[k % 2]
        P.dma("sp" if k % 2 == 0 else "act", st[:, 0:ncols], w_in_d[k * 128:(k + 1) * 128, col0:col0 + ncols])
        dst_fn(k, st)


def cpow(P, es, name, lr, th, jv, G, J, order="gj"):
    shp = [128, G, J] if order == "gj" else [128, J, G]
    Pr = P.sb(es, name + "_r", shp, F32)
    Pi = P.sb(es, name + "_i", shp, F32)
    tmp = ExitStack()
    mag = P.sb(tmp, name + "_mag", shp, F32)
    ang = P.sb(tmp, name + "_ang", shp, F32)
    t1 = P.sb(tmp, name + "_t1", shp, F32)
    ti = P.sb(tmp, name + "_ti", shp, I32)
    if order == "gj":
        lb, jb = lr.pat(0, [(1, G), (0, J)]), jv.pat(0, [(0, G), (1, J)])
        tb = th.pat(0, [(1, G), (0, J)])
    else:
        lb, jb = lr.pat(0, [(0, J), (1, G)]), jv.pat(0, [(1, J), (0, G)])
        tb = th.pat(0, [(0, J), (1, G)])
    P.tt("dve", mag.all(), lb, jb, ALU.mult)
    P.act(mag.all(), mag.all(), AF.Exp)
    fl = "p a b -> p (a b)"
    for dst, shift in ((Pi, 0.0), (Pr, math.pi / 2)):
        P.tt("dve", ang.all(), tb, jb, ALU.mult)
        if shift:
            P.ts("dve", ang.all(), ang.all(), shift, ALU.add)
        range_reduce_sin(P, dst.all().rr(fl), ang.all().rr(fl), t1.all().rr(fl), ti.all().rr(fl))
        P.tt("dve", dst.all(), dst.all(), mag.all(), ALU.mult)
    tmp.close()
    return Pr, Pi


def kappa(P, es, name, ar, ai, lr, th, G):
    kr = P.sb(es, name + "_kr", [128, G], F32)
    ki = P.sb(es, name + "_ki", [128, G], F32)
    tmp = ExitStack()
    one = P.sb(tmp, name + "_one", [128, 1], F32)
    P.memset("dve", one.all(), 1.0)
    Ar, Ai = cpow(P, tmp, name + "_a1", lr, th, one.all(), G, 1)
    den = P.sb(tmp, name + "_den", [128, G], F32)
    t = P.sb(tmp, name + "_t", [128, G], F32)
    arm = P.sb(tmp, name + "_arm", [128, G], F32)
    A_r, A_i = Ar.all().rr("p g j -> p (g j)"), Ai.all().rr("p g j -> p (g j)")
    P.tt("dve", den.all(), ar, ar, ALU.mult)
    P.tt("dve", t.all(), ai, ai, ALU.mult)
    P.tt("dve", den.all(), den.all(), t.all(), ALU.add)
    P.op("dve", "reciprocal", out=den.all(), in_=den.all())
    P.ts("dve", arm.all(), A_r, -1.0, ALU.add)
    P.tt("dve", kr.all(), arm.all(), ar, ALU.mult)
    P.tt("dve", t.all(), A_i, ai, ALU.mult)
    P.tt("dve", kr.all(), kr.all(), t.all(), ALU.add)
    P.tt("dve", kr.all(), kr.all(), den.all(), ALU.mult)
    P.tt("dve", ki.all(), A_i, ar, ALU.mult)
    P.tt("dve", t.all(), arm.all(), ai, ALU.mult)
    P.tt("dve", ki.all(), ki.all(), t.all(), ALU.subtract)
    P.tt("dve", ki.all(), ki.all(), den.all(), ALU.mult)
    tmp.close()
    return kr, ki


def ssm_phase(P, c, S, uys, sp):
    NCH = S // LCH
    psb = c.psb
    uT = ysT = y2 = uys
    ph = ExitStack()
    sg = P.sb(ph, "sg", [128, 1], F32)
    nsg = P.sb(ph, "nsg", [128, 1], F32)
    P.ts("dve", sg.all(), c.pid_f.all(), 63.5, ALU.is_gt, -2.0, ALU.mult)
    P.ts("dve", sg.all(), sg.all(), 1.0, ALU.add)
    P.ts("dve", nsg.all(), sg.all(), -1.0, ALU.mult)
    jv = P.sb(ph, "jv", [128, 256], F32)
    jvi = P.sb(ph, "jvi", [128, 256], I32)
    P.op("pool", "iota", out=jvi.all(), pattern=[[1, 256]], base=0, channel_multiplier=0)
    P.copy("dve", jv.all(), jvi.all())
    jrev = P.sb(ph, "jrev", [128, 16], F32)
    P.ts("dve", jrev.all(), jv[:, 0:16], -1.0, ALU.mult, 15.0, ALU.add)
    bm = P.sb(ph, "bm", [128, 8], F32)
    t8 = P.sb(ph, "t8", [128, 8], F32)
    P.ts("dve", t8.all(), jv[:, 0:8], 16.0, ALU.mult)
    P.ts("dve", bm.all(), t8.all(), c.pid_f[:, 0:1], ALU.subtract)
    P.ts("dve", t8.all(), bm.all(), 0.5, ALU.is_gt, -1.0, ALU.mult)
    P.ts("dve", t8.all(), t8.all(), 1.0, ALU.add)
    P.ts("dve", bm.all(), bm.all(), -15.5, ALU.is_gt)
    P.tt("dve", bm.all(), bm.all(), t8.all(), ALU.mult)
    eye8 = P.sb(ph, "eye8", [128, 8, 8], F32)
    P.tt("dve", eye8.all(), jv[:, 0:8].pat(0, [(1, 8), (0, 8)]), jv[:, 0:8].pat(0, [(0, 8), (1, 8)]), ALU.is_equal)
    pswapb = P.sb(ph, "pswapb", [128, 128], BF16)
    pswap = P.sb(ph, "pswap", [128, 128], F32)
    P.ts("dve", pswap.all(), c.iota_f.all(), c.pid_f[:, 0:1], ALU.subtract)
    P.tt("dve", pswap.all(), pswap.all(), pswap.all(), ALU.mult)
    P.ts("dve", pswap.all(), pswap.all(), 4096.0, ALU.is_equal)
    P.copy("dve", pswapb.all(), pswap.all())
    dsk = P.sb(ph, "dsk", [128, 4], F32)
    P.dma("sp", dsk.all(), sp["dskip"].all())
    wglu = P.sb(ph, "wglu", [128, 4, 512], BF16)
    stgw = P.sb(ph, "stgw", [128, 512], F32)
    for k in range(4):
        P.dma("sp", stgw.all(), sp["w_glu"][k * 128:(k + 1) * 128, :])
        P.copy("act", wglu[:, k, :], stgw.all())

    for o in range(4):
        oc = ExitStack()
        BD = P.sb(oc, "BD", [128, 16, 128], BF16)
        Wb = P.sb(oc, "Wb", [128, 8, 16, 128], BF16)
        Wc = P.sb(oc, "Wc", [128, 8, 16, 128], BF16)
        tc_ = P.sb(oc, "tabc", [128, 8, NCH], BF16)
        tsn = P.sb(oc, "tabs", [128, 8, NCH], BF16)
        rho = P.sb(oc, "rho", [128, 8], F32)
        pr = ExitStack()
        prm = P.sb(pr, "prm", [128, 3, 8], F32)
        for i, nm in enumerate(("ar_sm", "ai_sm", "ldt_sm")):
            P.dma("sp", prm[:, i, :], sp[nm][:, o * 8:(o + 1) * 8])
        dt = P.sb(pr, "dt", [128, 8], F32)
        lr = P.sb(pr, "lr", [128, 8], F32)
        th = P.sb(pr, "th", [128, 8], F32)
        P.act(dt.all(), prm[:, 2, :], AF.Exp)
        P.tt("dve", lr.all(), prm[:, 0, :], dt.all(), ALU.mult)
        P.tt("dve", th.all(), prm[:, 1, :], dt.all(), ALU.mult)
        kr, ki = kappa(P, pr, "ksm", prm[:, 0, :], prm[:, 1, :], lr.all(), th.all(), 8)
        Ar, Ai = cpow(P, pr, "apw", lr.all(), th.all(), jv[:, 0:17], 8, 17)
        prA = pr
        pr = ExitStack()
        U = P.sb(pr, "U12", [128, 2, 8, 16], F32)
        T12 = P.sb(pr, "T12", [128, 2, 8, 16], F32)
        for i, nm in enumerate(("bU1", "bU2")):
            P.dma("sp", U[:, i], sp[nm][:, o * 8:(o + 1) * 8, :])
        for i, nm in enumerate(("cT1", "cT2")):
            P.dma("act", T12[:, i], sp[nm][:, o * 8:(o + 1) * 8, :])
        X = P.sb(pr, "X", [128, 8, 16], F32)
        tx = P.sb(pr, "tx", [128, 8, 16], F32)
        kib = P.sb(pr, "kib", [128, 8], F32)
        P.ts("dve", kib.all(), ki.all(), nsg[:, 0:1], ALU.mult)
        P.tt("dve", X.all(), U[:, 0], kr.all().pat(0, [(1, 8), (0, 16)]), ALU.mult)
        P.tt("dve", tx.all(), U[:, 1], kib.all().pat(0, [(1, 8), (0, 16)]), ALU.mult)
        P.tt("dve", X.all(), X.all(), tx.all(), ALU.add)
        if getattr(c, "dump", None) and o == 0 and DUMP2:
            c.dump("kr", kr.all(), F32)
            c.dump("ki", ki.all(), F32)
            c.dump("X", X.all().rr("p a b -> p (a b)"), F32)
            c.dump("Ar", Ar.all().rr("p a b -> p (a b)"), F32)
        Y = P.sb(pr, "Y", [128, 8, 17, 16], F32)
        ty = P.sb(pr, "ty", [128, 8, 17, 16], F32)
        for g in range(8):
            P.tt("dve", Y[:, g], T12[:, 0, g, :].pat(0, [(0, 17), (1, 16)]), Ar[:, g, :].pat(0, [(1, 17), (0, 16)]), ALU.mult)
            P.tt("dve", ty[:, g], T12[:, 1, g, :].pat(0, [(0, 17), (1, 16)]), Ai[:, g, :].pat(0, [(1, 17), (0, 16)]), ALU.mult)
        P.stt("dve", Y.all().rr("p g t c -> p (g t c)"), Y.all().rr("p g t c -> p (g t c)"), sg[:, 0:1],
              ty.all().rr("p g t c -> p (g t c)"), ALU.mult, ALU.subtract)
        if getattr(c, "dump", None) and o == 0 and DUMP2:
            c.dump("Y", Y.all().rr("p a b c -> p (a b c)"), F32)
        for g in range(8):
            P.tt("dve", Wc[:, g].rr("p t (a c) -> p t a c", a=8),
                 Y[:, g, 1:17, :].pat(0, [(16, 16), (0, 8), (1, 16)]),
                 eye8[:, g, :].pat(0, [(0, 16), (1, 8), (0, 16)]), ALU.mult)
        Xpad = P.sb(pr, "Xpad", [128, 8, 8, 16], BF16)
        Yb = P.sb(pr, "Yb", [128, 8, 16, 16], BF16)
        P.copy("dve", Yb.all(), Y[:, :, 0:16, :])
        for g in range(8):
            P.tt("dve", Xpad[:, g], X[:, g, :].pat(0, [(0, 8), (1, 16)]), eye8[:, g, :].pat(0, [(1, 8), (0, 16)]), ALU.mult)
        for g in range(8):
            P.mm(psb[0][:, 0:256], Xpad[:, g].rr("p a c -> p (a c)"), Yb[:, g].rr("p t c -> p (t c)"), g == 0, g == 7)
        Rsb = P.sb(pr, "Rsb", [128, 16, 16], F32)
        P.copy("act", Rsb.all().rr("p t c -> p (t c)"), psb[0][:, 0:256])
        if getattr(c, "dump", None) and o == 0 and DUMP2:
            c.dump("Rsb", Rsb.all().rr("p a b -> p (a b)"), F32)
            c.dump("Xpad", Xpad.all().rr("p a b c -> p (a b c)"), BF16)
        for tau in range(16):
            P.tt("dve", BD[:, tau, :].rr("p (a c) -> p a c", a=8), Rsb[:, tau, :].pat(0, [(0, 8), (1, 16)]),
                 bm.all().pat(0, [(1, 8), (0, 16)]), ALU.mult)
        pr.close()
        pr = ExitStack()
        l16 = P.sb(pr, "l16", [128, 8], F32)
        P.act(rho.all(), lr.all(), AF.Exp, scale=float(LCH))
        P.ts("dve", l16.all(), th.all(), float(LCH), ALU.mult)
        zero8 = P.sb(pr, "zero8", [128, 8], F32)
        P.memset("dve", zero8.all(), 0.0)
        assert NCH <= 256
        for h4 in range(2):
            pq = ExitStack()
            Er, Ei = cpow(P, pq, f"rot{h4}", zero8[:, h4 * 4:h4 * 4 + 4], l16[:, h4 * 4:h4 * 4 + 4], jv[:, 0:NCH], 4, NCH)
            P.copy("act", tc_[:, h4 * 4:h4 * 4 + 4, :], Er.all())
            P.copy("act", tsn[:, h4 * 4:h4 * 4 + 4, :], Ei.all())
            pq.close()
        pr.close()
        pr = ExitStack()
        pcm = P.sb(pr, "pcm", [128, 5, 64], F32)
        for i, nm in enumerate(("ar_cm", "ai_cm", "ldt_cm", "br_cm", "bi_cm")):
            P.dma("sp", pcm[:, i, :], sp[nm][:, o, :])
        dtc = P.sb(pr, "dtc", [128, 64], F32)
        lrc = P.sb(pr, "lrc", [128, 64], F32)
        thc = P.sb(pr, "thc", [128, 64], F32)
        P.act(dtc.all(), pcm[:, 2, :], AF.Exp)
        P.tt("dve", lrc.all(), pcm[:, 0, :], dtc.all(), ALU.mult)
        P.tt("dve", thc.all(), pcm[:, 1, :], dtc.all(), ALU.mult)
        krc, kic = kappa(P, pr, "kcm", pcm[:, 0, :], pcm[:, 1, :], lrc.all(), thc.all(), 64)
        Bbr = P.sb(pr, "Bbr", [128, 64], F32)
        Bbi = P.sb(pr, "Bbi", [128, 64], F32)
        tb = P.sb(pr, "tb", [128, 64], F32)
        P.tt("dve", Bbr.all(), krc.all(), pcm[:, 3, :], ALU.mult)
        P.tt("dve", tb.all(), kic.all(), pcm[:, 4, :], ALU.mult)
        P.tt("dve", Bbr.all(), Bbr.all(), tb.all(), ALU.subtract)
        P.tt("dve", Bbi.all(), krc.all(), pcm[:, 4, :], ALU.mult)
        P.tt("dve", tb.all(), kic.all(), pcm[:, 3, :], ALU.mult)
        P.tt("dve", Bbi.all(), Bbi.all(), tb.all(), ALU.add)
        Pr_, Pi_ = cpow(P, pr, "apc", lrc.all(), thc.all(), jrev.all(), 64, 16, order="jg")
        Z = P.sb(pr, "Z", [128, 16, 2, 64], F32)
        tz = P.sb(pr, "tz", [128, 16, 64], F32)
        bb = lambda t: t.all().pat(0, [(0, 16), (1, 64)])
        P.tt("dve", Z[:, :, 0, :], Pr_.all(), bb(Bbr), ALU.mult)
        P.tt("dve", tz.all(), Pi_.all(), bb(Bbi), ALU.mult)
        P.tt("dve", Z[:, :, 0, :], Z[:, :, 0, :], tz.all(), ALU.subtract)
        P.tt("dve", Z[:, :, 1, :], Pr_.all(), bb(Bbi), ALU.mult)
        P.tt("dve", tz.all(), Pi_.all(), bb(Bbr), ALU.mult)
        P.tt("dve", Z[:, :, 1, :], Z[:, :, 1, :], tz.all(), ALU.add)
        if getattr(c, "dump", None) and o == 0 and DUMP2:
            c.dump("Z", Z.all().rr("p s r m -> p (s r m)"), F32)
            c.dump("Bbr", Bbr.all(), F32)
            c.dump("Prc", Pr_.all().rr("p a b -> p (a b)"), F32)
            c.dump("krc", krc.all(), F32)
            c.dump("pcm", pcm.all().rr("p a b -> p (a b)"), F32)
            c.dump("lrc", lrc.all(), F32)
        for g in range(8):
            P.ts("dve", Wb[:, g].rr("p s m -> p (s m)"), Z.all().rr("p s r m -> p (s r m)"), bm[:, g:g + 1], ALU.mult)
        pr.close()
        prA.close()
        if getattr(c, "dump", None) and o == 0:
            c.dump("BD", BD.all().rr("p a b -> p (a b)"), BF16)
            c.dump("Wb", Wb.all().rr("p a b m -> p (a b m)"), BF16)
            c.dump("Wc", Wc.all().rr("p a b m -> p (a b m)"), BF16)
            c.dump("tabc", tc_.all().rr("p a b -> p (a b)"), BF16)
            c.dump("tabs", tsn.all().rr("p a b -> p (a b)"), BF16)
            c.dump("rho", rho.all(), F32)

        wk = ExitStack()
        SA = P.sb(wk, "SA", [128, 8, NCH], F32)
        VA = P.sb(wk, "VA", [128, 8, NCH], F32)
        VB = P.sb(wk, "VB", [128, 8, NCH], F32)
        t1 = P.sb(wk, "l2t1", [128, 8, NCH], F32)
        t2 = P.sb(wk, "l2t2", [128, 8, NCH], F32)
        H = P.sb(wk, "H", [128, 8, NCH], BF16)
        SAh = P.sb(wk, "SAh", [128, 8, NCH], BF16)
        SAl = P.sb(wk, "SAl", [128, 8, NCH], BF16)
        uo = uT[:, o, :].rr("p (k s) -> p s k", s=LCH)
        for g in range(8):
            bank = psb[1 + (g % 2)]
            for q0 in range(0, NCH, 512):
                qn = min(512, NCH - q0)
                for s_ in range(LCH):
                    P.mm(bank[:, 0:qn], Wb[:, g, s_, :], uo[:, s_, q0:q0 + qn], s_ == 0, s_ == LCH - 1)
                P.copy("act", SA[:, g, q0:q0 + qn], bank[:, 0:qn])
        fl = "p g k -> p (g k)"
        cb, sb_ = tc_.all().rr(fl), tsn.all().rr(fl)
        for g in range(8):
            for q0 in range(0, NCH, 512):
                qn = min(512, NCH - q0)
                bank = psb[3 + (g % 2)]
                P.copy("dve", SAh[:, g, q0:q0 + qn], SA[:, g, q0:q0 + qn])
                P.tt("dve", SAl[:, g, q0:q0 + qn], SA[:, g, q0:q0 + qn], SAh[:, g, q0:q0 + qn], ALU.subtract)
                P.mm(bank[:, 0:qn], pswapb.all(), SAh[:, g, q0:q0 + qn], True, False)
                P.mm(bank[:, 0:qn], pswapb.all(), SAl[:, g, q0:q0 + qn], False, True)
                sl = slice(q0, q0 + qn)
                A_, B_ = SA[:, g, sl], bank[:, 0:qn]
                cg, sgn_ = tc_[:, g, sl], tsn[:, g, sl]
                P.tt("dve", t1[:, g, sl], A_, cg, ALU.mult)
                P.tt("dve", t2[:, g, sl], B_, sgn_, ALU.mult)
                P.stt("dve", VA[:, g, sl], t2[:, g, sl], sg[:, 0:1], t1[:, g, sl], ALU.mult, ALU.add)
                P.tt("dve", t1[:, g, sl], B_, cg, ALU.mult)
                P.tt("dve", t2[:, g, sl], A_, sgn_, ALU.mult)
                P.stt("dve", VB[:, g, sl], t2[:, g, sl], nsg[:, 0:1], t1[:, g, sl], ALU.mult, ALU.add)
        for g in range(8):
            rb = rho[:, g:g + 1].pat(0, [(0, NCH)])
            P.op("dve", "tensor_tensor_scan", out=VA[:, g, :], data0=rb, data1=VA[:, g, :], initial=0.0, op0=ALU.mult, op1=ALU.add)
            P.op("dve", "tensor_tensor_scan", out=VB[:, g, :], data0=rb, data1=VB[:, g, :], initial=0.0, op0=ALU.mult, op1=ALU.add)
        P.tt("dve", t1.all().rr(fl), VA.all().rr(fl), cb, ALU.mult)
        P.tt("dve", t2.all().rr(fl), VB.all().rr(fl), sb_, ALU.mult)
        P.stt("dve", H.all().rr(fl), t2.all().rr(fl), nsg[:, 0:1], t1.all().rr(fl), ALU.mult, ALU.add)
        if getattr(c, "dump", None) and o == 0:
            c.dump("SA", SA.all().rr("p a b -> p (a b)"), F32)
            c.dump("H", H.all().rr("p a b -> p (a b)"), BF16)
        yo = uo
        for t in range(LCH - 1, -1, -1):
            bank = psb[5 + (t % 3)]
            for q0 in range(0, NCH, 512):
                qn = min(512, NCH - q0)
                for s_ in range(t + 1):
                    P.mm(bank[:, 0:qn], BD[:, t - s_, :], uo[:, s_, q0:q0 + qn], s_ == 0, False)
                for g in range(8):
                    lo = 1 if q0 == 0 else 0
                    P.mm(bank[:, lo:qn], Wc[:, g, t, :], H[:, g, q0 + lo - 1:q0 + qn - 1], False, g == 7)
                P.stt("dve", yo[:, t, q0:q0 + qn], uo[:, t, q0:q0 + qn], dsk[:, o:o + 1], bank[:, 0:qn], ALU.mult, ALU.add)
        wk.close()
        oc.close()
    if getattr(c, "dump", None):
        c.dump("ypre", y2.all().rr("p a b -> p (a b)"), BF16)
    gl = ExitStack()
    g1 = P.sb(gl, "g1", [128, S], F32)
    g2 = P.sb(gl, "g2", [128, S], F32)
    for o in range(4):
        P.act(y2[:, o, :], y2[:, o, :], AF.Gelu_apprx_tanh)
    gate = P.sb(gl, "gate", [128, 4, 512], BF16)
    for q0 in range(0, S, 512):
        for n in range(4):
            bank = psb[n]
            for k in range(4):
                P.mm(bank[:, 0:512], wglu[:, k, n * 128:(n + 1) * 128], y2[:, k, q0:q0 + 512], k == 0, k == 3)
            P.act(gate[:, n, :], bank[:, 0:512], AF.Sigmoid)
        P.tt("dve", ysT[:, :, q0:q0 + 512], gate.all(), y2[:, :, q0:q0 + 512], ALU.mult)
    gl.close()
    ph.close()


def gelu_inplace(P, x, t1, t2, eng="dve"):
    P.tt(eng, t1, x, x, ALU.mult)
    P.ts(eng, t1, t1, 0.044715 * 1.5957691216, ALU.mult, 1.5957691216, ALU.add)
    P.tt(eng, t1, t1, x, ALU.mult)
    P.act(t2, t1, AF.Sigmoid)
    P.tt(eng, x, x, t2, ALU.mult)


def attn_phase(P, c, S, x_d, w_in_d, g1c, ng1c, kTd, kiT4, vaug, ya_d, stop_at=None, dump=None):
    NT = S // 128
    TOPK = min(256, S // 4)
    psb, ident = c.psb, c.ident
    ph = ExitStack()
    rope_tables(P, ph, S, c)
    w_q = P.sb(ph, "w_q", [128, 8, 8, 128], BF16)
    w_qi = P.sb(ph, "w_qi", [128, 8, 6, 128], BF16)
    P.memset("pool", w_qi.all(), 0.0)
    w_wi = P.sb(ph, "w_wi", [128, 8, 8], BF16)
    wst = ExitStack()
    stg = [P.sb(wst, f"astg{i}", [128, 512], F32) for i in range(2)]

    def cvt(dst, src, k, neg=False):
        P.act(dst, src, AF.Copy, scale=(ng1c if neg else g1c)[:, k:k + 1])

    def q_cvt(k, st):
        for m in range(4):
            cvt(w_q[:, k, m, :], st[:, m * 128:(m + 1) * 128], k)
            for e in range(2):
                base = m * 128 + e * 64
                cvt(w_q[:, k, 4 + m, e * 64:e * 64 + 32], st[:, base + 32:base + 64], k, neg=True)
                cvt(w_q[:, k, 4 + m, e * 64 + 32:e * 64 + 64], st[:, base:base + 32], k)
    load_w_cols(P, c, q_cvt, OFF_Q, 512, w_in_d, g1c, stg)

    def qi_cvt(k, st):
        for h in range(8):
            tl, pos = h // 3, h % 3
            cvt(w_qi[:, k, tl, pos * 32:(pos + 1) * 32], st[:, h * 32:(h + 1) * 32], k)
            cvt(w_qi[:, k, 3 + tl, pos * 32:pos * 32 + 16], st[:, h * 32 + 16:h * 32 + 32], k, neg=True)
            cvt(w_qi[:, k, 3 + tl, pos * 32 + 16:pos * 32 + 32], st[:, h * 32:h * 32 + 16], k)
    load_w_cols(P, c, qi_cvt, OFF_QI, 256, w_in_d, g1c, stg)
    load_w_cols(P, c, lambda k, st: cvt(w_wi[:, k, :], st[:, 0:8], k), OFF_WI, 8, w_in_d, g1c, stg)
    wst.close()

    xt = [P.sb(ph, "axt0", [128, D], F32)] * 2
    xh = P.sb(ph, "axh", [128, D], BF16)
    xhT = P.sb(ph, "axhT", [128, 8, 128], BF16)
    junk = P.sb(ph, "ajunk", [128, D], BF16)
    ss = P.sb(ph, "ass", [128, 1], F32)
    rstd = P.sb(ph, "arstd", [128, 1], F32)
    qT = P.sb(ph, "qT", [128, 4, 128], BF16)
    qiT = P.sb(ph, "qiT", [128, 3, 128], BF16)
    t1 = P.sb(ph, "at1", [128, 4, 128], F32)
    t2 = P.sb(ph, "at2", [128, 4, 128], F32)
    wsg = P.sb(ph, "wsg", [128, 8], F32)
    wsc = P.sb(ph, "wsc", [128, 8], F32)
    acc = P.sb(ph, "acc", [128, S], F32)
    msk = P.sb(ph, "msk", [128, S], BF16)
    mT = P.sb(ph, "mT", [128, NT, 128], BF16)
    rr = [P.sb(ph, f"rr{i}", [128, 512], F32) for i in range(2)]
    lo = P.sb(ph, "lo", [128, 1], F32)
    mid = P.sb(ph, "mid", [128, 1], F32)
    cnt = P.sb(ph, "cnt", [128, 1], F32)
    cntb = P.sb(ph, "cntb", [128, 1], F32)
    ge = P.sb(ph, "ge", [128, 1], F32)
    eT = [[P.sb(ph, f"eT{i}{n}", [128, 4, 128], BF16) for n in range(2)] for i in range(2)]
    pT = [[P.sb(ph, f"pT{i}{n}", [128, 4, 128], BF16) for n in range(2)] for i in range(2)]
    rden = P.sb(ph, "rden", [128, 512], F32)
    rdh = P.sb(ph, "rdh", [128, 512], BF16)
    rdl = P.sb(ph, "rdl", [128, 512], BF16)
    ones_bf = P.sb(ph, "ones_bf", [128, 64], BF16)
    P.memset("dve", ones_bf.all(), 1.0)
    bcs = P.sb(ph, "bcs", [64, 512], F32)
    ya = [P.sb(ph, f"ya{i}", [64, 8, 128], BF16) for i in range(2)]
    m4 = "p (m t) -> p m t"

    qTs = [qT, P.sb(ph, "qT1", [128, 4, 128], BF16)]
    sjunk = P.sb(ph, "sjunk", [128, 2432], BF16)
    PIPE = stop_at is None

    def stage_I(b):
        tok = slice(b * 128, (b + 1) * 128)
        Sc = (b + 1) * 128
        qTb = qTs[b % 2]
        xb = xt[b % 2]
        P.dma("sp", xb.all(), x_d[tok, :])
        c.norm_and_transpose(b, xb, xh, xhT, junk, ss, rstd, psb[0])
        for m in range(8):
            bank = psb[1] if m < 4 else psb[2]
            for k in range(8):
                P.mm(bank[:, (m % 4) * 128:(m % 4 + 1) * 128], w_q[:, k, m, :], xhT[:, k, :], k == 0, k == 7)
        P.tt("dve", t1.all(), psb[1].all().rr(m4, m=4), c.rope["cosA"][:, tok].pat(0, [(0, 4), (1, 128)]), ALU.mult)
        P.tt("dve", t2.all(), psb[2].all().rr(m4, m=4), c.rope["sinA"][:, tok].pat(0, [(0, 4), (1, 128)]), ALU.mult)
        P.tt("dve", qTb.all(), t1.all(), t2.all(), ALU.add)
        for m in range(6):
            bank = psb[3] if m < 3 else psb[6]
            for k in range(8):
                P.mm(bank[:, (m % 3) * 128:(m % 3 + 1) * 128], w_qi[:, k, m, :], xhT[:, k, :], k == 0, k == 7)
        P.tt("dve", t1[:, 0:3, :], psb[3][:, 0:384].rr(m4, m=3), c.rope["cosI"][:, tok].pat(0, [(0, 3), (1, 128)]), ALU.mult)
        P.tt("dve", t2[:, 0:3, :], psb[6][:, 0:384].rr(m4, m=3), c.rope["sinI"][:, tok].pat(0, [(0, 3), (1, 128)]), ALU.mult)
        P.tt("dve", qiT.all(), t1[:, 0:3, :], t2[:, 0:3, :], ALU.add)
        for k in range(8):
            P.mm(psb[7][:, 0:8], xhT[:, k, :], w_wi[:, k, :], k == 0, k == 7)
        P.ts("dve", wsg.all(), psb[7][:, 0:8], 0.0, ALU.is_gt, 2.0, ALU.mult)
        P.ts("dve", wsg.all(), wsg.all(), -1.0, ALU.add)
        P.tt("dve", wsc.all(), psb[7][:, 0:8], wsg.all(), ALU.mult)
        P.ts("dve", wsc.all(), wsc.all(), 1.0 / 16.0, ALU.mult)
        if stop_at == "proj":
            return
        ibanks = [psb[1], psb[2], psb[3], psb[6]]
        ci = 0
        for q0 in range(0, Sc, 512):
            qn = min(512, Sc - q0)
            for h in range(8):
                bank, r = ibanks[h % 3], rr[ci % 2]
                ci += 1
                pb = 32 * (h % 3)
                P.mm(bank[:, 0:qn], qiT[pb:pb + 32, h // 3, :], kiT4[pb:pb + 32, q0:q0 + qn], True, True)
                P.act(r[:, 0:qn], bank[:, 0:qn], AF.Relu, scale=wsc[:, h:h + 1])
                if h == 0:
                    P.ts("dve", acc[:, q0:q0 + qn], r[:, 0:qn], wsg[:, 0:1], ALU.mult)
                else:
                    P.stt("dve", acc[:, q0:q0 + qn], r[:, 0:qn], wsg[:, h:h + 1], acc[:, q0:q0 + qn], ALU.mult, ALU.add)
        P.tt("dve", acc[:, b * 128:Sc], acc[:, b * 128:Sc], c.caus.all(), ALU.add)

    def stage_B(b):
        Sc = (b + 1) * 128
        if Sc > TOPK:
            c1 = (int(Sc * 0.42) // 128) * 128 if Sc >= 1024 else Sc
            nB = Sc - c1
            half = 8.0
            P.memset("dve", mid.all(), 0.0)
            NSTEP = 19
            for it in range(NSTEP):
                P.op("dve", "tensor_scalar", out=msk[:, 0:c1], in0=acc[:, 0:c1], scalar1=mid[:, 0:1], scalar2=0.0,
                     op0=ALU.is_ge, op1=ALU.add, accum_out=cnt.all())
                if nB:
                    P.act(sjunk[:, 0:nB], acc[:, c1:Sc], AF.Sign, bias=mid[:, 0:1], scale=-1.0, accum_out=cntb.all())
                    P.stt("dve", cnt.all(), cntb.all(), -0.5, cnt.all(), ALU.mult, ALU.add)
                P.ts("dve", ge.all(), cnt.all(), TOPK - 0.5 - nB / 2.0, ALU.is_ge, half, ALU.mult)
                nxt = half / 2 if it < NSTEP - 1 else half
                P.stt("dve", mid.all(), ge.all(), -nxt, mid.all(), ALU.add, ALU.add)
                half = half / 2
                yield
            P.ts("dve", msk[:, 0:Sc], acc[:, 0:Sc], mid[:, 0:1], ALU.is_ge)
        else:
            P.ts("dve", msk[:, 0:Sc], acc[:, 0:Sc], -1.0e29, ALU.is_ge)

    def stage_T(b):
        pbf = psb[0].all().cast(BF16)
        for j0 in range(0, b + 1, 8):
            jn = min(8, b + 1 - j0)
            for jj in range(jn):
                P.tr(pbf[:, jj * 128:(jj + 1) * 128], msk[:, (j0 + jj) * 128:(j0 + jj + 1) * 128], ident.all())
            P.copy("act", mT[:, j0:j0 + jn, :].rr("p j t -> p (j t)"), pbf[:, 0:jn * 128])

    def stage_A(b):
        tok = slice(b * 128, (b + 1) * 128)
        qTb = qTs[b % 2]
        obank = [psb[4], psb[5]]
        dbank = [psb[7], psb[0]]
        for j in range(b + 1):
            meng = "pool" if (PIPE and mstate["bisect"]) else "dve"
            ks = slice(j * 128, (j + 1) * 128)
            lb = [psb[1], psb[2]] if j % 2 == 0 else [psb[3], psb[6]]
            for e in range(2):
                for n in range(2):
                    P.mm(lb[e][:, 2 * n * 128:(2 * n + 2) * 128], kTd[64 * e:64 * e + 64, n, ks],
                         qTb[64 * e:64 * e + 64, 2 * n:2 * n + 2, :].rr("p m t -> p (m t)"), True, True)
            for e in range(2):
                et, pt = eT[j % 2][e], pT[j % 2][e]
                P.act(et.all().rr("p h t -> p (h t)"), lb[e].all(), AF.Exp, scale=0.125)
                P.tt(meng, pt.all(), et.all(), mT[:, j, :].pat(0, [(0, 4), (1, 128)]), ALU.mult)
            if stop_at != "lg":
                for e in range(2):
                    pt = pT[j % 2][e]
                    for n in range(2):
                        rhs = pt[:, 2 * n:2 * n + 2, :].rr("p h t -> p (h t)")
                        cs = slice(2 * e * 128, (2 * e + 2) * 128)
                        st_, sp_ = (j == 0 and e == 0), (j == b and e == 1)
                        P.mm(obank[n][0:64, cs], vaug[:, j, n, 0:64], rhs, st_, sp_, skip_group_check=True)
                        P.mm(dbank[n][0:64, cs], ones_bf.all(), rhs, st_, sp_, skip_group_check=True)
            yield
        if stop_at in ("pv", "lg"):
            return
        yab = ya[b % 2]
        for n in range(2):
            P.op("dve", "reciprocal", out=bcs.all(), in_=dbank[n][0:64, :])
            for e in range(2):
                cs = slice(2 * e * 128, (2 * e + 2) * 128)
                P.tt("dve", yab[:, 4 * n + e:4 * n + e + 3:2, :], obank[n][0:64, cs].rr("p (i t) -> p i t", i=2),
                     bcs[:, cs].rr("p (i t) -> p i t", i=2), ALU.mult)
        P.dma("sp", ya_d[:, :, tok], yab.all())

    mstate = {"bisect": False}

    def run(gen):
        if gen is not None:
            for _ in gen:
                pass

    def step(gen):
        if gen is None:
            return None
        try:
            next(gen)
            return gen
        except StopIteration:
            return None

    if not PIPE:
        for b in range(NT):
            stage_I(b)
            if stop_at in ("proj", "idx"):
                continue
            run(stage_B(b))
            if stop_at == "thr":
                continue
            stage_T(b)
            if stop_at == "mt":
                continue
            run(stage_A(b))
    else:
        stage_I(0)
        run(stage_B(0))
        stage_T(0)
        for b in range(NT):
            gB = None
            if b + 1 < NT:
                stage_I(b + 1)
                gB = stage_B(b + 1)
            gA = stage_A(b)
            while gA is not None or gB is not None:
                mstate["bisect"] = gB is not None
                gA = step(gA)
                gB = step(gB)
                gB = step(gB)
            if b + 1 < NT:
                stage_T(b + 1)
    if dump is not None:
        dump("qT", qT.all().rr("p a b -> p (a b)"), BF16)
        dump("qiT", qiT.all().rr("p a b -> p (a b)"), BF16)
        dump("wsc", wsc.all(), F32)
        dump("wsg", wsg.all(), F32)
        if stop_at != "proj":
            dump("acc", acc.all(), F32)
        if stop_at not in ("proj", "idx"):
            dump("msk", msk.all(), BF16)
            dump("lo", mid.all(), F32)
        if stop_at == "lg":
            dump("pT", pT[(NT - 1) % 2][1].all().rr("p a b -> p (a b)"), BF16)
        if stop_at not in ("proj", "idx", "thr"):
            dump("mT", mT.all().rr("p a b -> p (a b)"), BF16)
        if stop_at == "pv":
            for n in range(2):
                P.copy("act", rden.all(), psb[4 + n].all())
                dump(f"oT{n}", rden.all(), F32)
    ph.close()


def merge_phase(P, c, S, x_d, w_in_d, g1c, ysT, ya_d, h_d, wd, uvb_d=None):
    NT = S // 128
    psb = c.psb
    ph = ExitStack()
    w_gs = P.sb(ph, "w_gs", [128, 8, 1024], BF16)
    w_ga = P.sb(ph, "w_ga", [128, 8, 1024], BF16)
    w_sup = P.sb(ph, "w_sup", [128, 4, 1024], BF16)
    w_aup = P.sb(ph, "w_aup", [64, 8, 1024], BF16)
    w_out = P.sb(ph, "w_out", [128, 8, 1024], BF16)
    stg = [P.sb(ph, f"bstg{i}", [128, 1024], F32) for i in range(2)]
    qs = ("sp", "act")

    def cvt(dst, src, k):
        P.act(dst, src, AF.Copy, scale=g1c[:, k:k + 1])
    load_w_cols(P, c, lambda k, st: cvt(w_gs[:, k, :], st[:, 0:1024], k), OFF_GS, 1024, w_in_d, g1c, stg)
    load_w_cols(P, c, lambda k, st: cvt(w_ga[:, k, :], st[:, 0:1024], k), OFF_GA, 1024, w_in_d, g1c, stg)
    for k in range(4):
        P.dma(qs[k % 2], stg[k % 2].all(), wd["w_ssm_up"][k * 128:(k + 1) * 128, :])
        P.copy("act", w_sup[:, k, :], stg[k % 2].all())
    for h in range(8):
        P.dma(qs[h % 2], stg[h % 2][0:64, :], wd["w_attn_up"][h * 64:(h + 1) * 64, :])
        P.copy("act", w_aup[:, h, :], stg[h % 2][0:64, :])
    for k in range(8):
        P.dma(qs[k % 2], stg[k % 2].all(), wd["w_out"][k * 128:(k + 1) * 128, :])
        P.copy("act", w_out[:, k, :], stg[k % 2].all())

    xt = [P.sb(ph, f"bxt{i}", [128, D], F32) for i in range(2)]
    xh = P.sb(ph, "bxh", [128, D], BF16)
    xhT = P.sb(ph, "bxhT", [128, 8, 128], BF16)
    junk = P.sb(ph, "bjunk", [128, D], BF16)
    ss = P.sb(ph, "bss", [128, 1], F32)
    rstd = P.sb(ph, "brstd", [128, 1], F32)
    yat = [P.sb(ph, f"yat{i}", [64, 8, 128], BF16) for i in range(2)]
    sgs = P.sb(ph, "sgs", [128, 512], F32)
    sga = P.sb(ph, "sga", [128, 512], F32)
    m1 = P.sb(ph, "m1", [128, 512], F32)
    m2 = P.sb(ph, "m2", [128, 512], F32)
    merged = P.sb(ph, "merged", [128, 8, 128], BF16)
    ht = [P.sb(ph, f"bht{i}", [128, D], F32) for i in range(2)]

    NCONV = 16384 // 128
    cvf = [P.sb(ph, f"cvf{i}", [128, 2048], F32) for i in range(2)]
    cvb = [P.sb(ph, f"cvb{i}", [128, 2048], BF16) for i in range(2)]

    def conv_step(i):
        rows = slice(i * 128, (i + 1) * 128)
        P.dma("pool", cvf[i % 2].all(), wd["peer_uv"][rows, :])
        P.copy("act", cvb[i % 2].all(), cvf[i % 2].all())
        P.dma("pool", uvb_d[rows, :], cvb[i % 2].all())
    conv_per_tile = (NCONV + NT - 1) // NT
    conv_i = 0

    for b in range(NT):
        for _ in range(conv_per_tile):
            if uvb_d is not None and conv_i < NCONV:
                conv_step(conv_i)
                conv_i += 1
        tok = slice(b * 128, (b + 1) * 128)
        xb = xt[b % 2]
        P.dma("sp", xb.all(), x_d[tok, :])
        ya = yat[b % 2]
        P.dma("sp", ya.all(), ya_d[:, :, tok])
        c.norm_and_transpose(b, xb, xh, xhT, junk, ss, rstd, psb[0])
        for half in range(2):
            for cc in range(4):
                ci = half * 4 + cc
                cols = slice(ci * 128, (ci + 1) * 128)
                reg = slice(cc * 128, (cc + 1) * 128)
                for k in range(8):
                    P.mm(psb[1][:, reg], w_gs[:, k, cols], xhT[:, k, :], k == 0, k == 7)
                for k in range(8):
                    P.mm(psb[2][:, reg], w_ga[:, k, cols], xhT[:, k, :], k == 0, k == 7)
                for k in range(4):
                    P.mm(psb[3][:, reg], w_sup[:, k, cols], ysT[:, k, tok], k == 0, k == 3)
                for h in range(8):
                    P.mm(psb[4][:, reg], w_aup[:, h, cols], ya[:, h, :], h == 0, h == 7)
            P.act(sgs.all(), psb[1].all(), AF.Sigmoid)
            P.act(sga.all(), psb[2].all(), AF.Sigmoid)
            P.tt("dve", m1.all(), sgs.all(), psb[3].all(), ALU.mult)
            P.tt("dve", m2.all(), sga.all(), psb[4].all(), ALU.mult)
            P.tt("dve", merged[:, half * 4:half * 4 + 4, :].rr("p c t -> p (c t)"), m1.all(), m2.all(), ALU.add)
        hb = ht[b % 2]
        for n2 in range(2):
            cs = slice(n2 * 512, (n2 + 1) * 512)
            for k in range(8):
                P.mm(psb[5 + n2].all(), merged[:, k, :], w_out[:, k, cs], k == 0, k == 7)
            P.tt("dve", hb[:, cs], psb[5 + n2].all(), xb[:, cs], ALU.add)
        P.dma("sp", h_d[tok, :], hb.all())
    ph.close()


def top16(P, src, vals_out, idx_out, tl, par):
    m8a, i8a, m8b, i8b = tl["m8a"][par], tl["i8a"][par], tl["m8b"][par], tl["i8b"][par]
    n = src.shape[-1]
    sc2 = tl["sc2"][par][:, 0:n]
    P.op("dve", "max", out=m8a.all(), in_=src)
    P.op("dve", "max_index", out=i8a.all(), in_max=m8a.all(), in_values=src)
    P.op("dve", "match_replace", out=sc2, in_to_replace=m8a.all(), in_values=src, imm_value=-1.0e30)
    P.op("dve", "max", out=m8b.all(), in_=sc2)
    P.op("dve", "max_index", out=i8b.all(), in_max=m8b.all(), in_values=sc2)
    P.copy("act", vals_out[:, 0:8], m8a.all())
    P.copy("act", vals_out[:, 8:16], m8b.all())
    P.copy("dve", idx_out[:, 0:8], i8a.all().cast(I32))
    P.copy("dve", idx_out[:, 8:16], i8b.all().cast(I32))


def peer_phase(P, c, S, h_d, out_d, pd, uvb_d, NB=16, dump=None):
    NT = S // 128
    psb, ident = c.psb, c.ident
    ph = ExitStack()
    wq = P.sb(ph, "wq", [128, 8, 2048], BF16)
    pk = P.sb(ph, "pk", [128, 16, 128], BF16)
    g2c = P.sb(ph, "g2c", [128, 8], F32)
    g2b = P.sb(ph, "g2b", [128, D], F32)
    gfb = P.sb(ph, "gfb", [128, D], F32)
    P.dma("sp", g2c.all(), pd["g2c"].all())
    P.dma("sp", g2b.all(), pd["g2b"].all())
    P.dma("sp", gfb.all(), pd["gfb"].all())
    hts = [P.sb(ph, f"cht{i}", [128, D], F32) for i in range(2)]
    hh = P.sb(ph, "chh", [128, D], BF16)
    hT = P.sb(ph, "chT", [128, 8, 128], BF16)
    junk = P.sb(ph, "cjunk", [128, D], BF16)
    hn = P.sb(ph, "chn", [128, D], F32)
    ss = P.sb(ph, "css", [128, 1], F32)
    rstd = P.sb(ph, "crstd", [128, 1], F32)
    qT = P.sb(ph, "cqT", [128, 16, 128], BF16)
    SC = P.sb(ph, "SC", [128, 16, 128], F32)
    V16 = P.sb(ph, "V16", [128, 16, 16], F32)
    I16 = P.sb(ph, "I16", [128, 16, 16], F32)
    CS = P.sb(ph, "CS", [128, 8, 256], F32)
    TS = P.sb(ph, "TS", [128, 8, 16], F32)
    POSf = P.sb(ph, "POSf", [128, 8, 16], F32)
    POSi = P.sb(ph, "POSi", [128, 8, 16], I32)
    rowi = P.sb(ph, "rowi", [128, 8, 16], I32)
    coli = P.sb(ph, "coli", [128, 8, 16], I32)
    rowf = P.sb(ph, "rowf", [128, 8, 16], F32)
    colf = P.sb(ph, "colf", [128, 8, 16], F32)
    E = P.sb(ph, "E", [128, 8, 16], F32)
    G = P.sb(ph, "G", [128, 8, 16], F32)
    sm = P.sb(ph, "sm", [128, 8], F32)
    OH = P.sb(ph, "OH", [128, 8, 16, 16], F32)
    OH2 = P.sb(ph, "OH2", [128, 8, 16, 16], F32)
    i1s = P.sb(ph, "i1s", [128, 8, 16], F32)
    i2s = P.sb(ph, "i2s", [128, 8, 16], F32)
    eidf = P.sb(ph, "eidf", [128, 128], F32)
    EID = P.sb(ph, "EID", [128, 128], I32)
    dots = P.sb(ph, "dots", [128, 128], F32)
    actw = P.sb(ph, "actw", [128, 128], F32)
    resd = P.sb(ph, "cres", [128, D], F32)
    outt = [P.sb(ph, f"cout{i}", [128, D], F32) for i in range(2)]
    tl = {k: [P.sb(ph, f"{k}{i}", [128, 8], dt) for i in range(2)]
          for k, dt in (("m8a", F32), ("i8a", U32), ("m8b", F32), ("i8b", U32))}
    tl["sc2"] = [P.sb(ph, f"sc2{i}", [128, 256], F32) for i in range(2)]
    wst = ExitStack()
    stg = [P.sb(wst, f"cstg{i}", [128, 2048], F32) for i in range(2)]
    qs = ("sp", "act")
    for k in range(8):
        P.dma(qs[k % 2], stg[k % 2].all(), pd["peer_wq"][k * 128:(k + 1) * 128, :])
        P.act(wq[:, k, :], stg[k % 2].all(), AF.Copy, scale=g2c[:, k:k + 1])
    P.dma("sp", stg[0].all(), pd["pk"][0:128].rr("p a n -> p (a n)"))
    P.copy("act", pk.all().rr("p a n -> p (a n)"), stg[0].all())
    wst.close()
    UV = [P.sb(ph, f"UV{i}", [128, 2048], BF16) for i in range(NB)]
    dg = [P.sb(ph, f"dg{i}", [128, 128], BF16) for i in range(4)]
    hnb = P.sb(ph, "hnb", [128, D], BF16)
    junkb = P.sb(ph, "cjunkb", [128, D], F32)
    tuv = uvb_d.all()

    EIDs = [EID, P.sb(ph, "EID1", [128, 128], I32)]
    Gs = [G, P.sb(ph, "G1", [128, 8, 16], F32)]
    hnbs = [hnb, P.sb(ph, "hnb1", [128, D], BF16)]
    ssf = P.sb(ph, "cssf", [128, 1], F32)
    rstdf = P.sb(ph, "crstdf", [128, 1], F32)
    pacc = [psb[6], psb[7]]

    def front(b):
        tok = slice(b * 128, (b + 1) * 128)
        ht, EIDb, Gb, hnbb = hts[b % 2], EIDs[b % 2], Gs[b % 2], hnbs[b % 2]
        P.dma("sp", ht.all(), h_d[tok, :])
        P.act(junk.all(), ht.all(), AF.Square, accum_out=ss.all())
        P.ts("dve", ss.all(), ss.all(), 1.0 / D, ALU.mult, EPS, ALU.add)
        P.act(ss.all(), ss.all(), AF.Sqrt)
        P.op("dve", "reciprocal", out=rstd.all(), in_=ss.all())
        P.ts("dve", hh.all(), ht.all(), rstd[:, 0:1], ALU.mult)
        yield
        P.stt("dve", hn.all(), ht.all(), rstd[:, 0:1], g2b.all(), ALU.mult, ALU.mult)
        P.copy("act", hnbb.all(), hn.all())
        pbf = psb[0].all().cast(BF16)
        for k in range(8):
            P.tr(pbf[:, k * 128:(k + 1) * 128], hh[:, k * 128:(k + 1) * 128], ident.all())
        P.copy("act", hT.all().rr("p k t -> p (k t)"), pbf)
        yield
        for ch in range(16):
            bank = psb[1 + (ch // 4) % 2]
            for k in range(8):
                P.mm(bank[:, (ch % 4) * 128:(ch % 4 + 1) * 128], wq[:, k, ch * 128:(ch + 1) * 128], hT[:, k, :], k == 0, k == 7)
            if ch % 4 == 3:
                P.copy("act", qT[:, ch - 3:ch + 1, :].rr("p a t -> p (a t)"), bank.all())
                yield
        sbanks = [psb[3], psb[4], psb[5], psb[0]]
        for ch in range(16):
            bank = sbanks[ch // 4]
            P.mm(bank[:, (ch % 4) * 128:(ch % 4 + 1) * 128], qT[:, ch, :], pk[:, ch, :], True, True)
            if ch % 4 == 3:
                P.copy("act", SC[:, ch - 3:ch + 1, :].rr("p a n -> p (a n)"), bank.all())
        yield
        for ch in range(16):
            top16(P, SC[:, ch, :], V16[:, ch, :], I16[:, ch, :], tl, ch % 2)
            if ch % 2 == 1:
                yield
        P.tt("dve", CS.all().rr("p h (i j) -> p h i j", i=16), V16.all().pat(0, [(32, 8), (1, 16), (0, 16)]),
             V16.all().pat(16, [(32, 8), (0, 16), (1, 16)]), ALU.add)
        for h in range(8):
            top16(P, CS[:, h, :], TS[:, h, :], POSf[:, h, :], tl, h % 2)
            if h % 2 == 1:
                yield
        P.tt("dve", E.all(), TS.all(), TS.all().pat(0, [(16, 8), (0, 16)]), ALU.subtract)
        P.act(E.all(), E.all(), AF.Exp)
        P.op("dve", "tensor_reduce", out=sm.all(), in_=E.all(), axis=AX.X, op=ALU.add)
        P.op("dve", "reciprocal", out=sm.all(), in_=sm.all())
        P.tt("dve", Gb.all(), E.all(), sm.all().pat(0, [(1, 8), (0, 16)]), ALU.mult)
        yield
        P.copy("dve", POSi.all(), POSf.all())
        P.op("dve", "tensor_single_scalar", out=rowi.all(), in_=POSi.all(), scalar=4, op=ALU.arith_shift_right)
        P.op("dve", "tensor_single_scalar", out=coli.all(), in_=POSi.all(), scalar=15, op=ALU.bitwise_and)
        P.copy("dve", rowf.all(), rowi.all())
        P.copy("dve", colf.all(), coli.all())
        io16 = c.iota_f[:, 0:16].pat(0, [(0, 8), (0, 16), (1, 16)])
        for src, off, dst, oh in ((rowf, 0, i1s, OH), (colf, 16, i2s, OH2)):
            P.tt("dve", oh.all(), src.all().pat(0, [(16, 8), (1, 16), (0, 16)]), io16, ALU.is_equal)
            P.tt("dve", oh.all(), oh.all(), I16.all().pat(off, [(32, 8), (0, 16), (1, 16)]), ALU.mult)
            P.op("dve", "tensor_reduce", out=dst.all().rr("p h k -> p (h k)"), in_=oh.all().rr("p h k i -> p (h k) i"),
                 axis=AX.X, op=ALU.add)
            yield
        P.stt("dve", eidf.all(), i1s.all().rr("p h k -> p (h k)"), 128.0, i2s.all().rr("p h k -> p (h k)"), ALU.mult, ALU.add)
        P.copy("dve", EIDb.all(), eidf.all())

    def advance(gen, n):
        if gen is None:
            return None
        try:
            for _ in range(n):
                next(gen)
        except StopIteration:
            return None
        return gen

    GS = NB // 2
    NGRP = 128 // GS
    advance(front(0), 1000)
    for b in range(NT):
        tok = slice(b * 128, (b + 1) * 128)
        ht, EIDb, hnbb = hts[b % 2], EIDs[b % 2], hnbs[b % 2]
        Gf = Gs[b % 2].all().rr("p a b -> p (a b)")
        nxt = front(b + 1) if b + 1 < NT else None
        for g in range(NGRP):
            gs = slice(g * GS, (g + 1) * GS)
            for e in range(g * GS, (g + 1) * GS):
                uv = UV[e % NB]
                P.gather(uv.all(), tuv, EIDb[:, e:e + 1])
                P.stt("dve", junkb.all(), uv[:, 0:D], 1.0, hnbb.all(), ALU.mult, ALU.mult, accum_out=dots[:, e:e + 1])
            P.act(actw[:, gs], dots[:, gs], AF.Gelu_apprx_tanh)
            P.tt("dve", actw[:, gs], actw[:, gs], Gf[:, gs], ALU.mult)
            for e in range(g * GS, (g + 1) * GS):
                uv, dgt = UV[e % NB], dg[e % 4]
                P.act(dgt.all(), ident.all(), AF.Copy, scale=actw[:, e:e + 1])
                for n2 in range(2):
                    P.mm(pacc[n2].all(), dgt.all(), uv[:, D + n2 * 512:D + (n2 + 1) * 512], e == 0, e == 127)
            nxt = advance(nxt, 2)
        advance(nxt, 1000)
        for n2 in range(2):
            P.tt("dve", resd[:, n2 * 512:(n2 + 1) * 512], pacc[n2].all(), ht[:, n2 * 512:(n2 + 1) * 512], ALU.add)
        P.act(junk.all(), resd.all(), AF.Square, accum_out=ssf.all())
        P.ts("dve", ssf.all(), ssf.all(), 1.0 / D, ALU.mult, EPS, ALU.add)
        P.act(ssf.all(), ssf.all(), AF.Sqrt)
        P.op("dve", "reciprocal", out=rstdf.all(), in_=ssf.all())
        ot = outt[b % 2]
        P.stt("dve", ot.all(), resd.all(), rstdf[:, 0:1], gfb.all(), ALU.mult, ALU.mult)
        P.dma("sp", out_d[tok, :], ot.all())
    ph.close()


def late_param_shapes():
    return {"w_ssm_up": [513, 1024], "w_attn_up": [513, 1024], "w_out": [1025, 1024], "g2c": [128, 8],
            "g2b": [128, 1024], "gfb": [128, 1024], "peer_wq": [1025, 2048], "pk": [129, 16, 128],
            "peer_uv": [16385, 2048]}


def late_host_layout(inp):
    g2 = np.asarray(inp["norm2_g"], dtype=np.float32)[0]
    gf = np.asarray(inp["norm_f_g"], dtype=np.float32)
    k1, k2 = np.asarray(inp["peer_k1"])[0], np.asarray(inp["peer_k2"])[0]
    pk = np.stack([k1, k2], 1).reshape(16, 128, 128).transpose(2, 0, 1)
    d = {"w_ssm_up": np.asarray(inp["w_ssm_up"])[0], "w_attn_up": np.asarray(inp["w_attn_up"])[0],
         "w_out": np.asarray(inp["w_out"])[0], "g2c": g2.reshape(8, 128).T,
         "g2b": np.broadcast_to(g2[None, :], (128, 1024)), "gfb": np.broadcast_to(gf[None, :], (128, 1024)),
         "peer_wq": np.asarray(inp["peer_wq"])[0], "pk": pk,
         "peer_uv": np.concatenate([np.asarray(inp["peer_u"])[0], np.asarray(inp["peer_v"])[0]], 1)}
    return {k: np.ascontiguousarray(v, dtype=np.float32) for k, v in d.items()}


def ssm_param_shapes():
    return {"ar_sm": [128, 32], "ai_sm": [128, 32], "ldt_sm": [128, 32],
            "bU1": [128, 32, 16], "bU2": [128, 32, 16], "cT1": [128, 32, 16], "cT2": [128, 32, 16],
            "ar_cm": [128, 4, 64], "ai_cm": [128, 4, 64], "ldt_cm": [128, 4, 64],
            "br_cm": [128, 4, 64], "bi_cm": [128, 4, 64], "dskip": [128, 4], "w_glu": [513, 512]}


def ssm_host_layout(inp):
    a_re, a_im, log_dt = inp["a_re"][0], inp["a_im"][0], inp["log_dt"][0]
    b_re, b_im, c_re, c_im = inp["b_re"][0], inp["b_im"][0], inp["c_re"][0], inp["c_im"][0]
    d = {}
    d["ar_sm"] = np.concatenate([a_re.T, a_re.T], 0)
    d["ai_sm"] = np.concatenate([a_im.T, a_im.T], 0)
    d["ldt_sm"] = np.broadcast_to(log_dt[None, :], (128, 32))
    brT, biT = b_re.transpose(1, 0, 2), b_im.transpose(1, 0, 2)
    d["bU1"] = np.concatenate([brT, biT], 0)
    d["bU2"] = np.concatenate([biT, brT], 0)
    crT, ciT = c_re.transpose(2, 0, 1), c_im.transpose(2, 0, 1)
    d["cT1"] = np.concatenate([crT, ciT], 0)
    d["cT2"] = np.concatenate([ciT, crT], 0)
    q = np.arange(128)
    gq = (np.arange(4)[None, :] * 8 + (q // 16)[:, None])
    d["ar_cm"] = a_re[gq]
    d["ai_cm"] = a_im[gq]
    d["ldt_cm"] = np.broadcast_to(log_dt[gq][:, :, None], (128, 4, 64))
    d["br_cm"] = b_re[gq, :, (q % 16)[:, None]]
    d["bi_cm"] = b_im[gq, :, (q % 16)[:, None]]
    d["dskip"] = inp["d_skip"][0].reshape(4, 128).T
    d["w_glu"] = inp["w_glu"][0]
    return {k: np.ascontiguousarray(v, dtype=np.float32) for k, v in d.items()}


def build(nc, S, stage_stop=None, dbg=None):
    NT = S // 128
    NCH = S // LCH
    TOPK = min(256, S // 4)
    es = ExitStack()
    P = Prog(nc, es)
    c = Ctx()
    c.P = P
    dbg = dbg if dbg is not None else {}

    x_d = P.dram("x", [S, D], F32, "ExternalInput")
    g1c_d = P.dram("g1c", [128, 8], F32, "ExternalInput")
    w_in_d = P.dram("w_in", [D + 1, IN_W], F32, "ExternalInput")
    out_d = P.dram("out", [S, D], F32, "ExternalOutput")

    def dbg_out(name, shape, dt=F32):
        t = P.dram("dbg_" + name, shape, dt, "ExternalOutput")
        dbg[name] = t
        return t

    blk = es.enter_context(nc.Block())
    holder = {}

    def body(_sync):
        glob = ExitStack()
        ident = P.sb(glob, "ident", [128, 128], BF16)
        identf = P.sb(glob, "identf", [128, 128], F32)
        iota_i = P.sb(glob, "iota_i", [128, 128], I32)
        pid_i = P.sb(glob, "pid_i", [128, 1], I32)
        pid_f = P.sb(glob, "pid_f", [128, 1], F32)
        iota_f = P.sb(glob, "iota_f", [128, 128], F32)
        P.op("pool", "iota", out=iota_i.all(), pattern=[[1, 128]], base=0, channel_multiplier=0)
        P.op("pool", "iota", out=pid_i.all(), pattern=[[0, 1]], base=0, channel_multiplier=1)
        P.copy("dve", iota_f.all(), iota_i.all())
        P.copy("dve", pid_f.all(), pid_i.all())
        P.ts("dve", identf.all(), iota_f.all(), pid_f[:, 0:1], ALU.is_equal)
        P.copy("dve", ident.all(), identf.all())
        caus = P.sb(glob, "caus", [128, 128], F32)
        P.ts("dve", caus.all(), iota_f.all(), pid_f[:, 0:1], ALU.is_gt, NEG, ALU.mult)
        g1c = P.sb(glob, "g1c", [128, 8], F32)
        ng1c = P.sb(glob, "ng1c", [128, 8], F32)
        P.dma("sp", g1c.all(), g1c_d.all())
        P.ts("dve", ng1c.all(), g1c.all(), -1.0, ALU.mult)
        c.ident, c.identf, c.iota_f, c.pid_f, c.caus = ident, identf, iota_f, pid_f, caus

        psb = [P.ps(glob, f"psb{i}", [128, 512], F32) for i in range(8)]
        c.psb = psb

        res = ExitStack()
        uT = P.sb(res, "uys", [128, 4, S], BF16)
        res_a = ExitStack()
        res_a_close = res_a.close
        kTd = P.sb(res_a, "kTd", [128, 2, S], BF16)
        kiT4 = P.sb(res_a, "kiT4", [128, S], BF16)
        vaug = P.sb(res_a, "vaug", [128, NT, 2, 80], BF16)
        P.memset("pool", vaug.all(), 1.0)

        s1 = ExitStack()
        rope_tables(P, s1, S, c)
        stg = [P.sb(s1, f"stg{i}", [128, 1024], F32) for i in range(2)]
        w_u = P.sb(s1, "w_u", [128, 8, 512], BF16)
        w_kd = P.sb(s1, "w_kd", [128, 8, 4, 128], BF16)
        w_ki = P.sb(s1, "w_ki", [128, 8, 2, 128], BF16)
        w_v = P.sb(s1, "w_v", [128, 8, 128], BF16)

        def cvt(dst, src, k, neg=False):
            P.act(dst, src, AF.Copy, scale=(ng1c if neg else g1c)[:, k:k + 1])

        load_w_cols(P, c, lambda k, st: cvt(w_u[:, k, :], st[:, 0:512], k), OFF_U, 512, w_in_d, g1c, stg)

        def k_cvt(k, st):
            for n in range(2):
                for dup in range(2):
                    cvt(w_kd[:, k, n, dup * 64:(dup + 1) * 64], st[:, n * 64:(n + 1) * 64], k)
                    cvt(w_kd[:, k, 2 + n, dup * 64:dup * 64 + 32], st[:, n * 64 + 32:n * 64 + 64], k, neg=True)
                    cvt(w_kd[:, k, 2 + n, dup * 64 + 32:dup * 64 + 64], st[:, n * 64:n * 64 + 32], k)
        load_w_cols(P, c, k_cvt, OFF_K, 128, w_in_d, g1c, stg)

        def ki_cvt(k, st):
            for r in range(4):
                cvt(w_ki[:, k, 0, r * 32:(r + 1) * 32], st[:, 0:32], k)
                cvt(w_ki[:, k, 1, r * 32:r * 32 + 16], st[:, 16:32], k, neg=True)
                cvt(w_ki[:, k, 1, r * 32 + 16:r * 32 + 32], st[:, 0:16], k)
        load_w_cols(P, c, ki_cvt, OFF_KI, 32, w_in_d, g1c, stg)
        load_w_cols(P, c, lambda k, st: cvt(w_v[:, k, :], st[:, 0:128], k), OFF_V, 128, w_in_d, g1c, stg)

        xt = [P.sb(s1, f"xt{i}", [128, D], F32) for i in range(2)]
        xh = P.sb(s1, "xh", [128, D], BF16)
        xhT = P.sb(s1, "xhT", [128, 8, 128], BF16)
        junk = P.sb(s1, "junk", [128, D], BF16)
        ss = P.sb(s1, "ss", [128, 1], F32)
        rstd = P.sb(s1, "rstd", [128, 1], F32)
        r1 = P.sb(s1, "r1", [128, 128], F32)
        r2 = P.sb(s1, "r2", [128, 128], F32)

        def norm_and_transpose(b, xt_b, xh, xhT, junk, ss, rstd, psT):
            P.act(junk.all(), xt_b.all(), AF.Square, accum_out=ss.all())
            P.act(ss.all(), ss.all(), AF.Sqrt, scale=1.0 / D, bias=EPS) if False else None
            P.ts("dve", ss.all(), ss.all(), 1.0 / D, ALU.mult, EPS, ALU.add)
            P.act(ss.all(), ss.all(), AF.Sqrt)
            P.op("dve", "reciprocal", out=rstd.all(), in_=ss.all())
            P.ts("dve", xh.all(), xt_b.all(), rstd[:, 0:1], ALU.mult)
            pb = psT.all().cast(BF16)
            for k in range(8):
                P.tr(pb[:, k * 128:(k + 1) * 128], xh[:, k * 128:(k + 1) * 128], ident.all())
            P.copy("act", xhT.all().rr("p k t -> p (k t)"), pb)
        c.norm_and_transpose = norm_and_transpose

        for b in range(NT):
            xb = xt[b % 2]
            P.dma("sp", xb.all(), x_d[b * 128:(b + 1) * 128, :])
            norm_and_transpose(b, xb, xh, xhT, junk, ss, rstd, psb[0])
            tok = slice(b * 128, (b + 1) * 128)
            for m in range(4):
                for k in range(8):
                    P.mm(psb[1][:, m * 128:(m + 1) * 128], w_u[:, k, m * 128:(m + 1) * 128], xhT[:, k, :], k == 0, k == 7)
            P.copy("act", uT[:, :, tok], psb[1].all().rr("p (m t) -> p m t", m=4))
            for m in range(4):
                for k in range(8):
                    P.mm(psb[2][:, m * 128:(m + 1) * 128], w_kd[:, k, m, :], xhT[:, k, :], k == 0, k == 7)
            for n in range(2):
                P.tt("dve", r1.all(), psb[2][:, n * 128:(n + 1) * 128], c.rope["cosA"][:, tok], ALU.mult)
                P.tt("dve", r2.all(), psb[2][:, (2 + n) * 128:(3 + n) * 128], c.rope["sinA"][:, tok], ALU.mult)
                P.tt("dve", kTd[:, n, tok], r1.all(), r2.all(), ALU.add)
            for m in range(2):
                for k in range(8):
                    P.mm(psb[3][:, m * 128:(m + 1) * 128], w_ki[:, k, m, :], xhT[:, k, :], k == 0, k == 7)
            for k in range(8):
                P.mm(psb[3][:, 256:384], xhT[:, k, :], w_v[:, k, :], k == 0, k == 7)
            P.tt("dve", r1.all(), psb[3][:, 0:128], c.rope["cosI"][:, tok], ALU.mult)
            P.tt("dve", r2.all(), psb[3][:, 128:256], c.rope["sinI"][:, tok], ALU.mult)
            P.tt("dve", kiT4[:, tok], r1.all(), r2.all(), ALU.add)
            P.copy("act", vaug[:, b, :, 0:64], psb[3][:, 256:384].rr("p (n d) -> p n d", n=2))
        if stage_stop == "s1":
            for nm in ("cosA", "sinA", "cosI", "sinI"):
                t = dbg_out(nm, [128, S], BF16)
                P.dma("sp", t.all(), c.rope[nm].all())
        s1.close()

        if stage_stop == "s1":
            t = dbg_out("uT", [128, 4 * S], BF16)
            P.dma("sp", t.all(), uT.all().rr("p m t -> p (m t)"))
            t = dbg_out("kTd", [128, 2 * S], BF16)
            P.dma("sp", t.all(), kTd.all().rr("p m t -> p (m t)"))
            t = dbg_out("kiT4", [128, S], BF16)
            P.dma("sp", t.all(), kiT4.all())
            t = dbg_out("vaug", [128, NT * 160], BF16)
            P.dma("sp", t.all(), vaug.all().rr("p a n d -> p (a n d)"))
            P.finish(list(dbg.values()))
            res_a.close()
            res.close()
            glob.close()
            return

        sp = {nm: P.dram(nm, shp, F32, "ExternalInput") for nm, shp in ssm_param_shapes().items()}
        if stage_stop == "s2":
            def dump(name, view, dt):
                t = dbg_out(name, list(view.shape), dt)
                P.dma("sp", t.all(), view)
            c.dump = dump
        ssm_phase(P, c, S, uT, sp)
        if stage_stop == "s2":
            t = dbg_out("ysT", [128, 4 * S], BF16)
            P.dma("sp", t.all(), uT.all().rr("p m t -> p (m t)"))
            P.finish(list(dbg.values()))
            res_a.close()
            res.close()
            glob.close()
            return
        ya_d = P.dram("ya_scr", [64, 8, S], BF16, "ExternalOutput" if stage_stop == "a" else "Internal")
        astop = stage_stop[2:] if (stage_stop or "").startswith("a:") else None

        def adump(name, view, dt):
            t = dbg_out(name, list(view.shape), dt)
            P.dma("sp", t.all(), view)
        attn_phase(P, c, S, x_d, w_in_d, g1c, ng1c, kTd, kiT4, vaug, ya_d, stop_at=astop, dump=adump if astop else None)
        res_a.close()
        if astop:
            P.finish(list(dbg.values()))
            res.close()
            glob.close()
            return
        if stage_stop == "a":
            dbg["ya_scr"] = ya_d
            P.finish([ya_d])
            res.close()
            glob.close()
            return
        pd = {nm: P.dram(nm, shp, F32, "ExternalInput") for nm, shp in late_param_shapes().items()}
        h_d = P.dram("h_scr", [S, D], F32, "ExternalOutput" if stage_stop == "b" else "Internal")
        uvb_d = P.dram("uvb_scr", [16384, 2048], BF16, "ExternalOutput" if stage_stop == "cdbg" else "Internal")
        merge_phase(P, c, S, x_d, w_in_d, g1c, uT, ya_d, h_d, pd, uvb_d)
        res.close()
        if stage_stop == "b":
            dbg["h_scr"] = h_d
            P.finish([h_d])
            glob.close()
            return
        def cdump(name, view, dt):
            t = dbg_out(name, list(view.shape), dt)
            P.dma("sp", t.all(), view)
        peer_phase(P, c, S, h_d, out_d, pd, uvb_d, dump=cdump if stage_stop == "cdbg" else None)
        P.finish([out_d] + list(dbg.values()) + ([uvb_d] if stage_stop == "cdbg" else []))
        glob.close()

    holder["rest"] = lambda P, c, env: None
    blk.sync(body)
    es.close()
    return P, dbg


PADDED = ("w_in", "w_glu", "w_ssm_up", "w_attn_up", "w_out", "peer_wq", "pk", "peer_uv")


def core_inputs(shared, xb):
    im = dict(shared)
    im["x"] = np.ascontiguousarray(xb, dtype=np.float32)
    flat = im["x"].reshape(-1)
    for nm in PADDED:
        a = shared[nm]
        row = flat[:a[0].size].reshape((1,) + a.shape[1:])
        im[nm] = np.concatenate([a, row], 0)
    return im


def kernel(**inputs):
    inputs = {k: np.asarray(v) for k, v in inputs.items()}
    B, S, _ = inputs["x"].shape
    assert B == NCORES
    g1 = inputs["norm1_g"].astype(np.float32)[0]
    shared = {"g1c": np.ascontiguousarray(g1.reshape(8, 128).T),
              "w_in": np.ascontiguousarray(inputs["w_in"].astype(np.float32)[0])}
    shared.update(ssm_host_layout(inputs))
    shared.update(late_host_layout(inputs))
    nc = bass.Bass("TRN2", target_bir_lowering=False)
    build(nc, S)
    x = inputs["x"].astype(np.float32)
    in_maps = [core_inputs(shared, x[b]) for b in range(NCORES)]
    res = run_bass_kernel_spmd(nc, in_maps, core_ids=list(range(NCORES)))
    out = np.stack([np.asarray(res.results[b]["out"], dtype=np.float32) for b in range(NCORES)], 0)
    return out
```
